# Optimizing a Trainium2 kernel written in Bass

```python
import math
import jax, jax.numpy as jnp
from jax import lax
import numpy as np

D_MODEL = 1024
BATCH = 8
SEQ = 2048
DEPTH = 2

N_A = DEPTH // 2
N_B = DEPTH - N_A

RET_HEADS = 4
RET_QK_DIM = 128
RET_V_DIM = 192
RET_CHUNK = 128
RET_QK_W = RET_HEADS * RET_QK_DIM
RET_V_W = RET_HEADS * RET_V_DIM

MOBA_HEADS = 12
MOBA_HEAD_DIM = 64
MOBA_W = MOBA_HEADS * MOBA_HEAD_DIM
MOBA_BLOCK = 256
MOBA_TOPK = 3
MOBA_QCHUNK = 8

MEM_LEN = 256
MEM_HEADS = 4
MEM_HEAD_DIM = 64
MEM_W = MEM_HEADS * MEM_HEAD_DIM

A_IN_W = 2 * RET_QK_W + 2 * RET_V_W + MEM_W
A_MIX_W = RET_V_W + MEM_W
B_IN_W = MOBA_W + MEM_W
B_MIX_W = MOBA_W + MEM_W
D_FF = 4 * D_MODEL
EPS = 1e-6
NEG_INF = -1e30

kernel_name = "yoco_retention_moba_hybrid"


def alibi_slopes(n):
    def pow2(m):
        return [2.0 ** (-8.0 * (i + 1) / m) for i in range(m)]
    p = 2 ** int(math.floor(math.log2(n)))
    s = pow2(p)
    if p < n:
        s = s + pow2(2 * p)[0::2][: n - p]
    return np.asarray(s, dtype=np.float32)


def rmsnorm(x, g):
    xf = x.astype(jnp.float32)
    y = xf * lax.rsqrt(jnp.mean(xf * xf, axis=-1, keepdims=True) + EPS)
    return (y * g.astype(jnp.float32)).astype(x.dtype)


def split_heads(x, n_heads):
    b, s, w = x.shape
    return x.reshape(b, s, n_heads, w // n_heads).transpose(0, 2, 1, 3)


def merge_heads(x):
    b, h, s, d = x.shape
    return x.transpose(0, 2, 1, 3).reshape(b, s, h * d)


def memory_attention(q_m, mem, w_mkv):
    mk, mv = jnp.split(mem @ w_mkv, 2, axis=-1)
    q = split_heads(q_m, MEM_HEADS)
    k = split_heads(mk, MEM_HEADS)
    v = split_heads(mv, MEM_HEADS)
    s = jnp.einsum("bhsd,bhmd->bhsm", q, k).astype(jnp.float32) * (MEM_HEAD_DIM ** -0.5)
    p = jax.nn.softmax(s, axis=-1).astype(v.dtype)
    return merge_heads(jnp.einsum("bhsm,bhmd->bhsd", p, v))


def retention(q, k, v):
    b, h, s, dk = q.shape
    dv = v.shape[-1]
    c = RET_CHUNK
    nc = s // c
    q = q.astype(jnp.float32)
    k = k.astype(jnp.float32) * (dk ** -0.5)
    v = v.astype(jnp.float32)
    log_g = jnp.log1p(-jnp.exp2(-5.0 - jnp.arange(h, dtype=jnp.float32)))
    qc = q.reshape(b, h, nc, c, dk)
    kc = k.reshape(b, h, nc, c, dk)
    vc = v.reshape(b, h, nc, c, dv)
    pos = jnp.arange(c, dtype=jnp.float32)
    rel = pos[:, None] - pos[None, :]
    d_intra = jnp.where(rel >= 0, jnp.exp(log_g[:, None, None] * jnp.maximum(rel, 0.0)), 0.0)
    inner = jnp.einsum("bhncd,bhnkd->bhnck", qc, kc) * d_intra[None, :, None]
    inner_out = jnp.einsum("bhnck,bhnkv->bhncv", inner, vc)
    zeta = jnp.exp(log_g[:, None] * (c - 1.0 - pos))
    kv_chunk = jnp.einsum("bhnkd,bhnkv->bhndv", kc * zeta[None, :, None, :, None], vc)
    chunk_decay = jnp.exp(log_g * c)[None, :, None, None]

    def step(state, kv_n):
        return state * chunk_decay + kv_n, state

    _, prev = lax.scan(step, jnp.zeros((b, h, dk, dv), jnp.float32), jnp.moveaxis(kv_chunk, 2, 0))
    prev = jnp.moveaxis(prev, 0, 2)
    xi = jnp.exp(log_g[:, None] * (pos + 1.0))
    cross = jnp.einsum("bhncd,bhndv->bhncv", qc * xi[None, :, None, :, None], prev)
    return (inner_out + cross).reshape(b, h, s, dv)


def moba_attention(q, k, v, slopes):
    b, h, s, dh = q.shape
    bs = MOBA_BLOCK
    nb = -(-s // bs)
    sp = nb * bs
    pad = sp - s
    if pad:
        widths = ((0, 0), (0, 0), (0, pad), (0, 0))
        q, k, v = jnp.pad(q, widths), jnp.pad(k, widths), jnp.pad(v, widths)
    kb = k.reshape(b, h, nb, bs, dh)
    vb = v.reshape(b, h, nb, bs, dh)
    t = jnp.arange(sp, dtype=jnp.int32)
    qblk = t // bs
    own = jnp.broadcast_to(qblk, (b, h, sp))[..., None]
    kk = min(MOBA_TOPK, nb - 1)
    if kk > 0:
        kmean = jnp.mean(kb.astype(jnp.float32), axis=3)
        gate = jnp.einsum("bhsd,bhnd->bhsn", q.astype(jnp.float32), kmean)
        past = jnp.arange(nb, dtype=jnp.int32)[None, :] < qblk[:, None]
        gate = jnp.where(past, gate, NEG_INF)
        _, top = lax.top_k(gate, kk)
        idx = jnp.concatenate([top.astype(jnp.int32), own], axis=-1)
        valid = jnp.concatenate([jnp.arange(kk, dtype=jnp.int32)[None, :] < qblk[:, None],
                                 jnp.ones((sp, 1), bool)], axis=-1)
    else:
        idx = own
        valid = jnp.ones((sp, 1), bool)
    r = idx.shape[-1]
    qc_n = MOBA_QCHUNK
    nq = sp // qc_n

    def to_chunks(a):
        a = a.reshape(b, h, nq, qc_n, *a.shape[3:])
        return jnp.moveaxis(a, 2, 0)

    bi = jnp.arange(b)[:, None, None, None]
    hi = jnp.arange(h)[None, :, None, None]
    scale = dh ** -0.5
    blk_pos = jnp.arange(bs, dtype=jnp.int32)

    def one_chunk(args):
        qc, ic, tc, vc = args
        kg = kb[bi, hi, ic]
        vg = vb[bi, hi, ic]
        sc = jnp.einsum("bhqd,bhqrkd->bhqrk", qc, kg).astype(jnp.float32) * scale
        kpos = ic[..., None] * bs + blk_pos
        dist = tc[None, None, :, None, None] - kpos
        sc = sc - slopes[None, :, None, None, None] * dist.astype(jnp.float32)
        mask = (dist >= 0) & vc[None, None, :, :, None]
        sc = jnp.where(mask, sc, NEG_INF)
        p = jax.nn.softmax(sc.reshape(b, h, qc_n, r * bs), axis=-1).reshape(sc.shape)
        return jnp.einsum("bhqrk,bhqrkd->bhqd", p.astype(vg.dtype), vg)

    out = lax.map(one_chunk, (to_chunks(q), to_chunks(idx), t.reshape(nq, qc_n), valid.reshape(nq, qc_n, r)))
    out = jnp.moveaxis(out, 0, 2).reshape(b, h, sp, dh)
    return out[:, :, :s]


def retention_mixer(hn, mem, w_in, gn_gain, w_out, w_mkv):
    proj = hn @ w_in
    q_r, k_r, v_r, g_r, q_m = jnp.split(
        proj, [RET_QK_W, 2 * RET_QK_W, 2 * RET_QK_W + RET_V_W, 2 * RET_QK_W + 2 * RET_V_W], axis=-1)
    y = retention(split_heads(q_r, RET_HEADS), split_heads(k_r, RET_HEADS), split_heads(v_r, RET_HEADS))
    y = y * lax.rsqrt(jnp.mean(y * y, axis=-1, keepdims=True) + EPS)
    y = (merge_heads(y) * gn_gain.astype(jnp.float32)).astype(hn.dtype)
    y = jax.nn.silu(g_r) * y
    m = memory_attention(q_m, mem, w_mkv)
    return jnp.concatenate([y, m], axis=-1) @ w_out


def moba_mixer(hn, mem, k_sh, v_sh, slopes, w_in, w_out, w_mkv):
    q_b, q_m = jnp.split(hn @ w_in, [MOBA_W], axis=-1)
    a = merge_heads(moba_attention(split_heads(q_b, MOBA_HEADS), k_sh, v_sh, slopes))
    m = memory_attention(q_m, mem, w_mkv)
    return jnp.concatenate([a, m], axis=-1) @ w_out


def squared_relu_mlp(hn, w_up, w_down):
    return jnp.square(jax.nn.relu(hn @ w_up)) @ w_down


def setup_inputs(seed: int = 0) -> dict:
    key = jax.random.key(seed)
    ks = jax.random.split(key, 16)

    def w(k, shape):
        return jax.random.normal(k, shape, jnp.float32) * (shape[-2] ** -0.5)

    def gain(k, shape):
        return 1.0 + 0.1 * jax.random.normal(k, shape, jnp.float32)

    return {
        "x": jax.random.normal(ks[0], (BATCH, SEQ, D_MODEL), jnp.float32),
        "mem": jax.random.normal(ks[1], (BATCH, MEM_LEN, D_MODEL), jnp.float32),
        "w_in_a": w(ks[2], (N_A, D_MODEL, A_IN_W)),
        "ret_norm_gain": gain(ks[3], (N_A, RET_V_W)),
        "w_out_a": w(ks[4], (N_A, A_MIX_W, D_MODEL)),
        "kv_norm_gain": gain(ks[5], (D_MODEL,)),
        "w_kv_shared": w(ks[6], (D_MODEL, 2 * MOBA_W)),
        "w_in_b": w(ks[7], (N_B, D_MODEL, B_IN_W)),
        "w_out_b": w(ks[8], (N_B, B_MIX_W, D_MODEL)),
        "w_mem_kv": w(ks[9], (DEPTH, D_MODEL, 2 * MEM_W)),
        "norm_pre_mix": gain(ks[10], (DEPTH, D_MODEL)),
        "norm_post_mix": gain(ks[11], (DEPTH, D_MODEL)),
        "norm_pre_mlp": gain(ks[12], (DEPTH, D_MODEL)),
        "norm_post_mlp": gain(ks[13], (DEPTH, D_MODEL)),
        "w_up": w(ks[14], (DEPTH, D_MODEL, D_FF)),
        "w_down": w(ks[15], (DEPTH, D_FF, D_MODEL)),
    }


def reference(x, mem, w_in_a, ret_norm_gain, w_out_a, kv_norm_gain, w_kv_shared, w_in_b, w_out_b,
              w_mem_kv, norm_pre_mix, norm_post_mix, norm_pre_mlp, norm_post_mlp, w_up, w_down):
    slopes = jnp.asarray(alibi_slopes(MOBA_HEADS))
    h = x
    k_sh = None
    v_sh = None
    for l in range(DEPTH):
        hn = rmsnorm(h, norm_pre_mix[l])
        if l < N_A:
            mix = retention_mixer(hn, mem, w_in_a[l], ret_norm_gain[l], w_out_a[l], w_mem_kv[l])
        else:
            if l == N_A:
                k_all, v_all = jnp.split(rmsnorm(h, kv_norm_gain) @ w_kv_shared, 2, axis=-1)
                k_sh = split_heads(k_all, MOBA_HEADS)
                v_sh = split_heads(v_all, MOBA_HEADS)
            j = l - N_A
            mix = moba_mixer(hn, mem, k_sh, v_sh, slopes, w_in_b[j], w_out_b[j], w_mem_kv[l])
        h = h + rmsnorm(mix, norm_post_mix[l])
        y = squared_relu_mlp(rmsnorm(h, norm_pre_mlp[l]), w_up[l], w_down[l])
        h = h + rmsnorm(y, norm_post_mlp[l])
    return h
```

```python
import math
from contextlib import ExitStack
import numpy as np
import concourse.bass as bass
import concourse.mybir as mybir
from concourse.alu_op_type import AluOpType as ALU
from concourse.bass_utils import run_bass_kernel_spmd

F32 = mybir.dt.float32
BF16 = mybir.dt.bfloat16
AF = mybir.ActivationFunctionType
AX = mybir.AxisListType

D = 1024
S = 2048
T = 1024
NT = 8
NH = 2
DFF = 4096
EPS = 1e-6
NRING = 3
ENGS = ("pe", "act", "dve", "pool", "sp")


def alibi_slopes(n):
    def pow2(m):
        return [2.0 ** (-8.0 * (i + 1) / m) for i in range(m)]
    p = 2 ** int(math.floor(math.log2(n)))
    s = pow2(p)
    if p < n:
        s = s + pow2(2 * p)[0::2][: n - p]
    return np.asarray(s, dtype=np.float64)


class Ins:
    __slots__ = ("eng", "fn", "waits", "stream", "sidx", "milestone", "count", "is_dma")


class Sched:
    def __init__(self):
        self.cells = {}
        self.eng_list = {e: [] for e in ENGS}
        self.streams = {}
        self.seen = {e: {} for e in ENGS}

    def add(self, eng, fn, reads=(), writes=(), dma=None):
        ins = Ins()
        ins.eng = eng
        ins.fn = fn
        ins.is_dma = dma is not None
        ins.stream = dma if dma else eng
        need = {}

        def dep(d, war):
            if d is None:
                return
            if (not ins.is_dma) and (not d.is_dma) and d.eng == eng:
                if eng == "pe" or war:
                    return
            if need.get(d.stream, -1) < d.sidx:
                need[d.stream] = d.sidx

        for c in reads:
            cell = self.cells.get(c)
            if cell:
                dep(cell[0], False)
        for c in writes:
            cell = self.cells.get(c)
            if cell:
                dep(cell[0], False)
                for r in cell[1].values():
                    dep(r, True)
        waits = []
        seen = self.seen[eng]
        for s, i in need.items():
            if seen.get(s, -1) >= i:
                continue
            seen[s] = i
            waits.append((s, i))
            self.streams[s][i].milestone = True
        ins.waits = waits
        lst = self.streams.setdefault(ins.stream, [])
        ins.sidx = len(lst)
        lst.append(ins)
        ins.milestone = ins.is_dma
        self.eng_list[eng].append(ins)
        for c in writes:
            self.cells[c] = [ins, {}]
        for c in reads:
            cell = self.cells.setdefault(c, [None, {}])
            cell[1][ins.stream] = ins
        return ins

    def emit(self, nc, es):
        sems = {}
        for s, lst in self.streams.items():
            sems[s] = es.enter_context(nc.semaphore("s_" + s))
            c = 0
            for ins in lst:
                if ins.is_dma:
                    c += 16
                elif ins.milestone:
                    c += 1
                ins.count = c
        streams = self.streams

        def run(eng_name, eng):
            for ins in self.eng_list[eng_name]:
                for (s, i) in ins.waits:
                    eng.wait_ge(sems[s], streams[s][i].count)
                if ins.fn is not None:
                    h = ins.fn(eng)
                    if ins.is_dma:
                        h.then_inc(sems[ins.stream], 16)
                    elif ins.milestone:
                        h.then_inc(sems[ins.stream], 1)

        with nc.Block() as block:
            @block.tensor
            def _(e):
                run("pe", e)

            @block.scalar
            def _(e):
                run("act", e)

            @block.vector
            def _(e):
                run("dve", e)

            @block.gpsimd
            def _(e):
                run("pool", e)

            @block.sync
            def _(e):
                run("sp", e)


def make_consts():
    c = {}
    c["ident"] = np.eye(128, dtype=np.float32)
    k = np.arange(128)[:, None]
    q = np.arange(128)[None, :]
    tri = (q >= k).astype(np.float32)
    c["tri4"] = np.tile(tri, (1, 4)).astype(np.float32)
    hh = np.arange(4, dtype=np.float64)
    log_g = np.log1p(-np.exp2(-5.0 - hh))
    p = np.arange(128, dtype=np.float64)
    xi = np.exp(log_g[None, :] * (p[:, None] + 1.0))
    rv = np.zeros((128, 8), np.float32)
    rv[:, 0:4] = (1.0 / xi) * (128.0 ** -0.5)
    rv[:, 4:8] = EPS / (xi * xi)
    c["retv"] = rv
    c["ret_decay"] = [float(np.exp(log_g[i] * 128.0)) for i in range(4)]
    sl = alibi_slopes(12)
    ab = np.zeros((128, 12, 16), np.float32)
    for h in range(12):
        for d in range(16):
            ab[:, h, d] = sl[h] * (p - 64.0 - 128.0 * d)
    c["alibi"] = ab.reshape(128, 192)
    return c


class Builder:
    def __init__(self, stop=99):
        self.stop = stop
        self.nc = bass.Bass("TRN2", target_bir_lowering=False)
        self.sc = Sched()
        self.es = ExitStack()
        self.ps_i = 0
        self.ring_i = 0
        self.cnt = {}
        self.consts = make_consts()

    def sb(self, name, shape, dt):
        return self.es.enter_context(self.nc.sbuf_tensor(name, shape, dt))

    def dram(self, name, shape, dt=F32, kind="ExternalInput"):
        return self.nc.dram_tensor(name, shape, dt, kind=kind).ap()

    def rot(self, name, n):
        i = self.cnt.get(name, 0)
        self.cnt[name] = i + 1
        return i % n

    def psum(self):
        i = self.ps_i % 8
        self.ps_i += 1
        return self.PS[i], ("ps", i)

    def add(self, *a, **k):
        return self.sc.add(*a, **k)

    def setup(self):
        nc = self.nc
        d = self.dram
        self.x = d("x", [S, D])
        self.mem = d("mem", [256, D])
        self.w_in_a = d("w_in_a", [D, 2816])
        self.ret_gain = d("ret_norm_gain", [768])
        self.w_out_a = d("w_out_a", [D, D])
        self.kv_gain = d("kv_norm_gain", [D])
        self.w_kv = d("w_kv_shared", [D, 1536])
        self.w_in_b = d("w_in_b", [D, D])
        self.w_out_b = d("w_out_b", [D, D])
        self.w_mkv = d("w_mem_kv", [2, D, 512])
        self.g_pre_mix = d("norm_pre_mix", [2, D])
        self.g_post_mix = d("norm_post_mix", [2, D])
        self.g_pre_mlp = d("norm_pre_mlp", [2, D])
        self.g_post_mlp = d("norm_post_mlp", [2, D])
        self.w_up = d("w_up", [2, D, DFF])
        self.w_down = d("w_down", [2, DFF, D])
        self.c_ident = d("c_ident", [128, 128])
        self.c_tri4 = d("c_tri4", [128, 512])
        self.c_retv = d("c_retv", [128, 8])
        self.c_alibi = d("c_alibi", [128, 192])
        self.out = d("out", [S, D], kind="ExternalOutput")

        sb = self.sb
        self.h = sb("h", [128, NT, D], F32)
        self.KT = sb("KT", [128, 6, S], BF16)
        self.V = sb("V", [128, 16, 12, 65], BF16)
        self.ring = sb("ring", [128, NRING, 8, 512], BF16)
        self.B1 = sb("B1", [128, 8, 1024], BF16)
        self.B2 = sb("B2", [128, 8, 1024], BF16)
        self.B3 = sb("B3", [128, 8 * 1024], F32)
        self.y = self.B3[:].rearrange("p (t n) -> p t n", n=1024)
        self.B3b = self.B3[:].bitcast(BF16).rearrange("p (t n) -> p t n", n=2048)
        self.qmT = sb("qmT", [128, 2, 1024], BF16)
        self.memT = sb("memT", [128, 8, 256], BF16)
        self.mkT = sb("mkT", [128, 2, 256], BF16)
        self.mv = sb("mv", [128, 2, 4, 65], BF16)
        self.gain = sb("gain", [128, 1, 1024], F32)
        self.state = sb("state", [128, 4, 192], F32)
        self.state_bf = sb("state_bf", [128, 4, 192], BF16)
        self.tmpf = sb("tmpf", [128, 2, 512], F32)
        self.hnb = sb("hnb", [128, 1, 1024], BF16)
        self.junk = sb("junk", [128, 192], BF16)
        self.PT = sb("PT", [128, 8, 512], BF16)
        self.ident = sb("ident", [128, 128], BF16)
        self.tri4 = sb("tri4", [128, 512], BF16)
        self.retv = sb("retv", [128, 8], F32)
        self.alibi = sb("alibi", [128, 192], F32)
        self.ksum = sb("ksum", [128, 6, 8], F32)
        self.kmT = sb("kmT", [128, 6, 8], BF16)
        self.st = sb("st", [128, 64], F32)
        self.gsb = sb("gsb", [128, 12, 8], F32)
        self.top8 = sb("top8", [128, 12, 8], F32)
        self.sel = sb("sel", [128, 12, 8], F32)
        self.acc = sb("acc", [128, 4, 65], F32)
        self.PS = [self.es.enter_context(nc.psum_tensor("ps%d" % i, [128, 512], F32)) for i in range(8)]

        add = self.add
        ycell = lambda t: [("B3", t, s_) for s_ in range(8)]
        add("sp", lambda e: e.dma_start(out=self.y[:, 2, 0:128], in_=self.c_ident), writes=ycell(2), dma="d_c0")
        add("sp", lambda e: e.dma_start(out=self.y[:, 3, 0:512], in_=self.c_tri4), writes=ycell(3), dma="d_c1")
        add("sp", lambda e: e.dma_start(out=self.retv[:], in_=self.c_retv), writes=[("c", 2)], dma="d_c2")
        add("sp", lambda e: e.dma_start(out=self.alibi[:], in_=self.c_alibi), writes=[("c", 3)], dma="d_c3")
        add("dve", lambda e: e.tensor_copy(out=self.ident[:], in_=self.y[:, 2, 0:128]), reads=ycell(2), writes=[("ident",)])
        add("dve", lambda e: e.tensor_copy(out=self.tri4[:], in_=self.y[:, 3, 0:512]), reads=ycell(3), writes=[("tri4",)])
        add("dve", lambda e: e.memset(self.V[:, :, :, 64:65], 1.0), writes=[("Vones",)])
        add("dve", lambda e: e.memset(self.mv[:, :, :, 64:65], 1.0), writes=[("mvones",)])
        add("dve", lambda e: e.memset(self.state[:], 0.0), writes=[("state",)])
        add("dve", lambda e: e.memset(self.state_bf[:], 0.0), writes=[("state_bf",)])
        for mt in range(2):
            add("sp", lambda e, mt=mt: e.dma_start(out=self.y[:, mt, :], in_=self.mem[mt * 128:(mt + 1) * 128, :]),
                writes=ycell(mt), dma="d_mem%d" % mt)
            add("dve", lambda e, mt=mt: e.tensor_copy(out=self.hnb[:, 0, :], in_=self.y[:, mt, :]),
                reads=ycell(mt), writes=[("hnb", 0)])
            self.transpose8(self.hnb[:, 0, :], [("hnb", 0)], self.memT[:, :, mt * 128:(mt + 1) * 128], [("memT", mt)])

    def transpose8(self, src, src_cells, dst, dst_cells, eng="act"):
        ps, pc = self.psum()
        psb = ps[:].bitcast(BF16)
        for kc in range(8):
            self.add("pe", lambda e, kc=kc: e.transpose(out=psb[:, kc * 128:(kc + 1) * 128],
                                                        in_=src[:, kc * 128:(kc + 1) * 128], identity=self.ident[:]),
                     reads=list(src_cells) + [("ident",)], writes=[pc])
        pin = psb[:, 0:1024].rearrange("p (k c) -> p k c", c=128)
        if eng == "act":
            self.add("act", lambda e: e.copy(out=dst, in_=pin), reads=[pc], writes=dst_cells)
        else:
            self.add("dve", lambda e: e.tensor_copy(out=dst, in_=pin), reads=[pc], writes=dst_cells)

    def load_slab(self, wap, ncols=512):
        r = self.ring_i % NRING
        self.ring_i += 1
        src = wap.rearrange("(kc p) n -> p kc n", p=128)
        self.add("pool", lambda e: e.dma_start(out=self.ring[:, r, :, 0:ncols], in_=src),
                 writes=[("ring", r)], dma="d_ring%d" % r)
        return r

    def load_gain(self, gap, n=1024):
        sl = 0
        self.add("sp", lambda e: e.dma_start(out=self.gain[:, sl, 0:n], in_=gap.partition_broadcast(128)),
                 writes=[("gain", sl)], dma="d_gain%d" % sl)
        return sl

    def stat(self):
        i = self.rot("st", 16)
        return self.st[:, i * 4:(i + 1) * 4], ("st", i)

    def norm_T(self, src, src_cells, gsl, dstT_ap, dst_cells):
        add = self.add
        st, stc = self.stat()
        sl = 0
        add("act", lambda e: e.activation(out=self.hnb[:, sl, :], in_=src, func=AF.Square, accum_out=st[:, 0:1]),
            reads=src_cells, writes=[stc, ("hnb", sl)])
        add("act", lambda e: e.activation(out=st[:, 1:2], in_=st[:, 0:1], func=AF.Sqrt, bias=self.epsc[:, 0:1], scale=1.0 / D),
            reads=[stc, ("epsc",)], writes=[stc])
        add("dve", lambda e: e.reciprocal(out=st[:, 2:3], in_=st[:, 1:2]), reads=[stc], writes=[stc])
        add("dve", lambda e: e.scalar_tensor_tensor(out=self.hnb[:, sl, :], in0=src, scalar=st[:, 2:3],
                                                    in1=self.gain[:, gsl, :], op0=ALU.mult, op1=ALU.mult),
            reads=list(src_cells) + [stc, ("gain", gsl)], writes=[("hnb", sl)])
        self.transpose8(self.hnb[:, sl, :], [("hnb", sl)], dstT_ap, dst_cells)

    def post_res(self, t, gsl):
        add = self.add
        st, stc = self.stat()
        ycells = [("B3", t, s) for s in range(8)]
        add("act", lambda e: e.activation(out=self.hnb[:, 0, :], in_=self.y[:, t, :], func=AF.Square, accum_out=st[:, 0:1]),
            reads=ycells, writes=[stc, ("hnb", 0)])
        add("act", lambda e: e.activation(out=st[:, 1:2], in_=st[:, 0:1], func=AF.Sqrt, bias=self.epsc[:, 0:1], scale=1.0 / D),
            reads=[stc, ("epsc",)], writes=[stc])
        add("dve", lambda e: e.reciprocal(out=st[:, 2:3], in_=st[:, 1:2]), reads=[stc], writes=[stc])
        add("dve", lambda e: e.scalar_tensor_tensor(out=self.y[:, t, :], in0=self.y[:, t, :], scalar=st[:, 2:3],
                                                    in1=self.gain[:, gsl, :], op0=ALU.mult, op1=ALU.mult),
            reads=ycells + [stc, ("gain", gsl)], writes=ycells)
        add("dve", lambda e: e.tensor_tensor(out=self.h[:, t, :], in0=self.h[:, t, :], in1=self.y[:, t, :], op=ALU.add),
            reads=[("h", t)] + ycells, writes=[("h", t)])

    def mm_B(self, r, j, xT, xcells_fn, grp, ncol=512, nk=8):
        ps, pc = self.psum()
        for kc in range(nk):
            self.add("pe", lambda e, kc=kc: e.matmul(ps[:, 0:ncol], lhsT=self.ring[:, r, kc, j * 128:(j + 1) * 128],
                                                     rhs=xT[:, kc, grp * ncol:(grp + 1) * ncol],
                                                     start=(kc == 0), stop=(kc == nk - 1)),
                     reads=[("ring", r)] + xcells_fn(kc, grp), writes=[pc])
        return ps, pc

    def mm_A(self, r, c0, n, xT, xcells_fn, t, nk=8):
        ps, pc = self.psum()
        for kc in range(nk):
            self.add("pe", lambda e, kc=kc: e.matmul(ps[:, 0:n], lhsT=xT[:, kc, t * 128:(t + 1) * 128],
                                                     rhs=self.ring[:, r, kc, c0:c0 + n],
                                                     start=(kc == 0), stop=(kc == nk - 1)),
                     reads=[("ring", r)] + xcells_fn(kc, t), writes=[pc])
        return ps, pc

    @staticmethod
    def cellsB1_grp(kc, grp):
        return [("B1", kc, grp * 4 + i) for i in range(4)]

    @staticmethod
    def cellsB1_t(kc, t):
        return [("B1", kc, t)]

    @staticmethod
    def cellsB2_grp(kc, grp):
        return [("B2", kc, grp * 4 + i) for i in range(4)]

    @staticmethod
    def cellsB2_t(kc, t):
        return [("B2", kc, t)]

    def mem_kv(self, l):
        add = self.add
        r = self.load_slab(self.w_mkv[l])
        for j in range(2):
            ps, pc = self.psum()
            for kc in range(8):
                add("pe", lambda e, kc=kc, ps=ps, j=j: e.matmul(ps[:, 0:256], lhsT=self.ring[:, r, kc, j * 128:(j + 1) * 128],
                                                               rhs=self.memT[:, kc, :], start=(kc == 0), stop=(kc == 7)),
                    reads=[("ring", r), ("memT", 0), ("memT", 1)], writes=[pc])
            add("act", lambda e, ps=ps, j=j: e.copy(out=self.mkT[:, j, :], in_=ps[:, 0:256]), reads=[pc], writes=[("mkT", j)])
        for mt in range(2):
            ps, pc = self.psum()
            for kc in range(8):
                add("pe", lambda e, kc=kc, ps=ps, mt=mt: e.matmul(ps[:, 0:256], lhsT=self.memT[:, kc, mt * 128:(mt + 1) * 128],
                                                                 rhs=self.ring[:, r, kc, 256:512], start=(kc == 0), stop=(kc == 7)),
                    reads=[("ring", r), ("memT", mt)], writes=[pc])
            add("act", lambda e, ps=ps, mt=mt: e.copy(out=self.mv[:, mt, :, 0:64],
                                                     in_=ps[:, 0:256].rearrange("p (h c) -> p h c", c=64)),
                reads=[pc, ("mvones",)], writes=[("mv", mt)])

    def mem_attn(self, t):
        add = self.add
        pts = []
        for hh in range(2):
            ps, pc = self.psum()
            po = hh * 64
            for half in range(2):
                for mt in range(2):
                    sl = half * 2 + mt
                    add("pe", lambda e, ps=ps, sl=sl, po=po, half=half, mt=mt: e.matmul(
                        ps[:, sl * 128:(sl + 1) * 128], lhsT=self.mkT[po:po + 64, half, mt * 128:(mt + 1) * 128],
                        rhs=self.qmT[po:po + 64, half, t * 128:(t + 1) * 128], start=True, stop=True),
                        reads=[("mkT", half), ("qmT", half, t)], writes=[pc])
            psl = self.rot("PT", 8)
            add("act", lambda e, ps=ps, psl=psl: e.activation(out=self.PT[:, psl, :], in_=ps[:, 0:512], func=AF.Exp, scale=0.125),
                reads=[pc], writes=[("PT", psl)])
            pts.append(psl)
        pso, poc = self.psum()
        for hd in range(4):
            psl = pts[hd % 2]
            for mt in range(2):
                sl = (hd // 2) * 2 + mt
                add("pe", lambda e, hd=hd, psl=psl, sl=sl, mt=mt: e.matmul(
                    pso[:, hd * 65:(hd + 1) * 65], lhsT=self.PT[:, psl, sl * 128:(sl + 1) * 128],
                    rhs=self.mv[:, mt, hd, :], start=(mt == 0), stop=(mt == 1)),
                    reads=[("PT", psl), ("mv", mt), ("mvones",)], writes=[poc])
        st, stc = self.stat()
        pv = pso[:, 0:260].rearrange("p (h c) -> p h c", c=65)
        add("dve", lambda e: e.reciprocal(out=st[:, 0:4], in_=pv[:, :, 64]), reads=[poc], writes=[stc])
        for hd in range(4):
            add("dve", lambda e, hd=hd: e.tensor_scalar(out=self.B1[:, t, 768 + hd * 64:768 + (hd + 1) * 64], in0=pv[:, hd, 0:64],
                                                        scalar1=st[:, hd:hd + 1], scalar2=None, op0=ALU.mult),
                reads=[poc, stc], writes=[("B1", t, 6 + hd // 2)])

    def cat_to_T(self):
        for t in range(NT):
            self.transpose8(self.B1[:, t, :], [("B1", t, j) for j in range(8)],
                            self.B2[:, :, t * 128:(t + 1) * 128], [("B2", kc, t) for kc in range(8)])

    def out_proj(self, wap, g_post):
        add = self.add
        gsl = self.load_gain(g_post)
        for s in range(2):
            r = self.load_slab(wap[:, s * 512:(s + 1) * 512])
            for t in range(NT):
                ps, pc = self.mm_A(r, 0, 512, self.B2, self.cellsB2_t, t)
                add("act", lambda e, ps=ps, t=t, s=s: e.copy(out=self.y[:, t, s * 512:(s + 1) * 512], in_=ps[:, 0:512]),
                    reads=[pc], writes=[("B3", t, s * 4 + i) for i in range(4)])
                if s == 1:
                    self.post_res(t, gsl)

    def mlp(self, l):
        add = self.add
        gsl = self.load_gain(self.g_pre_mlp[l])
        for t in range(NT):
            self.norm_T(self.h[:, t, :], [("h", t)], gsl, self.B1[:, :, t * 128:(t + 1) * 128], [("B1", kc, t) for kc in range(8)])
        gpost = self.load_gain(self.g_post_mlp[l])
        for b in range(4):
            for s in range(2):
                r = self.load_slab(self.w_up[l][:, b * 1024 + s * 512: b * 1024 + (s + 1) * 512])
                for j in range(4):
                    for grp in range(2):
                        ps, pc = self.mm_B(r, j, self.B1, self.cellsB1_grp, grp)
                        sl = self.rot("tmpf", 2)
                        add("act", lambda e, ps=ps, sl=sl: e.activation(out=self.tmpf[:, sl, 0:512], in_=ps[:, 0:512], func=AF.Relu),
                            reads=[pc], writes=[("tmpf", sl)])
                        add("dve", lambda e, ps=ps, sl=sl, s=s, j=j, grp=grp: e.tensor_tensor(
                            out=self.B2[:, s * 4 + j, grp * 512:(grp + 1) * 512], in0=self.tmpf[:, sl, 0:512], in1=ps[:, 0:512], op=ALU.mult),
                            reads=[pc, ("tmpf", sl)], writes=[("B2", s * 4 + j, grp * 4 + i) for i in range(4)])
            for s in range(2):
                r = self.load_slab(self.w_down[l][b * 1024:(b + 1) * 1024, s * 512:(s + 1) * 512])
                for t in range(NT):
                    ps, pc = self.mm_A(r, 0, 512, self.B2, self.cellsB2_t, t)
                    yc = [("B3", t, s * 4 + i) for i in range(4)]
                    if b == 0:
                        add("act", lambda e, ps=ps, t=t, s=s: e.copy(out=self.y[:, t, s * 512:(s + 1) * 512], in_=ps[:, 0:512]),
                            reads=[pc], writes=yc)
                    else:
                        add("dve", lambda e, ps=ps, t=t, s=s: e.tensor_tensor(out=self.y[:, t, s * 512:(s + 1) * 512],
                                                                             in0=self.y[:, t, s * 512:(s + 1) * 512], in1=ps[:, 0:512], op=ALU.add),
                            reads=[pc] + yc, writes=yc)
                    if b == 3 and s == 1:
                        self.post_res(t, gpost)

    def layer0_mix(self, hf):
        add = self.add
        gsl = self.load_gain(self.g_pre_mix[0])
        for t in range(NT):
            self.norm_T(self.h[:, t, :], [("h", t)], gsl, self.B1[:, :, t * 128:(t + 1) * 128], [("B1", kc, t) for kc in range(8)])
        if self.stop < 0.15: return
        self.mem_kv(0)
        if self.stop < 0.25: return
        W = self.w_in_a
        r = self.load_slab(W[:, 0:512])
        for j in range(4):
            for grp in range(2):
                ps, pc = self.mm_B(r, j, self.B1, self.cellsB1_grp, grp)
                add("act", lambda e, ps=ps, j=j, grp=grp: e.copy(out=self.B2[:, j, grp * 512:(grp + 1) * 512], in_=ps[:, 0:512]),
                    reads=[pc], writes=[("B2", j, grp * 4 + i) for i in range(4)])
        if self.stop < 0.35: return
        r = self.load_slab(W[:, 512:1024])
        for j in range(4):
            for grp in range(2):
                ps, pc = self.mm_B(r, j, self.B1, self.cellsB1_grp, grp)
                add("act", lambda e, ps=ps, j=j, grp=grp: e.copy(out=self.B2[:, 4 + j, grp * 512:(grp + 1) * 512], in_=ps[:, 0:512]),
                    reads=[pc], writes=[("B2", 4 + j, grp * 4 + i) for i in range(4)])
        for t in range(NT):
            ps, pc = self.mm_A(r, 0, 512, self.B1, self.cellsB1_t, t)
            add("dve", lambda e, ps=ps, t=t: e.tensor_copy(out=self.B3b[:, t, 0:512], in_=ps[:, 0:512]),
                reads=[pc], writes=[("B3", t, 0), ("B3", t, 1)])
        if self.stop < 0.45: return
        self.load_gain(self.ret_gain, 768)
        for si in range(2, 5):
            r = self.load_slab(W[:, si * 512:(si + 1) * 512])
            for t in range(NT):
                ps, pc = self.mm_A(r, 0, 512, self.B1, self.cellsB1_t, t)
                c0 = si * 512 - 1024
                bounds = sorted(set([c0, c0 + 512] + [b for b in range(0, 1537, 192) if c0 < b < c0 + 512]))
                for a, bnd in zip(bounds[:-1], bounds[1:]):
                    lo, hi = a - c0, bnd - c0
                    if a < 768:
                        hd = a // 192
                        add("act", lambda e, ps=ps, t=t, lo=lo, hi=hi, a=a, bnd=bnd, hd=hd: e.activation(
                            out=self.B3b[:, t, 512 + a:512 + bnd], in_=ps[:, lo:hi], func=AF.Copy, scale=self.retv[:, hd:hd + 1]),
                            reads=[pc, ("c", 2)], writes=[("B3", t, s) for s in range((512 + a) // 256, (512 + bnd - 1) // 256 + 1)])
                    else:
                        ga, gb = a - 768, bnd - 768
                        sl = self.rot("tmpf", 2)
                        add("act", lambda e, ps=ps, lo=lo, hi=hi, sl=sl: e.activation(out=self.tmpf[:, sl, 0:hi - lo], in_=ps[:, lo:hi], func=AF.Silu),
                            reads=[pc], writes=[("tmpf", sl)])
                        add("dve", lambda e, t=t, ga=ga, gb=gb, sl=sl: e.tensor_tensor(
                            out=self.B3b[:, t, 1280 + ga:1280 + gb], in0=self.tmpf[:, sl, 0:gb - ga], in1=self.gain[:, 0, ga:gb], op=ALU.mult),
                            reads=[("tmpf", sl), ("gain", 0)], writes=[("B3", t, s) for s in range((1280 + ga) // 256, (1280 + gb - 1) // 256 + 1)])
        if self.stop < 0.55: return
        r = self.load_slab(W[:, 2560:2816], ncols=256)
        for j in range(2):
            for grp in range(2):
                ps, pc = self.mm_B(r, j, self.B1, self.cellsB1_grp, grp)
                add("act", lambda e, ps=ps, j=j, grp=grp: e.copy(out=self.qmT[:, j, grp * 512:(grp + 1) * 512], in_=ps[:, 0:512]),
                    reads=[pc], writes=[("qmT", j, grp * 4 + i) for i in range(4)])
        if self.stop < 0.65: return
        dec = self.consts["ret_decay"]
        for t in range(NT):
            tok = slice(t * 128, (t + 1) * 128)
            ps, pc = self.psum()
            for hd in range(4):
                add("pe", lambda e, ps=ps, hd=hd, tok=tok: e.matmul(ps[:, hd * 128:(hd + 1) * 128], lhsT=self.B2[:, 4 + hd, tok],
                                                                   rhs=self.B2[:, hd, tok], start=True, stop=True),
                    reads=[("B2", 4 + hd, t), ("B2", hd, t)], writes=[pc])
            psl = self.rot("PT", 8)
            add("dve", lambda e, ps=ps, psl=psl: e.tensor_tensor(out=self.PT[:, psl, :], in0=ps[:, 0:512], in1=self.tri4[:], op=ALU.mult),
                reads=[pc, ("tri4",)], writes=[("PT", psl)])
            if self.stop < 0.66: continue
            vcells = [("B3", t, 2), ("B3", t, 3), ("B3", t, 4)]
            pos = []
            for pair in range(2):
                po_, poc = self.psum()
                pos.append((po_, poc))
                for hh in range(2):
                    hd = pair * 2 + hh
                    add("pe", lambda e, po_=po_, hd=hd, hh=hh, psl=psl, t=t: e.matmul(
                        po_[:, hh * 192:(hh + 1) * 192], lhsT=self.PT[:, psl, hd * 128:(hd + 1) * 128],
                        rhs=self.B3b[:, t, 512 + hd * 192:512 + (hd + 1) * 192], start=True, stop=False),
                        reads=[("PT", psl)] + vcells, writes=[poc])
                    add("pe", lambda e, po_=po_, hd=hd, hh=hh, tok=tok: e.matmul(
                        po_[:, hh * 192:(hh + 1) * 192], lhsT=self.B2[:, hd, tok], rhs=self.state_bf[:, hd, :], start=False, stop=True),
                        reads=[("B2", hd, t), ("state_bf",)], writes=[poc])
            if self.stop < 0.67: continue
            for pair in range(2):
                pk, pkc = self.psum()
                for hh in range(2):
                    hd = pair * 2 + hh
                    add("pe", lambda e, pk=pk, hd=hd, hh=hh, t=t: e.matmul(
                        pk[:, hh * 192:(hh + 1) * 192], lhsT=self.B3b[:, t, hd * 128:(hd + 1) * 128],
                        rhs=self.B3b[:, t, 512 + hd * 192:512 + (hd + 1) * 192], start=True, stop=True),
                        reads=[("B3", t, 0), ("B3", t, 1)] + vcells, writes=[pkc])
                for hh in range(2):
                    hd = pair * 2 + hh
                    add("dve", lambda e, pk=pk, hd=hd, hh=hh: e.scalar_tensor_tensor(
                        out=self.state[:, hd, :], in0=pk[:, hh * 192:(hh + 1) * 192], scalar=1.0, in1=self.state[:, hd, :],
                        op0=ALU.mult, op1=ALU.add), reads=[pkc, ("state",)], writes=[("state",)])
                    add("dve", lambda e, hd=hd: e.tensor_scalar(out=self.state[:, hd, :], in0=self.state[:, hd, :], scalar1=dec[hd],
                                                               scalar2=None, op0=ALU.mult), reads=[("state",)], writes=[("state",)])
                    add("dve", lambda e, hd=hd: e.tensor_copy(out=self.state_bf[:, hd, :], in_=self.state[:, hd, :]),
                        reads=[("state",)], writes=[("state_bf",)])
            if self.stop < 0.68: continue
            st, stc = self.stat()
            for hd in range(4):
                po_, poc = pos[hd // 2]
                hh = hd % 2
                add("act", lambda e, po_=po_, hh=hh, hd=hd: e.activation(out=self.junk[:, 0:192], in_=po_[:, hh * 192:(hh + 1) * 192],
                                                                       func=AF.Square, accum_out=st[:, hd:hd + 1]),
                    reads=[poc], writes=[stc])
            st2, stc2 = self.stat()
            add("dve", lambda e: e.scalar_tensor_tensor(out=st2[:, 0:4], in0=st[:, 0:4], scalar=1.0 / 192.0, in1=self.retv[:, 4:8],
                                                        op0=ALU.mult, op1=ALU.add), reads=[stc, ("c", 2)], writes=[stc2])
            add("act", lambda e: e.activation(out=st2[:, 0:4], in_=st2[:, 0:4], func=AF.Sqrt), reads=[stc2], writes=[stc2])
            st3, stc3 = self.stat()
            add("dve", lambda e: e.reciprocal(out=st3[:, 0:4], in_=st2[:, 0:4]), reads=[stc2], writes=[stc3])
            for hd in range(4):
                po_, poc = pos[hd // 2]
                hh = hd % 2
                c0, c1 = hd * 192, (hd + 1) * 192
                add("dve", lambda e, po_=po_, hh=hh, hd=hd, c0=c0, c1=c1, t=t: e.scalar_tensor_tensor(
                    out=self.B1[:, t, c0:c1], in0=po_[:, hh * 192:(hh + 1) * 192], scalar=st3[:, hd:hd + 1],
                    in1=self.B3b[:, t, 1280 + c0:1280 + c1], op0=ALU.mult, op1=ALU.mult),
                    reads=[poc, stc3, ("B3", t, 5), ("B3", t, 6), ("B3", t, 7)],
                    writes=[("B1", t, j) for j in range(c0 // 128, (c1 - 1) // 128 + 1)])
            if self.stop < 0.69: continue
            self.mem_attn(t)
        if self.stop < 0.75: return
        self.cat_to_T()
        if self.stop < 0.85: return
        self.out_proj(self.w_out_a, self.g_post_mix[0])

    def layer1_mix(self, hf):
        add = self.add
        gsl = self.load_gain(self.kv_gain)
        for t in range(NT):
            self.norm_T(self.h[:, t, :], [("h", t)], gsl, self.B1[:, :, t * 128:(t + 1) * 128], [("B1", kc, t) for kc in range(8)])
        W = self.w_kv
        for si in range(3):
            r = self.load_slab(W[:, si * 512:(si + 1) * 512])
            for j in range(4):
                col = si * 512 + j * 128
                if col >= 768:
                    continue
                c = col // 128
                for grp in range(2):
                    ps, pc = self.mm_B(r, j, self.B1, self.cellsB1_grp, grp)
                    for bb in range(2):
                        blk = hf * 4 + grp * 2 + bb
                        g0 = hf * T + grp * 512 + bb * 256
                        add("act", lambda e, ps=ps, c=c, g0=g0, bb=bb, blk=blk: e.activation(
                            out=self.KT[:, c, g0:g0 + 256], in_=ps[:, bb * 256:(bb + 1) * 256], func=AF.Copy,
                            accum_out=self.ksum[:, c, blk:blk + 1]),
                            reads=[pc], writes=[("KT", c, blk), ("ksum", c, blk)])
            v0 = max(si * 512, 768)
            v1 = (si + 1) * 512
            if v1 > v0:
                n = v1 - v0
                h0 = (v0 - 768) // 64
                nh = n // 64
                for t in range(NT):
                    gt = hf * NT + t
                    ps, pc = self.mm_A(r, v0 - si * 512, n, self.B1, self.cellsB1_t, t)
                    add("dve", lambda e, ps=ps, gt=gt, h0=h0, nh=nh, n=n: e.tensor_copy(
                        out=self.V[:, gt, h0:h0 + nh, 0:64], in_=ps[:, 0:n].rearrange("p (h c) -> p h c", c=64)),
                        reads=[pc, ("Vones",)], writes=[("V", gt, h0 // 4 + i) for i in range(nh // 4)])
        gsl = self.load_gain(self.g_pre_mix[1])
        for t in range(NT):
            self.norm_T(self.h[:, t, :], [("h", t)], gsl, self.B1[:, :, t * 128:(t + 1) * 128], [("B1", kc, t) for kc in range(8)])
        self.mem_kv(1)
        W = self.w_in_b
        for si in range(2):
            r = self.load_slab(W[:, si * 512:(si + 1) * 512])
            for j in range(4):
                col = si * 512 + j * 128
                for grp in range(2):
                    ps, pc = self.mm_B(r, j, self.B1, self.cellsB1_grp, grp)
                    if col < 768:
                        c = col // 128
                        add("act", lambda e, ps=ps, c=c, grp=grp: e.copy(out=self.B2[:, c, grp * 512:(grp + 1) * 512], in_=ps[:, 0:512]),
                            reads=[pc], writes=[("B2", c, grp * 4 + i) for i in range(4)])
                    else:
                        c = (col - 768) // 128
                        add("act", lambda e, ps=ps, c=c, grp=grp: e.copy(out=self.qmT[:, c, grp * 512:(grp + 1) * 512], in_=ps[:, 0:512]),
                            reads=[pc], writes=[("qmT", c, grp * 4 + i) for i in range(4)])
        if hf == 1:
            kc_all = [("ksum", c, b) for c in range(6) for b in range(8)]
            add("act", lambda e: e.activation(out=self.kmT[:], in_=self.ksum[:], func=AF.Copy, scale=1.0 / 256.0),
                reads=kc_all, writes=[("kmT",)])
        for t in range(NT):
            gt = hf * NT + t
            qb = gt // 2
            if hf == 1:
                self.moba_gate(t, qb)
            for hd in range(12):
                self.moba_head(hf, t, hd)
            self.mem_attn(t)
        self.cat_to_T()
        self.out_proj(self.w_out_b, self.g_post_mix[1])

    def moba_gate(self, t, qb):
        add = self.add
        gv = self.gsb[:].rearrange("p (c two) b -> p c two b", two=2)
        for hh in range(2):
            ps, pc = self.psum()
            po = hh * 64
            for c in range(6):
                add("pe", lambda e, ps=ps, c=c, po=po: e.matmul(ps[:, c * 8:(c + 1) * 8], lhsT=self.B2[po:po + 64, c, t * 128:(t + 1) * 128],
                                                               rhs=self.kmT[po:po + 64, c, :], start=True, stop=True),
                    reads=[("B2", c, t), ("kmT",)], writes=[pc])
            add("act", lambda e, ps=ps, hh=hh: e.copy(out=gv[:, :, hh, :], in_=ps[:, 0:48].rearrange("p (c b) -> p c b", b=8)),
                reads=[pc], writes=[("gsb",)])
        if qb < 8:
            add("dve", lambda e: e.memset(self.gsb[:, :, qb:8], -1e30), reads=[("gsb",)], writes=[("gsb",)])
        for hd in range(12):
            add("dve", lambda e, hd=hd: e.max(out=self.top8[:, hd, :], in_=self.gsb[:, hd, :]), reads=[("gsb",)], writes=[("top8", hd)])
            add("dve", lambda e, hd=hd: e.tensor_scalar(out=self.sel[:, hd, :], in0=self.gsb[:, hd, :], scalar1=self.top8[:, hd, 2:3],
                                                        scalar2=None, op0=ALU.is_ge), reads=[("gsb",), ("top8", hd)], writes=[("sel", hd)])

    def moba_head(self, hf, t, hd):
        add = self.add
        gt = hf * NT + t
        qb = gt // 2
        c, po = hd // 2, (hd % 2) * 64
        qap = self.B2[po:po + 64, c, t * 128:(t + 1) * 128]
        kts = list(range(gt + 1))
        pt_of = {}
        for i0 in range(0, len(kts), 4):
            grp = kts[i0:i0 + 4]
            ps, pc = self.psum()
            for i, kt in enumerate(grp):
                add("pe", lambda e, ps=ps, i=i, kt=kt: e.matmul(ps[:, i * 128:(i + 1) * 128], lhsT=self.KT[po:po + 64, c, kt * 128:(kt + 1) * 128],
                                                               rhs=qap, start=True, stop=True),
                    reads=[("KT", c, kt // 2), ("B2", c, t)], writes=[pc])
            psl = self.rot("PT", 8)
            for i, kt in enumerate(grp):
                dd = gt - kt
                add("act", lambda e, ps=ps, i=i, psl=psl, dd=dd: e.activation(
                    out=self.PT[:, psl, i * 128:(i + 1) * 128], in_=ps[:, i * 128:(i + 1) * 128], func=AF.Exp,
                    bias=self.alibi[:, hd * 16 + dd:hd * 16 + dd + 1], scale=0.125),
                    reads=[pc, ("c", 3)], writes=[("PT", psl)])
                if kt == gt:
                    add("dve", lambda e, psl=psl, i=i: e.tensor_tensor(out=self.PT[:, psl, i * 128:(i + 1) * 128],
                                                                       in0=self.PT[:, psl, i * 128:(i + 1) * 128], in1=self.tri4[:, 0:128], op=ALU.mult),
                        reads=[("PT", psl), ("tri4",)], writes=[("PT", psl)])
                pt_of[kt] = (psl, i)
        pso, poc = self.psum()
        asl = self.rot("acc", 4)
        if hf == 0:
            for n_, kt in enumerate(kts):
                psl, i = pt_of[kt]
                add("pe", lambda e, psl=psl, i=i, kt=kt, n_=n_: e.matmul(pso[:, 0:65], lhsT=self.PT[:, psl, i * 128:(i + 1) * 128],
                                                                        rhs=self.V[:, kt, hd, :], start=(n_ == 0), stop=(n_ == len(kts) - 1)),
                    reads=[("PT", psl), ("V", kt, hd // 4), ("Vones",)], writes=[poc])
            src = pso
            srcc = poc
            res = pso[:, 0:65]
        else:
            psB, pbc = self.psum()
            own = [kt for kt in kts if kt // 2 == qb]
            for n_, kt in enumerate(own):
                psl, i = pt_of[kt]
                add("pe", lambda e, psl=psl, i=i, kt=kt, n_=n_: e.matmul(psB[:, 0:65], lhsT=self.PT[:, psl, i * 128:(i + 1) * 128],
                                                                        rhs=self.V[:, kt, hd, :], start=(n_ == 0), stop=(n_ == len(own) - 1)),
                    reads=[("PT", psl), ("V", kt, hd // 4), ("Vones",)], writes=[pbc])
            for b in range(qb):
                for n_, kt in enumerate((2 * b, 2 * b + 1)):
                    psl, i = pt_of[kt]
                    add("pe", lambda e, psl=psl, i=i, kt=kt, n_=n_, b=b: e.matmul(pso[:, b * 65:(b + 1) * 65], lhsT=self.PT[:, psl, i * 128:(i + 1) * 128],
                                                                                 rhs=self.V[:, kt, hd, :], start=(n_ == 0), stop=(n_ == 1)),
                        reads=[("PT", psl), ("V", kt, hd // 4), ("Vones",)], writes=[poc])
            accap = self.acc[:, asl, :]
            add("act", lambda e: e.copy(out=accap, in_=psB[:, 0:65]), reads=[pbc], writes=[("acc", asl)])
            for b in range(qb):
                add("dve", lambda e, b=b: e.scalar_tensor_tensor(out=accap, in0=pso[:, b * 65:(b + 1) * 65], scalar=self.sel[:, hd, b:b + 1],
                                                                 in1=accap, op0=ALU.mult, op1=ALU.add),
                    reads=[poc, ("sel", hd), ("acc", asl)], writes=[("acc", asl)])
            res = accap
            srcc = ("acc", asl)
        st, stc = self.stat()
        add("dve", lambda e: e.reciprocal(out=st[:, 0:1], in_=res[:, 64:65]), reads=[srcc], writes=[stc])
        add("dve", lambda e: e.tensor_scalar(out=self.B1[:, t, hd * 64:(hd + 1) * 64], in0=res[:, 0:64], scalar1=st[:, 0:1],
                                             scalar2=None, op0=ALU.mult), reads=[srcc, stc], writes=[("B1", t, hd // 2)])

    def build(self):
        add = self.add
        self.setup()
        self.epsc = self.sb("epsc", [128, 1], F32)
        add("dve", lambda e: e.memset(self.epsc[:], EPS), writes=[("epsc",)])
        for hf in range(NH):
            for t in range(NT):
                gt = hf * NT + t
                add("sp", lambda e, t=t, gt=gt: e.dma_start(out=self.h[:, t, :], in_=self.x[gt * 128:(gt + 1) * 128, :]),
                    writes=[("h", t)], dma="d_h%d" % t)
            if self.stop > 0:
                self.layer0_mix(hf)
            if self.stop >= 2:
                self.mlp(0)
            if self.stop >= 3:
                self.layer1_mix(hf)
            if self.stop >= 4:
                self.mlp(1)
            for t in range(NT):
                gt = hf * NT + t
                add("sp", lambda e, t=t, gt=gt: e.dma_start(out=self.out[gt * 128:(gt + 1) * 128, :], in_=self.h[:, t, :]),
                    reads=[("h", t)], dma="d_o%d" % t)
        add("sp", None, writes=[("h", t) for t in range(NT)])
        self.sc.emit(self.nc, self.es)
        self.es.close()
        return self.nc


_CACHE = {}


def get_nc(stop=99):
    if stop not in _CACHE:
        _CACHE[stop] = Builder(stop).build()
    return _CACHE[stop]


def kernel(stop=99, ncores=8, **inputs):
    stop = float(stop)
    nc = get_nc(stop)
    cs = make_consts()
    f = lambda a: np.ascontiguousarray(np.asarray(a, dtype=np.float32))
    shared = {
        "w_in_a": f(inputs["w_in_a"][0]), "ret_norm_gain": f(inputs["ret_norm_gain"][0]), "w_out_a": f(inputs["w_out_a"][0]),
        "kv_norm_gain": f(inputs["kv_norm_gain"]), "w_kv_shared": f(inputs["w_kv_shared"]), "w_in_b": f(inputs["w_in_b"][0]),
        "w_out_b": f(inputs["w_out_b"][0]), "w_mem_kv": f(inputs["w_mem_kv"]), "norm_pre_mix": f(inputs["norm_pre_mix"]),
        "norm_post_mix": f(inputs["norm_post_mix"]), "norm_pre_mlp": f(inputs["norm_pre_mlp"]),
        "norm_post_mlp": f(inputs["norm_post_mlp"]), "w_up": f(inputs["w_up"]), "w_down": f(inputs["w_down"]),
        "c_ident": cs["ident"], "c_tri4": cs["tri4"], "c_retv": cs["retv"], "c_alibi": cs["alibi"],
    }
    x = f(inputs["x"])
    mem = f(inputs["mem"])
    in_maps = []
    for b in range(ncores):
        m = dict(shared)
        m["x"] = x[b]
        m["mem"] = mem[b]
        in_maps.append(m)
    res = run_bass_kernel_spmd(nc, in_maps, core_ids=list(range(ncores)))
    return np.stack([np.asarray(r["out"], dtype=np.float32) for r in res.results], axis=0)
```

```python
import math
from contextlib import ExitStack
import numpy as np
import concourse.bass as bass
import concourse.mybir as mybir
from concourse.alu_op_type import AluOpType as ALU
from concourse.bass_utils import run_bass_kernel_spmd

F32 = mybir.dt.float32
BF16 = mybir.dt.bfloat16
AF = mybir.ActivationFunctionType
AX = mybir.AxisListType

D = 1024
S = 2048
T = 1024
NT = 8
NH = 2
DFF = 4096
EPS = 1e-6
NRING = 3
ENGS = ("pe", "act", "dve", "pool", "sp")


def alibi_slopes(n):
    def pow2(m):
        return [2.0 ** (-8.0 * (i + 1) / m) for i in range(m)]
    p = 2 ** int(math.floor(math.log2(n)))
    s = pow2(p)
    if p < n:
        s = s + pow2(2 * p)[0::2][: n - p]
    return np.asarray(s, dtype=np.float64)


class Ins:
    __slots__ = ("eng", "fn", "waits", "stream", "sidx", "milestone", "count", "is_dma")


class Sched:
    def __init__(self):
        self.cells = {}
        self.eng_list = {e: [] for e in ENGS}
        self.streams = {}
        self.seen = {e: {} for e in ENGS}

    def add(self, eng, fn, reads=(), writes=(), dma=None):
        ins = Ins()
        ins.eng = eng
        ins.fn = fn
        ins.is_dma = dma is not None
        ins.stream = dma if dma else eng
        need = {}

        def dep(d, war):
            if d is None:
                return
            if (not ins.is_dma) and (not d.is_dma) and d.eng == eng:
                if eng == "pe" or war:
                    return
            if need.get(d.stream, -1) < d.sidx:
                need[d.stream] = d.sidx

        for c in reads:
            cell = self.cells.get(c)
            if cell:
                dep(cell[0], False)
        for c in writes:
            cell = self.cells.get(c)
            if cell:
                dep(cell[0], False)
                for r in cell[1].values():
                    dep(r, True)
        waits = []
        seen = self.seen[eng]
        for s, i in need.items():
            if seen.get(s, -1) >= i:
                continue
            seen[s] = i
            waits.append((s, i))
            self.streams[s][i].milestone = True
        ins.waits = waits
        lst = self.streams.setdefault(ins.stream, [])
        ins.sidx = len(lst)
        lst.append(ins)
        ins.milestone = ins.is_dma
        self.eng_list[eng].append(ins)
        for c in writes:
            self.cells[c] = [ins, {}]
        for c in reads:
            cell = self.cells.setdefault(c, [None, {}])
            cell[1][ins.stream] = ins
        return ins

    def emit(self, nc, es):
        sems = {}
        for s, lst in self.streams.items():
            sems[s] = es.enter_context(nc.semaphore("s_" + s))
            c = 0
            for ins in lst:
                if ins.is_dma:
                    c += 16
                elif ins.milestone:
                    c += 1
                ins.count = c
        streams = self.streams

        def run(eng_name, eng):
            for ins in self.eng_list[eng_name]:
                for (s, i) in ins.waits:
                    eng.wait_ge(sems[s], streams[s][i].count)
                if ins.fn is not None:
                    h = ins.fn(eng)
                    if ins.is_dma:
                        h.then_inc(sems[ins.stream], 16)
                    elif ins.milestone:
                        h.then_inc(sems[ins.stream], 1)

        with nc.Block() as block:
            @block.tensor
            def _(e):
                run("pe", e)

            @block.scalar
            def _(e):
                run("act", e)

            @block.vector
            def _(e):
                run("dve", e)

            @block.gpsimd
            def _(e):
                run("pool", e)

            @block.sync
            def _(e):
                run("sp", e)


def make_consts():
    c = {}
    c["ident"] = np.eye(128, dtype=np.float32)
    k = np.arange(128)[:, None]
    q = np.arange(128)[None, :]
    tri = (q >= k).astype(np.float32)
    c["tri4"] = np.tile(tri, (1, 4)).astype(np.float32)
    hh = np.arange(4, dtype=np.float64)
    log_g = np.log1p(-np.exp2(-5.0 - hh))
    p = np.arange(128, dtype=np.float64)
    xi = np.exp(log_g[None, :] * (p[:, None] + 1.0))
    rv = np.zeros((128, 8), np.float32)
    rv[:, 0:4] = (1.0 / xi) * (128.0 ** -0.5)
    rv[:, 4:8] = EPS / (xi * xi)
    c["retv"] = rv
    c["ret_decay"] = [float(np.exp(log_g[i] * 128.0)) for i in range(4)]
    sl = alibi_slopes(12)
    ab = np.zeros((128, 12, 16), np.float32)
    for h in range(12):
        for d in range(16):
            ab[:, h, d] = sl[h] * (p - 64.0 - 128.0 * d)
    c["alibi"] = ab.reshape(128, 192)
    return c


class Builder:
    def __init__(self, stop=99):
        self.stop = stop
        self.nc = bass.Bass("TRN2", target_bir_lowering=False, dynamic_dma_scratch_size=8192)
        self.sc = Sched()
        self.es = ExitStack()
        self.ps_i = 0
        self.ring_i = 0
        self.cnt = {}
        self.consts = make_consts()

    def sb(self, name, shape, dt):
        return self.es.enter_context(self.nc.sbuf_tensor(name, shape, dt))

    def dram(self, name, shape, dt=F32, kind="ExternalInput"):
        return self.nc.dram_tensor(name, shape, dt, kind=kind).ap()

    def rot(self, name, n):
        i = self.cnt.get(name, 0)
        self.cnt[name] = i + 1
        return i % n

    def psum(self):
        i = self.ps_i % 8
        self.ps_i += 1
        return self.PS[i], ("ps", i)

    def add(self, *a, **k):
        return self.sc.add(*a, **k)

    def setup(self):
        nc = self.nc
        d = self.dram
        self.x = d("x", [S, D])
        self.mem = d("mem", [256, D])
        self.w_in_a = d("w_in_a", [D, 2816])
        self.ret_gain = d("ret_norm_gain", [768])
        self.w_out_a = d("w_out_a", [D, D])
        self.kv_gain = d("kv_norm_gain", [D])
        self.w_kv = d("w_kv_shared", [D, 1536])
        self.w_in_b = d("w_in_b", [D, D])
        self.w_out_b = d("w_out_b", [D, D])
        self.w_mkv = d("w_mem_kv", [2, D, 512])
        self.g_pre_mix = d("norm_pre_mix", [2, D])
        self.g_post_mix = d("norm_post_mix", [2, D])
        self.g_pre_mlp = d("norm_pre_mlp", [2, D])
        self.g_post_mlp = d("norm_post_mlp", [2, D])
        self.w_up = d("w_up", [2, D, DFF])
        self.w_down = d("w_down", [2, DFF, D])
        self.c_ident = d("c_ident", [128, 128])
        self.c_tri4 = d("c_tri4", [128, 512])
        self.c_retv = d("c_retv", [128, 8])
        self.c_alibi = d("c_alibi", [128, 192])
        self.out = d("out", [S, D], kind="ExternalOutput")

        sb = self.sb
        self.h = sb("h", [128, NT, D], F32)
        self.KT = sb("KT", [128, 6, S], BF16)
        self.V = sb("V", [128, 16, 12, 65], BF16)
        self.ring = sb("ring", [128, NRING, 8, 512], BF16)
        self.B1 = sb("B1", [128, 8, 1024], BF16)
        self.B2 = sb("B2", [128, 8, 1024], BF16)
        self.B3 = sb("B3", [128, 8 * 1024], F32)
        self.y = self.B3[:].rearrange("p (t n) -> p t n", n=1024)
        self.B3b = self.B3[:].bitcast(BF16).rearrange("p (t n) -> p t n", n=2048)
        self.qmT = sb("qmT", [128, 2, 1024], BF16)
        self.memT = sb("memT", [128, 8, 256], BF16)
        self.mkT = sb("mkT", [128, 2, 256], BF16)
        self.mv = sb("mv", [128, 2, 4, 65], BF16)
        self.gain = sb("gain", [128, 1, 1024], F32)
        self.state = sb("state", [128, 4, 192], F32)
        self.state_bf = sb("state_bf", [128, 4, 192], BF16)
        self.tmpf = sb("tmpf", [128, 2, 512], F32)
        self.hnb = sb("hnb", [128, 1, 1024], BF16)
        self.junk = sb("junk", [128, 192], BF16)
        self.PT = sb("PT", [128, 8, 512], BF16)
        self.PTm = sb("PTm", [128, 4, 512], BF16)
        self.ident = sb("ident", [128, 128], BF16)
        self.tri4 = sb("tri4", [128, 512], BF16)
        self.retv = sb("retv", [128, 8], F32)
        self.alibi = sb("alibi", [128, 192], F32)
        self.ksum = sb("ksum", [128, 6, 8], F32)
        self.kmT = sb("kmT", [128, 6, 8], BF16)
        self.st = sb("st", [128, 64], F32)
        self.gsb = sb("gsb", [128, 12, 8], F32)
        self.top8 = sb("top8", [128, 12, 8], F32)
        self.sel = sb("sel", [128, 2, 12, 8], F32)
        self.acc = sb("acc", [128, 4, 65], F32)
        self.PS = [self.es.enter_context(nc.psum_tensor("ps%d" % i, [128, 512], F32)) for i in range(8)]

        add = self.add
        ycell = lambda t: [("B3", t, s_) for s_ in range(8)]
        add("sp", lambda e: e.dma_start(out=self.y[:, 2, 0:128], in_=self.c_ident), writes=ycell(2), dma="d_c0")
        add("sp", lambda e: e.dma_start(out=self.y[:, 3, 0:512], in_=self.c_tri4), writes=ycell(3), dma="d_c1")
        add("sp", lambda e: e.dma_start(out=self.retv[:], in_=self.c_retv), writes=[("c", 2)], dma="d_c2")
        add("sp", lambda e: e.dma_start(out=self.alibi[:], in_=self.c_alibi), writes=[("c", 3)], dma="d_c3")
        add("dve", lambda e: e.tensor_copy(out=self.ident[:], in_=self.y[:, 2, 0:128]), reads=ycell(2), writes=[("ident",)])
        add("dve", lambda e: e.tensor_copy(out=self.tri4[:], in_=self.y[:, 3, 0:512]), reads=ycell(3), writes=[("tri4",)])
        add("dve", lambda e: e.memset(self.V[:, :, :, 64:65], 1.0), writes=[("Vones",)])
        add("dve", lambda e: e.memset(self.mv[:, :, :, 64:65], 1.0), writes=[("mvones",)])
        add("dve", lambda e: e.memset(self.state[:], 0.0), writes=[("state",)])
        add("dve", lambda e: e.memset(self.state_bf[:], 0.0), writes=[("state_bf",)])
        for mt in range(2):
            add("sp", lambda e, mt=mt: e.dma_start(out=self.y[:, mt, :], in_=self.mem[mt * 128:(mt + 1) * 128, :]),
                writes=ycell(mt), dma="d_mem%d" % mt)
            add("dve", lambda e, mt=mt: e.tensor_copy(out=self.hnb[:, 0, :], in_=self.y[:, mt, :]),
                reads=ycell(mt), writes=[("hnb", 0)])
            self.transpose8(self.hnb[:, 0, :], [("hnb", 0)], self.memT[:, :, mt * 128:(mt + 1) * 128], [("memT", mt)])

    def transpose8(self, src, src_cells, dst, dst_cells, eng="act"):
        ps, pc = self.psum()
        psb = ps[:].bitcast(BF16)
        for kc in range(8):
            self.add("pe", lambda e, kc=kc: e.transpose(out=psb[:, kc * 128:(kc + 1) * 128],
                                                        in_=src[:, kc * 128:(kc + 1) * 128], identity=self.ident[:]),
                     reads=list(src_cells) + [("ident",)], writes=[pc])
        pin = psb[:, 0:1024].rearrange("p (k c) -> p k c", c=128)
        if eng == "act":
            self.add("act", lambda e: e.copy(out=dst, in_=pin), reads=[pc], writes=dst_cells)
        else:
            self.add("dve", lambda e: e.tensor_copy(out=dst, in_=pin), reads=[pc], writes=dst_cells)

    def load_slab(self, wap, ncols=512):
        r = self.ring_i % NRING
        self.ring_i += 1
        src = wap.rearrange("(kc p) n -> p kc n", p=128)
        self.add("pool", lambda e: e.dma_start(out=self.ring[:, r, :, 0:ncols], in_=src),
                 writes=[("ring", r)], dma="d_ring%d" % r)
        return r

    def load_gain(self, gap, n=1024):
        sl = 0
        self.add("sp", lambda e: e.dma_start(out=self.gain[:, sl, 0:n], in_=gap.partition_broadcast(128)),
                 writes=[("gain", sl)], dma="d_gain%d" % sl)
        return sl

    def stat(self):
        i = self.rot("st", 16)
        return self.st[:, i * 4:(i + 1) * 4], ("st", i)

    def norm_T(self, src, src_cells, gsl, dstT_ap, dst_cells):
        add = self.add
        st, stc = self.stat()
        sl = 0
        add("act", lambda e: e.activation(out=self.hnb[:, sl, :], in_=src, func=AF.Square, accum_out=st[:, 0:1]),
            reads=src_cells, writes=[stc, ("hnb", sl)])
        add("act", lambda e: e.activation(out=st[:, 1:2], in_=st[:, 0:1], func=AF.Sqrt, bias=self.epsc[:, 0:1], scale=1.0 / D),
            reads=[stc, ("epsc",)], writes=[stc])
        add("dve", lambda e: e.reciprocal(out=st[:, 2:3], in_=st[:, 1:2]), reads=[stc], writes=[stc])
        add("dve", lambda e: e.scalar_tensor_tensor(out=self.hnb[:, sl, :], in0=src, scalar=st[:, 2:3],
                                                    in1=self.gain[:, gsl, :], op0=ALU.mult, op1=ALU.mult),
            reads=list(src_cells) + [stc, ("gain", gsl)], writes=[("hnb", sl)])
        self.transpose8(self.hnb[:, sl, :], [("hnb", sl)], dstT_ap, dst_cells)

    def post_res(self, t, gsl):
        add = self.add
        st, stc = self.stat()
        ycells = [("B3", t, s) for s in range(8)]
        add("act", lambda e: e.activation(out=self.hnb[:, 0, :], in_=self.y[:, t, :], func=AF.Square, accum_out=st[:, 0:1]),
            reads=ycells, writes=[stc, ("hnb", 0)])
        add("act", lambda e: e.activation(out=st[:, 1:2], in_=st[:, 0:1], func=AF.Sqrt, bias=self.epsc[:, 0:1], scale=1.0 / D),
            reads=[stc, ("epsc",)], writes=[stc])
        add("dve", lambda e: e.reciprocal(out=st[:, 2:3], in_=st[:, 1:2]), reads=[stc], writes=[stc])
        add("dve", lambda e: e.scalar_tensor_tensor(out=self.y[:, t, :], in0=self.y[:, t, :], scalar=st[:, 2:3],
                                                    in1=self.gain[:, gsl, :], op0=ALU.mult, op1=ALU.mult),
            reads=ycells + [stc, ("gain", gsl)], writes=ycells)
        add("dve", lambda e: e.tensor_tensor(out=self.h[:, t, :], in0=self.h[:, t, :], in1=self.y[:, t, :], op=ALU.add),
            reads=[("h", t)] + ycells, writes=[("h", t)])

    def mm_B(self, r, j, xT, xcells_fn, grp, ncol=512, nk=8):
        ps, pc = self.psum()
        for kc in range(nk):
            self.add("pe", lambda e, kc=kc: e.matmul(ps[:, 0:ncol], lhsT=self.ring[:, r, kc, j * 128:(j + 1) * 128],
                                                     rhs=xT[:, kc, grp * ncol:(grp + 1) * ncol],
                                                     start=(kc == 0), stop=(kc == nk - 1)),
                     reads=[("ring", r)] + xcells_fn(kc, grp), writes=[pc])
        return ps, pc

    def mm_A(self, r, c0, n, xT, xcells_fn, t, nk=8):
        ps, pc = self.psum()
        for kc in range(nk):
            self.add("pe", lambda e, kc=kc: e.matmul(ps[:, 0:n], lhsT=xT[:, kc, t * 128:(t + 1) * 128],
                                                     rhs=self.ring[:, r, kc, c0:c0 + n],
                                                     start=(kc == 0), stop=(kc == nk - 1)),
                     reads=[("ring", r)] + xcells_fn(kc, t), writes=[pc])
        return ps, pc

    @staticmethod
    def cellsB1_grp(kc, grp):
        return [("B1", kc, grp * 4 + i) for i in range(4)]

    @staticmethod
    def cellsB1_t(kc, t):
        return [("B1", kc, t)]

    @staticmethod
    def cellsB2_grp(kc, grp):
        return [("B2", kc, grp * 4 + i) for i in range(4)]

    @staticmethod
    def cellsB2_t(kc, t):
        return [("B2", kc, t)]

    def mem_kv(self, l):
        add = self.add
        r = self.load_slab(self.w_mkv[l])
        for j in range(2):
            ps, pc = self.psum()
            for kc in range(8):
                add("pe", lambda e, kc=kc, ps=ps, j=j: e.matmul(ps[:, 0:256], lhsT=self.ring[:, r, kc, j * 128:(j + 1) * 128],
                                                               rhs=self.memT[:, kc, :], start=(kc == 0), stop=(kc == 7)),
                    reads=[("ring", r), ("memT", 0), ("memT", 1)], writes=[pc])
            add("act", lambda e, ps=ps, j=j: e.copy(out=self.mkT[:, j, :], in_=ps[:, 0:256]), reads=[pc], writes=[("mkT", j)])
        for mt in range(2):
            ps, pc = self.psum()
            for kc in range(8):
                add("pe", lambda e, kc=kc, ps=ps, mt=mt: e.matmul(ps[:, 0:256], lhsT=self.memT[:, kc, mt * 128:(mt + 1) * 128],
                                                                 rhs=self.ring[:, r, kc, 256:512], start=(kc == 0), stop=(kc == 7)),
                    reads=[("ring", r), ("memT", mt)], writes=[pc])
            add("act", lambda e, ps=ps, mt=mt: e.copy(out=self.mv[:, mt, :, 0:64],
                                                     in_=ps[:, 0:256].rearrange("p (h c) -> p h c", c=64)),
                reads=[pc, ("mvones",)], writes=[("mv", mt)])

    def mem_front(self, t):
        add = self.add
        pts = []
        for hh in range(2):
            ps, pc = self.psum()
            po = hh * 64
            for half in range(2):
                for mt in range(2):
                    sl = half * 2 + mt
                    add("pe", lambda e, ps=ps, sl=sl, po=po, half=half, mt=mt: e.matmul(
                        ps[:, sl * 128:(sl + 1) * 128], lhsT=self.mkT[po:po + 64, half, mt * 128:(mt + 1) * 128],
                        rhs=self.qmT[po:po + 64, half, t * 128:(t + 1) * 128], start=True, stop=True),
                        reads=[("mkT", half), ("qmT", half, t)], writes=[pc])
            psl = (t % 2) * 2 + hh
            add("act", lambda e, ps=ps, psl=psl: e.activation(out=self.PTm[:, psl, :], in_=ps[:, 0:512], func=AF.Exp, scale=0.125),
                reads=[pc], writes=[("PTm", psl)])
            pts.append(psl)
        return pts

    def mem_back(self, t, pts):
        add = self.add
        pso, poc = self.psum()
        for hd in range(4):
            psl = pts[hd % 2]
            for mt in range(2):
                sl = (hd // 2) * 2 + mt
                add("pe", lambda e, hd=hd, psl=psl, sl=sl, mt=mt: e.matmul(
                    pso[:, hd * 65:(hd + 1) * 65], lhsT=self.PTm[:, psl, sl * 128:(sl + 1) * 128],
                    rhs=self.mv[:, mt, hd, :], start=(mt == 0), stop=(mt == 1)),
                    reads=[("PTm", psl), ("mv", mt), ("mvones",)], writes=[poc])
        st, stc = self.stat()
        pv = pso[:, 0:260].rearrange("p (h c) -> p h c", c=65)
        add("dve", lambda e: e.reciprocal(out=st[:, 0:4], in_=pv[:, :, 64]), reads=[poc], writes=[stc])
        for hd in range(4):
            add("dve", lambda e, hd=hd: e.tensor_scalar(out=self.B1[:, t, 768 + hd * 64:768 + (hd + 1) * 64], in0=pv[:, hd, 0:64],
                                                        scalar1=st[:, hd:hd + 1], scalar2=None, op0=ALU.mult),
                reads=[poc, stc], writes=[("B1", t, 6 + hd // 2)])

    def mem_attn(self, t):
        self.mem_back(t, self.mem_front(t))

    def cat_to_T(self):
        for t in range(NT):
            self.transpose8(self.B1[:, t, :], [("B1", t, j) for j in range(8)],
                            self.B2[:, :, t * 128:(t + 1) * 128], [("B2", kc, t) for kc in range(8)])

    def out_proj(self, wap, g_post):
        add = self.add
        gsl = self.load_gain(g_post)
        for s in range(2):
            r = self.load_slab(wap[:, s * 512:(s + 1) * 512])
            for t in range(NT):
                ps, pc = self.mm_A(r, 0, 512, self.B2, self.cellsB2_t, t)
                add("act", lambda e, ps=ps, t=t, s=s: e.copy(out=self.y[:, t, s * 512:(s + 1) * 512], in_=ps[:, 0:512]),
                    reads=[pc], writes=[("B3", t, s * 4 + i) for i in range(4)])
                if s == 1:
                    self.post_res(t, gsl)

    def mlp(self, l):
        add = self.add
        gsl = self.load_gain(self.g_pre_mlp[l])
        for t in range(NT):
            self.norm_T(self.h[:, t, :], [("h", t)], gsl, self.B1[:, :, t * 128:(t + 1) * 128], [("B1", kc, t) for kc in range(8)])
        gpost = self.load_gain(self.g_post_mlp[l])
        for b in range(4):
            for s in range(2):
                r = self.load_slab(self.w_up[l][:, b * 1024 + s * 512: b * 1024 + (s + 1) * 512])
                for j in range(4):
                    for grp in range(2):
                        ps, pc = self.mm_B(r, j, self.B1, self.cellsB1_grp, grp)
                        sl = self.rot("tmpf", 2)
                        add("act", lambda e, ps=ps, sl=sl: e.activation(out=self.tmpf[:, sl, 0:512], in_=ps[:, 0:512], func=AF.Relu),
                            reads=[pc], writes=[("tmpf", sl)])
                        add("dve", lambda e, ps=ps, sl=sl, s=s, j=j, grp=grp: e.tensor_tensor(
                            out=self.B2[:, s * 4 + j, grp * 512:(grp + 1) * 512], in0=self.tmpf[:, sl, 0:512], in1=ps[:, 0:512], op=ALU.mult),
                            reads=[pc, ("tmpf", sl)], writes=[("B2", s * 4 + j, grp * 4 + i) for i in range(4)])
            for s in range(2):
                r = self.load_slab(self.w_down[l][b * 1024:(b + 1) * 1024, s * 512:(s + 1) * 512])
                for t in range(NT):
                    ps, pc = self.mm_A(r, 0, 512, self.B2, self.cellsB2_t, t)
                    yc = [("B3", t, s * 4 + i) for i in range(4)]
                    if b == 0:
                        add("act", lambda e, ps=ps, t=t, s=s: e.copy(out=self.y[:, t, s * 512:(s + 1) * 512], in_=ps[:, 0:512]),
                            reads=[pc], writes=yc)
                    else:
                        add("dve", lambda e, ps=ps, t=t, s=s: e.tensor_tensor(out=self.y[:, t, s * 512:(s + 1) * 512],
                                                                             in0=self.y[:, t, s * 512:(s + 1) * 512], in1=ps[:, 0:512], op=ALU.add),
                            reads=[pc] + yc, writes=yc)
                    if b == 3 and s == 1:
                        self.post_res(t, gpost)

    def layer0_mix(self, hf):
        add = self.add
        gsl = self.load_gain(self.g_pre_mix[0])
        for t in range(NT):
            self.norm_T(self.h[:, t, :], [("h", t)], gsl, self.B1[:, :, t * 128:(t + 1) * 128], [("B1", kc, t) for kc in range(8)])
        if self.stop < 0.15: return
        self.mem_kv(0)
        if self.stop < 0.25: return
        W = self.w_in_a
        r = self.load_slab(W[:, 0:512])
        for j in range(4):
            for grp in range(2):
                ps, pc = self.mm_B(r, j, self.B1, self.cellsB1_grp, grp)
                add("act", lambda e, ps=ps, j=j, grp=grp: e.copy(out=self.B2[:, j, grp * 512:(grp + 1) * 512], in_=ps[:, 0:512]),
                    reads=[pc], writes=[("B2", j, grp * 4 + i) for i in range(4)])
        if self.stop < 0.35: return
        r = self.load_slab(W[:, 512:1024])
        for j in range(4):
            for grp in range(2):
                ps, pc = self.mm_B(r, j, self.B1, self.cellsB1_grp, grp)
                add("act", lambda e, ps=ps, j=j, grp=grp: e.copy(out=self.B2[:, 4 + j, grp * 512:(grp + 1) * 512], in_=ps[:, 0:512]),
                    reads=[pc], writes=[("B2", 4 + j, grp * 4 + i) for i in range(4)])
        for t in range(NT):
            ps, pc = self.mm_A(r, 0, 512, self.B1, self.cellsB1_t, t)
            add("dve", lambda e, ps=ps, t=t: e.tensor_copy(out=self.B3b[:, t, 0:512], in_=ps[:, 0:512]),
                reads=[pc], writes=[("B3", t, 0), ("B3", t, 1)])
        if self.stop < 0.45: return
        self.load_gain(self.ret_gain, 768)
        for si in range(2, 5):
            r = self.load_slab(W[:, si * 512:(si + 1) * 512])
            for t in range(NT):
                ps, pc = self.mm_A(r, 0, 512, self.B1, self.cellsB1_t, t)
                c0 = si * 512 - 1024
                bounds = sorted(set([c0, c0 + 512] + [b for b in range(0, 1537, 192) if c0 < b < c0 + 512]))
                for a, bnd in zip(bounds[:-1], bounds[1:]):
                    lo, hi = a - c0, bnd - c0
                    if a < 768:
                        hd = a // 192
                        add("act", lambda e, ps=ps, t=t, lo=lo, hi=hi, a=a, bnd=bnd, hd=hd: e.activation(
                            out=self.B3b[:, t, 512 + a:512 + bnd], in_=ps[:, lo:hi], func=AF.Copy, scale=self.retv[:, hd:hd + 1]),
                            reads=[pc, ("c", 2)], writes=[("B3", t, s) for s in range((512 + a) // 256, (512 + bnd - 1) // 256 + 1)])
                    else:
                        ga, gb = a - 768, bnd - 768
                        sl = self.rot("tmpf", 2)
                        add("act", lambda e, ps=ps, lo=lo, hi=hi, sl=sl: e.activation(out=self.tmpf[:, sl, 0:hi - lo], in_=ps[:, lo:hi], func=AF.Silu),
                            reads=[pc], writes=[("tmpf", sl)])
                        add("dve", lambda e, t=t, ga=ga, gb=gb, sl=sl: e.tensor_tensor(
                            out=self.B3b[:, t, 1280 + ga:1280 + gb], in0=self.tmpf[:, sl, 0:gb - ga], in1=self.gain[:, 0, ga:gb], op=ALU.mult),
                            reads=[("tmpf", sl), ("gain", 0)], writes=[("B3", t, s) for s in range((1280 + ga) // 256, (1280 + gb - 1) // 256 + 1)])
        if self.stop < 0.55: return
        r = self.load_slab(W[:, 2560:2816], ncols=256)
        for j in range(2):
            for grp in range(2):
                ps, pc = self.mm_B(r, j, self.B1, self.cellsB1_grp, grp)
                add("act", lambda e, ps=ps, j=j, grp=grp: e.copy(out=self.qmT[:, j, grp * 512:(grp + 1) * 512], in_=ps[:, 0:512]),
                    reads=[pc], writes=[("qmT", j, grp * 4 + i) for i in range(4)])
        if self.stop < 0.65: return
        dec = self.consts["ret_decay"]
        def front(t):
            tok = slice(t * 128, (t + 1) * 128)
            ps, pc = self.psum()
            for hd in range(4):
                add("pe", lambda e, ps=ps, hd=hd, tok=tok: e.matmul(ps[:, hd * 128:(hd + 1) * 128], lhsT=self.B2[:, 4 + hd, tok],
                                                                   rhs=self.B2[:, hd, tok], start=True, stop=True),
                    reads=[("B2", 4 + hd, t), ("B2", hd, t)], writes=[pc])
            psl = self.rot("PT", 8)
            add("dve", lambda e, ps=ps, psl=psl: e.tensor_tensor(out=self.PT[:, psl, :], in0=ps[:, 0:512], in1=self.tri4[:], op=ALU.mult),
                reads=[pc, ("tri4",)], writes=[("PT", psl)])
            return psl, self.mem_front(t)

        def back(t, psl, mf):
            tok = slice(t * 128, (t + 1) * 128)
            vcells = [("B3", t, 2), ("B3", t, 3), ("B3", t, 4)]
            pos = []
            for pair in range(2):
                po_, poc = self.psum()
                pos.append((po_, poc))
                for hh in range(2):
                    hd = pair * 2 + hh
                    add("pe", lambda e, po_=po_, hd=hd, hh=hh, psl=psl, t=t: e.matmul(
                        po_[:, hh * 192:(hh + 1) * 192], lhsT=self.PT[:, psl, hd * 128:(hd + 1) * 128],
                        rhs=self.B3b[:, t, 512 + hd * 192:512 + (hd + 1) * 192], start=True, stop=False),
                        reads=[("PT", psl)] + vcells, writes=[poc])
                    add("pe", lambda e, po_=po_, hd=hd, hh=hh, tok=tok: e.matmul(
                        po_[:, hh * 192:(hh + 1) * 192], lhsT=self.B2[:, hd, tok], rhs=self.state_bf[:, hd, :], start=False, stop=True),
                        reads=[("B2", hd, t), ("state_bf",)], writes=[poc])
            for pair in range(2):
                pk, pkc = self.psum()
                for hh in range(2):
                    hd = pair * 2 + hh
                    add("pe", lambda e, pk=pk, hd=hd, hh=hh, t=t: e.matmul(
                        pk[:, hh * 192:(hh + 1) * 192], lhsT=self.B3b[:, t, hd * 128:(hd + 1) * 128],
                        rhs=self.B3b[:, t, 512 + hd * 192:512 + (hd + 1) * 192], start=True, stop=True),
                        reads=[("B3", t, 0), ("B3", t, 1)] + vcells, writes=[pkc])
                for hh in range(2):
                    hd = pair * 2 + hh
                    add("dve", lambda e, pk=pk, hd=hd, hh=hh: e.scalar_tensor_tensor(
                        out=self.state[:, hd, :], in0=pk[:, hh * 192:(hh + 1) * 192], scalar=1.0, in1=self.state[:, hd, :],
                        op0=ALU.mult, op1=ALU.add), reads=[pkc, ("state",)], writes=[("state",)])
                    add("dve", lambda e, hd=hd: e.tensor_scalar(out=self.state[:, hd, :], in0=self.state[:, hd, :], scalar1=dec[hd],
                                                               scalar2=None, op0=ALU.mult), reads=[("state",)], writes=[("state",)])
                    add("dve", lambda e, hd=hd: e.tensor_copy(out=self.state_bf[:, hd, :], in_=self.state[:, hd, :]),
                        reads=[("state",)], writes=[("state_bf",)])
            st, stc = self.stat()
            for hd in range(4):
                po_, poc = pos[hd // 2]
                hh = hd % 2
                add("act", lambda e, po_=po_, hh=hh, hd=hd: e.activation(out=self.junk[:, 0:192], in_=po_[:, hh * 192:(hh + 1) * 192],
                                                                       func=AF.Square, accum_out=st[:, hd:hd + 1]),
                    reads=[poc], writes=[stc])
            st2, stc2 = self.stat()
            add("dve", lambda e: e.scalar_tensor_tensor(out=st2[:, 0:4], in0=st[:, 0:4], scalar=1.0 / 192.0, in1=self.retv[:, 4:8],
                                                        op0=ALU.mult, op1=ALU.add), reads=[stc, ("c", 2)], writes=[stc2])
            add("act", lambda e: e.activation(out=st2[:, 0:4], in_=st2[:, 0:4], func=AF.Sqrt), reads=[stc2], writes=[stc2])
            st3, stc3 = self.stat()
            add("dve", lambda e: e.reciprocal(out=st3[:, 0:4], in_=st2[:, 0:4]), reads=[stc2], writes=[stc3])
            for hd in range(4):
                po_, poc = pos[hd // 2]
                hh = hd % 2
                c0, c1 = hd * 192, (hd + 1) * 192
                add("dve", lambda e, po_=po_, hh=hh, hd=hd, c0=c0, c1=c1, t=t: e.scalar_tensor_tensor(
                    out=self.B1[:, t, c0:c1], in0=po_[:, hh * 192:(hh + 1) * 192], scalar=st3[:, hd:hd + 1],
                    in1=self.B3b[:, t, 1280 + c0:1280 + c1], op0=ALU.mult, op1=ALU.mult),
                    reads=[poc, stc3, ("B3", t, 5), ("B3", t, 6), ("B3", t, 7)],
                    writes=[("B1", t, j) for j in range(c0 // 128, (c1 - 1) // 128 + 1)])
            self.mem_back(t, mf)

        f = front(0)
        for t in range(NT):
            fn = front(t + 1) if t + 1 < NT else None
            back(t, *f)
            f = fn
        if self.stop < 0.75: return
        self.cat_to_T()
        if self.stop < 0.85: return
        self.out_proj(self.w_out_a, self.g_post_mix[0])

    def layer1_mix(self, hf):
        add = self.add
        gsl = self.load_gain(self.kv_gain)
        for t in range(NT):
            self.norm_T(self.h[:, t, :], [("h", t)], gsl, self.B1[:, :, t * 128:(t + 1) * 128], [("B1", kc, t) for kc in range(8)])
        W = self.w_kv
        for si in range(3):
            r = self.load_slab(W[:, si * 512:(si + 1) * 512])
            for j in range(4):
                col = si * 512 + j * 128
                if col >= 768:
                    continue
                c = col // 128
                for grp in range(2):
                    ps, pc = self.mm_B(r, j, self.B1, self.cellsB1_grp, grp)
                    for bb in range(2):
                        blk = hf * 4 + grp * 2 + bb
                        g0 = hf * T + grp * 512 + bb * 256
                        add("act", lambda e, ps=ps, c=c, g0=g0, bb=bb, blk=blk: e.activation(
                            out=self.KT[:, c, g0:g0 + 256], in_=ps[:, bb * 256:(bb + 1) * 256], func=AF.Copy,
                            accum_out=self.ksum[:, c, blk:blk + 1]),
                            reads=[pc], writes=[("KT", c, blk), ("ksum", c, blk)])
            v0 = max(si * 512, 768)
            v1 = (si + 1) * 512
            if v1 > v0:
                n = v1 - v0
                h0 = (v0 - 768) // 64
                nh = n // 64
                for t in range(NT):
                    gt = hf * NT + t
                    ps, pc = self.mm_A(r, v0 - si * 512, n, self.B1, self.cellsB1_t, t)
                    add("dve", lambda e, ps=ps, gt=gt, h0=h0, nh=nh, n=n: e.tensor_copy(
                        out=self.V[:, gt, h0:h0 + nh, 0:64], in_=ps[:, 0:n].rearrange("p (h c) -> p h c", c=64)),
                        reads=[pc, ("Vones",)], writes=[("V", gt, h0 // 4 + i) for i in range(nh // 4)])
        gsl = self.load_gain(self.g_pre_mix[1])
        for t in range(NT):
            self.norm_T(self.h[:, t, :], [("h", t)], gsl, self.B1[:, :, t * 128:(t + 1) * 128], [("B1", kc, t) for kc in range(8)])
        self.mem_kv(1)
        W = self.w_in_b
        for si in range(2):
            r = self.load_slab(W[:, si * 512:(si + 1) * 512])
            for j in range(4):
                col = si * 512 + j * 128
                for grp in range(2):
                    ps, pc = self.mm_B(r, j, self.B1, self.cellsB1_grp, grp)
                    if col < 768:
                        c = col // 128
                        add("act", lambda e, ps=ps, c=c, grp=grp: e.copy(out=self.B2[:, c, grp * 512:(grp + 1) * 512], in_=ps[:, 0:512]),
                            reads=[pc], writes=[("B2", c, grp * 4 + i) for i in range(4)])
                    else:
                        c = (col - 768) // 128
                        add("act", lambda e, ps=ps, c=c, grp=grp: e.copy(out=self.qmT[:, c, grp * 512:(grp + 1) * 512], in_=ps[:, 0:512]),
                            reads=[pc], writes=[("qmT", c, grp * 4 + i) for i in range(4)])
        if hf == 1:
            kc_all = [("ksum", c, b) for c in range(6) for b in range(8)]
            add("act", lambda e: e.activation(out=self.kmT[:], in_=self.ksum[:], func=AF.Copy, scale=1.0 / 256.0),
                reads=kc_all, writes=[("kmT",)])
        prev = None
        for t in range(NT):
            gt = hf * NT + t
            qb = gt // 2
            if hf == 1:
                self.moba_gate(t, qb)
            mf = self.mem_front(t)
            for hd in range(12):
                cur = self.moba_front(hf, t, hd)
                if prev is not None:
                    self.moba_back(*prev)
                prev = (hf, t, hd, cur)
            self.mem_back(t, mf)
        self.moba_back(*prev)
        self.cat_to_T()
        self.out_proj(self.w_out_b, self.g_post_mix[1])

    def moba_gate(self, t, qb):
        add = self.add
        gv = self.gsb[:].rearrange("p (c two) b -> p c two b", two=2)
        for hh in range(2):
            ps, pc = self.psum()
            po = hh * 64
            for c in range(6):
                add("pe", lambda e, ps=ps, c=c, po=po: e.matmul(ps[:, c * 8:(c + 1) * 8], lhsT=self.B2[po:po + 64, c, t * 128:(t + 1) * 128],
                                                               rhs=self.kmT[po:po + 64, c, :], start=True, stop=True),
                    reads=[("B2", c, t), ("kmT",)], writes=[pc])
            add("act", lambda e, ps=ps, hh=hh: e.copy(out=gv[:, :, hh, :], in_=ps[:, 0:48].rearrange("p (c b) -> p c b", b=8)),
                reads=[pc], writes=[("gsb",)])
        if qb < 8:
            add("dve", lambda e: e.memset(self.gsb[:, :, qb:8], -1e30), reads=[("gsb",)], writes=[("gsb",)])
        for hd in range(12):
            add("dve", lambda e, hd=hd: e.max(out=self.top8[:, hd, :], in_=self.gsb[:, hd, :]), reads=[("gsb",)], writes=[("top8", hd)])
            add("dve", lambda e, hd=hd: e.tensor_scalar(out=self.sel[:, t % 2, hd, :], in0=self.gsb[:, hd, :], scalar1=self.top8[:, hd, 2:3],
                                                        scalar2=None, op0=ALU.is_ge), reads=[("gsb",), ("top8", hd)], writes=[("sel", t % 2, hd)])

    def moba_front(self, hf, t, hd):
        add = self.add
        gt = hf * NT + t
        qb = gt // 2
        c, po = hd // 2, (hd % 2) * 64
        qap = self.B2[po:po + 64, c, t * 128:(t + 1) * 128]
        kts = list(range(gt + 1))
        pt_of = {}
        for i0 in range(0, len(kts), 4):
            grp = kts[i0:i0 + 4]
            ps, pc = self.psum()
            for i, kt in enumerate(grp):
                add("pe", lambda e, ps=ps, i=i, kt=kt: e.matmul(ps[:, i * 128:(i + 1) * 128], lhsT=self.KT[po:po + 64, c, kt * 128:(kt + 1) * 128],
                                                               rhs=qap, start=True, stop=True),
                    reads=[("KT", c, kt // 2), ("B2", c, t)], writes=[pc])
            psl = self.rot("PT", 8)
            for i, kt in enumerate(grp):
                dd = gt - kt
                add("act", lambda e, ps=ps, i=i, psl=psl, dd=dd: e.activation(
                    out=self.PT[:, psl, i * 128:(i + 1) * 128], in_=ps[:, i * 128:(i + 1) * 128], func=AF.Exp,
                    bias=self.alibi[:, hd * 16 + dd:hd * 16 + dd + 1], scale=0.125),
                    reads=[pc, ("c", 3)], writes=[("PT", psl)])
                if kt == gt:
                    add("dve", lambda e, psl=psl, i=i: e.tensor_tensor(out=self.PT[:, psl, i * 128:(i + 1) * 128],
                                                                       in0=self.PT[:, psl, i * 128:(i + 1) * 128], in1=self.tri4[:, 0:128], op=ALU.mult),
                        reads=[("PT", psl), ("tri4",)], writes=[("PT", psl)])
                pt_of[kt] = (psl, i)
        return pt_of

    def moba_back(self, hf, t, hd, pt_of):
        add = self.add
        gt = hf * NT + t
        qb = gt // 2
        kts = list(range(gt + 1))
        pso, poc = self.psum()
        asl = self.rot("acc", 4)
        if hf == 0:
            for n_, kt in enumerate(kts):
                psl, i = pt_of[kt]
                add("pe", lambda e, psl=psl, i=i, kt=kt, n_=n_: e.matmul(pso[:, 0:65], lhsT=self.PT[:, psl, i * 128:(i + 1) * 128],
                                                                        rhs=self.V[:, kt, hd, :], start=(n_ == 0), stop=(n_ == len(kts) - 1)),
                    reads=[("PT", psl), ("V", kt, hd // 4), ("Vones",)], writes=[poc])
            src = pso
            srcc = poc
            res = pso[:, 0:65]
        else:
            psB, pbc = self.psum()
            own = [kt for kt in kts if kt // 2 == qb]
            for n_, kt in enumerate(own):
                psl, i = pt_of[kt]
                add("pe", lambda e, psl=psl, i=i, kt=kt, n_=n_: e.matmul(psB[:, 0:65], lhsT=self.PT[:, psl, i * 128:(i + 1) * 128],
                                                                        rhs=self.V[:, kt, hd, :], start=(n_ == 0), stop=(n_ == len(own) - 1)),
                    reads=[("PT", psl), ("V", kt, hd // 4), ("Vones",)], writes=[pbc])
            for b in range(qb):
                for n_, kt in enumerate((2 * b, 2 * b + 1)):
                    psl, i = pt_of[kt]
                    add("pe", lambda e, psl=psl, i=i, kt=kt, n_=n_, b=b: e.matmul(pso[:, b * 65:(b + 1) * 65], lhsT=self.PT[:, psl, i * 128:(i + 1) * 128],
                                                                                 rhs=self.V[:, kt, hd, :], start=(n_ == 0), stop=(n_ == 1)),
                        reads=[("PT", psl), ("V", kt, hd // 4), ("Vones",)], writes=[poc])
            accap = self.acc[:, asl, :]
            add("act", lambda e: e.copy(out=accap, in_=psB[:, 0:65]), reads=[pbc], writes=[("acc", asl)])
            for b in range(qb):
                add("dve", lambda e, b=b: e.scalar_tensor_tensor(out=accap, in0=pso[:, b * 65:(b + 1) * 65], scalar=self.sel[:, t % 2, hd, b:b + 1],
                                                                 in1=accap, op0=ALU.mult, op1=ALU.add),
                    reads=[poc, ("sel", t % 2, hd), ("acc", asl)], writes=[("acc", asl)])
            res = accap
            srcc = ("acc", asl)
        st, stc = self.stat()
        add("dve", lambda e: e.reciprocal(out=st[:, 0:1], in_=res[:, 64:65]), reads=[srcc], writes=[stc])
        add("dve", lambda e: e.tensor_scalar(out=self.B1[:, t, hd * 64:(hd + 1) * 64], in0=res[:, 0:64], scalar1=st[:, 0:1],
                                             scalar2=None, op0=ALU.mult), reads=[srcc, stc], writes=[("B1", t, hd // 2)])

    def build(self):
        add = self.add
        self.setup()
        self.epsc = self.sb("epsc", [128, 1], F32)
        add("dve", lambda e: e.memset(self.epsc[:], EPS), writes=[("epsc",)])
        for hf in range(NH):
            for t in range(NT):
                gt = hf * NT + t
                add("sp", lambda e, t=t, gt=gt: e.dma_start(out=self.h[:, t, :], in_=self.x[gt * 128:(gt + 1) * 128, :]),
                    writes=[("h", t)], dma="d_h%d" % t)
            if self.stop > 0:
                self.layer0_mix(hf)
            if self.stop >= 2:
                self.mlp(0)
            if self.stop >= 3:
                self.layer1_mix(hf)
            if self.stop >= 4:
                self.mlp(1)
            for t in range(NT):
                gt = hf * NT + t
                add("sp", lambda e, t=t, gt=gt: e.dma_start(out=self.out[gt * 128:(gt + 1) * 128, :], in_=self.h[:, t, :]),
                    reads=[("h", t)], dma="d_o%d" % t)
        add("sp", None, writes=[("h", t) for t in range(NT)])
        self.sc.emit(self.nc, self.es)
        self.es.close()
        return self.nc


_CACHE = {}


def get_nc(stop=99):
    if stop not in _CACHE:
        _CACHE[stop] = Builder(stop).build()
    return _CACHE[stop]


def kernel(stop=99, ncores=8, **inputs):
    stop = float(stop)
    nc = get_nc(stop)
    cs = make_consts()
    f = lambda a: np.ascontiguousarray(np.asarray(a, dtype=np.float32))
    shared = {
        "w_in_a": f(inputs["w_in_a"][0]), "ret_norm_gain": f(inputs["ret_norm_gain"][0]), "w_out_a": f(inputs["w_out_a"][0]),
        "kv_norm_gain": f(inputs["kv_norm_gain"]), "w_kv_shared": f(inputs["w_kv_shared"]), "w_in_b": f(inputs["w_in_b"][0]),
        "w_out_b": f(inputs["w_out_b"][0]), "w_mem_kv": f(inputs["w_mem_kv"]), "norm_pre_mix": f(inputs["norm_pre_mix"]),
        "norm_post_mix": f(inputs["norm_post_mix"]), "norm_pre_mlp": f(inputs["norm_pre_mlp"]),
        "norm_post_mlp": f(inputs["norm_post_mlp"]), "w_up": f(inputs["w_up"]), "w_down": f(inputs["w_down"]),
        "c_ident": cs["ident"], "c_tri4": cs["tri4"], "c_retv": cs["retv"], "c_alibi": cs["alibi"],
    }
    x = f(inputs["x"])
    mem = f(inputs["mem"])
    in_maps = []
    for b in range(ncores):
        m = dict(shared)
        m["x"] = x[b]
        m["mem"] = mem[b]
        in_maps.append(m)
    res = run_bass_kernel_spmd(nc, in_maps, core_ids=list(range(ncores)))
    return np.stack([np.asarray(r["out"], dtype=np.float32) for r in res.results], axis=0)
```

```python
import math
from contextlib import ExitStack
import numpy as np
import concourse.bass as bass
import concourse.mybir as mybir
from concourse.alu_op_type import AluOpType as ALU
from concourse.bass_utils import run_bass_kernel_spmd

F32 = mybir.dt.float32
BF16 = mybir.dt.bfloat16
AF = mybir.ActivationFunctionType
AX = mybir.AxisListType

D = 1024
S = 2048
T = 1024
NT = 8
NH = 2
DFF = 4096
EPS = 1e-6
NRING = 3
ENGS = ("pe", "act", "dve", "pool", "sp")


def alibi_slopes(n):
    def pow2(m):
        return [2.0 ** (-8.0 * (i + 1) / m) for i in range(m)]
    p = 2 ** int(math.floor(math.log2(n)))
    s = pow2(p)
    if p < n:
        s = s + pow2(2 * p)[0::2][: n - p]
    return np.asarray(s, dtype=np.float64)


class Ins:
    __slots__ = ("eng", "fn", "waits", "stream", "sidx", "milestone", "count", "is_dma")


class Sched:
    def __init__(self):
        self.cells = {}
        self.eng_list = {e: [] for e in ENGS}
        self.streams = {}
        self.seen = {e: {} for e in ENGS}

    def add(self, eng, fn, reads=(), writes=(), dma=None):
        ins = Ins()
        ins.eng = eng
        ins.fn = fn
        ins.is_dma = dma is not None
        ins.stream = dma if dma else eng
        need = {}

        def dep(d, war):
            if d is None:
                return
            if (not ins.is_dma) and (not d.is_dma) and d.eng == eng:
                if eng == "pe" or war:
                    return
            if need.get(d.stream, -1) < d.sidx:
                need[d.stream] = d.sidx

        for c in reads:
            cell = self.cells.get(c)
            if cell:
                dep(cell[0], False)
        for c in writes:
            cell = self.cells.get(c)
            if cell:
                dep(cell[0], False)
                for r in cell[1].values():
                    dep(r, True)
        waits = []
        seen = self.seen[eng]
        for s, i in need.items():
            if seen.get(s, -1) >= i:
                continue
            seen[s] = i
            waits.append((s, i))
            self.streams[s][i].milestone = True
        ins.waits = waits
        lst = self.streams.setdefault(ins.stream, [])
        ins.sidx = len(lst)
        lst.append(ins)
        ins.milestone = ins.is_dma
        self.eng_list[eng].append(ins)
        for c in writes:
            self.cells[c] = [ins, {}]
        for c in reads:
            cell = self.cells.setdefault(c, [None, {}])
            cell[1][ins.stream] = ins
        return ins

    def emit(self, nc, es):
        sems = {}
        for s, lst in self.streams.items():
            sems[s] = es.enter_context(nc.semaphore("s_" + s))
            c = 0
            for ins in lst:
                if ins.is_dma:
                    c += 16
                elif ins.milestone:
                    c += 1
                ins.count = c
        streams = self.streams

        def run(eng_name, eng):
            for ins in self.eng_list[eng_name]:
                for (s, i) in ins.waits:
                    eng.wait_ge(sems[s], streams[s][i].count)
                if ins.fn is not None:
                    h = ins.fn(eng)
                    if ins.is_dma:
                        h.then_inc(sems[ins.stream], 16)
                    elif ins.milestone:
                        h.then_inc(sems[ins.stream], 1)

        with nc.Block() as block:
            @block.tensor
            def _(e):
                run("pe", e)

            @block.scalar
            def _(e):
                run("act", e)

            @block.vector
            def _(e):
                run("dve", e)

            @block.gpsimd
            def _(e):
                run("pool", e)

            @block.sync
            def _(e):
                run("sp", e)


def make_consts():
    c = {}
    c["ident"] = np.eye(128, dtype=np.float32)
    k = np.arange(128)[:, None]
    q = np.arange(128)[None, :]
    tri = (q >= k).astype(np.float32)
    c["tri4"] = np.tile(tri, (1, 4)).astype(np.float32)
    hh = np.arange(4, dtype=np.float64)
    log_g = np.log1p(-np.exp2(-5.0 - hh))
    p = np.arange(128, dtype=np.float64)
    xi = np.exp(log_g[None, :] * (p[:, None] + 1.0))
    rv = np.zeros((128, 8), np.float32)
    rv[:, 0:4] = (1.0 / xi) * (128.0 ** -0.5)
    rv[:, 4:8] = EPS / (xi * xi)
    c["retv"] = rv
    c["ret_decay"] = [float(np.exp(log_g[i] * 128.0)) for i in range(4)]
    sl = alibi_slopes(12)
    ab = np.zeros((128, 12, 16), np.float32)
    for h in range(12):
        for d in range(16):
            ab[:, h, d] = sl[h] * (p - 64.0 - 128.0 * d)
    ef = np.exp(sl[None, :] * (p[:, None] - 64.0)).astype(np.float32)
    c["alibi"] = np.concatenate([ab.reshape(128, 192), ef], axis=1).astype(np.float32)
    c["dbias"] = [[float(-128.0 * sl[h] * d) for d in range(16)] for h in range(12)]
    return c


class Builder:
    def __init__(self, stop=99):
        self.stop = stop
        self.nc = bass.Bass("TRN2", target_bir_lowering=False, dynamic_dma_scratch_size=8192)
        self.sc = Sched()
        self.es = ExitStack()
        self.ps_i = 0
        self.ring_i = 0
        self.cnt = {}
        self.consts = make_consts()

    def sb(self, name, shape, dt):
        return self.es.enter_context(self.nc.sbuf_tensor(name, shape, dt))

    def dram(self, name, shape, dt=F32, kind="ExternalInput"):
        return self.nc.dram_tensor(name, shape, dt, kind=kind).ap()

    def rot(self, name, n):
        i = self.cnt.get(name, 0)
        self.cnt[name] = i + 1
        return i % n

    def psum(self):
        i = self.ps_i % 8
        self.ps_i += 1
        return self.PS[i], ("ps", i)

    def add(self, *a, **k):
        return self.sc.add(*a, **k)

    def setup(self):
        nc = self.nc
        d = self.dram
        self.x = d("x", [S, D])
        self.mem = d("mem", [256, D])
        self.w_in_a = d("w_in_a", [D, 2816])
        self.ret_gain = d("ret_norm_gain", [768])
        self.w_out_a = d("w_out_a", [D, D])
        self.kv_gain = d("kv_norm_gain", [D])
        self.w_kv = d("w_kv_shared", [D, 1536])
        self.w_in_b = d("w_in_b", [D, D])
        self.w_out_b = d("w_out_b", [D, D])
        self.w_mkv = d("w_mem_kv", [2, D, 512])
        self.g_pre_mix = d("norm_pre_mix", [2, D])
        self.g_post_mix = d("norm_post_mix", [2, D])
        self.g_pre_mlp = d("norm_pre_mlp", [2, D])
        self.g_post_mlp = d("norm_post_mlp", [2, D])
        self.w_up = d("w_up", [2, D, DFF])
        self.w_down = d("w_down", [2, DFF, D])
        self.c_ident = d("c_ident", [128, 128])
        self.c_tri4 = d("c_tri4", [128, 512])
        self.c_retv = d("c_retv", [128, 8])
        self.c_alibi = d("c_alibi", [128, 204])
        self.out = d("out", [S, D], kind="ExternalOutput")

        sb = self.sb
        self.h = sb("h", [128, NT, D], F32)
        self.KT = sb("KT", [128, 6, S], BF16)
        self.V = sb("V", [128, 16, 12, 65], BF16)
        self.ring = sb("ring", [128, NRING, 8, 512], BF16)
        self.B1 = sb("B1", [128, 8, 1024], BF16)
        self.B2 = sb("B2", [128, 8, 1024], BF16)
        self.B3 = sb("B3", [128, 8 * 1024], F32)
        self.y = self.B3[:].rearrange("p (t n) -> p t n", n=1024)
        self.B3b = self.B3[:].bitcast(BF16).rearrange("p (t n) -> p t n", n=2048)
        self.qmT = sb("qmT", [128, 2, 1024], BF16)
        self.memT = sb("memT", [128, 8, 256], BF16)
        self.mkT = sb("mkT", [128, 2, 256], BF16)
        self.mv = sb("mv", [128, 2, 4, 65], BF16)
        self.gain = sb("gain", [128, 1, 1024], F32)
        self.state = sb("state", [128, 4, 192], F32)
        self.state_bf = sb("state_bf", [128, 4, 192], BF16)
        self.tmpf = sb("tmpf", [128, 2, 512], F32)
        self.hnb = sb("hnb", [128, 2, 1024], BF16)
        self.junk2 = sb("junk2", [128, 1024], BF16)
        self.junk = sb("junk", [128, 192], BF16)
        self.PT = sb("PT", [128, 8, 512], BF16)
        self.PTm = sb("PTm", [128, 4, 512], BF16)
        self.ident = sb("ident", [128, 128], BF16)
        self.tri4 = sb("tri4", [128, 512], BF16)
        self.retv = sb("retv", [128, 8], F32)
        self.alibi = sb("alibi", [128, 204], F32)
        self.ksum = sb("ksum", [128, 6, 8], F32)
        self.kmT = sb("kmT", [128, 6, 8], BF16)
        self.st = sb("st", [128, 64], F32)
        self.gsb = sb("gsb", [128, 12, 8], F32)
        self.top8 = sb("top8", [128, 12, 8], F32)
        self.sel = sb("sel", [128, 2, 12, 8], F32)
        self.acc = sb("acc", [128, 4, 65], F32)
        self.PS = [self.es.enter_context(nc.psum_tensor("ps%d" % i, [128, 512], F32)) for i in range(8)]

        add = self.add
        ycell = lambda t: [("B3", t, s_) for s_ in range(8)]
        add("sp", lambda e: e.dma_start(out=self.y[:, 2, 0:128], in_=self.c_ident), writes=ycell(2), dma="d_c0")
        add("sp", lambda e: e.dma_start(out=self.y[:, 3, 0:512], in_=self.c_tri4), writes=ycell(3), dma="d_c1")
        add("sp", lambda e: e.dma_start(out=self.retv[:], in_=self.c_retv), writes=[("c", 2)], dma="d_c2")
        add("sp", lambda e: e.dma_start(out=self.alibi[:], in_=self.c_alibi), writes=[("c", 3)], dma="d_c3")
        add("dve", lambda e: e.tensor_copy(out=self.ident[:], in_=self.y[:, 2, 0:128]), reads=ycell(2), writes=[("ident",)])
        add("dve", lambda e: e.tensor_copy(out=self.tri4[:], in_=self.y[:, 3, 0:512]), reads=ycell(3), writes=[("tri4",)])
        for gt_ in range(16):
            add("dve", lambda e, gt_=gt_: e.tensor_copy(out=self.V[:, gt_, :, 64], in_=self.alibi[:, 192:204]),
                reads=[("c", 3)], writes=[("Vones", gt_)])
        add("dve", lambda e: e.memset(self.mv[:, :, :, 64:65], 1.0), writes=[("mvones",)])
        add("dve", lambda e: e.memset(self.state[:], 0.0), writes=[("state",)])
        add("dve", lambda e: e.memset(self.state_bf[:], 0.0), writes=[("state_bf",)])
        for mt in range(2):
            add("sp", lambda e, mt=mt: e.dma_start(out=self.y[:, mt, :], in_=self.mem[mt * 128:(mt + 1) * 128, :]),
                writes=ycell(mt), dma="d_mem%d" % mt)
            add("dve", lambda e, mt=mt: e.tensor_copy(out=self.hnb[:, 0, :], in_=self.y[:, mt, :]),
                reads=ycell(mt), writes=[("hnb", 0)])
            self.transpose8(self.hnb[:, 0, :], [("hnb", 0)], self.memT[:, :, mt * 128:(mt + 1) * 128], [("memT", mt)])

    def transpose8(self, src, src_cells, dst, dst_cells, eng="act"):
        ps, pc = self.psum()
        psb = ps[:].bitcast(BF16)
        for kc in range(8):
            self.add("pe", lambda e, kc=kc: e.transpose(out=psb[:, kc * 128:(kc + 1) * 128],
                                                        in_=src[:, kc * 128:(kc + 1) * 128], identity=self.ident[:]),
                     reads=list(src_cells) + [("ident",)], writes=[pc])
        pin = psb[:, 0:1024].rearrange("p (k c) -> p k c", c=128)
        if eng == "act":
            self.add("act", lambda e: e.copy(out=dst, in_=pin), reads=[pc], writes=dst_cells)
        else:
            self.add("dve", lambda e: e.tensor_copy(out=dst, in_=pin), reads=[pc], writes=dst_cells)

    def load_slab(self, wap, ncols=512):
        r = self.ring_i % NRING
        self.ring_i += 1
        src = wap.rearrange("(kc p) n -> p kc n", p=128)
        self.add("pool", lambda e: e.dma_start(out=self.ring[:, r, :, 0:ncols], in_=src),
                 writes=[("ring", r)], dma="d_ring%d" % r)
        return r

    def load_gain(self, gap, n=1024):
        sl = 0
        self.add("sp", lambda e: e.dma_start(out=self.gain[:, sl, 0:n], in_=gap.partition_broadcast(128)),
                 writes=[("gain", sl)], dma="d_gain%d" % sl)
        return sl

    def stat(self):
        i = self.rot("st", 16)
        return self.st[:, i * 4:(i + 1) * 4], ("st", i)

    def norm_T(self, src, src_cells, gsl, dstT_ap, dst_cells):
        add = self.add
        st, stc = self.stat()
        sl = self.rot("hnb", 2)
        add("act", lambda e: e.activation(out=self.junk2[:], in_=src, func=AF.Square, accum_out=st[:, 0:1]),
            reads=src_cells, writes=[stc])
        add("act", lambda e: e.activation(out=st[:, 1:2], in_=st[:, 0:1], func=AF.Sqrt, bias=self.epsc[:, 0:1], scale=1.0 / D),
            reads=[stc, ("epsc",)], writes=[stc])
        add("dve", lambda e: e.reciprocal(out=st[:, 2:3], in_=st[:, 1:2]), reads=[stc], writes=[stc])
        add("dve", lambda e: e.scalar_tensor_tensor(out=self.hnb[:, sl, :], in0=src, scalar=st[:, 2:3],
                                                    in1=self.gain[:, gsl, :], op0=ALU.mult, op1=ALU.mult),
            reads=list(src_cells) + [stc, ("gain", gsl)], writes=[("hnb", sl)])
        self.transpose8(self.hnb[:, sl, :], [("hnb", sl)], dstT_ap, dst_cells)

    def post_res(self, t, gsl):
        add = self.add
        st, stc = self.stat()
        ycells = [("B3", t, s) for s in range(8)]
        add("act", lambda e: e.activation(out=self.junk2[:], in_=self.y[:, t, :], func=AF.Square, accum_out=st[:, 0:1]),
            reads=ycells, writes=[stc])
        add("act", lambda e: e.activation(out=st[:, 1:2], in_=st[:, 0:1], func=AF.Sqrt, bias=self.epsc[:, 0:1], scale=1.0 / D),
            reads=[stc, ("epsc",)], writes=[stc])
        add("dve", lambda e: e.reciprocal(out=st[:, 2:3], in_=st[:, 1:2]), reads=[stc], writes=[stc])
        add("dve", lambda e: e.scalar_tensor_tensor(out=self.y[:, t, :], in0=self.y[:, t, :], scalar=st[:, 2:3],
                                                    in1=self.gain[:, gsl, :], op0=ALU.mult, op1=ALU.mult),
            reads=ycells + [stc, ("gain", gsl)], writes=ycells)
        add("dve", lambda e: e.tensor_tensor(out=self.h[:, t, :], in0=self.h[:, t, :], in1=self.y[:, t, :], op=ALU.add),
            reads=[("h", t)] + ycells, writes=[("h", t)])

    def mm_B(self, r, j, xT, xcells_fn, grp, ncol=512, nk=8):
        ps, pc = self.psum()
        for kc in range(nk):
            self.add("pe", lambda e, kc=kc: e.matmul(ps[:, 0:ncol], lhsT=self.ring[:, r, kc, j * 128:(j + 1) * 128],
                                                     rhs=xT[:, kc, grp * ncol:(grp + 1) * ncol],
                                                     start=(kc == 0), stop=(kc == nk - 1)),
                     reads=[("ring", r)] + xcells_fn(kc, grp), writes=[pc])
        return ps, pc

    def mm_A(self, r, c0, n, xT, xcells_fn, t, nk=8):
        ps, pc = self.psum()
        for kc in range(nk):
            self.add("pe", lambda e, kc=kc: e.matmul(ps[:, 0:n], lhsT=xT[:, kc, t * 128:(t + 1) * 128],
                                                     rhs=self.ring[:, r, kc, c0:c0 + n],
                                                     start=(kc == 0), stop=(kc == nk - 1)),
                     reads=[("ring", r)] + xcells_fn(kc, t), writes=[pc])
        return ps, pc

    @staticmethod
    def cellsB1_grp(kc, grp):
        return [("B1", kc, grp * 4 + i) for i in range(4)]

    @staticmethod
    def cellsB1_t(kc, t):
        return [("B1", kc, t)]

    @staticmethod
    def cellsB2_grp(kc, grp):
        return [("B2", kc, grp * 4 + i) for i in range(4)]

    @staticmethod
    def cellsB2_t(kc, t):
        return [("B2", kc, t)]

    def mem_kv(self, l):
        add = self.add
        r = self.load_slab(self.w_mkv[l])
        for j in range(2):
            ps, pc = self.psum()
            for kc in range(8):
                add("pe", lambda e, kc=kc, ps=ps, j=j: e.matmul(ps[:, 0:256], lhsT=self.ring[:, r, kc, j * 128:(j + 1) * 128],
                                                               rhs=self.memT[:, kc, :], start=(kc == 0), stop=(kc == 7)),
                    reads=[("ring", r), ("memT", 0), ("memT", 1)], writes=[pc])
            add("act", lambda e, ps=ps, j=j: e.copy(out=self.mkT[:, j, :], in_=ps[:, 0:256]), reads=[pc], writes=[("mkT", j)])
        for mt in range(2):
            ps, pc = self.psum()
            for kc in range(8):
                add("pe", lambda e, kc=kc, ps=ps, mt=mt: e.matmul(ps[:, 0:256], lhsT=self.memT[:, kc, mt * 128:(mt + 1) * 128],
                                                                 rhs=self.ring[:, r, kc, 256:512], start=(kc == 0), stop=(kc == 7)),
                    reads=[("ring", r), ("memT", mt)], writes=[pc])
            add("act", lambda e, ps=ps, mt=mt: e.copy(out=self.mv[:, mt, :, 0:64],
                                                     in_=ps[:, 0:256].rearrange("p (h c) -> p h c", c=64)),
                reads=[pc, ("mvones",)], writes=[("mv", mt)])

    def mem_front(self, t):
        add = self.add
        pts = []
        for hh in range(2):
            ps, pc = self.psum()
            po = hh * 64
            for half in range(2):
                for mt in range(2):
                    sl = half * 2 + mt
                    add("pe", lambda e, ps=ps, sl=sl, po=po, half=half, mt=mt: e.matmul(
                        ps[:, sl * 128:(sl + 1) * 128], lhsT=self.mkT[po:po + 64, half, mt * 128:(mt + 1) * 128],
                        rhs=self.qmT[po:po + 64, half, t * 128:(t + 1) * 128], start=True, stop=True),
                        reads=[("mkT", half), ("qmT", half, t)], writes=[pc])
            psl = (t % 2) * 2 + hh
            add("act", lambda e, ps=ps, psl=psl: e.activation(out=self.PTm[:, psl, :], in_=ps[:, 0:512], func=AF.Exp, scale=0.125),
                reads=[pc], writes=[("PTm", psl)])
            pts.append(psl)
        return pts

    def mem_back(self, t, pts):
        add = self.add
        pso, poc = self.psum()
        for hd in range(4):
            psl = pts[hd % 2]
            for mt in range(2):
                sl = (hd // 2) * 2 + mt
                add("pe", lambda e, hd=hd, psl=psl, sl=sl, mt=mt: e.matmul(
                    pso[:, hd * 65:(hd + 1) * 65], lhsT=self.PTm[:, psl, sl * 128:(sl + 1) * 128],
                    rhs=self.mv[:, mt, hd, :], start=(mt == 0), stop=(mt == 1)),
                    reads=[("PTm", psl), ("mv", mt), ("mvones",)], writes=[poc])
        st, stc = self.stat()
        pv = pso[:, 0:260].rearrange("p (h c) -> p h c", c=65)
        add("dve", lambda e: e.reciprocal(out=st[:, 0:4], in_=pv[:, :, 64]), reads=[poc], writes=[stc])
        for hd in range(4):
            add("dve", lambda e, hd=hd: e.tensor_scalar(out=self.B1[:, t, 768 + hd * 64:768 + (hd + 1) * 64], in0=pv[:, hd, 0:64],
                                                        scalar1=st[:, hd:hd + 1], scalar2=None, op0=ALU.mult),
                reads=[poc, stc], writes=[("B1", t, 6 + hd // 2)])

    def mem_attn(self, t):
        self.mem_back(t, self.mem_front(t))

    def cat_to_T(self):
        for t in range(NT):
            self.transpose8(self.B1[:, t, :], [("B1", t, j) for j in range(8)],
                            self.B2[:, :, t * 128:(t + 1) * 128], [("B2", kc, t) for kc in range(8)])

    def out_proj(self, wap, g_post):
        add = self.add
        gsl = self.load_gain(g_post)
        for s in range(2):
            r = self.load_slab(wap[:, s * 512:(s + 1) * 512])
            for t in range(NT):
                ps, pc = self.mm_A(r, 0, 512, self.B2, self.cellsB2_t, t)
                add("act", lambda e, ps=ps, t=t, s=s: e.copy(out=self.y[:, t, s * 512:(s + 1) * 512], in_=ps[:, 0:512]),
                    reads=[pc], writes=[("B3", t, s * 4 + i) for i in range(4)])
                if s == 1:
                    self.post_res(t, gsl)

    def mlp(self, l):
        add = self.add
        gsl = self.load_gain(self.g_pre_mlp[l])
        for t in range(NT):
            self.norm_T(self.h[:, t, :], [("h", t)], gsl, self.B1[:, :, t * 128:(t + 1) * 128], [("B1", kc, t) for kc in range(8)])
        gpost = self.load_gain(self.g_post_mlp[l])
        for b in range(4):
            for s in range(2):
                r = self.load_slab(self.w_up[l][:, b * 1024 + s * 512: b * 1024 + (s + 1) * 512])
                for j in range(4):
                    for grp in range(2):
                        ps, pc = self.mm_B(r, j, self.B1, self.cellsB1_grp, grp)
                        sl = self.rot("tmpf", 2)
                        add("act", lambda e, ps=ps, sl=sl: e.activation(out=self.tmpf[:, sl, 0:512], in_=ps[:, 0:512], func=AF.Relu),
                            reads=[pc], writes=[("tmpf", sl)])
                        add("dve", lambda e, ps=ps, sl=sl, s=s, j=j, grp=grp: e.tensor_tensor(
                            out=self.B2[:, s * 4 + j, grp * 512:(grp + 1) * 512], in0=self.tmpf[:, sl, 0:512], in1=ps[:, 0:512], op=ALU.mult),
                            reads=[pc, ("tmpf", sl)], writes=[("B2", s * 4 + j, grp * 4 + i) for i in range(4)])
            for s in range(2):
                r = self.load_slab(self.w_down[l][b * 1024:(b + 1) * 1024, s * 512:(s + 1) * 512])
                for t in range(NT):
                    ps, pc = self.mm_A(r, 0, 512, self.B2, self.cellsB2_t, t)
                    yc = [("B3", t, s * 4 + i) for i in range(4)]
                    if b == 0:
                        add("act", lambda e, ps=ps, t=t, s=s: e.copy(out=self.y[:, t, s * 512:(s + 1) * 512], in_=ps[:, 0:512]),
                            reads=[pc], writes=yc)
                    else:
                        add("dve", lambda e, ps=ps, t=t, s=s: e.tensor_tensor(out=self.y[:, t, s * 512:(s + 1) * 512],
                                                                             in0=self.y[:, t, s * 512:(s + 1) * 512], in1=ps[:, 0:512], op=ALU.add),
                            reads=[pc] + yc, writes=yc)
                    if b == 3 and s == 1:
                        self.post_res(t, gpost)

    def layer0_mix(self, hf):
        add = self.add
        gsl = self.load_gain(self.g_pre_mix[0])
        for t in range(NT):
            self.norm_T(self.h[:, t, :], [("h", t)], gsl, self.B1[:, :, t * 128:(t + 1) * 128], [("B1", kc, t) for kc in range(8)])
        if self.stop < 0.15: return
        self.mem_kv(0)
        if self.stop < 0.25: return
        W = self.w_in_a
        r = self.load_slab(W[:, 0:512])
        for j in range(4):
            for grp in range(2):
                ps, pc = self.mm_B(r, j, self.B1, self.cellsB1_grp, grp)
                add("act", lambda e, ps=ps, j=j, grp=grp: e.copy(out=self.B2[:, j, grp * 512:(grp + 1) * 512], in_=ps[:, 0:512]),
                    reads=[pc], writes=[("B2", j, grp * 4 + i) for i in range(4)])
        if self.stop < 0.35: return
        r = self.load_slab(W[:, 512:1024])
        for j in range(4):
            for grp in range(2):
                ps, pc = self.mm_B(r, j, self.B1, self.cellsB1_grp, grp)
                add("act", lambda e, ps=ps, j=j, grp=grp: e.copy(out=self.B2[:, 4 + j, grp * 512:(grp + 1) * 512], in_=ps[:, 0:512]),
                    reads=[pc], writes=[("B2", 4 + j, grp * 4 + i) for i in range(4)])
        for t in range(NT):
            ps, pc = self.mm_A(r, 0, 512, self.B1, self.cellsB1_t, t)
            add("dve", lambda e, ps=ps, t=t: e.tensor_copy(out=self.B3b[:, t, 0:512], in_=ps[:, 0:512]),
                reads=[pc], writes=[("B3", t, 0), ("B3", t, 1)])
        if self.stop < 0.45: return
        self.load_gain(self.ret_gain, 768)
        for si in range(2, 5):
            r = self.load_slab(W[:, si * 512:(si + 1) * 512])
            for t in range(NT):
                ps, pc = self.mm_A(r, 0, 512, self.B1, self.cellsB1_t, t)
                c0 = si * 512 - 1024
                bounds = sorted(set([c0, c0 + 512] + [b for b in range(0, 1537, 192) if c0 < b < c0 + 512]))
                for a, bnd in zip(bounds[:-1], bounds[1:]):
                    lo, hi = a - c0, bnd - c0
                    if a < 768:
                        hd = a // 192
                        add("act", lambda e, ps=ps, t=t, lo=lo, hi=hi, a=a, bnd=bnd, hd=hd: e.activation(
                            out=self.B3b[:, t, 512 + a:512 + bnd], in_=ps[:, lo:hi], func=AF.Copy, scale=self.retv[:, hd:hd + 1]),
                            reads=[pc, ("c", 2)], writes=[("B3", t, s) for s in range((512 + a) // 256, (512 + bnd - 1) // 256 + 1)])
                    else:
                        ga, gb = a - 768, bnd - 768
                        sl = self.rot("tmpf", 2)
                        add("act", lambda e, ps=ps, lo=lo, hi=hi, sl=sl: e.activation(out=self.tmpf[:, sl, 0:hi - lo], in_=ps[:, lo:hi], func=AF.Silu),
                            reads=[pc], writes=[("tmpf", sl)])
                        add("dve", lambda e, t=t, ga=ga, gb=gb, sl=sl: e.tensor_tensor(
                            out=self.B3b[:, t, 1280 + ga:1280 + gb], in0=self.tmpf[:, sl, 0:gb - ga], in1=self.gain[:, 0, ga:gb], op=ALU.mult),
                            reads=[("tmpf", sl), ("gain", 0)], writes=[("B3", t, s) for s in range((1280 + ga) // 256, (1280 + gb - 1) // 256 + 1)])
        if self.stop < 0.55: return
        r = self.load_slab(W[:, 2560:2816], ncols=256)
        for j in range(2):
            for grp in range(2):
                ps, pc = self.mm_B(r, j, self.B1, self.cellsB1_grp, grp)
                add("act", lambda e, ps=ps, j=j, grp=grp: e.copy(out=self.qmT[:, j, grp * 512:(grp + 1) * 512], in_=ps[:, 0:512]),
                    reads=[pc], writes=[("qmT", j, grp * 4 + i) for i in range(4)])
        if self.stop < 0.65: return
        dec = self.consts["ret_decay"]
        def front(t):
            tok = slice(t * 128, (t + 1) * 128)
            ps, pc = self.psum()
            for hd in range(4):
                add("pe", lambda e, ps=ps, hd=hd, tok=tok: e.matmul(ps[:, hd * 128:(hd + 1) * 128], lhsT=self.B2[:, 4 + hd, tok],
                                                                   rhs=self.B2[:, hd, tok], start=True, stop=True),
                    reads=[("B2", 4 + hd, t), ("B2", hd, t)], writes=[pc])
            psl = self.rot("PT", 8)
            add("dve", lambda e, ps=ps, psl=psl: e.tensor_tensor(out=self.PT[:, psl, :], in0=ps[:, 0:512], in1=self.tri4[:], op=ALU.mult),
                reads=[pc, ("tri4",)], writes=[("PT", psl)])
            return psl, self.mem_front(t)

        def back(t, psl, mf):
            tok = slice(t * 128, (t + 1) * 128)
            vcells = [("B3", t, 2), ("B3", t, 3), ("B3", t, 4)]
            pos = []
            for pair in range(2):
                po_, poc = self.psum()
                pos.append((po_, poc))
                for hh in range(2):
                    hd = pair * 2 + hh
                    add("pe", lambda e, po_=po_, hd=hd, hh=hh, psl=psl, t=t: e.matmul(
                        po_[:, hh * 192:(hh + 1) * 192], lhsT=self.PT[:, psl, hd * 128:(hd + 1) * 128],
                        rhs=self.B3b[:, t, 512 + hd * 192:512 + (hd + 1) * 192], start=True, stop=False),
                        reads=[("PT", psl)] + vcells, writes=[poc])
                    add("pe", lambda e, po_=po_, hd=hd, hh=hh, tok=tok: e.matmul(
                        po_[:, hh * 192:(hh + 1) * 192], lhsT=self.B2[:, hd, tok], rhs=self.state_bf[:, hd, :], start=False, stop=True),
                        reads=[("B2", hd, t), ("state_bf",)], writes=[poc])
            for pair in range(2):
                pk, pkc = self.psum()
                for hh in range(2):
                    hd = pair * 2 + hh
                    add("pe", lambda e, pk=pk, hd=hd, hh=hh, t=t: e.matmul(
                        pk[:, hh * 192:(hh + 1) * 192], lhsT=self.B3b[:, t, hd * 128:(hd + 1) * 128],
                        rhs=self.B3b[:, t, 512 + hd * 192:512 + (hd + 1) * 192], start=True, stop=True),
                        reads=[("B3", t, 0), ("B3", t, 1)] + vcells, writes=[pkc])
                for hh in range(2):
                    hd = pair * 2 + hh
                    add("dve", lambda e, pk=pk, hd=hd, hh=hh: e.scalar_tensor_tensor(
                        out=self.state[:, hd, :], in0=pk[:, hh * 192:(hh + 1) * 192], scalar=1.0, in1=self.state[:, hd, :],
                        op0=ALU.mult, op1=ALU.add), reads=[pkc, ("state",)], writes=[("state",)])
                    add("dve", lambda e, hd=hd: e.tensor_scalar(out=self.state[:, hd, :], in0=self.state[:, hd, :], scalar1=dec[hd],
                                                               scalar2=None, op0=ALU.mult), reads=[("state",)], writes=[("state",)])
                    add("dve", lambda e, hd=hd: e.tensor_copy(out=self.state_bf[:, hd, :], in_=self.state[:, hd, :]),
                        reads=[("state",)], writes=[("state_bf",)])
            st, stc = self.stat()
            for hd in range(4):
                po_, poc = pos[hd // 2]
                hh = hd % 2
                add("act", lambda e, po_=po_, hh=hh, hd=hd: e.activation(out=self.junk[:, 0:192], in_=po_[:, hh * 192:(hh + 1) * 192],
                                                                       func=AF.Square, accum_out=st[:, hd:hd + 1]),
                    reads=[poc], writes=[stc])
            st2, stc2 = self.stat()
            add("dve", lambda e: e.scalar_tensor_tensor(out=st2[:, 0:4], in0=st[:, 0:4], scalar=1.0 / 192.0, in1=self.retv[:, 4:8],
                                                        op0=ALU.mult, op1=ALU.add), reads=[stc, ("c", 2)], writes=[stc2])
            add("act", lambda e: e.activation(out=st2[:, 0:4], in_=st2[:, 0:4], func=AF.Sqrt), reads=[stc2], writes=[stc2])
            st3, stc3 = self.stat()
            add("dve", lambda e: e.reciprocal(out=st3[:, 0:4], in_=st2[:, 0:4]), reads=[stc2], writes=[stc3])
            for hd in range(4):
                po_, poc = pos[hd // 2]
                hh = hd % 2
                c0, c1 = hd * 192, (hd + 1) * 192
                add("dve", lambda e, po_=po_, hh=hh, hd=hd, c0=c0, c1=c1, t=t: e.scalar_tensor_tensor(
                    out=self.B1[:, t, c0:c1], in0=po_[:, hh * 192:(hh + 1) * 192], scalar=st3[:, hd:hd + 1],
                    in1=self.B3b[:, t, 1280 + c0:1280 + c1], op0=ALU.mult, op1=ALU.mult),
                    reads=[poc, stc3, ("B3", t, 5), ("B3", t, 6), ("B3", t, 7)],
                    writes=[("B1", t, j) for j in range(c0 // 128, (c1 - 1) // 128 + 1)])
            self.mem_back(t, mf)

        f = front(0)
        for t in range(NT):
            fn = front(t + 1) if t + 1 < NT else None
            back(t, *f)
            f = fn
        if self.stop < 0.75: return
        self.cat_to_T()
        if self.stop < 0.85: return
        self.out_proj(self.w_out_a, self.g_post_mix[0])

    def layer1_mix(self, hf):
        add = self.add
        gsl = self.load_gain(self.kv_gain)
        for t in range(NT):
            self.norm_T(self.h[:, t, :], [("h", t)], gsl, self.B1[:, :, t * 128:(t + 1) * 128], [("B1", kc, t) for kc in range(8)])
        W = self.w_kv
        for si in range(3):
            r = self.load_slab(W[:, si * 512:(si + 1) * 512])
            for j in range(4):
                col = si * 512 + j * 128
                if col >= 768:
                    continue
                c = col // 128
                for grp in range(2):
                    ps, pc = self.mm_B(r, j, self.B1, self.cellsB1_grp, grp)
                    for bb in range(2):
                        blk = hf * 4 + grp * 2 + bb
                        g0 = hf * T + grp * 512 + bb * 256
                        add("act", lambda e, ps=ps, c=c, g0=g0, bb=bb, blk=blk: e.activation(
                            out=self.KT[:, c, g0:g0 + 256], in_=ps[:, bb * 256:(bb + 1) * 256], func=AF.Copy,
                            accum_out=self.ksum[:, c, blk:blk + 1]),
                            reads=[pc], writes=[("KT", c, blk), ("ksum", c, blk)])
            v0 = max(si * 512, 768)
            v1 = (si + 1) * 512
            if v1 > v0:
                n = v1 - v0
                h0 = (v0 - 768) // 64
                nh = n // 64
                for t in range(NT):
                    gt = hf * NT + t
                    ps, pc = self.mm_A(r, v0 - si * 512, n, self.B1, self.cellsB1_t, t)
                    for j_ in range(nh):
                        hd_ = h0 + j_
                        add("dve", lambda e, ps=ps, gt=gt, hd_=hd_, j_=j_: e.tensor_scalar(
                            out=self.V[:, gt, hd_, 0:64], in0=ps[:, j_ * 64:(j_ + 1) * 64], scalar1=self.alibi[:, 192 + hd_:193 + hd_],
                            scalar2=None, op0=ALU.mult),
                            reads=[pc, ("c", 3)], writes=[("V", gt, hd_ // 4)])
        gsl = self.load_gain(self.g_pre_mix[1])
        for t in range(NT):
            self.norm_T(self.h[:, t, :], [("h", t)], gsl, self.B1[:, :, t * 128:(t + 1) * 128], [("B1", kc, t) for kc in range(8)])
        self.mem_kv(1)
        W = self.w_in_b
        for si in range(2):
            r = self.load_slab(W[:, si * 512:(si + 1) * 512])
            for j in range(4):
                col = si * 512 + j * 128
                for grp in range(2):
                    ps, pc = self.mm_B(r, j, self.B1, self.cellsB1_grp, grp)
                    if col < 768:
                        c = col // 128
                        add("act", lambda e, ps=ps, c=c, grp=grp: e.copy(out=self.B2[:, c, grp * 512:(grp + 1) * 512], in_=ps[:, 0:512]),
                            reads=[pc], writes=[("B2", c, grp * 4 + i) for i in range(4)])
                    else:
                        c = (col - 768) // 128
                        add("act", lambda e, ps=ps, c=c, grp=grp: e.copy(out=self.qmT[:, c, grp * 512:(grp + 1) * 512], in_=ps[:, 0:512]),
                            reads=[pc], writes=[("qmT", c, grp * 4 + i) for i in range(4)])
        if hf == 1:
            kc_all = [("ksum", c, b) for c in range(6) for b in range(8)]
            add("act", lambda e: e.activation(out=self.kmT[:], in_=self.ksum[:], func=AF.Copy, scale=1.0 / 256.0),
                reads=kc_all, writes=[("kmT",)])
        prev = None
        for t in range(NT):
            gt = hf * NT + t
            qb = gt // 2
            if hf == 1:
                self.moba_gate(t, qb)
            mf = self.mem_front(t)
            for hd in range(12):
                cur = self.moba_front(hf, t, hd)
                if prev is not None:
                    self.moba_back(*prev)
                prev = (hf, t, hd, cur)
            self.mem_back(t, mf)
        self.moba_back(*prev)
        self.cat_to_T()
        self.out_proj(self.w_out_b, self.g_post_mix[1])

    def moba_gate(self, t, qb):
        add = self.add
        gv = self.gsb[:].rearrange("p (c two) b -> p c two b", two=2)
        for hh in range(2):
            ps, pc = self.psum()
            po = hh * 64
            for c in range(6):
                add("pe", lambda e, ps=ps, c=c, po=po: e.matmul(ps[:, c * 8:(c + 1) * 8], lhsT=self.B2[po:po + 64, c, t * 128:(t + 1) * 128],
                                                               rhs=self.kmT[po:po + 64, c, :], start=True, stop=True),
                    reads=[("B2", c, t), ("kmT",)], writes=[pc])
            add("act", lambda e, ps=ps, hh=hh: e.copy(out=gv[:, :, hh, :], in_=ps[:, 0:48].rearrange("p (c b) -> p c b", b=8)),
                reads=[pc], writes=[("gsb",)])
        if qb < 8:
            add("dve", lambda e: e.memset(self.gsb[:, :, qb:8], -1e30), reads=[("gsb",)], writes=[("gsb",)])
        for hd in range(12):
            add("dve", lambda e, hd=hd: e.max(out=self.top8[:, hd, :], in_=self.gsb[:, hd, :]), reads=[("gsb",)], writes=[("top8", hd)])
            add("dve", lambda e, hd=hd: e.tensor_scalar(out=self.sel[:, t % 2, hd, :], in0=self.gsb[:, hd, :], scalar1=self.top8[:, hd, 2:3],
                                                        scalar2=None, op0=ALU.is_ge), reads=[("gsb",), ("top8", hd)], writes=[("sel", t % 2, hd)])

    def moba_front(self, hf, t, hd):
        add = self.add
        gt = hf * NT + t
        qb = gt // 2
        c, po = hd // 2, (hd % 2) * 64
        qap = self.B2[po:po + 64, c, t * 128:(t + 1) * 128]
        kts = list(range(gt + 1))
        pt_of = {}
        for i0 in range(0, len(kts), 4):
            grp = kts[i0:i0 + 4]
            ps, pc = self.psum()
            for i, kt in enumerate(grp):
                add("pe", lambda e, ps=ps, i=i, kt=kt: e.matmul(ps[:, i * 128:(i + 1) * 128], lhsT=self.KT[po:po + 64, c, kt * 128:(kt + 1) * 128],
                                                               rhs=qap, start=True, stop=True),
                    reads=[("KT", c, kt // 2), ("B2", c, t)], writes=[pc])
            psl = self.rot("PT", 8)
            for i, kt in enumerate(grp):
                dd = gt - kt
                add("act", lambda e, ps=ps, i=i, psl=psl, dd=dd: e.activation(
                    out=self.PT[:, psl, i * 128:(i + 1) * 128], in_=ps[:, i * 128:(i + 1) * 128], func=AF.Exp,
                    bias=self.consts["dbias"][hd][dd], scale=0.125),
                    reads=[pc], writes=[("PT", psl)])
                if kt == gt:
                    add("dve", lambda e, psl=psl, i=i: e.tensor_tensor(out=self.PT[:, psl, i * 128:(i + 1) * 128],
                                                                       in0=self.PT[:, psl, i * 128:(i + 1) * 128], in1=self.tri4[:, 0:128], op=ALU.mult),
                        reads=[("PT", psl), ("tri4",)], writes=[("PT", psl)])
                pt_of[kt] = (psl, i)
        return pt_of

    def moba_back(self, hf, t, hd, pt_of):
        add = self.add
        gt = hf * NT + t
        qb = gt // 2
        kts = list(range(gt + 1))
        pso, poc = self.psum()
        asl = self.rot("acc", 4)
        if hf == 0:
            for n_, kt in enumerate(kts):
                psl, i = pt_of[kt]
                add("pe", lambda e, psl=psl, i=i, kt=kt, n_=n_: e.matmul(pso[:, 0:65], lhsT=self.PT[:, psl, i * 128:(i + 1) * 128],
                                                                        rhs=self.V[:, kt, hd, :], start=(n_ == 0), stop=(n_ == len(kts) - 1)),
                    reads=[("PT", psl), ("V", kt, hd // 4), ("Vones", kt)], writes=[poc])
            src = pso
            srcc = poc
            res = pso[:, 0:65]
        else:
            psB, pbc = self.psum()
            own = [kt for kt in kts if kt // 2 == qb]
            for n_, kt in enumerate(own):
                psl, i = pt_of[kt]
                add("pe", lambda e, psl=psl, i=i, kt=kt, n_=n_: e.matmul(psB[:, 0:65], lhsT=self.PT[:, psl, i * 128:(i + 1) * 128],
                                                                        rhs=self.V[:, kt, hd, :], start=(n_ == 0), stop=(n_ == len(own) - 1)),
                    reads=[("PT", psl), ("V", kt, hd // 4), ("Vones", kt)], writes=[pbc])
            for b in range(qb):
                for n_, kt in enumerate((2 * b, 2 * b + 1)):
                    psl, i = pt_of[kt]
                    add("pe", lambda e, psl=psl, i=i, kt=kt, n_=n_, b=b: e.matmul(pso[:, b * 65:(b + 1) * 65], lhsT=self.PT[:, psl, i * 128:(i + 1) * 128],
                                                                                 rhs=self.V[:, kt, hd, :], start=(n_ == 0), stop=(n_ == 1)),
                        reads=[("PT", psl), ("V", kt, hd // 4), ("Vones", kt)], writes=[poc])
            accap = self.acc[:, asl, :]
            add("act", lambda e: e.copy(out=accap, in_=psB[:, 0:65]), reads=[pbc], writes=[("acc", asl)])
            for b in range(qb):
                add("dve", lambda e, b=b: e.scalar_tensor_tensor(out=accap, in0=pso[:, b * 65:(b + 1) * 65], scalar=self.sel[:, t % 2, hd, b:b + 1],
                                                                 in1=accap, op0=ALU.mult, op1=ALU.add),
                    reads=[poc, ("sel", t % 2, hd), ("acc", asl)], writes=[("acc", asl)])
            res = accap
            srcc = ("acc", asl)
        st, stc = self.stat()
        add("dve", lambda e: e.reciprocal(out=st[:, 0:1], in_=res[:, 64:65]), reads=[srcc], writes=[stc])
        add("dve", lambda e: e.tensor_scalar(out=self.B1[:, t, hd * 64:(hd + 1) * 64], in0=res[:, 0:64], scalar1=st[:, 0:1],
                                             scalar2=None, op0=ALU.mult), reads=[srcc, stc], writes=[("B1", t, hd // 2)])

    def build(self):
        add = self.add
        self.setup()
        self.epsc = self.sb("epsc", [128, 1], F32)
        add("dve", lambda e: e.memset(self.epsc[:], EPS), writes=[("epsc",)])
        for hf in range(NH):
            for t in range(NT):
                gt = hf * NT + t
                add("sp", lambda e, t=t, gt=gt: e.dma_start(out=self.h[:, t, :], in_=self.x[gt * 128:(gt + 1) * 128, :]),
                    writes=[("h", t)], dma="d_h%d" % t)
            if self.stop > 0:
                self.layer0_mix(hf)
            if self.stop >= 2:
                self.mlp(0)
            if self.stop >= 3:
                self.layer1_mix(hf)
            if self.stop >= 4:
                self.mlp(1)
            for t in range(NT):
                gt = hf * NT + t
                add("sp", lambda e, t=t, gt=gt: e.dma_start(out=self.out[gt * 128:(gt + 1) * 128, :], in_=self.h[:, t, :]),
                    reads=[("h", t)], dma="d_o%d" % t)
        add("sp", None, writes=[("h", t) for t in range(NT)])
        self.sc.emit(self.nc, self.es)
        self.es.close()
        return self.nc


_CACHE = {}


def get_nc(stop=99):
    if stop not in _CACHE:
        _CACHE[stop] = Builder(stop).build()
    return _CACHE[stop]


def kernel(stop=99, ncores=8, **inputs):
    stop = float(stop)
    nc = get_nc(stop)
    cs = make_consts()
    f = lambda a: np.ascontiguousarray(np.asarray(a, dtype=np.float32))
    shared = {
        "w_in_a": f(inputs["w_in_a"][0]), "ret_norm_gain": f(inputs["ret_norm_gain"][0]), "w_out_a": f(inputs["w_out_a"][0]),
        "kv_norm_gain": f(inputs["kv_norm_gain"]), "w_kv_shared": f(inputs["w_kv_shared"]), "w_in_b": f(inputs["w_in_b"][0]),
        "w_out_b": f(inputs["w_out_b"][0]), "w_mem_kv": f(inputs["w_mem_kv"]), "norm_pre_mix": f(inputs["norm_pre_mix"]),
        "norm_post_mix": f(inputs["norm_post_mix"]), "norm_pre_mlp": f(inputs["norm_pre_mlp"]),
        "norm_post_mlp": f(inputs["norm_post_mlp"]), "w_up": f(inputs["w_up"]), "w_down": f(inputs["w_down"]),
        "c_ident": cs["ident"], "c_tri4": cs["tri4"], "c_retv": cs["retv"], "c_alibi": cs["alibi"],
    }
    x = f(inputs["x"])
    mem = f(inputs["mem"])
    in_maps = []
    for b in range(ncores):
        m = dict(shared)
        m["x"] = x[b]
        m["mem"] = mem[b]
        in_maps.append(m)
    res = run_bass_kernel_spmd(nc, in_maps, core_ids=list(range(ncores)))
    return np.stack([np.asarray(r["out"], dtype=np.float32) for r in res.results], axis=0)
```

```python
import math
from contextlib import ExitStack
import numpy as np
import concourse.bass as bass
import concourse.mybir as mybir
from concourse.alu_op_type import AluOpType as ALU
from concourse.bass_utils import run_bass_kernel_spmd

F32 = mybir.dt.float32
BF16 = mybir.dt.bfloat16
AF = mybir.ActivationFunctionType
AX = mybir.AxisListType

D = 1024
S = 2048
T = 1024
NT = 8
NH = 2
DFF = 4096
EPS = 1e-6
NRING = 3
ENGS = ("pe", "act", "dve", "pool", "sp")


def alibi_slopes(n):
    def pow2(m):
        return [2.0 ** (-8.0 * (i + 1) / m) for i in range(m)]
    p = 2 ** int(math.floor(math.log2(n)))
    s = pow2(p)
    if p < n:
        s = s + pow2(2 * p)[0::2][: n - p]
    return np.asarray(s, dtype=np.float64)


class Ins:
    __slots__ = ("eng", "fn", "waits", "stream", "sidx", "milestone", "count", "is_dma")


class Sched:
    def __init__(self):
        self.cells = {}
        self.eng_list = {e: [] for e in ENGS}
        self.streams = {}
        self.seen = {e: {} for e in ENGS}

    def add(self, eng, fn, reads=(), writes=(), dma=None):
        ins = Ins()
        ins.eng = eng
        ins.fn = fn
        ins.is_dma = dma is not None
        ins.stream = dma if dma else eng
        need = {}

        def dep(d, war):
            if d is None:
                return
            if (not ins.is_dma) and (not d.is_dma) and d.eng == eng:
                if eng == "pe" or war:
                    return
            if need.get(d.stream, -1) < d.sidx:
                need[d.stream] = d.sidx

        for c in reads:
            cell = self.cells.get(c)
            if cell:
                dep(cell[0], False)
        for c in writes:
            cell = self.cells.get(c)
            if cell:
                dep(cell[0], False)
                for r in cell[1].values():
                    dep(r, True)
        waits = []
        seen = self.seen[eng]
        for s, i in need.items():
            if seen.get(s, -1) >= i:
                continue
            seen[s] = i
            waits.append((s, i))
            self.streams[s][i].milestone = True
        ins.waits = waits
        lst = self.streams.setdefault(ins.stream, [])
        ins.sidx = len(lst)
        lst.append(ins)
        ins.milestone = ins.is_dma
        self.eng_list[eng].append(ins)
        for c in writes:
            self.cells[c] = [ins, {}]
        for c in reads:
            cell = self.cells.setdefault(c, [None, {}])
            cell[1][ins.stream] = ins
        return ins

    def emit(self, nc, es):
        sems = {}
        for s, lst in self.streams.items():
            sems[s] = es.enter_context(nc.semaphore("s_" + s))
            c = 0
            for ins in lst:
                if ins.is_dma:
                    c += 16
                elif ins.milestone:
                    c += 1
                ins.count = c
        streams = self.streams

        def run(eng_name, eng):
            for ins in self.eng_list[eng_name]:
                for (s, i) in ins.waits:
                    eng.wait_ge(sems[s], streams[s][i].count)
                if ins.fn is not None:
                    h = ins.fn(eng)
                    if ins.is_dma:
                        h.then_inc(sems[ins.stream], 16)
                    elif ins.milestone:
                        h.then_inc(sems[ins.stream], 1)

        with nc.Block() as block:
            @block.tensor
            def _(e):
                run("pe", e)

            @block.scalar
            def _(e):
                run("act", e)

            @block.vector
            def _(e):
                run("dve", e)

            @block.gpsimd
            def _(e):
                run("pool", e)

            @block.sync
            def _(e):
                run("sp", e)


def make_consts():
    c = {}
    c["ident"] = np.eye(128, dtype=np.float32)
    k = np.arange(128)[:, None]
    q = np.arange(128)[None, :]
    tri = (q >= k).astype(np.float32)
    c["tri4"] = np.tile(tri, (1, 4)).astype(np.float32)
    hh = np.arange(4, dtype=np.float64)
    log_g = np.log1p(-np.exp2(-5.0 - hh))
    p = np.arange(128, dtype=np.float64)
    xi = np.exp(log_g[None, :] * (p[:, None] + 1.0))
    rv = np.zeros((128, 8), np.float32)
    rv[:, 0:4] = (1.0 / xi) * (128.0 ** -0.5)
    rv[:, 4:8] = EPS / (xi * xi)
    c["retv"] = rv
    c["ret_decay"] = [float(np.exp(log_g[i] * 128.0)) for i in range(4)]
    sl = alibi_slopes(12)
    ab = np.zeros((128, 12, 16), np.float32)
    for h in range(12):
        for d in range(16):
            ab[:, h, d] = sl[h] * (p - 64.0 - 128.0 * d)
    ef = np.exp(sl[None, :] * (p[:, None] - 64.0)).astype(np.float32)
    c["alibi"] = np.concatenate([ab.reshape(128, 192), ef], axis=1).astype(np.float32)
    c["dbias"] = [[float(-128.0 * sl[h] * d) for d in range(16)] for h in range(12)]
    return c


class Builder:
    def __init__(self, stop=99):
        self.stop = stop
        self.nc = bass.Bass("TRN2", target_bir_lowering=False, dynamic_dma_scratch_size=8192)
        self.sc = Sched()
        self.es = ExitStack()
        self.ps_i = 0
        self.ring_i = 0
        self.cnt = {}
        self.consts = make_consts()

    def sb(self, name, shape, dt):
        return self.es.enter_context(self.nc.sbuf_tensor(name, shape, dt))

    def dram(self, name, shape, dt=F32, kind="ExternalInput"):
        return self.nc.dram_tensor(name, shape, dt, kind=kind).ap()

    def rot(self, name, n):
        i = self.cnt.get(name, 0)
        self.cnt[name] = i + 1
        return i % n

    def psum(self):
        i = self.ps_i % 8
        self.ps_i += 1
        return self.PS[i], ("ps", i)

    def add(self, *a, **k):
        return self.sc.add(*a, **k)

    def setup(self):
        nc = self.nc
        d = self.dram
        self.x = d("x", [S, D])
        self.mem = d("mem", [256, D])
        self.w_in_a = d("w_in_a", [D, 2816])
        self.ret_gain = d("ret_norm_gain", [768])
        self.w_out_a = d("w_out_a", [D, D])
        self.kv_gain = d("kv_norm_gain", [D])
        self.w_kv = d("w_kv_shared", [D, 1536])
        self.w_in_b = d("w_in_b", [D, D])
        self.w_out_b = d("w_out_b", [D, D])
        self.w_mkv = d("w_mem_kv", [2, D, 512])
        self.g_pre_mix = d("norm_pre_mix", [2, D])
        self.g_post_mix = d("norm_post_mix", [2, D])
        self.g_pre_mlp = d("norm_pre_mlp", [2, D])
        self.g_post_mlp = d("norm_post_mlp", [2, D])
        self.w_up = d("w_up", [2, D, DFF])
        self.w_down = d("w_down", [2, DFF, D])
        self.c_ident = d("c_ident", [128, 128])
        self.c_tri4 = d("c_tri4", [128, 512])
        self.c_retv = d("c_retv", [128, 8])
        self.c_alibi = d("c_alibi", [128, 204])
        self.out = d("out", [S, D], kind="ExternalOutput")

        sb = self.sb
        self.h = sb("h", [128, NT, D], F32)
        self.KT = sb("KT", [128, 6, S], BF16)
        self.V = sb("V", [128, 16, 12, 65], BF16)
        self.ring = sb("ring", [128, NRING, 8, 512], BF16)
        self.B1 = sb("B1", [128, 8, 1024], BF16)
        self.B2 = sb("B2", [128, 8, 1024], BF16)
        self.B3 = sb("B3", [128, 8 * 1024], F32)
        self.y = self.B3[:].rearrange("p (t n) -> p t n", n=1024)
        self.B3b = self.B3[:].bitcast(BF16).rearrange("p (t n) -> p t n", n=2048)
        self.qmT = sb("qmT", [128, 2, 1024], BF16)
        self.memT = sb("memT", [128, 8, 256], BF16)
        self.mkT = sb("mkT", [128, 2, 256], BF16)
        self.mv = sb("mv", [128, 2, 4, 65], BF16)
        self.gain = sb("gain", [128, 2, 1024], F32)
        self.state = sb("state", [128, 4, 192], F32)
        self.state_bf = sb("state_bf", [128, 4, 192], BF16)
        self.tmpf = sb("tmpf", [128, 1, 512], F32)
        self.hnb = sb("hnb", [128, 2, 1024], BF16)
        self.junk = sb("junk", [128, 192], BF16)
        self.PT = sb("PT", [128, 8, 512], BF16)
        self.PTm = sb("PTm", [128, 4, 512], BF16)
        self.ident = sb("ident", [128, 128], BF16)
        self.tri4 = sb("tri4", [128, 512], BF16)
        self.retv = sb("retv", [128, 8], F32)
        self.alibi = sb("alibi", [128, 204], F32)
        self.ksum = sb("ksum", [128, 6, 8], F32)
        self.kmT = sb("kmT", [128, 6, 8], BF16)
        self.st = sb("st", [128, 64], F32)
        self.gsb = sb("gsb", [128, 12, 8], F32)
        self.top8 = sb("top8", [128, 12, 8], F32)
        self.sel = sb("sel", [128, 2, 12, 8], F32)
        self.acc = sb("acc", [128, 4, 65], F32)
        self.PS = [self.es.enter_context(nc.psum_tensor("ps%d" % i, [128, 512], F32)) for i in range(8)]

        add = self.add
        ycell = lambda t: [("B3", t, s_) for s_ in range(8)]
        add("sp", lambda e: e.dma_start(out=self.y[:, 2, 0:128], in_=self.c_ident), writes=ycell(2), dma="d_c0")
        add("sp", lambda e: e.dma_start(out=self.y[:, 3, 0:512], in_=self.c_tri4), writes=ycell(3), dma="d_c1")
        add("sp", lambda e: e.dma_start(out=self.retv[:], in_=self.c_retv), writes=[("c", 2)], dma="d_c2")
        add("sp", lambda e: e.dma_start(out=self.alibi[:], in_=self.c_alibi), writes=[("c", 3)], dma="d_c3")
        add("dve", lambda e: e.tensor_copy(out=self.ident[:], in_=self.y[:, 2, 0:128]), reads=ycell(2), writes=[("ident",)])
        add("dve", lambda e: e.tensor_copy(out=self.tri4[:], in_=self.y[:, 3, 0:512]), reads=ycell(3), writes=[("tri4",)])
        for gt_ in range(16):
            add("dve", lambda e, gt_=gt_: e.tensor_copy(out=self.V[:, gt_, :, 64], in_=self.alibi[:, 192:204]),
                reads=[("c", 3)], writes=[("Vones", gt_)])
        add("dve", lambda e: e.memset(self.mv[:, :, :, 64:65], 1.0), writes=[("mvones",)])
        add("dve", lambda e: e.memset(self.state[:], 0.0), writes=[("state",)])
        add("dve", lambda e: e.memset(self.state_bf[:], 0.0), writes=[("state_bf",)])
        for mt in range(2):
            add("sp", lambda e, mt=mt: e.dma_start(out=self.y[:, mt, :], in_=self.mem[mt * 128:(mt + 1) * 128, :]),
                writes=ycell(mt), dma="d_mem%d" % mt)
            add("dve", lambda e, mt=mt: e.tensor_copy(out=self.hnb[:, 0, :], in_=self.y[:, mt, :]),
                reads=ycell(mt), writes=[("hnb", 0)])
            self.transpose8(self.hnb[:, 0, :], [("hnb", 0)], self.memT[:, :, mt * 128:(mt + 1) * 128], [("memT", mt)])

    def transpose8(self, src, src_cells, dst, dst_cells, eng="act"):
        ps, pc = self.psum()
        psb = ps[:].bitcast(BF16)
        for kc in range(8):
            self.add("pe", lambda e, kc=kc: e.transpose(out=psb[:, kc * 128:(kc + 1) * 128],
                                                        in_=src[:, kc * 128:(kc + 1) * 128], identity=self.ident[:]),
                     reads=list(src_cells) + [("ident",)], writes=[pc])
        pin = psb[:, 0:1024].rearrange("p (k c) -> p k c", c=128)
        if eng == "act":
            self.add("act", lambda e: e.copy(out=dst, in_=pin), reads=[pc], writes=dst_cells)
        else:
            self.add("dve", lambda e: e.tensor_copy(out=dst, in_=pin), reads=[pc], writes=dst_cells)

    def load_slab(self, wap, ncols=512):
        r = self.ring_i % NRING
        self.ring_i += 1
        src = wap.rearrange("(kc p) n -> p kc n", p=128)
        self.add("pool", lambda e: e.dma_start(out=self.ring[:, r, :, 0:ncols], in_=src),
                 writes=[("ring", r)], dma="d_ring%d" % r)
        return r

    def load_gain(self, gap, n=1024):
        sl = self.rot("gain", 2)
        self.add("sp", lambda e: e.dma_start(out=self.gain[:, sl, 0:n], in_=gap.partition_broadcast(128)),
                 writes=[("gain", sl)], dma="d_gain%d" % sl)
        return sl

    def stat(self):
        i = self.rot("st", 16)
        return self.st[:, i * 4:(i + 1) * 4], ("st", i)

    def norm_T(self, src, src_cells, gsl, dstT_ap, dst_cells):
        add = self.add
        st, stc = self.stat()
        sl = self.rot("hnb", 2)
        add("act", lambda e: e.activation(out=self.hnb[:, sl, :], in_=src, func=AF.Square, accum_out=st[:, 0:1]),
            reads=src_cells, writes=[stc, ("hnb", sl)])
        add("act", lambda e: e.activation(out=st[:, 1:2], in_=st[:, 0:1], func=AF.Sqrt, bias=self.epsc[:, 0:1], scale=1.0 / D),
            reads=[stc, ("epsc",)], writes=[stc])
        add("dve", lambda e: e.reciprocal(out=st[:, 2:3], in_=st[:, 1:2]), reads=[stc], writes=[stc])
        add("dve", lambda e: e.scalar_tensor_tensor(out=self.hnb[:, sl, :], in0=src, scalar=st[:, 2:3],
                                                    in1=self.gain[:, gsl, :], op0=ALU.mult, op1=ALU.mult),
            reads=list(src_cells) + [stc, ("gain", gsl)], writes=[("hnb", sl)])
        self.transpose8(self.hnb[:, sl, :], [("hnb", sl)], dstT_ap, dst_cells)

    def post_res(self, t, gsl):
        add = self.add
        st, stc = self.stat()
        ycells = [("B3", t, s) for s in range(8)]
        jsl = self.rot("hnb", 2)
        add("act", lambda e: e.activation(out=self.hnb[:, jsl, :], in_=self.y[:, t, :], func=AF.Square, accum_out=st[:, 0:1]),
            reads=ycells, writes=[stc, ("hnb", jsl)])
        add("act", lambda e: e.activation(out=st[:, 1:2], in_=st[:, 0:1], func=AF.Sqrt, bias=self.epsc[:, 0:1], scale=1.0 / D),
            reads=[stc, ("epsc",)], writes=[stc])
        add("dve", lambda e: e.reciprocal(out=st[:, 2:3], in_=st[:, 1:2]), reads=[stc], writes=[stc])
        add("dve", lambda e: e.scalar_tensor_tensor(out=self.y[:, t, :], in0=self.y[:, t, :], scalar=st[:, 2:3],
                                                    in1=self.gain[:, gsl, :], op0=ALU.mult, op1=ALU.mult),
            reads=ycells + [stc, ("gain", gsl)], writes=ycells)
        add("dve", lambda e: e.tensor_tensor(out=self.h[:, t, :], in0=self.h[:, t, :], in1=self.y[:, t, :], op=ALU.add),
            reads=[("h", t)] + ycells, writes=[("h", t)])

    def mm_B(self, r, j, xT, xcells_fn, grp, ncol=512, nk=8):
        ps, pc = self.psum()
        for kc in range(nk):
            self.add("pe", lambda e, kc=kc: e.matmul(ps[:, 0:ncol], lhsT=self.ring[:, r, kc, j * 128:(j + 1) * 128],
                                                     rhs=xT[:, kc, grp * ncol:(grp + 1) * ncol],
                                                     start=(kc == 0), stop=(kc == nk - 1)),
                     reads=[("ring", r)] + xcells_fn(kc, grp), writes=[pc])
        return ps, pc

    def mm_A(self, r, c0, n, xT, xcells_fn, t, nk=8):
        ps, pc = self.psum()
        for kc in range(nk):
            self.add("pe", lambda e, kc=kc: e.matmul(ps[:, 0:n], lhsT=xT[:, kc, t * 128:(t + 1) * 128],
                                                     rhs=self.ring[:, r, kc, c0:c0 + n],
                                                     start=(kc == 0), stop=(kc == nk - 1)),
                     reads=[("ring", r)] + xcells_fn(kc, t), writes=[pc])
        return ps, pc

    @staticmethod
    def cellsB1_grp(kc, grp):
        return [("B1", kc, grp * 4 + i) for i in range(4)]

    @staticmethod
    def cellsB1_t(kc, t):
        return [("B1", kc, t)]

    @staticmethod
    def cellsB2_grp(kc, grp):
        return [("B2", kc, grp * 4 + i) for i in range(4)]

    @staticmethod
    def cellsB2_t(kc, t):
        return [("B2", kc, t)]

    def mem_kv(self, l):
        add = self.add
        r = self.load_slab(self.w_mkv[l])
        for j in range(2):
            ps, pc = self.psum()
            for kc in range(8):
                add("pe", lambda e, kc=kc, ps=ps, j=j: e.matmul(ps[:, 0:256], lhsT=self.ring[:, r, kc, j * 128:(j + 1) * 128],
                                                               rhs=self.memT[:, kc, :], start=(kc == 0), stop=(kc == 7)),
                    reads=[("ring", r), ("memT", 0), ("memT", 1)], writes=[pc])
            add("act", lambda e, ps=ps, j=j: e.copy(out=self.mkT[:, j, :], in_=ps[:, 0:256]), reads=[pc], writes=[("mkT", j)])
        for mt in range(2):
            ps, pc = self.psum()
            for kc in range(8):
                add("pe", lambda e, kc=kc, ps=ps, mt=mt: e.matmul(ps[:, 0:256], lhsT=self.memT[:, kc, mt * 128:(mt + 1) * 128],
                                                                 rhs=self.ring[:, r, kc, 256:512], start=(kc == 0), stop=(kc == 7)),
                    reads=[("ring", r), ("memT", mt)], writes=[pc])
            add("act", lambda e, ps=ps, mt=mt: e.copy(out=self.mv[:, mt, :, 0:64],
                                                     in_=ps[:, 0:256].rearrange("p (h c) -> p h c", c=64)),
                reads=[pc, ("mvones",)], writes=[("mv", mt)])

    def mem_front(self, t):
        add = self.add
        pts = []
        for hh in range(2):
            ps, pc = self.psum()
            po = hh * 64
            for half in range(2):
                for mt in range(2):
                    sl = half * 2 + mt
                    add("pe", lambda e, ps=ps, sl=sl, po=po, half=half, mt=mt: e.matmul(
                        ps[:, sl * 128:(sl + 1) * 128], lhsT=self.mkT[po:po + 64, half, mt * 128:(mt + 1) * 128],
                        rhs=self.qmT[po:po + 64, half, t * 128:(t + 1) * 128], start=True, stop=True),
                        reads=[("mkT", half), ("qmT", half, t)], writes=[pc])
            psl = (t % 2) * 2 + hh
            add("act", lambda e, ps=ps, psl=psl: e.activation(out=self.PTm[:, psl, :], in_=ps[:, 0:512], func=AF.Exp, scale=0.125),
                reads=[pc], writes=[("PTm", psl)])
            pts.append(psl)
        return pts

    def mem_back(self, t, pts):
        add = self.add
        pso, poc = self.psum()
        for hd in range(4):
            psl = pts[hd % 2]
            for mt in range(2):
                sl = (hd // 2) * 2 + mt
                add("pe", lambda e, hd=hd, psl=psl, sl=sl, mt=mt: e.matmul(
                    pso[:, hd * 65:(hd + 1) * 65], lhsT=self.PTm[:, psl, sl * 128:(sl + 1) * 128],
                    rhs=self.mv[:, mt, hd, :], start=(mt == 0), stop=(mt == 1)),
                    reads=[("PTm", psl), ("mv", mt), ("mvones",)], writes=[poc])
        st, stc = self.stat()
        pv = pso[:, 0:260].rearrange("p (h c) -> p h c", c=65)
        add("dve", lambda e: e.reciprocal(out=st[:, 0:4], in_=pv[:, :, 64]), reads=[poc], writes=[stc])
        for hd in range(4):
            add("dve", lambda e, hd=hd: e.tensor_scalar(out=self.B1[:, t, 768 + hd * 64:768 + (hd + 1) * 64], in0=pv[:, hd, 0:64],
                                                        scalar1=st[:, hd:hd + 1], scalar2=None, op0=ALU.mult),
                reads=[poc, stc], writes=[("B1", t, 6 + hd // 2)])

    def mem_attn(self, t):
        self.mem_back(t, self.mem_front(t))

    def cat_to_T(self):
        for t in range(NT):
            self.transpose8(self.B1[:, t, :], [("B1", t, j) for j in range(8)],
                            self.B2[:, :, t * 128:(t + 1) * 128], [("B2", kc, t) for kc in range(8)])

    def out_proj(self, wap, g_post):
        add = self.add
        gsl = self.load_gain(g_post)
        for s in range(2):
            r = self.load_slab(wap[:, s * 512:(s + 1) * 512])
            for t in range(NT):
                ps, pc = self.mm_A(r, 0, 512, self.B2, self.cellsB2_t, t)
                add("act", lambda e, ps=ps, t=t, s=s: e.copy(out=self.y[:, t, s * 512:(s + 1) * 512], in_=ps[:, 0:512]),
                    reads=[pc], writes=[("B3", t, s * 4 + i) for i in range(4)])
                if s == 1:
                    self.post_res(t, gsl)

    def mlp(self, l):
        add = self.add
        gsl = self.load_gain(self.g_pre_mlp[l])
        for t in range(NT):
            self.norm_T(self.h[:, t, :], [("h", t)], gsl, self.B1[:, :, t * 128:(t + 1) * 128], [("B1", kc, t) for kc in range(8)])
        gpost = self.load_gain(self.g_post_mlp[l])
        for b in range(4):
            for s in range(2):
                r = self.load_slab(self.w_up[l][:, b * 1024 + s * 512: b * 1024 + (s + 1) * 512])
                for j in range(4):
                    for grp in range(2):
                        ps, pc = self.mm_B(r, j, self.B1, self.cellsB1_grp, grp)
                        dst = self.B2[:, s * 4 + j, grp * 512:(grp + 1) * 512]
                        dcells = [("B2", s * 4 + j, grp * 4 + i) for i in range(4)]
                        add("act", lambda e, ps=ps: e.activation(out=self.tmpf[:, 0, 0:512], in_=ps[:, 0:512], func=AF.Relu),
                            reads=[pc], writes=[("tmpf", 0)])
                        add("dve", lambda e, ps=ps, dst=dst: e.tensor_tensor(out=dst, in0=self.tmpf[:, 0, 0:512], in1=ps[:, 0:512], op=ALU.mult),
                            reads=[pc, ("tmpf", 0)], writes=dcells)
            for s in range(2):
                r = self.load_slab(self.w_down[l][b * 1024:(b + 1) * 1024, s * 512:(s + 1) * 512])
                for t in range(NT):
                    ps, pc = self.mm_A(r, 0, 512, self.B2, self.cellsB2_t, t)
                    yc = [("B3", t, s * 4 + i) for i in range(4)]
                    if b == 0:
                        add("act", lambda e, ps=ps, t=t, s=s: e.copy(out=self.y[:, t, s * 512:(s + 1) * 512], in_=ps[:, 0:512]),
                            reads=[pc], writes=yc)
                    else:
                        add("dve", lambda e, ps=ps, t=t, s=s: e.tensor_tensor(out=self.y[:, t, s * 512:(s + 1) * 512],
                                                                             in0=self.y[:, t, s * 512:(s + 1) * 512], in1=ps[:, 0:512], op=ALU.add),
                            reads=[pc] + yc, writes=yc)
                    if b == 3 and s == 1:
                        self.post_res(t, gpost)

    def layer0_mix(self, hf):
        add = self.add
        gsl = self.load_gain(self.g_pre_mix[0])
        for t in range(NT):
            self.norm_T(self.h[:, t, :], [("h", t)], gsl, self.B1[:, :, t * 128:(t + 1) * 128], [("B1", kc, t) for kc in range(8)])
        if self.stop < 0.15: return
        self.mem_kv(0)
        if self.stop < 0.25: return
        W = self.w_in_a
        r = self.load_slab(W[:, 0:512])
        for j in range(4):
            for grp in range(2):
                ps, pc = self.mm_B(r, j, self.B1, self.cellsB1_grp, grp)
                add("act", lambda e, ps=ps, j=j, grp=grp: e.copy(out=self.B2[:, j, grp * 512:(grp + 1) * 512], in_=ps[:, 0:512]),
                    reads=[pc], writes=[("B2", j, grp * 4 + i) for i in range(4)])
        if self.stop < 0.35: return
        r = self.load_slab(W[:, 512:1024])
        for j in range(4):
            for grp in range(2):
                ps, pc = self.mm_B(r, j, self.B1, self.cellsB1_grp, grp)
                add("act", lambda e, ps=ps, j=j, grp=grp: e.copy(out=self.B2[:, 4 + j, grp * 512:(grp + 1) * 512], in_=ps[:, 0:512]),
                    reads=[pc], writes=[("B2", 4 + j, grp * 4 + i) for i in range(4)])
        for t in range(NT):
            ps, pc = self.mm_A(r, 0, 512, self.B1, self.cellsB1_t, t)
            add("dve", lambda e, ps=ps, t=t: e.tensor_copy(out=self.B3b[:, t, 0:512], in_=ps[:, 0:512]),
                reads=[pc], writes=[("B3", t, 0), ("B3", t, 1)])
        if self.stop < 0.45: return
        rgs = self.load_gain(self.ret_gain, 768)
        for si in range(2, 5):
            r = self.load_slab(W[:, si * 512:(si + 1) * 512])
            for t in range(NT):
                ps, pc = self.mm_A(r, 0, 512, self.B1, self.cellsB1_t, t)
                c0 = si * 512 - 1024
                bounds = sorted(set([c0, c0 + 512] + [b for b in range(0, 1537, 192) if c0 < b < c0 + 512]))
                for a, bnd in zip(bounds[:-1], bounds[1:]):
                    lo, hi = a - c0, bnd - c0
                    if a < 768:
                        hd = a // 192
                        add("act", lambda e, ps=ps, t=t, lo=lo, hi=hi, a=a, bnd=bnd, hd=hd: e.activation(
                            out=self.B3b[:, t, 512 + a:512 + bnd], in_=ps[:, lo:hi], func=AF.Copy, scale=self.retv[:, hd:hd + 1]),
                            reads=[pc, ("c", 2)], writes=[("B3", t, s) for s in range((512 + a) // 256, (512 + bnd - 1) // 256 + 1)])
                    else:
                        ga, gb = a - 768, bnd - 768
                        sl = 0
                        add("act", lambda e, ps=ps, lo=lo, hi=hi, sl=sl: e.activation(out=self.tmpf[:, sl, 0:hi - lo], in_=ps[:, lo:hi], func=AF.Silu),
                            reads=[pc], writes=[("tmpf", sl)])
                        add("dve", lambda e, t=t, ga=ga, gb=gb, sl=sl: e.tensor_tensor(
                            out=self.B3b[:, t, 1280 + ga:1280 + gb], in0=self.tmpf[:, sl, 0:gb - ga], in1=self.gain[:, rgs, ga:gb], op=ALU.mult),
                            reads=[("tmpf", sl), ("gain", rgs)], writes=[("B3", t, s) for s in range((1280 + ga) // 256, (1280 + gb - 1) // 256 + 1)])
        if self.stop < 0.55: return
        r = self.load_slab(W[:, 2560:2816], ncols=256)
        for j in range(2):
            for grp in range(2):
                ps, pc = self.mm_B(r, j, self.B1, self.cellsB1_grp, grp)
                add("act", lambda e, ps=ps, j=j, grp=grp: e.copy(out=self.qmT[:, j, grp * 512:(grp + 1) * 512], in_=ps[:, 0:512]),
                    reads=[pc], writes=[("qmT", j, grp * 4 + i) for i in range(4)])
        if self.stop < 0.65: return
        dec = self.consts["ret_decay"]
        def front(t):
            tok = slice(t * 128, (t + 1) * 128)
            ps, pc = self.psum()
            for hd in range(4):
                add("pe", lambda e, ps=ps, hd=hd, tok=tok: e.matmul(ps[:, hd * 128:(hd + 1) * 128], lhsT=self.B2[:, 4 + hd, tok],
                                                                   rhs=self.B2[:, hd, tok], start=True, stop=True),
                    reads=[("B2", 4 + hd, t), ("B2", hd, t)], writes=[pc])
            psl = self.rot("PT", 8)
            add("dve", lambda e, ps=ps, psl=psl: e.tensor_tensor(out=self.PT[:, psl, :], in0=ps[:, 0:512], in1=self.tri4[:], op=ALU.mult),
                reads=[pc, ("tri4",)], writes=[("PT", psl)])
            return psl, self.mem_front(t)

        def back(t, psl, mf):
            tok = slice(t * 128, (t + 1) * 128)
            vcells = [("B3", t, 2), ("B3", t, 3), ("B3", t, 4)]
            pos = []
            for pair in range(2):
                po_, poc = self.psum()
                pos.append((po_, poc))
                for hh in range(2):
                    hd = pair * 2 + hh
                    add("pe", lambda e, po_=po_, hd=hd, hh=hh, psl=psl, t=t: e.matmul(
                        po_[:, hh * 192:(hh + 1) * 192], lhsT=self.PT[:, psl, hd * 128:(hd + 1) * 128],
                        rhs=self.B3b[:, t, 512 + hd * 192:512 + (hd + 1) * 192], start=True, stop=False),
                        reads=[("PT", psl)] + vcells, writes=[poc])
                    add("pe", lambda e, po_=po_, hd=hd, hh=hh, tok=tok: e.matmul(
                        po_[:, hh * 192:(hh + 1) * 192], lhsT=self.B2[:, hd, tok], rhs=self.state_bf[:, hd, :], start=False, stop=True),
                        reads=[("B2", hd, t), ("state_bf",)], writes=[poc])
            for pair in range(2):
                pk, pkc = self.psum()
                for hh in range(2):
                    hd = pair * 2 + hh
                    add("pe", lambda e, pk=pk, hd=hd, hh=hh, t=t: e.matmul(
                        pk[:, hh * 192:(hh + 1) * 192], lhsT=self.B3b[:, t, hd * 128:(hd + 1) * 128],
                        rhs=self.B3b[:, t, 512 + hd * 192:512 + (hd + 1) * 192], start=True, stop=True),
                        reads=[("B3", t, 0), ("B3", t, 1)] + vcells, writes=[pkc])
                for hh in range(2):
                    hd = pair * 2 + hh
                    add("dve", lambda e, pk=pk, hd=hd, hh=hh: e.scalar_tensor_tensor(
                        out=self.state[:, hd, :], in0=pk[:, hh * 192:(hh + 1) * 192], scalar=1.0, in1=self.state[:, hd, :],
                        op0=ALU.mult, op1=ALU.add), reads=[pkc, ("state",)], writes=[("state",)])
                    add("dve", lambda e, hd=hd: e.tensor_scalar(out=self.state[:, hd, :], in0=self.state[:, hd, :], scalar1=dec[hd],
                                                               scalar2=None, op0=ALU.mult), reads=[("state",)], writes=[("state",)])
                    add("dve", lambda e, hd=hd: e.tensor_copy(out=self.state_bf[:, hd, :], in_=self.state[:, hd, :]),
                        reads=[("state",)], writes=[("state_bf",)])
            st, stc = self.stat()
            for hd in range(4):
                po_, poc = pos[hd // 2]
                hh = hd % 2
                add("act", lambda e, po_=po_, hh=hh, hd=hd: e.activation(out=self.junk[:, 0:192], in_=po_[:, hh * 192:(hh + 1) * 192],
                                                                       func=AF.Square, accum_out=st[:, hd:hd + 1]),
                    reads=[poc], writes=[stc])
            st2, stc2 = self.stat()
            add("dve", lambda e: e.scalar_tensor_tensor(out=st2[:, 0:4], in0=st[:, 0:4], scalar=1.0 / 192.0, in1=self.retv[:, 4:8],
                                                        op0=ALU.mult, op1=ALU.add), reads=[stc, ("c", 2)], writes=[stc2])
            add("act", lambda e: e.activation(out=st2[:, 0:4], in_=st2[:, 0:4], func=AF.Sqrt), reads=[stc2], writes=[stc2])
            st3, stc3 = self.stat()
            add("dve", lambda e: e.reciprocal(out=st3[:, 0:4], in_=st2[:, 0:4]), reads=[stc2], writes=[stc3])
            for hd in range(4):
                po_, poc = pos[hd // 2]
                hh = hd % 2
                c0, c1 = hd * 192, (hd + 1) * 192
                add("dve", lambda e, po_=po_, hh=hh, hd=hd, c0=c0, c1=c1, t=t: e.scalar_tensor_tensor(
                    out=self.B1[:, t, c0:c1], in0=po_[:, hh * 192:(hh + 1) * 192], scalar=st3[:, hd:hd + 1],
                    in1=self.B3b[:, t, 1280 + c0:1280 + c1], op0=ALU.mult, op1=ALU.mult),
                    reads=[poc, stc3, ("B3", t, 5), ("B3", t, 6), ("B3", t, 7)],
                    writes=[("B1", t, j) for j in range(c0 // 128, (c1 - 1) // 128 + 1)])
            self.mem_back(t, mf)

        f = front(0)
        for t in range(NT):
            fn = front(t + 1) if t + 1 < NT else None
            back(t, *f)
            f = fn
        if self.stop < 0.75: return
        self.cat_to_T()
        if self.stop < 0.85: return
        self.out_proj(self.w_out_a, self.g_post_mix[0])

    def layer1_mix(self, hf):
        add = self.add
        gsl = self.load_gain(self.kv_gain)
        for t in range(NT):
            self.norm_T(self.h[:, t, :], [("h", t)], gsl, self.B1[:, :, t * 128:(t + 1) * 128], [("B1", kc, t) for kc in range(8)])
        W = self.w_kv
        for si in range(3):
            r = self.load_slab(W[:, si * 512:(si + 1) * 512])
            for j in range(4):
                col = si * 512 + j * 128
                if col >= 768:
                    continue
                c = col // 128
                for grp in range(2):
                    ps, pc = self.mm_B(r, j, self.B1, self.cellsB1_grp, grp)
                    for bb in range(2):
                        blk = hf * 4 + grp * 2 + bb
                        g0 = hf * T + grp * 512 + bb * 256
                        add("act", lambda e, ps=ps, c=c, g0=g0, bb=bb, blk=blk: e.activation(
                            out=self.KT[:, c, g0:g0 + 256], in_=ps[:, bb * 256:(bb + 1) * 256], func=AF.Copy,
                            accum_out=self.ksum[:, c, blk:blk + 1]),
                            reads=[pc], writes=[("KT", c, blk), ("ksum", c, blk)])
            v0 = max(si * 512, 768)
            v1 = (si + 1) * 512
            if v1 > v0:
                n = v1 - v0
                h0 = (v0 - 768) // 64
                nh = n // 64
                for t in range(NT):
                    gt = hf * NT + t
                    ps, pc = self.mm_A(r, v0 - si * 512, n, self.B1, self.cellsB1_t, t)
                    for j_ in range(nh):
                        hd_ = h0 + j_
                        add("dve", lambda e, ps=ps, gt=gt, hd_=hd_, j_=j_: e.tensor_scalar(
                            out=self.V[:, gt, hd_, 0:64], in0=ps[:, j_ * 64:(j_ + 1) * 64], scalar1=self.alibi[:, 192 + hd_:193 + hd_],
                            scalar2=None, op0=ALU.mult),
                            reads=[pc, ("c", 3)], writes=[("V", gt, hd_ // 4)])
        gsl = self.load_gain(self.g_pre_mix[1])
        for t in range(NT):
            self.norm_T(self.h[:, t, :], [("h", t)], gsl, self.B1[:, :, t * 128:(t + 1) * 128], [("B1", kc, t) for kc in range(8)])
        self.mem_kv(1)
        W = self.w_in_b
        for si in range(2):
            r = self.load_slab(W[:, si * 512:(si + 1) * 512])
            for j in range(4):
                col = si * 512 + j * 128
                for grp in range(2):
                    ps, pc = self.mm_B(r, j, self.B1, self.cellsB1_grp, grp)
                    if col < 768:
                        c = col // 128
                        add("act", lambda e, ps=ps, c=c, grp=grp: e.copy(out=self.B2[:, c, grp * 512:(grp + 1) * 512], in_=ps[:, 0:512]),
                            reads=[pc], writes=[("B2", c, grp * 4 + i) for i in range(4)])
                    else:
                        c = (col - 768) // 128
                        add("act", lambda e, ps=ps, c=c, grp=grp: e.copy(out=self.qmT[:, c, grp * 512:(grp + 1) * 512], in_=ps[:, 0:512]),
                            reads=[pc], writes=[("qmT", c, grp * 4 + i) for i in range(4)])
        if hf == 1:
            kc_all = [("ksum", c, b) for c in range(6) for b in range(8)]
            add("act", lambda e: e.activation(out=self.kmT[:], in_=self.ksum[:], func=AF.Copy, scale=1.0 / 256.0),
                reads=kc_all, writes=[("kmT",)])
        prev = None
        for t in range(NT):
            gt = hf * NT + t
            qb = gt // 2
            if hf == 1:
                self.moba_gate(t, qb)
            mf = self.mem_front(t)
            for hd in range(12):
                cur = self.moba_front(hf, t, hd)
                if prev is not None:
                    self.moba_back(*prev)
                prev = (hf, t, hd, cur)
            self.mem_back(t, mf)
        self.moba_back(*prev)
        self.cat_to_T()
        self.out_proj(self.w_out_b, self.g_post_mix[1])

    def moba_gate(self, t, qb):
        add = self.add
        gv = self.gsb[:].rearrange("p (c two) b -> p c two b", two=2)
        for hh in range(2):
            ps, pc = self.psum()
            po = hh * 64
            for c in range(6):
                add("pe", lambda e, ps=ps, c=c, po=po: e.matmul(ps[:, c * 8:(c + 1) * 8], lhsT=self.B2[po:po + 64, c, t * 128:(t + 1) * 128],
                                                               rhs=self.kmT[po:po + 64, c, :], start=True, stop=True),
                    reads=[("B2", c, t), ("kmT",)], writes=[pc])
            add("act", lambda e, ps=ps, hh=hh: e.copy(out=gv[:, :, hh, :], in_=ps[:, 0:48].rearrange("p (c b) -> p c b", b=8)),
                reads=[pc], writes=[("gsb",)])
        if qb < 8:
            add("dve", lambda e: e.memset(self.gsb[:, :, qb:8], -1e30), reads=[("gsb",)], writes=[("gsb",)])
        for hd in range(12):
            add("dve", lambda e, hd=hd: e.max(out=self.top8[:, hd, :], in_=self.gsb[:, hd, :]), reads=[("gsb",)], writes=[("top8", hd)])
            add("dve", lambda e, hd=hd: e.tensor_scalar(out=self.sel[:, t % 2, hd, :], in0=self.gsb[:, hd, :], scalar1=self.top8[:, hd, 2:3],
                                                        scalar2=None, op0=ALU.is_ge), reads=[("gsb",), ("top8", hd)], writes=[("sel", t % 2, hd)])

    def moba_front(self, hf, t, hd):
        add = self.add
        gt = hf * NT + t
        qb = gt // 2
        c, po = hd // 2, (hd % 2) * 64
        qap = self.B2[po:po + 64, c, t * 128:(t + 1) * 128]
        kts = list(range(gt + 1))
        pt_of = {}
        for i0 in range(0, len(kts), 4):
            grp = kts[i0:i0 + 4]
            ps, pc = self.psum()
            for i, kt in enumerate(grp):
                add("pe", lambda e, ps=ps, i=i, kt=kt: e.matmul(ps[:, i * 128:(i + 1) * 128], lhsT=self.KT[po:po + 64, c, kt * 128:(kt + 1) * 128],
                                                               rhs=qap, start=True, stop=True),
                    reads=[("KT", c, kt // 2), ("B2", c, t)], writes=[pc])
            psl = self.rot("PT", 8)
            for i, kt in enumerate(grp):
                dd = gt - kt
                add("act", lambda e, ps=ps, i=i, psl=psl, dd=dd: e.activation(
                    out=self.PT[:, psl, i * 128:(i + 1) * 128], in_=ps[:, i * 128:(i + 1) * 128], func=AF.Exp,
                    bias=self.consts["dbias"][hd][dd], scale=0.125),
                    reads=[pc], writes=[("PT", psl)])
                if kt == gt:
                    add("dve", lambda e, psl=psl, i=i: e.tensor_tensor(out=self.PT[:, psl, i * 128:(i + 1) * 128],
                                                                       in0=self.PT[:, psl, i * 128:(i + 1) * 128], in1=self.tri4[:, 0:128], op=ALU.mult),
                        reads=[("PT", psl), ("tri4",)], writes=[("PT", psl)])
                pt_of[kt] = (psl, i)
        return pt_of

    def moba_back(self, hf, t, hd, pt_of):
        add = self.add
        gt = hf * NT + t
        qb = gt // 2
        kts = list(range(gt + 1))
        pso, poc = self.psum()
        asl = self.rot("acc", 4)
        if hf == 0:
            for n_, kt in enumerate(kts):
                psl, i = pt_of[kt]
                add("pe", lambda e, psl=psl, i=i, kt=kt, n_=n_: e.matmul(pso[:, 0:65], lhsT=self.PT[:, psl, i * 128:(i + 1) * 128],
                                                                        rhs=self.V[:, kt, hd, :], start=(n_ == 0), stop=(n_ == len(kts) - 1)),
                    reads=[("PT", psl), ("V", kt, hd // 4), ("Vones", kt)], writes=[poc])
            src = pso
            srcc = poc
            res = pso[:, 0:65]
        else:
            psB, pbc = self.psum()
            own = [kt for kt in kts if kt // 2 == qb]
            for n_, kt in enumerate(own):
                psl, i = pt_of[kt]
                add("pe", lambda e, psl=psl, i=i, kt=kt, n_=n_: e.matmul(psB[:, 0:65], lhsT=self.PT[:, psl, i * 128:(i + 1) * 128],
                                                                        rhs=self.V[:, kt, hd, :], start=(n_ == 0), stop=(n_ == len(own) - 1)),
                    reads=[("PT", psl), ("V", kt, hd // 4), ("Vones", kt)], writes=[pbc])
            for b in range(qb):
                for n_, kt in enumerate((2 * b, 2 * b + 1)):
                    psl, i = pt_of[kt]
                    add("pe", lambda e, psl=psl, i=i, kt=kt, n_=n_, b=b: e.matmul(pso[:, b * 65:(b + 1) * 65], lhsT=self.PT[:, psl, i * 128:(i + 1) * 128],
                                                                                 rhs=self.V[:, kt, hd, :], start=(n_ == 0), stop=(n_ == 1)),
                        reads=[("PT", psl), ("V", kt, hd // 4), ("Vones", kt)], writes=[poc])
            accap = self.acc[:, asl, :]
            add("dve", lambda e: e.tensor_copy(out=accap, in_=psB[:, 0:65]), reads=[pbc], writes=[("acc", asl)])
            for b in range(qb):
                add("dve", lambda e, b=b: e.scalar_tensor_tensor(out=accap, in0=pso[:, b * 65:(b + 1) * 65], scalar=self.sel[:, t % 2, hd, b:b + 1],
                                                                 in1=accap, op0=ALU.mult, op1=ALU.add),
                    reads=[poc, ("sel", t % 2, hd), ("acc", asl)], writes=[("acc", asl)])
            res = accap
            srcc = ("acc", asl)
        st, stc = self.stat()
        add("dve", lambda e: e.reciprocal(out=st[:, 0:1], in_=res[:, 64:65]), reads=[srcc], writes=[stc])
        add("dve", lambda e: e.tensor_scalar(out=self.B1[:, t, hd * 64:(hd + 1) * 64], in0=res[:, 0:64], scalar1=st[:, 0:1],
                                             scalar2=None, op0=ALU.mult), reads=[srcc, stc], writes=[("B1", t, hd // 2)])

    def build(self):
        add = self.add
        self.setup()
        self.epsc = self.sb("epsc", [128, 1], F32)
        add("dve", lambda e: e.memset(self.epsc[:], EPS), writes=[("epsc",)])
        for hf in range(NH):
            for t in range(NT):
                gt = hf * NT + t
                add("sp", lambda e, t=t, gt=gt: e.dma_start(out=self.h[:, t, :], in_=self.x[gt * 128:(gt + 1) * 128, :]),
                    writes=[("h", t)], dma="d_h%d" % t)
            if self.stop > 0:
                self.layer0_mix(hf)
            if self.stop >= 2:
                self.mlp(0)
            if self.stop >= 3:
                self.layer1_mix(hf)
            if self.stop >= 4:
                self.mlp(1)
            for t in range(NT):
                gt = hf * NT + t
                add("sp", lambda e, t=t, gt=gt: e.dma_start(out=self.out[gt * 128:(gt + 1) * 128, :], in_=self.h[:, t, :]),
                    reads=[("h", t)], dma="d_o%d" % t)
        add("sp", None, writes=[("h", t) for t in range(NT)])
        self.sc.emit(self.nc, self.es)
        self.es.close()
        return self.nc


_CACHE = {}


def get_nc(stop=99):
    if stop not in _CACHE:
        _CACHE[stop] = Builder(stop).build()
    return _CACHE[stop]


def kernel(stop=99, ncores=8, **inputs):
    stop = float(stop)
    nc = get_nc(stop)
    cs = make_consts()
    f = lambda a: np.ascontiguousarray(np.asarray(a, dtype=np.float32))
    shared = {
        "w_in_a": f(inputs["w_in_a"][0]), "ret_norm_gain": f(inputs["ret_norm_gain"][0]), "w_out_a": f(inputs["w_out_a"][0]),
        "kv_norm_gain": f(inputs["kv_norm_gain"]), "w_kv_shared": f(inputs["w_kv_shared"]), "w_in_b": f(inputs["w_in_b"][0]),
        "w_out_b": f(inputs["w_out_b"][0]), "w_mem_kv": f(inputs["w_mem_kv"]), "norm_pre_mix": f(inputs["norm_pre_mix"]),
        "norm_post_mix": f(inputs["norm_post_mix"]), "norm_pre_mlp": f(inputs["norm_pre_mlp"]),
        "norm_post_mlp": f(inputs["norm_post_mlp"]), "w_up": f(inputs["w_up"]), "w_down": f(inputs["w_down"]),
        "c_ident": cs["ident"], "c_tri4": cs["tri4"], "c_retv": cs["retv"], "c_alibi": cs["alibi"],
    }
    x = f(inputs["x"])
    mem = f(inputs["mem"])
    in_maps = []
    for b in range(ncores):
        m = dict(shared)
        m["x"] = x[b]
        m["mem"] = mem[b]
        in_maps.append(m)
    res = run_bass_kernel_spmd(nc, in_maps, core_ids=list(range(ncores)))
    return np.stack([np.asarray(r["out"], dtype=np.float32) for r in res.results], axis=0)
```

```python
import math
from contextlib import ExitStack
import numpy as np
import concourse.bass as bass
import concourse.mybir as mybir
from concourse.alu_op_type import AluOpType as ALU
from concourse.bass_utils import run_bass_kernel_spmd

F32 = mybir.dt.float32
BF16 = mybir.dt.bfloat16
AF = mybir.ActivationFunctionType
AX = mybir.AxisListType

D = 1024
S = 2048
T = 1024
NT = 8
NH = 2
DFF = 4096
EPS = 1e-6
NRING = 3
ENGS = ("pe", "act", "dve", "pool", "sp")


def alibi_slopes(n):
    def pow2(m):
        return [2.0 ** (-8.0 * (i + 1) / m) for i in range(m)]
    p = 2 ** int(math.floor(math.log2(n)))
    s = pow2(p)
    if p < n:
        s = s + pow2(2 * p)[0::2][: n - p]
    return np.asarray(s, dtype=np.float64)


class Ins:
    __slots__ = ("eng", "fn", "waits", "stream", "sidx", "milestone", "count", "is_dma")


class Sched:
    def __init__(self):
        self.cells = {}
        self.eng_list = {e: [] for e in ENGS}
        self.streams = {}
        self.seen = {e: {} for e in ENGS}

    def add(self, eng, fn, reads=(), writes=(), dma=None):
        ins = Ins()
        ins.eng = eng
        ins.fn = fn
        ins.is_dma = dma is not None
        ins.stream = dma if dma else eng
        need = {}

        def dep(d, war):
            if d is None:
                return
            if (not ins.is_dma) and (not d.is_dma) and d.eng == eng:
                if eng == "pe" or war:
                    return
            if need.get(d.stream, -1) < d.sidx:
                need[d.stream] = d.sidx

        for c in reads:
            cell = self.cells.get(c)
            if cell:
                dep(cell[0], False)
        for c in writes:
            cell = self.cells.get(c)
            if cell:
                dep(cell[0], False)
                for r in cell[1].values():
                    dep(r, True)
        waits = []
        seen = self.seen[eng]
        for s, i in need.items():
            if seen.get(s, -1) >= i:
                continue
            seen[s] = i
            waits.append((s, i))
            self.streams[s][i].milestone = True
        ins.waits = waits
        lst = self.streams.setdefault(ins.stream, [])
        ins.sidx = len(lst)
        lst.append(ins)
        ins.milestone = ins.is_dma
        self.eng_list[eng].append(ins)
        for c in writes:
            self.cells[c] = [ins, {}]
        for c in reads:
            cell = self.cells.setdefault(c, [None, {}])
            cell[1][ins.stream] = ins
        return ins

    def emit(self, nc, es):
        sems = {}
        for s, lst in self.streams.items():
            sems[s] = es.enter_context(nc.semaphore("s_" + s))
            c = 0
            for ins in lst:
                if ins.is_dma:
                    c += 16
                elif ins.milestone:
                    c += 1
                ins.count = c
        streams = self.streams

        def run(eng_name, eng):
            for ins in self.eng_list[eng_name]:
                for (s, i) in ins.waits:
                    eng.wait_ge(sems[s], streams[s][i].count)
                if ins.fn is not None:
                    h = ins.fn(eng)
                    if ins.is_dma:
                        h.then_inc(sems[ins.stream], 16)
                    elif ins.milestone:
                        h.then_inc(sems[ins.stream], 1)

        with nc.Block() as block:
            @block.tensor
            def _(e):
                run("pe", e)

            @block.scalar
            def _(e):
                run("act", e)

            @block.vector
            def _(e):
                run("dve", e)

            @block.gpsimd
            def _(e):
                run("pool", e)

            @block.sync
            def _(e):
                run("sp", e)


def make_consts():
    c = {}
    c["ident"] = np.eye(128, dtype=np.float32)
    k = np.arange(128)[:, None]
    q = np.arange(128)[None, :]
    tri = (q >= k).astype(np.float32)
    c["tri4"] = np.tile(tri, (1, 4)).astype(np.float32)
    hh = np.arange(4, dtype=np.float64)
    log_g = np.log1p(-np.exp2(-5.0 - hh))
    p = np.arange(128, dtype=np.float64)
    xi = np.exp(log_g[None, :] * (p[:, None] + 1.0))
    rv = np.zeros((128, 8), np.float32)
    rv[:, 0:4] = (1.0 / xi) * (128.0 ** -0.5)
    rv[:, 4:8] = EPS / (xi * xi)
    c["retv"] = rv
    c["ret_decay"] = [float(np.exp(log_g[i] * 128.0)) for i in range(4)]
    sl = alibi_slopes(12)
    ab = np.zeros((128, 12, 16), np.float32)
    for h in range(12):
        for d in range(16):
            ab[:, h, d] = sl[h] * (p - 64.0 - 128.0 * d)
    ef = np.exp(sl[None, :] * (p[:, None] - 64.0)).astype(np.float32)
    c["alibi"] = np.concatenate([ab.reshape(128, 192), ef], axis=1).astype(np.float32)
    c["dbias"] = [[float(-128.0 * sl[h] * d) for d in range(16)] for h in range(12)]
    return c


class Builder:
    def __init__(self, stop=99):
        self.stop = stop
        self.nc = bass.Bass("TRN2", target_bir_lowering=False, dynamic_dma_scratch_size=8192)
        self.sc = Sched()
        self.es = ExitStack()
        self.ps_i = 0
        self.ring_i = 0
        self.cnt = {}
        self.consts = make_consts()

    def sb(self, name, shape, dt):
        return self.es.enter_context(self.nc.sbuf_tensor(name, shape, dt))

    def dram(self, name, shape, dt=F32, kind="ExternalInput"):
        return self.nc.dram_tensor(name, shape, dt, kind=kind).ap()

    def rot(self, name, n):
        i = self.cnt.get(name, 0)
        self.cnt[name] = i + 1
        return i % n

    def psum(self):
        i = self.ps_i % 8
        self.ps_i += 1
        return self.PS[i], ("ps", i)

    def add(self, *a, **k):
        return self.sc.add(*a, **k)

    def setup(self):
        nc = self.nc
        d = self.dram
        self.x = d("x", [S, D])
        self.mem = d("mem", [256, D])
        self.w_in_a = d("w_in_a", [D, 2816])
        self.ret_gain = d("ret_norm_gain", [768])
        self.w_out_a = d("w_out_a", [D, D])
        self.kv_gain = d("kv_norm_gain", [D])
        self.w_kv = d("w_kv_shared", [D, 1536])
        self.w_in_b = d("w_in_b", [D, D])
        self.w_out_b = d("w_out_b", [D, D])
        self.w_mkv = d("w_mem_kv", [2, D, 512])
        self.g_pre_mix = d("norm_pre_mix", [2, D])
        self.g_post_mix = d("norm_post_mix", [2, D])
        self.g_pre_mlp = d("norm_pre_mlp", [2, D])
        self.g_post_mlp = d("norm_post_mlp", [2, D])
        self.w_up = d("w_up", [2, D, DFF])
        self.w_down = d("w_down", [2, DFF, D])
        self.c_ident = d("c_ident", [128, 128])
        self.c_tri4 = d("c_tri4", [128, 512])
        self.c_retv = d("c_retv", [128, 8])
        self.c_alibi = d("c_alibi", [128, 204])
        self.out = d("out", [S, D], kind="ExternalOutput")

        sb = self.sb
        self.h = sb("h", [128, NT, D], F32)
        self.KT = sb("KT", [128, 6, S], BF16)
        self.V = sb("V", [128, 16, 12, 65], BF16)
        self.ring = sb("ring", [128, NRING, 8, 512], BF16)
        self.B1 = sb("B1", [128, 8, 1024], BF16)
        self.B2 = sb("B2", [128, 8, 1024], BF16)
        self.B3 = sb("B3", [128, 8 * 1024], F32)
        self.y = self.B3[:].rearrange("p (t n) -> p t n", n=1024)
        self.B3b = self.B3[:].bitcast(BF16).rearrange("p (t n) -> p t n", n=2048)
        self.qmT = sb("qmT", [128, 2, 1024], BF16)
        self.memT = sb("memT", [128, 8, 256], BF16)
        self.mkT = sb("mkT", [128, 2, 256], BF16)
        self.mv = sb("mv", [128, 2, 4, 65], BF16)
        self.gain = sb("gain", [128, 2, 1024], F32)
        self.state = sb("state", [128, 4, 192], F32)
        self.state_bf = sb("state_bf", [128, 4, 192], BF16)
        self.tmpf = sb("tmpf", [128, 1, 512], F32)
        self.hnb = sb("hnb", [128, 2, 1024], BF16)
        self.junk = sb("junk", [128, 192], BF16)
        self.PT = sb("PT", [128, 8, 512], BF16)
        self.PTm = sb("PTm", [128, 4, 512], BF16)
        self.ident = sb("ident", [128, 128], BF16)
        self.tri4 = sb("tri4", [128, 512], BF16)
        self.retv = sb("retv", [128, 8], F32)
        self.alibi = sb("alibi", [128, 204], F32)
        self.ksum = sb("ksum", [128, 6, 8], F32)
        self.kmT = sb("kmT", [128, 6, 8], BF16)
        self.st = sb("st", [128, 64], F32)
        self.gsb = sb("gsb", [128, 12, 8], F32)
        self.top8 = sb("top8", [128, 12, 8], F32)
        self.sel = sb("sel", [128, 2, 12, 8], F32)
        self.acc = sb("acc", [128, 4, 65], F32)
        self.PS = [self.es.enter_context(nc.psum_tensor("ps%d" % i, [128, 512], F32)) for i in range(8)]

        add = self.add
        ycell = lambda t: [("B3", t, s_) for s_ in range(8)]
        add("sp", lambda e: e.dma_start(out=self.y[:, 2, 0:128], in_=self.c_ident), writes=ycell(2), dma="d_c0")
        add("sp", lambda e: e.dma_start(out=self.y[:, 3, 0:512], in_=self.c_tri4), writes=ycell(3), dma="d_c1")
        add("sp", lambda e: e.dma_start(out=self.retv[:], in_=self.c_retv), writes=[("c", 2)], dma="d_c2")
        add("sp", lambda e: e.dma_start(out=self.alibi[:], in_=self.c_alibi), writes=[("c", 3)], dma="d_c3")
        add("dve", lambda e: e.tensor_copy(out=self.ident[:], in_=self.y[:, 2, 0:128]), reads=ycell(2), writes=[("ident",)])
        add("dve", lambda e: e.tensor_copy(out=self.tri4[:], in_=self.y[:, 3, 0:512]), reads=ycell(3), writes=[("tri4",)])
        for gt_ in range(16):
            add("dve", lambda e, gt_=gt_: e.tensor_copy(out=self.V[:, gt_, :, 64], in_=self.alibi[:, 192:204]),
                reads=[("c", 3)], writes=[("Vones", gt_)])
        add("dve", lambda e: e.memset(self.mv[:, :, :, 64:65], 1.0), writes=[("mvones",)])
        add("dve", lambda e: e.memset(self.state[:], 0.0), writes=[("state",)])
        add("dve", lambda e: e.memset(self.state_bf[:], 0.0), writes=[("state_bf",)])
        for mt in range(2):
            add("sp", lambda e, mt=mt: e.dma_start(out=self.y[:, mt, :], in_=self.mem[mt * 128:(mt + 1) * 128, :]),
                writes=ycell(mt), dma="d_mem%d" % mt)
            add("dve", lambda e, mt=mt: e.tensor_copy(out=self.hnb[:, 0, :], in_=self.y[:, mt, :]),
                reads=ycell(mt), writes=[("hnb", 0)])
            self.transpose8(self.hnb[:, 0, :], [("hnb", 0)], self.memT[:, :, mt * 128:(mt + 1) * 128], [("memT", mt)])

    def transpose8(self, src, src_cells, dst, dst_cells, eng="act"):
        ps, pc = self.psum()
        psb = ps[:].bitcast(BF16)
        for kc in range(8):
            self.add("pe", lambda e, kc=kc: e.transpose(out=psb[:, kc * 128:(kc + 1) * 128],
                                                        in_=src[:, kc * 128:(kc + 1) * 128], identity=self.ident[:]),
                     reads=list(src_cells) + [("ident",)], writes=[pc])
        pin = psb[:, 0:1024].rearrange("p (k c) -> p k c", c=128)
        if eng == "act":
            self.add("act", lambda e: e.copy(out=dst, in_=pin), reads=[pc], writes=dst_cells)
        else:
            self.add("dve", lambda e: e.tensor_copy(out=dst, in_=pin), reads=[pc], writes=dst_cells)

    def load_slab(self, wap, ncols=512):
        r = self.ring_i % NRING
        self.ring_i += 1
        src = wap.rearrange("(kc p) n -> p kc n", p=128)
        self.add("pool", lambda e: e.dma_start(out=self.ring[:, r, :, 0:ncols], in_=src),
                 writes=[("ring", r)], dma="d_ring%d" % r)
        return r

    def load_gain(self, gap, n=1024):
        sl = self.rot("gain", 2)
        self.add("sp", lambda e: e.dma_start(out=self.gain[:, sl, 0:n], in_=gap.partition_broadcast(128)),
                 writes=[("gain", sl)], dma="d_gain%d" % sl)
        return sl

    def stat(self):
        i = self.rot("st", 16)
        return self.st[:, i * 4:(i + 1) * 4], ("st", i)

    def norm_A(self, src, src_cells, gsl):
        add = self.add
        st, stc = self.stat()
        sl = self.rot("hnb", 2)
        add("act", lambda e: e.activation(out=self.hnb[:, sl, :], in_=src, func=AF.Square, accum_out=st[:, 0:1]),
            reads=src_cells, writes=[stc, ("hnb", sl)])
        add("act", lambda e: e.activation(out=st[:, 1:2], in_=st[:, 0:1], func=AF.Sqrt, bias=self.epsc[:, 0:1], scale=1.0 / D),
            reads=[stc, ("epsc",)], writes=[stc])
        add("dve", lambda e: e.reciprocal(out=st[:, 2:3], in_=st[:, 1:2]), reads=[stc], writes=[stc])
        add("dve", lambda e: e.scalar_tensor_tensor(out=self.hnb[:, sl, :], in0=src, scalar=st[:, 2:3],
                                                    in1=self.gain[:, gsl, :], op0=ALU.mult, op1=ALU.mult),
            reads=list(src_cells) + [stc, ("gain", gsl)], writes=[("hnb", sl)])
        return sl

    def norm_phase(self, gsl):
        sl = self.norm_A(self.h[:, 0, :], [("h", 0)], gsl)
        for t in range(NT):
            nsl = self.norm_A(self.h[:, t + 1, :], [("h", t + 1)], gsl) if t + 1 < NT else None
            self.transpose8(self.hnb[:, sl, :], [("hnb", sl)], self.B1[:, :, t * 128:(t + 1) * 128],
                            [("B1", kc, t) for kc in range(8)], eng=("act" if t % 2 == 0 else "dve"))
            sl = nsl

    def post_res(self, t, gsl):
        add = self.add
        st, stc = self.stat()
        ycells = [("B3", t, s) for s in range(8)]
        jsl = self.rot("hnb", 2)
        add("act", lambda e: e.activation(out=self.hnb[:, jsl, :], in_=self.y[:, t, :], func=AF.Square, accum_out=st[:, 0:1]),
            reads=ycells, writes=[stc, ("hnb", jsl)])
        add("act", lambda e: e.activation(out=st[:, 1:2], in_=st[:, 0:1], func=AF.Sqrt, bias=self.epsc[:, 0:1], scale=1.0 / D),
            reads=[stc, ("epsc",)], writes=[stc])
        add("dve", lambda e: e.reciprocal(out=st[:, 2:3], in_=st[:, 1:2]), reads=[stc], writes=[stc])
        add("dve", lambda e: e.scalar_tensor_tensor(out=self.y[:, t, :], in0=self.y[:, t, :], scalar=st[:, 2:3],
                                                    in1=self.gain[:, gsl, :], op0=ALU.mult, op1=ALU.mult),
            reads=ycells + [stc, ("gain", gsl)], writes=ycells)
        add("dve", lambda e: e.tensor_tensor(out=self.h[:, t, :], in0=self.h[:, t, :], in1=self.y[:, t, :], op=ALU.add),
            reads=[("h", t)] + ycells, writes=[("h", t)])

    def mm_B(self, r, j, xT, xcells_fn, grp, ncol=512, nk=8):
        ps, pc = self.psum()
        for kc in range(nk):
            self.add("pe", lambda e, kc=kc: e.matmul(ps[:, 0:ncol], lhsT=self.ring[:, r, kc, j * 128:(j + 1) * 128],
                                                     rhs=xT[:, kc, grp * ncol:(grp + 1) * ncol],
                                                     start=(kc == 0), stop=(kc == nk - 1)),
                     reads=[("ring", r)] + xcells_fn(kc, grp), writes=[pc])
        return ps, pc

    def mm_A(self, r, c0, n, xT, xcells_fn, t, nk=8):
        ps, pc = self.psum()
        for kc in range(nk):
            self.add("pe", lambda e, kc=kc: e.matmul(ps[:, 0:n], lhsT=xT[:, kc, t * 128:(t + 1) * 128],
                                                     rhs=self.ring[:, r, kc, c0:c0 + n],
                                                     start=(kc == 0), stop=(kc == nk - 1)),
                     reads=[("ring", r)] + xcells_fn(kc, t), writes=[pc])
        return ps, pc

    @staticmethod
    def cellsB1_grp(kc, grp):
        return [("B1", kc, grp * 4 + i) for i in range(4)]

    @staticmethod
    def cellsB1_t(kc, t):
        return [("B1", kc, t)]

    @staticmethod
    def cellsB2_grp(kc, grp):
        return [("B2", kc, grp * 4 + i) for i in range(4)]

    @staticmethod
    def cellsB2_t(kc, t):
        return [("B2", kc, t)]

    def mem_kv(self, l):
        add = self.add
        r = self.load_slab(self.w_mkv[l])
        for j in range(2):
            ps, pc = self.psum()
            for kc in range(8):
                add("pe", lambda e, kc=kc, ps=ps, j=j: e.matmul(ps[:, 0:256], lhsT=self.ring[:, r, kc, j * 128:(j + 1) * 128],
                                                               rhs=self.memT[:, kc, :], start=(kc == 0), stop=(kc == 7)),
                    reads=[("ring", r), ("memT", 0), ("memT", 1)], writes=[pc])
            add("act", lambda e, ps=ps, j=j: e.copy(out=self.mkT[:, j, :], in_=ps[:, 0:256]), reads=[pc], writes=[("mkT", j)])
        for mt in range(2):
            ps, pc = self.psum()
            for kc in range(8):
                add("pe", lambda e, kc=kc, ps=ps, mt=mt: e.matmul(ps[:, 0:256], lhsT=self.memT[:, kc, mt * 128:(mt + 1) * 128],
                                                                 rhs=self.ring[:, r, kc, 256:512], start=(kc == 0), stop=(kc == 7)),
                    reads=[("ring", r), ("memT", mt)], writes=[pc])
            add("act", lambda e, ps=ps, mt=mt: e.copy(out=self.mv[:, mt, :, 0:64],
                                                     in_=ps[:, 0:256].rearrange("p (h c) -> p h c", c=64)),
                reads=[pc, ("mvones",)], writes=[("mv", mt)])

    def mem_front(self, t):
        add = self.add
        pts = []
        for hh in range(2):
            ps, pc = self.psum()
            po = hh * 64
            for half in range(2):
                for mt in range(2):
                    sl = half * 2 + mt
                    add("pe", lambda e, ps=ps, sl=sl, po=po, half=half, mt=mt: e.matmul(
                        ps[:, sl * 128:(sl + 1) * 128], lhsT=self.mkT[po:po + 64, half, mt * 128:(mt + 1) * 128],
                        rhs=self.qmT[po:po + 64, half, t * 128:(t + 1) * 128], start=True, stop=True),
                        reads=[("mkT", half), ("qmT", half, t)], writes=[pc])
            psl = (t % 2) * 2 + hh
            add("act", lambda e, ps=ps, psl=psl: e.activation(out=self.PTm[:, psl, :], in_=ps[:, 0:512], func=AF.Exp, scale=0.125),
                reads=[pc], writes=[("PTm", psl)])
            pts.append(psl)
        return pts

    def mem_back(self, t, pts):
        add = self.add
        pso, poc = self.psum()
        for hd in range(4):
            psl = pts[hd % 2]
            for mt in range(2):
                sl = (hd // 2) * 2 + mt
                add("pe", lambda e, hd=hd, psl=psl, sl=sl, mt=mt: e.matmul(
                    pso[:, hd * 65:(hd + 1) * 65], lhsT=self.PTm[:, psl, sl * 128:(sl + 1) * 128],
                    rhs=self.mv[:, mt, hd, :], start=(mt == 0), stop=(mt == 1)),
                    reads=[("PTm", psl), ("mv", mt), ("mvones",)], writes=[poc])
        st, stc = self.stat()
        pv = pso[:, 0:260].rearrange("p (h c) -> p h c", c=65)
        add("dve", lambda e: e.reciprocal(out=st[:, 0:4], in_=pv[:, :, 64]), reads=[poc], writes=[stc])
        for hd in range(4):
            add("dve", lambda e, hd=hd: e.tensor_scalar(out=self.B1[:, t, 768 + hd * 64:768 + (hd + 1) * 64], in0=pv[:, hd, 0:64],
                                                        scalar1=st[:, hd:hd + 1], scalar2=None, op0=ALU.mult),
                reads=[poc, stc], writes=[("B1", t, 6 + hd // 2)])

    def mem_attn(self, t):
        self.mem_back(t, self.mem_front(t))

    def cat_to_T(self):
        for t in range(NT):
            self.transpose8(self.B1[:, t, :], [("B1", t, j) for j in range(8)],
                            self.B2[:, :, t * 128:(t + 1) * 128], [("B2", kc, t) for kc in range(8)])

    def out_proj(self, wap, g_post):
        add = self.add
        gsl = self.load_gain(g_post)
        for s in range(2):
            r = self.load_slab(wap[:, s * 512:(s + 1) * 512])
            for t in range(NT):
                ps, pc = self.mm_A(r, 0, 512, self.B2, self.cellsB2_t, t)
                add("act", lambda e, ps=ps, t=t, s=s: e.copy(out=self.y[:, t, s * 512:(s + 1) * 512], in_=ps[:, 0:512]),
                    reads=[pc], writes=[("B3", t, s * 4 + i) for i in range(4)])
                if s == 1:
                    self.post_res(t, gsl)

    def mlp(self, l):
        add = self.add
        gsl = self.load_gain(self.g_pre_mlp[l])
        self.norm_phase(gsl)
        gpost = self.load_gain(self.g_post_mlp[l])
        for b in range(4):
            for s in range(2):
                r = self.load_slab(self.w_up[l][:, b * 1024 + s * 512: b * 1024 + (s + 1) * 512])
                for j in range(4):
                    for grp in range(2):
                        ps, pc = self.mm_B(r, j, self.B1, self.cellsB1_grp, grp)
                        dst = self.B2[:, s * 4 + j, grp * 512:(grp + 1) * 512]
                        dcells = [("B2", s * 4 + j, grp * 4 + i) for i in range(4)]
                        add("act", lambda e, ps=ps: e.activation(out=self.tmpf[:, 0, 0:512], in_=ps[:, 0:512], func=AF.Relu),
                            reads=[pc], writes=[("tmpf", 0)])
                        add("dve", lambda e, ps=ps, dst=dst: e.tensor_tensor(out=dst, in0=self.tmpf[:, 0, 0:512], in1=ps[:, 0:512], op=ALU.mult),
                            reads=[pc, ("tmpf", 0)], writes=dcells)
            for s in range(2):
                r = self.load_slab(self.w_down[l][b * 1024:(b + 1) * 1024, s * 512:(s + 1) * 512])
                for t in range(NT):
                    ps, pc = self.mm_A(r, 0, 512, self.B2, self.cellsB2_t, t)
                    yc = [("B3", t, s * 4 + i) for i in range(4)]
                    if b == 0:
                        add("act", lambda e, ps=ps, t=t, s=s: e.copy(out=self.y[:, t, s * 512:(s + 1) * 512], in_=ps[:, 0:512]),
                            reads=[pc], writes=yc)
                    else:
                        add("dve", lambda e, ps=ps, t=t, s=s: e.tensor_tensor(out=self.y[:, t, s * 512:(s + 1) * 512],
                                                                             in0=self.y[:, t, s * 512:(s + 1) * 512], in1=ps[:, 0:512], op=ALU.add),
                            reads=[pc] + yc, writes=yc)
                    if b == 3 and s == 1:
                        self.post_res(t, gpost)

    def layer0_mix(self, hf):
        add = self.add
        gsl = self.load_gain(self.g_pre_mix[0])
        self.norm_phase(gsl)
        if self.stop < 0.15: return
        self.mem_kv(0)
        if self.stop < 0.25: return
        W = self.w_in_a
        r = self.load_slab(W[:, 0:512])
        for j in range(4):
            for grp in range(2):
                ps, pc = self.mm_B(r, j, self.B1, self.cellsB1_grp, grp)
                add("act", lambda e, ps=ps, j=j, grp=grp: e.copy(out=self.B2[:, j, grp * 512:(grp + 1) * 512], in_=ps[:, 0:512]),
                    reads=[pc], writes=[("B2", j, grp * 4 + i) for i in range(4)])
        if self.stop < 0.35: return
        r = self.load_slab(W[:, 512:1024])
        for j in range(4):
            for grp in range(2):
                ps, pc = self.mm_B(r, j, self.B1, self.cellsB1_grp, grp)
                add("act", lambda e, ps=ps, j=j, grp=grp: e.copy(out=self.B2[:, 4 + j, grp * 512:(grp + 1) * 512], in_=ps[:, 0:512]),
                    reads=[pc], writes=[("B2", 4 + j, grp * 4 + i) for i in range(4)])
        for t in range(NT):
            ps, pc = self.mm_A(r, 0, 512, self.B1, self.cellsB1_t, t)
            add("dve", lambda e, ps=ps, t=t: e.tensor_copy(out=self.B3b[:, t, 0:512], in_=ps[:, 0:512]),
                reads=[pc], writes=[("B3", t, 0), ("B3", t, 1)])
        if self.stop < 0.45: return
        rgs = self.load_gain(self.ret_gain, 768)
        for si in range(2, 5):
            r = self.load_slab(W[:, si * 512:(si + 1) * 512])
            for t in range(NT):
                ps, pc = self.mm_A(r, 0, 512, self.B1, self.cellsB1_t, t)
                c0 = si * 512 - 1024
                bounds = sorted(set([c0, c0 + 512] + [b for b in range(0, 1537, 192) if c0 < b < c0 + 512]))
                for a, bnd in zip(bounds[:-1], bounds[1:]):
                    lo, hi = a - c0, bnd - c0
                    if a < 768:
                        hd = a // 192
                        add("act", lambda e, ps=ps, t=t, lo=lo, hi=hi, a=a, bnd=bnd, hd=hd: e.activation(
                            out=self.B3b[:, t, 512 + a:512 + bnd], in_=ps[:, lo:hi], func=AF.Copy, scale=self.retv[:, hd:hd + 1]),
                            reads=[pc, ("c", 2)], writes=[("B3", t, s) for s in range((512 + a) // 256, (512 + bnd - 1) // 256 + 1)])
                    else:
                        ga, gb = a - 768, bnd - 768
                        sl = 0
                        add("act", lambda e, ps=ps, lo=lo, hi=hi, sl=sl: e.activation(out=self.tmpf[:, sl, 0:hi - lo], in_=ps[:, lo:hi], func=AF.Silu),
                            reads=[pc], writes=[("tmpf", sl)])
                        add("dve", lambda e, t=t, ga=ga, gb=gb, sl=sl: e.tensor_tensor(
                            out=self.B3b[:, t, 1280 + ga:1280 + gb], in0=self.tmpf[:, sl, 0:gb - ga], in1=self.gain[:, rgs, ga:gb], op=ALU.mult),
                            reads=[("tmpf", sl), ("gain", rgs)], writes=[("B3", t, s) for s in range((1280 + ga) // 256, (1280 + gb - 1) // 256 + 1)])
        if self.stop < 0.55: return
        r = self.load_slab(W[:, 2560:2816], ncols=256)
        for j in range(2):
            for grp in range(2):
                ps, pc = self.mm_B(r, j, self.B1, self.cellsB1_grp, grp)
                add("act", lambda e, ps=ps, j=j, grp=grp: e.copy(out=self.qmT[:, j, grp * 512:(grp + 1) * 512], in_=ps[:, 0:512]),
                    reads=[pc], writes=[("qmT", j, grp * 4 + i) for i in range(4)])
        if self.stop < 0.65: return
        dec = self.consts["ret_decay"]
        def front(t):
            tok = slice(t * 128, (t + 1) * 128)
            ps, pc = self.psum()
            for hd in range(4):
                add("pe", lambda e, ps=ps, hd=hd, tok=tok: e.matmul(ps[:, hd * 128:(hd + 1) * 128], lhsT=self.B2[:, 4 + hd, tok],
                                                                   rhs=self.B2[:, hd, tok], start=True, stop=True),
                    reads=[("B2", 4 + hd, t), ("B2", hd, t)], writes=[pc])
            psl = self.rot("PT", 8)
            add("dve", lambda e, ps=ps, psl=psl: e.tensor_tensor(out=self.PT[:, psl, :], in0=ps[:, 0:512], in1=self.tri4[:], op=ALU.mult),
                reads=[pc, ("tri4",)], writes=[("PT", psl)])
            return psl, self.mem_front(t)

        def back(t, psl, mf):
            tok = slice(t * 128, (t + 1) * 128)
            vcells = [("B3", t, 2), ("B3", t, 3), ("B3", t, 4)]
            pos = []
            for pair in range(2):
                po_, poc = self.psum()
                pos.append((po_, poc))
                for hh in range(2):
                    hd = pair * 2 + hh
                    add("pe", lambda e, po_=po_, hd=hd, hh=hh, psl=psl, t=t: e.matmul(
                        po_[:, hh * 192:(hh + 1) * 192], lhsT=self.PT[:, psl, hd * 128:(hd + 1) * 128],
                        rhs=self.B3b[:, t, 512 + hd * 192:512 + (hd + 1) * 192], start=True, stop=False),
                        reads=[("PT", psl)] + vcells, writes=[poc])
                    add("pe", lambda e, po_=po_, hd=hd, hh=hh, tok=tok: e.matmul(
                        po_[:, hh * 192:(hh + 1) * 192], lhsT=self.B2[:, hd, tok], rhs=self.state_bf[:, hd, :], start=False, stop=True),
                        reads=[("B2", hd, t), ("state_bf",)], writes=[poc])
            for pair in range(2):
                pk, pkc = self.psum()
                for hh in range(2):
                    hd = pair * 2 + hh
                    add("pe", lambda e, pk=pk, hd=hd, hh=hh, t=t: e.matmul(
                        pk[:, hh * 192:(hh + 1) * 192], lhsT=self.B3b[:, t, hd * 128:(hd + 1) * 128],
                        rhs=self.B3b[:, t, 512 + hd * 192:512 + (hd + 1) * 192], start=True, stop=True),
                        reads=[("B3", t, 0), ("B3", t, 1)] + vcells, writes=[pkc])
                for hh in range(2):
                    hd = pair * 2 + hh
                    add("dve", lambda e, pk=pk, hd=hd, hh=hh: e.scalar_tensor_tensor(
                        out=self.state[:, hd, :], in0=pk[:, hh * 192:(hh + 1) * 192], scalar=1.0, in1=self.state[:, hd, :],
                        op0=ALU.mult, op1=ALU.add), reads=[pkc, ("state",)], writes=[("state",)])
                    add("dve", lambda e, hd=hd: e.tensor_scalar(out=self.state[:, hd, :], in0=self.state[:, hd, :], scalar1=dec[hd],
                                                               scalar2=None, op0=ALU.mult), reads=[("state",)], writes=[("state",)])
                    add("dve", lambda e, hd=hd: e.tensor_copy(out=self.state_bf[:, hd, :], in_=self.state[:, hd, :]),
                        reads=[("state",)], writes=[("state_bf",)])
            st, stc = self.stat()
            for hd in range(4):
                po_, poc = pos[hd // 2]
                hh = hd % 2
                add("act", lambda e, po_=po_, hh=hh, hd=hd: e.activation(out=self.junk[:, 0:192], in_=po_[:, hh * 192:(hh + 1) * 192],
                                                                       func=AF.Square, accum_out=st[:, hd:hd + 1]),
                    reads=[poc], writes=[stc])
            st2, stc2 = self.stat()
            add("dve", lambda e: e.scalar_tensor_tensor(out=st2[:, 0:4], in0=st[:, 0:4], scalar=1.0 / 192.0, in1=self.retv[:, 4:8],
                                                        op0=ALU.mult, op1=ALU.add), reads=[stc, ("c", 2)], writes=[stc2])
            add("act", lambda e: e.activation(out=st2[:, 0:4], in_=st2[:, 0:4], func=AF.Sqrt), reads=[stc2], writes=[stc2])
            st3, stc3 = self.stat()
            add("dve", lambda e: e.reciprocal(out=st3[:, 0:4], in_=st2[:, 0:4]), reads=[stc2], writes=[stc3])
            for hd in range(4):
                po_, poc = pos[hd // 2]
                hh = hd % 2
                c0, c1 = hd * 192, (hd + 1) * 192
                add("dve", lambda e, po_=po_, hh=hh, hd=hd, c0=c0, c1=c1, t=t: e.scalar_tensor_tensor(
                    out=self.B1[:, t, c0:c1], in0=po_[:, hh * 192:(hh + 1) * 192], scalar=st3[:, hd:hd + 1],
                    in1=self.B3b[:, t, 1280 + c0:1280 + c1], op0=ALU.mult, op1=ALU.mult),
                    reads=[poc, stc3, ("B3", t, 5), ("B3", t, 6), ("B3", t, 7)],
                    writes=[("B1", t, j) for j in range(c0 // 128, (c1 - 1) // 128 + 1)])
            self.mem_back(t, mf)

        f = front(0)
        for t in range(NT):
            fn = front(t + 1) if t + 1 < NT else None
            back(t, *f)
            f = fn
        if self.stop < 0.75: return
        self.cat_to_T()
        if self.stop < 0.85: return
        self.out_proj(self.w_out_a, self.g_post_mix[0])

    def layer1_mix(self, hf):
        add = self.add
        gsl = self.load_gain(self.kv_gain)
        self.norm_phase(gsl)
        W = self.w_kv
        for si in range(3):
            r = self.load_slab(W[:, si * 512:(si + 1) * 512])
            for j in range(4):
                col = si * 512 + j * 128
                if col >= 768:
                    continue
                c = col // 128
                for grp in range(2):
                    ps, pc = self.mm_B(r, j, self.B1, self.cellsB1_grp, grp)
                    for bb in range(2):
                        blk = hf * 4 + grp * 2 + bb
                        g0 = hf * T + grp * 512 + bb * 256
                        add("act", lambda e, ps=ps, c=c, g0=g0, bb=bb, blk=blk: e.activation(
                            out=self.KT[:, c, g0:g0 + 256], in_=ps[:, bb * 256:(bb + 1) * 256], func=AF.Copy,
                            accum_out=self.ksum[:, c, blk:blk + 1]),
                            reads=[pc], writes=[("KT", c, blk), ("ksum", c, blk)])
            v0 = max(si * 512, 768)
            v1 = (si + 1) * 512
            if v1 > v0:
                n = v1 - v0
                h0 = (v0 - 768) // 64
                nh = n // 64
                for t in range(NT):
                    gt = hf * NT + t
                    ps, pc = self.mm_A(r, v0 - si * 512, n, self.B1, self.cellsB1_t, t)
                    for j_ in range(nh):
                        hd_ = h0 + j_
                        add("dve", lambda e, ps=ps, gt=gt, hd_=hd_, j_=j_: e.tensor_scalar(
                            out=self.V[:, gt, hd_, 0:64], in0=ps[:, j_ * 64:(j_ + 1) * 64], scalar1=self.alibi[:, 192 + hd_:193 + hd_],
                            scalar2=None, op0=ALU.mult),
                            reads=[pc, ("c", 3)], writes=[("V", gt, hd_ // 4)])
        gsl = self.load_gain(self.g_pre_mix[1])
        self.norm_phase(gsl)
        self.mem_kv(1)
        W = self.w_in_b
        for si in range(2):
            r = self.load_slab(W[:, si * 512:(si + 1) * 512])
            for j in range(4):
                col = si * 512 + j * 128
                for grp in range(2):
                    ps, pc = self.mm_B(r, j, self.B1, self.cellsB1_grp, grp)
                    if col < 768:
                        c = col // 128
                        add("act", lambda e, ps=ps, c=c, grp=grp: e.copy(out=self.B2[:, c, grp * 512:(grp + 1) * 512], in_=ps[:, 0:512]),
                            reads=[pc], writes=[("B2", c, grp * 4 + i) for i in range(4)])
                    else:
                        c = (col - 768) // 128
                        add("act", lambda e, ps=ps, c=c, grp=grp: e.copy(out=self.qmT[:, c, grp * 512:(grp + 1) * 512], in_=ps[:, 0:512]),
                            reads=[pc], writes=[("qmT", c, grp * 4 + i) for i in range(4)])
        if hf == 1:
            kc_all = [("ksum", c, b) for c in range(6) for b in range(8)]
            add("act", lambda e: e.activation(out=self.kmT[:], in_=self.ksum[:], func=AF.Copy, scale=1.0 / 256.0),
                reads=kc_all, writes=[("kmT",)])
        prev = None
        for t in range(NT):
            gt = hf * NT + t
            qb = gt // 2
            if hf == 1:
                self.moba_gate(t, qb)
            mf = self.mem_front(t)
            for hd in range(12):
                cur = self.moba_front(hf, t, hd)
                if prev is not None:
                    self.moba_back(*prev)
                prev = (hf, t, hd, cur)
            self.mem_back(t, mf)
        self.moba_back(*prev)
        self.cat_to_T()
        self.out_proj(self.w_out_b, self.g_post_mix[1])

    def moba_gate(self, t, qb):
        add = self.add
        gv = self.gsb[:].rearrange("p (c two) b -> p c two b", two=2)
        for hh in range(2):
            ps, pc = self.psum()
            po = hh * 64
            for c in range(6):
                add("pe", lambda e, ps=ps, c=c, po=po: e.matmul(ps[:, c * 8:(c + 1) * 8], lhsT=self.B2[po:po + 64, c, t * 128:(t + 1) * 128],
                                                               rhs=self.kmT[po:po + 64, c, :], start=True, stop=True),
                    reads=[("B2", c, t), ("kmT",)], writes=[pc])
            add("act", lambda e, ps=ps, hh=hh: e.copy(out=gv[:, :, hh, :], in_=ps[:, 0:48].rearrange("p (c b) -> p c b", b=8)),
                reads=[pc], writes=[("gsb",)])
        if qb < 8:
            add("dve", lambda e: e.memset(self.gsb[:, :, qb:8], -1e30), reads=[("gsb",)], writes=[("gsb",)])
        for hd in range(12):
            add("dve", lambda e, hd=hd: e.max(out=self.top8[:, hd, :], in_=self.gsb[:, hd, :]), reads=[("gsb",)], writes=[("top8", hd)])
            add("dve", lambda e, hd=hd: e.tensor_scalar(out=self.sel[:, t % 2, hd, :], in0=self.gsb[:, hd, :], scalar1=self.top8[:, hd, 2:3],
                                                        scalar2=None, op0=ALU.is_ge), reads=[("gsb",), ("top8", hd)], writes=[("sel", t % 2, hd)])

    def moba_front(self, hf, t, hd):
        add = self.add
        gt = hf * NT + t
        qb = gt // 2
        c, po = hd // 2, (hd % 2) * 64
        qap = self.B2[po:po + 64, c, t * 128:(t + 1) * 128]
        kts = list(range(gt + 1))
        pt_of = {}
        for i0 in range(0, len(kts), 4):
            grp = kts[i0:i0 + 4]
            ps, pc = self.psum()
            for i, kt in enumerate(grp):
                add("pe", lambda e, ps=ps, i=i, kt=kt: e.matmul(ps[:, i * 128:(i + 1) * 128], lhsT=self.KT[po:po + 64, c, kt * 128:(kt + 1) * 128],
                                                               rhs=qap, start=True, stop=True),
                    reads=[("KT", c, kt // 2), ("B2", c, t)], writes=[pc])
            psl = self.rot("PT", 8)
            for i, kt in enumerate(grp):
                dd = gt - kt
                add("act", lambda e, ps=ps, i=i, psl=psl, dd=dd: e.activation(
                    out=self.PT[:, psl, i * 128:(i + 1) * 128], in_=ps[:, i * 128:(i + 1) * 128], func=AF.Exp,
                    bias=self.consts["dbias"][hd][dd], scale=0.125),
                    reads=[pc], writes=[("PT", psl)])
                if kt == gt:
                    add("dve", lambda e, psl=psl, i=i: e.tensor_tensor(out=self.PT[:, psl, i * 128:(i + 1) * 128],
                                                                       in0=self.PT[:, psl, i * 128:(i + 1) * 128], in1=self.tri4[:, 0:128], op=ALU.mult),
                        reads=[("PT", psl), ("tri4",)], writes=[("PT", psl)])
                pt_of[kt] = (psl, i)
        return pt_of

    def moba_back(self, hf, t, hd, pt_of):
        add = self.add
        gt = hf * NT + t
        qb = gt // 2
        kts = list(range(gt + 1))
        pso, poc = self.psum()
        asl = self.rot("acc", 4)
        if hf == 0:
            for n_, kt in enumerate(kts):
                psl, i = pt_of[kt]
                add("pe", lambda e, psl=psl, i=i, kt=kt, n_=n_: e.matmul(pso[:, 0:65], lhsT=self.PT[:, psl, i * 128:(i + 1) * 128],
                                                                        rhs=self.V[:, kt, hd, :], start=(n_ == 0), stop=(n_ == len(kts) - 1)),
                    reads=[("PT", psl), ("V", kt, hd // 4), ("Vones", kt)], writes=[poc])
            src = pso
            srcc = poc
            res = pso[:, 0:65]
        else:
            psB, pbc = self.psum()
            own = [kt for kt in kts if kt // 2 == qb]
            for n_, kt in enumerate(own):
                psl, i = pt_of[kt]
                add("pe", lambda e, psl=psl, i=i, kt=kt, n_=n_: e.matmul(psB[:, 0:65], lhsT=self.PT[:, psl, i * 128:(i + 1) * 128],
                                                                        rhs=self.V[:, kt, hd, :], start=(n_ == 0), stop=(n_ == len(own) - 1)),
                    reads=[("PT", psl), ("V", kt, hd // 4), ("Vones", kt)], writes=[pbc])
            for b in range(qb):
                for n_, kt in enumerate((2 * b, 2 * b + 1)):
                    psl, i = pt_of[kt]
                    add("pe", lambda e, psl=psl, i=i, kt=kt, n_=n_, b=b: e.matmul(pso[:, b * 65:(b + 1) * 65], lhsT=self.PT[:, psl, i * 128:(i + 1) * 128],
                                                                                 rhs=self.V[:, kt, hd, :], start=(n_ == 0), stop=(n_ == 1)),
                        reads=[("PT", psl), ("V", kt, hd // 4), ("Vones", kt)], writes=[poc])
            accap = self.acc[:, asl, :]
            add("dve", lambda e: e.tensor_copy(out=accap, in_=psB[:, 0:65]), reads=[pbc], writes=[("acc", asl)])
            for b in range(qb):
                add("dve", lambda e, b=b: e.scalar_tensor_tensor(out=accap, in0=pso[:, b * 65:(b + 1) * 65], scalar=self.sel[:, t % 2, hd, b:b + 1],
                                                                 in1=accap, op0=ALU.mult, op1=ALU.add),
                    reads=[poc, ("sel", t % 2, hd), ("acc", asl)], writes=[("acc", asl)])
            res = accap
            srcc = ("acc", asl)
        st, stc = self.stat()
        add("dve", lambda e: e.reciprocal(out=st[:, 0:1], in_=res[:, 64:65]), reads=[srcc], writes=[stc])
        add("dve", lambda e: e.tensor_scalar(out=self.B1[:, t, hd * 64:(hd + 1) * 64], in0=res[:, 0:64], scalar1=st[:, 0:1],
                                             scalar2=None, op0=ALU.mult), reads=[srcc, stc], writes=[("B1", t, hd // 2)])

    def build(self):
        add = self.add
        self.setup()
        self.epsc = self.sb("epsc", [128, 1], F32)
        add("dve", lambda e: e.memset(self.epsc[:], EPS), writes=[("epsc",)])
        for hf in range(NH):
            for t in range(NT):
                gt = hf * NT + t
                add("sp", lambda e, t=t, gt=gt: e.dma_start(out=self.h[:, t, :], in_=self.x[gt * 128:(gt + 1) * 128, :]),
                    writes=[("h", t)], dma="d_h%d" % t)
            if self.stop > 0:
                self.layer0_mix(hf)
            if self.stop >= 2:
                self.mlp(0)
            if self.stop >= 3:
                self.layer1_mix(hf)
            if self.stop >= 4:
                self.mlp(1)
            for t in range(NT):
                gt = hf * NT + t
                add("sp", lambda e, t=t, gt=gt: e.dma_start(out=self.out[gt * 128:(gt + 1) * 128, :], in_=self.h[:, t, :]),
                    reads=[("h", t)], dma="d_o%d" % t)
        add("sp", None, writes=[("h", t) for t in range(NT)])
        self.sc.emit(self.nc, self.es)
        self.es.close()
        return self.nc


_CACHE = {}


def get_nc(stop=99):
    if stop not in _CACHE:
        _CACHE[stop] = Builder(stop).build()
    return _CACHE[stop]


def kernel(stop=99, ncores=8, **inputs):
    stop = float(stop)
    nc = get_nc(stop)
    cs = make_consts()
    f = lambda a: np.ascontiguousarray(np.asarray(a, dtype=np.float32))
    shared = {
        "w_in_a": f(inputs["w_in_a"][0]), "ret_norm_gain": f(inputs["ret_norm_gain"][0]), "w_out_a": f(inputs["w_out_a"][0]),
        "kv_norm_gain": f(inputs["kv_norm_gain"]), "w_kv_shared": f(inputs["w_kv_shared"]), "w_in_b": f(inputs["w_in_b"][0]),
        "w_out_b": f(inputs["w_out_b"][0]), "w_mem_kv": f(inputs["w_mem_kv"]), "norm_pre_mix": f(inputs["norm_pre_mix"]),
        "norm_post_mix": f(inputs["norm_post_mix"]), "norm_pre_mlp": f(inputs["norm_pre_mlp"]),
        "norm_post_mlp": f(inputs["norm_post_mlp"]), "w_up": f(inputs["w_up"]), "w_down": f(inputs["w_down"]),
        "c_ident": cs["ident"], "c_tri4": cs["tri4"], "c_retv": cs["retv"], "c_alibi": cs["alibi"],
    }
    x = f(inputs["x"])
    mem = f(inputs["mem"])
    in_maps = []
    for b in range(ncores):
        m = dict(shared)
        m["x"] = x[b]
        m["mem"] = mem[b]
        in_maps.append(m)
    res = run_bass_kernel_spmd(nc, in_maps, core_ids=list(range(ncores)))
    return np.stack([np.asarray(r["out"], dtype=np.float32) for r in res.results], axis=0)
```

```python
import math
from contextlib import ExitStack
import numpy as np
import concourse.bass as bass
import concourse.mybir as mybir
from concourse.alu_op_type import AluOpType as ALU
from concourse.bass_utils import run_bass_kernel_spmd

F32 = mybir.dt.float32
BF16 = mybir.dt.bfloat16
AF = mybir.ActivationFunctionType
AX = mybir.AxisListType

D = 1024
S = 2048
T = 1024
NT = 8
NH = 2
DFF = 4096
EPS = 1e-6
NRING = 3
ENGS = ("pe", "act", "dve", "pool", "sp")


def alibi_slopes(n):
    def pow2(m):
        return [2.0 ** (-8.0 * (i + 1) / m) for i in range(m)]
    p = 2 ** int(math.floor(math.log2(n)))
    s = pow2(p)
    if p < n:
        s = s + pow2(2 * p)[0::2][: n - p]
    return np.asarray(s, dtype=np.float64)


class Ins:
    __slots__ = ("eng", "fn", "waits", "stream", "sidx", "milestone", "count", "is_dma")


class Sched:
    def __init__(self):
        self.cells = {}
        self.eng_list = {e: [] for e in ENGS}
        self.streams = {}
        self.seen = {e: {} for e in ENGS}

    def add(self, eng, fn, reads=(), writes=(), dma=None):
        ins = Ins()
        ins.eng = eng
        ins.fn = fn
        ins.is_dma = dma is not None
        ins.stream = dma if dma else eng
        need = {}

        def dep(d, war):
            if d is None:
                return
            if (not ins.is_dma) and (not d.is_dma) and d.eng == eng:
                if eng == "pe" or war:
                    return
            if need.get(d.stream, -1) < d.sidx:
                need[d.stream] = d.sidx

        for c in reads:
            cell = self.cells.get(c)
            if cell:
                dep(cell[0], False)
        for c in writes:
            cell = self.cells.get(c)
            if cell:
                dep(cell[0], False)
                for r in cell[1].values():
                    dep(r, True)
        waits = []
        seen = self.seen[eng]
        for s, i in need.items():
            if seen.get(s, -1) >= i:
                continue
            seen[s] = i
            waits.append((s, i))
            self.streams[s][i].milestone = True
        ins.waits = waits
        lst = self.streams.setdefault(ins.stream, [])
        ins.sidx = len(lst)
        lst.append(ins)
        ins.milestone = ins.is_dma
        self.eng_list[eng].append(ins)
        for c in writes:
            self.cells[c] = [ins, {}]
        for c in reads:
            cell = self.cells.setdefault(c, [None, {}])
            cell[1][ins.stream] = ins
        return ins

    def emit(self, nc, es):
        sems = {}
        for s, lst in self.streams.items():
            sems[s] = es.enter_context(nc.semaphore("s_" + s))
            c = 0
            for ins in lst:
                if ins.is_dma:
                    c += 16
                elif ins.milestone:
                    c += 1
                ins.count = c
        streams = self.streams

        def run(eng_name, eng):
            for ins in self.eng_list[eng_name]:
                for (s, i) in ins.waits:
                    eng.wait_ge(sems[s], streams[s][i].count)
                if ins.fn is not None:
                    h = ins.fn(eng)
                    if ins.is_dma:
                        h.then_inc(sems[ins.stream], 16)
                    elif ins.milestone:
                        h.then_inc(sems[ins.stream], 1)

        with nc.Block() as block:
            @block.tensor
            def _(e):
                run("pe", e)

            @block.scalar
            def _(e):
                run("act", e)

            @block.vector
            def _(e):
                run("dve", e)

            @block.gpsimd
            def _(e):
                run("pool", e)

            @block.sync
            def _(e):
                run("sp", e)


def make_consts():
    c = {}
    c["ident"] = np.eye(128, dtype=np.float32)
    k = np.arange(128)[:, None]
    q = np.arange(128)[None, :]
    tri = (q >= k).astype(np.float32)
    c["tri4"] = np.tile(tri, (1, 4)).astype(np.float32)
    hh = np.arange(4, dtype=np.float64)
    log_g = np.log1p(-np.exp2(-5.0 - hh))
    p = np.arange(128, dtype=np.float64)
    xi = np.exp(log_g[None, :] * (p[:, None] + 1.0))
    rv = np.zeros((128, 8), np.float32)
    rv[:, 0:4] = (1.0 / xi) * (128.0 ** -0.5)
    rv[:, 4:8] = EPS / (xi * xi)
    c["retv"] = rv
    c["ret_decay"] = [float(np.exp(log_g[i] * 128.0)) for i in range(4)]
    sl = alibi_slopes(12)
    ab = np.zeros((128, 12, 16), np.float32)
    for h in range(12):
        for d in range(16):
            ab[:, h, d] = sl[h] * (p - 64.0 - 128.0 * d)
    ef = np.exp(sl[None, :] * (p[:, None] - 64.0)).astype(np.float32)
    c["alibi"] = np.concatenate([ab.reshape(128, 192), ef], axis=1).astype(np.float32)
    c["dbias"] = [[float(-128.0 * sl[h] * d) for d in range(16)] for h in range(12)]
    return c


class Builder:
    def __init__(self, stop=99):
        self.stop = stop
        self.nc = bass.Bass("TRN2", target_bir_lowering=False, dynamic_dma_scratch_size=8192)
        self.sc = Sched()
        self.es = ExitStack()
        self.ps_i = 0
        self.ring_i = 0
        self.cnt = {}
        self.consts = make_consts()

    def sb(self, name, shape, dt):
        return self.es.enter_context(self.nc.sbuf_tensor(name, shape, dt))

    def dram(self, name, shape, dt=F32, kind="ExternalInput"):
        return self.nc.dram_tensor(name, shape, dt, kind=kind).ap()

    def rot(self, name, n):
        i = self.cnt.get(name, 0)
        self.cnt[name] = i + 1
        return i % n

    def psum(self):
        i = self.ps_i % 8
        self.ps_i += 1
        return self.PS[i], ("ps", i)

    def add(self, *a, **k):
        return self.sc.add(*a, **k)

    def setup(self):
        nc = self.nc
        d = self.dram
        self.x = d("x", [S, D])
        self.mem = d("mem", [256, D])
        self.w_in_a = d("w_in_a", [D, 2816])
        self.ret_gain = d("ret_norm_gain", [768])
        self.w_out_a = d("w_out_a", [D, D])
        self.kv_gain = d("kv_norm_gain", [D])
        self.w_kv = d("w_kv_shared", [D, 1536])
        self.w_in_b = d("w_in_b", [D, D])
        self.w_out_b = d("w_out_b", [D, D])
        self.w_mkv = d("w_mem_kv", [2, D, 512])
        self.g_pre_mix = d("norm_pre_mix", [2, D])
        self.g_post_mix = d("norm_post_mix", [2, D])
        self.g_pre_mlp = d("norm_pre_mlp", [2, D])
        self.g_post_mlp = d("norm_post_mlp", [2, D])
        self.w_up = d("w_up", [2, D, DFF])
        self.w_down = d("w_down", [2, DFF, D])
        self.c_ident = d("c_ident", [128, 128])
        self.c_tri4 = d("c_tri4", [128, 512])
        self.c_retv = d("c_retv", [128, 8])
        self.c_alibi = d("c_alibi", [128, 204])
        self.out = d("out", [S, D], kind="ExternalOutput")

        sb = self.sb
        self.h = sb("h", [128, NT, D], F32)
        self.KT = sb("KT", [128, 6, S], BF16)
        self.V = sb("V", [128, 16, 12, 65], BF16)
        self.ring = sb("ring", [128, NRING, 8, 512], BF16)
        self.B1 = sb("B1", [128, 8, 1024], BF16)
        self.B2 = sb("B2", [128, 8, 1024], BF16)
        self.B3 = sb("B3", [128, 8 * 1024], F32)
        self.y = self.B3[:].rearrange("p (t n) -> p t n", n=1024)
        self.B3b = self.B3[:].bitcast(BF16).rearrange("p (t n) -> p t n", n=2048)
        self.qmT = sb("qmT", [128, 2, 1024], BF16)
        self.memT = sb("memT", [128, 8, 256], BF16)
        self.mkT = sb("mkT", [128, 2, 256], BF16)
        self.mv = sb("mv", [128, 2, 4, 65], BF16)
        self.gain = sb("gain", [128, 2, 1024], F32)
        self.state = sb("state", [128, 4, 192], F32)
        self.state_bf = sb("state_bf", [128, 2, 4, 192], BF16)
        self.tmpf = sb("tmpf", [128, 1, 512], F32)
        self.hnb = sb("hnb", [128, 2, 1024], BF16)
        self.junk = sb("junk", [128, 192], BF16)
        self.PT = sb("PT", [128, 8, 512], BF16)
        self.PTm = sb("PTm", [128, 4, 512], BF16)
        self.ident = sb("ident", [128, 128], BF16)
        self.tri4 = sb("tri4", [128, 512], BF16)
        self.retv = sb("retv", [128, 8], F32)
        self.alibi = sb("alibi", [128, 204], F32)
        self.ksum = sb("ksum", [128, 6, 8], F32)
        self.kmT = sb("kmT", [128, 6, 8], BF16)
        self.st = sb("st", [128, 64], F32)
        self.gsb = sb("gsb", [128, 12, 8], F32)
        self.top8 = sb("top8", [128, 12, 8], F32)
        self.sel = sb("sel", [128, 2, 12, 8], F32)
        self.acc = sb("acc", [128, 2, 65], F32)
        self.PS = [self.es.enter_context(nc.psum_tensor("ps%d" % i, [128, 512], F32)) for i in range(8)]

        add = self.add
        ycell = lambda t: [("B3", t, s_) for s_ in range(8)]
        add("sp", lambda e: e.dma_start(out=self.y[:, 2, 0:128], in_=self.c_ident), writes=ycell(2), dma="d_c0")
        add("sp", lambda e: e.dma_start(out=self.y[:, 3, 0:512], in_=self.c_tri4), writes=ycell(3), dma="d_c1")
        add("sp", lambda e: e.dma_start(out=self.retv[:], in_=self.c_retv), writes=[("c", 2)], dma="d_c2")
        add("sp", lambda e: e.dma_start(out=self.alibi[:], in_=self.c_alibi), writes=[("c", 3)], dma="d_c3")
        add("dve", lambda e: e.tensor_copy(out=self.ident[:], in_=self.y[:, 2, 0:128]), reads=ycell(2), writes=[("ident",)])
        add("dve", lambda e: e.tensor_copy(out=self.tri4[:], in_=self.y[:, 3, 0:512]), reads=ycell(3), writes=[("tri4",)])
        for gt_ in range(16):
            add("dve", lambda e, gt_=gt_: e.tensor_copy(out=self.V[:, gt_, :, 64], in_=self.alibi[:, 192:204]),
                reads=[("c", 3)], writes=[("Vones", gt_)])
        add("dve", lambda e: e.memset(self.mv[:, :, :, 64:65], 1.0), writes=[("mvones",)])
        add("dve", lambda e: e.memset(self.state[:], 0.0), writes=[("state", i_) for i_ in range(4)])
        add("dve", lambda e: e.memset(self.state_bf[:], 0.0), writes=[("state_bf", p_, i_) for p_ in range(2) for i_ in range(4)])
        for mt in range(2):
            add("sp", lambda e, mt=mt: e.dma_start(out=self.y[:, mt, :], in_=self.mem[mt * 128:(mt + 1) * 128, :]),
                writes=ycell(mt), dma="d_mem%d" % mt)
            add("dve", lambda e, mt=mt: e.tensor_copy(out=self.hnb[:, 0, :], in_=self.y[:, mt, :]),
                reads=ycell(mt), writes=[("hnb", 0)])
            self.transpose8(self.hnb[:, 0, :], [("hnb", 0)], self.memT[:, :, mt * 128:(mt + 1) * 128], [("memT", mt)])

    def transpose8(self, src, src_cells, dst, dst_cells, eng="act"):
        ps, pc = self.psum()
        psb = ps[:].bitcast(BF16)
        for kc in range(8):
            self.add("pe", lambda e, kc=kc: e.transpose(out=psb[:, kc * 128:(kc + 1) * 128],
                                                        in_=src[:, kc * 128:(kc + 1) * 128], identity=self.ident[:]),
                     reads=list(src_cells) + [("ident",)], writes=[pc])
        pin = psb[:, 0:1024].rearrange("p (k c) -> p k c", c=128)
        if eng == "act":
            self.add("act", lambda e: e.copy(out=dst, in_=pin), reads=[pc], writes=dst_cells)
        else:
            self.add("dve", lambda e: e.tensor_copy(out=dst, in_=pin), reads=[pc], writes=dst_cells)

    def load_slab(self, wap, ncols=512):
        r = self.ring_i % NRING
        self.ring_i += 1
        src = wap.rearrange("(kc p) n -> p kc n", p=128)
        self.add("pool", lambda e: e.dma_start(out=self.ring[:, r, :, 0:ncols], in_=src),
                 writes=[("ring", r)], dma="d_ring%d" % r)
        return r

    def load_gain(self, gap, n=1024):
        sl = self.rot("gain", 2)
        self.add("sp", lambda e: e.dma_start(out=self.gain[:, sl, 0:n], in_=gap.partition_broadcast(128)),
                 writes=[("gain", sl)], dma="d_gain%d" % sl)
        return sl

    def stat(self):
        i = self.rot("st", 16)
        return self.st[:, i * 4:(i + 1) * 4], ("st", i)

    def norm_A(self, src, src_cells, gsl):
        add = self.add
        st, stc = self.stat()
        sl = self.rot("hnb", 2)
        add("act", lambda e: e.activation(out=self.hnb[:, sl, :], in_=src, func=AF.Square, accum_out=st[:, 0:1]),
            reads=src_cells, writes=[stc, ("hnb", sl)])
        add("act", lambda e: e.activation(out=st[:, 1:2], in_=st[:, 0:1], func=AF.Sqrt, bias=self.epsc[:, 0:1], scale=1.0 / D),
            reads=[stc, ("epsc",)], writes=[stc])
        add("dve", lambda e: e.reciprocal(out=st[:, 2:3], in_=st[:, 1:2]), reads=[stc], writes=[stc])
        add("dve", lambda e: e.scalar_tensor_tensor(out=self.hnb[:, sl, :], in0=src, scalar=st[:, 2:3],
                                                    in1=self.gain[:, gsl, :], op0=ALU.mult, op1=ALU.mult),
            reads=list(src_cells) + [stc, ("gain", gsl)], writes=[("hnb", sl)])
        return sl

    def norm_phase(self, gsl):
        sl = self.norm_A(self.h[:, 0, :], [("h", 0)], gsl)
        for t in range(NT):
            nsl = self.norm_A(self.h[:, t + 1, :], [("h", t + 1)], gsl) if t + 1 < NT else None
            self.transpose8(self.hnb[:, sl, :], [("hnb", sl)], self.B1[:, :, t * 128:(t + 1) * 128],
                            [("B1", kc, t) for kc in range(8)], eng=("act" if t % 2 == 0 else "dve"))
            sl = nsl

    def post_res(self, t, gsl):
        add = self.add
        st, stc = self.stat()
        ycells = [("B3", t, s) for s in range(8)]
        jsl = self.rot("hnb", 2)
        add("act", lambda e: e.activation(out=self.hnb[:, jsl, :], in_=self.y[:, t, :], func=AF.Square, accum_out=st[:, 0:1]),
            reads=ycells, writes=[stc, ("hnb", jsl)])
        add("act", lambda e: e.activation(out=st[:, 1:2], in_=st[:, 0:1], func=AF.Sqrt, bias=self.epsc[:, 0:1], scale=1.0 / D),
            reads=[stc, ("epsc",)], writes=[stc])
        add("dve", lambda e: e.reciprocal(out=st[:, 2:3], in_=st[:, 1:2]), reads=[stc], writes=[stc])
        add("dve", lambda e: e.scalar_tensor_tensor(out=self.y[:, t, :], in0=self.y[:, t, :], scalar=st[:, 2:3],
                                                    in1=self.gain[:, gsl, :], op0=ALU.mult, op1=ALU.mult),
            reads=ycells + [stc, ("gain", gsl)], writes=ycells)
        add("dve", lambda e: e.tensor_tensor(out=self.h[:, t, :], in0=self.h[:, t, :], in1=self.y[:, t, :], op=ALU.add),
            reads=[("h", t)] + ycells, writes=[("h", t)])

    def mm_B(self, r, j, xT, xcells_fn, grp, ncol=512, nk=8):
        ps, pc = self.psum()
        for kc in range(nk):
            self.add("pe", lambda e, kc=kc: e.matmul(ps[:, 0:ncol], lhsT=self.ring[:, r, kc, j * 128:(j + 1) * 128],
                                                     rhs=xT[:, kc, grp * ncol:(grp + 1) * ncol],
                                                     start=(kc == 0), stop=(kc == nk - 1)),
                     reads=[("ring", r)] + xcells_fn(kc, grp), writes=[pc])
        return ps, pc

    def mm_A(self, r, c0, n, xT, xcells_fn, t, nk=8):
        ps, pc = self.psum()
        for kc in range(nk):
            self.add("pe", lambda e, kc=kc: e.matmul(ps[:, 0:n], lhsT=xT[:, kc, t * 128:(t + 1) * 128],
                                                     rhs=self.ring[:, r, kc, c0:c0 + n],
                                                     start=(kc == 0), stop=(kc == nk - 1)),
                     reads=[("ring", r)] + xcells_fn(kc, t), writes=[pc])
        return ps, pc

    @staticmethod
    def cellsB1_grp(kc, grp):
        return [("B1", kc, grp * 4 + i) for i in range(4)]

    @staticmethod
    def cellsB1_t(kc, t):
        return [("B1", kc, t)]

    @staticmethod
    def cellsB2_grp(kc, grp):
        return [("B2", kc, grp * 4 + i) for i in range(4)]

    @staticmethod
    def cellsB2_t(kc, t):
        return [("B2", kc, t)]

    def mem_kv(self, l):
        add = self.add
        r = self.load_slab(self.w_mkv[l])
        for j in range(2):
            ps, pc = self.psum()
            for kc in range(8):
                add("pe", lambda e, kc=kc, ps=ps, j=j: e.matmul(ps[:, 0:256], lhsT=self.ring[:, r, kc, j * 128:(j + 1) * 128],
                                                               rhs=self.memT[:, kc, :], start=(kc == 0), stop=(kc == 7)),
                    reads=[("ring", r), ("memT", 0), ("memT", 1)], writes=[pc])
            add("act", lambda e, ps=ps, j=j: e.copy(out=self.mkT[:, j, :], in_=ps[:, 0:256]), reads=[pc], writes=[("mkT", j)])
        for mt in range(2):
            ps, pc = self.psum()
            for kc in range(8):
                add("pe", lambda e, kc=kc, ps=ps, mt=mt: e.matmul(ps[:, 0:256], lhsT=self.memT[:, kc, mt * 128:(mt + 1) * 128],
                                                                 rhs=self.ring[:, r, kc, 256:512], start=(kc == 0), stop=(kc == 7)),
                    reads=[("ring", r), ("memT", mt)], writes=[pc])
            add("act", lambda e, ps=ps, mt=mt: e.copy(out=self.mv[:, mt, :, 0:64],
                                                     in_=ps[:, 0:256].rearrange("p (h c) -> p h c", c=64)),
                reads=[pc, ("mvones",)], writes=[("mv", mt)])

    def mem_front(self, t):
        add = self.add
        pts = []
        for hh in range(2):
            ps, pc = self.psum()
            po = hh * 64
            for half in range(2):
                for mt in range(2):
                    sl = half * 2 + mt
                    add("pe", lambda e, ps=ps, sl=sl, po=po, half=half, mt=mt: e.matmul(
                        ps[:, sl * 128:(sl + 1) * 128], lhsT=self.mkT[po:po + 64, half, mt * 128:(mt + 1) * 128],
                        rhs=self.qmT[po:po + 64, half, t * 128:(t + 1) * 128], start=True, stop=True),
                        reads=[("mkT", half), ("qmT", half, t)], writes=[pc])
            psl = (t % 2) * 2 + hh
            add("act", lambda e, ps=ps, psl=psl: e.activation(out=self.PTm[:, psl, :], in_=ps[:, 0:512], func=AF.Exp, scale=0.125),
                reads=[pc], writes=[("PTm", psl)])
            pts.append(psl)
        return pts

    def mem_back(self, t, pts):
        add = self.add
        pso, poc = self.psum()
        for hd in range(4):
            psl = pts[hd % 2]
            for mt in range(2):
                sl = (hd // 2) * 2 + mt
                add("pe", lambda e, hd=hd, psl=psl, sl=sl, mt=mt: e.matmul(
                    pso[:, hd * 65:(hd + 1) * 65], lhsT=self.PTm[:, psl, sl * 128:(sl + 1) * 128],
                    rhs=self.mv[:, mt, hd, :], start=(mt == 0), stop=(mt == 1)),
                    reads=[("PTm", psl), ("mv", mt), ("mvones",)], writes=[poc])
        st, stc = self.stat()
        pv = pso[:, 0:260].rearrange("p (h c) -> p h c", c=65)
        add("dve", lambda e: e.reciprocal(out=st[:, 0:4], in_=pv[:, :, 64]), reads=[poc], writes=[stc])
        for hd in range(4):
            add("dve", lambda e, hd=hd: e.tensor_scalar(out=self.B1[:, t, 768 + hd * 64:768 + (hd + 1) * 64], in0=pv[:, hd, 0:64],
                                                        scalar1=st[:, hd:hd + 1], scalar2=None, op0=ALU.mult),
                reads=[poc, stc], writes=[("B1", t, 6 + hd // 2)])

    def mem_attn(self, t):
        self.mem_back(t, self.mem_front(t))

    def cat_to_T(self):
        for t in range(NT):
            self.transpose8(self.B1[:, t, :], [("B1", t, j) for j in range(8)],
                            self.B2[:, :, t * 128:(t + 1) * 128], [("B2", kc, t) for kc in range(8)])

    def out_proj(self, wap, g_post):
        add = self.add
        gsl = self.load_gain(g_post)
        for s in range(2):
            r = self.load_slab(wap[:, s * 512:(s + 1) * 512])
            for t in range(NT):
                ps, pc = self.mm_A(r, 0, 512, self.B2, self.cellsB2_t, t)
                add("act", lambda e, ps=ps, t=t, s=s: e.copy(out=self.y[:, t, s * 512:(s + 1) * 512], in_=ps[:, 0:512]),
                    reads=[pc], writes=[("B3", t, s * 4 + i) for i in range(4)])
                if s == 1:
                    self.post_res(t, gsl)

    def mlp(self, l):
        add = self.add
        gsl = self.load_gain(self.g_pre_mlp[l])
        self.norm_phase(gsl)
        gpost = self.load_gain(self.g_post_mlp[l])
        for b in range(4):
            for s in range(2):
                r = self.load_slab(self.w_up[l][:, b * 1024 + s * 512: b * 1024 + (s + 1) * 512])
                for j in range(4):
                    for grp in range(2):
                        ps, pc = self.mm_B(r, j, self.B1, self.cellsB1_grp, grp)
                        dst = self.B2[:, s * 4 + j, grp * 512:(grp + 1) * 512]
                        dcells = [("B2", s * 4 + j, grp * 4 + i) for i in range(4)]
                        add("act", lambda e, ps=ps: e.activation(out=self.tmpf[:, 0, 0:512], in_=ps[:, 0:512], func=AF.Relu),
                            reads=[pc], writes=[("tmpf", 0)])
                        add("dve", lambda e, ps=ps, dst=dst: e.tensor_tensor(out=dst, in0=self.tmpf[:, 0, 0:512], in1=ps[:, 0:512], op=ALU.mult),
                            reads=[pc, ("tmpf", 0)], writes=dcells)
            for s in range(2):
                r = self.load_slab(self.w_down[l][b * 1024:(b + 1) * 1024, s * 512:(s + 1) * 512])
                for t in range(NT):
                    ps, pc = self.mm_A(r, 0, 512, self.B2, self.cellsB2_t, t)
                    yc = [("B3", t, s * 4 + i) for i in range(4)]
                    if b == 0:
                        add("act", lambda e, ps=ps, t=t, s=s: e.copy(out=self.y[:, t, s * 512:(s + 1) * 512], in_=ps[:, 0:512]),
                            reads=[pc], writes=yc)
                    else:
                        add("dve", lambda e, ps=ps, t=t, s=s: e.tensor_tensor(out=self.y[:, t, s * 512:(s + 1) * 512],
                                                                             in0=self.y[:, t, s * 512:(s + 1) * 512], in1=ps[:, 0:512], op=ALU.add),
                            reads=[pc] + yc, writes=yc)
                    if b == 3 and s == 1:
                        self.post_res(t, gpost)

    def layer0_mix(self, hf):
        add = self.add
        gsl = self.load_gain(self.g_pre_mix[0])
        self.norm_phase(gsl)
        if self.stop < 0.15: return
        self.mem_kv(0)
        if self.stop < 0.25: return
        W = self.w_in_a
        r = self.load_slab(W[:, 0:512])
        for j in range(4):
            for grp in range(2):
                ps, pc = self.mm_B(r, j, self.B1, self.cellsB1_grp, grp)
                add("act", lambda e, ps=ps, j=j, grp=grp: e.copy(out=self.B2[:, j, grp * 512:(grp + 1) * 512], in_=ps[:, 0:512]),
                    reads=[pc], writes=[("B2", j, grp * 4 + i) for i in range(4)])
        if self.stop < 0.35: return
        r = self.load_slab(W[:, 512:1024])
        for j in range(4):
            for grp in range(2):
                ps, pc = self.mm_B(r, j, self.B1, self.cellsB1_grp, grp)
                add("act", lambda e, ps=ps, j=j, grp=grp: e.copy(out=self.B2[:, 4 + j, grp * 512:(grp + 1) * 512], in_=ps[:, 0:512]),
                    reads=[pc], writes=[("B2", 4 + j, grp * 4 + i) for i in range(4)])
        for t in range(NT):
            ps, pc = self.mm_A(r, 0, 512, self.B1, self.cellsB1_t, t)
            add("dve", lambda e, ps=ps, t=t: e.tensor_copy(out=self.B3b[:, t, 0:512], in_=ps[:, 0:512]),
                reads=[pc], writes=[("B3", t, 0), ("B3", t, 1)])
        if self.stop < 0.45: return
        rgs = self.load_gain(self.ret_gain, 768)
        for si in range(2, 5):
            r = self.load_slab(W[:, si * 512:(si + 1) * 512])
            for t in range(NT):
                ps, pc = self.mm_A(r, 0, 512, self.B1, self.cellsB1_t, t)
                c0 = si * 512 - 1024
                bounds = sorted(set([c0, c0 + 512] + [b for b in range(0, 1537, 192) if c0 < b < c0 + 512]))
                for a, bnd in zip(bounds[:-1], bounds[1:]):
                    lo, hi = a - c0, bnd - c0
                    if a < 768:
                        hd = a // 192
                        add("act", lambda e, ps=ps, t=t, lo=lo, hi=hi, a=a, bnd=bnd, hd=hd: e.activation(
                            out=self.B3b[:, t, 512 + a:512 + bnd], in_=ps[:, lo:hi], func=AF.Copy, scale=self.retv[:, hd:hd + 1]),
                            reads=[pc, ("c", 2)], writes=[("B3", t, s) for s in range((512 + a) // 256, (512 + bnd - 1) // 256 + 1)])
                    else:
                        ga, gb = a - 768, bnd - 768
                        sl = 0
                        add("act", lambda e, ps=ps, lo=lo, hi=hi, sl=sl: e.activation(out=self.tmpf[:, sl, 0:hi - lo], in_=ps[:, lo:hi], func=AF.Silu),
                            reads=[pc], writes=[("tmpf", sl)])
                        add("dve", lambda e, t=t, ga=ga, gb=gb, sl=sl: e.tensor_tensor(
                            out=self.B3b[:, t, 1280 + ga:1280 + gb], in0=self.tmpf[:, sl, 0:gb - ga], in1=self.gain[:, rgs, ga:gb], op=ALU.mult),
                            reads=[("tmpf", sl), ("gain", rgs)], writes=[("B3", t, s) for s in range((1280 + ga) // 256, (1280 + gb - 1) // 256 + 1)])
        if self.stop < 0.55: return
        r = self.load_slab(W[:, 2560:2816], ncols=256)
        for j in range(2):
            for grp in range(2):
                ps, pc = self.mm_B(r, j, self.B1, self.cellsB1_grp, grp)
                add("act", lambda e, ps=ps, j=j, grp=grp: e.copy(out=self.qmT[:, j, grp * 512:(grp + 1) * 512], in_=ps[:, 0:512]),
                    reads=[pc], writes=[("qmT", j, grp * 4 + i) for i in range(4)])
        if self.stop < 0.65: return
        dec = self.consts["ret_decay"]
        def kvupd(t):
            par = (hf * NT + t + 1) % 2
            vcells = [("B3", t, 2), ("B3", t, 3), ("B3", t, 4)]
            for pair in range(2):
                pk, pkc = self.psum()
                for hh in range(2):
                    hd = pair * 2 + hh
                    add("pe", lambda e, pk=pk, hd=hd, hh=hh, t=t: e.matmul(
                        pk[:, hh * 192:(hh + 1) * 192], lhsT=self.B3b[:, t, hd * 128:(hd + 1) * 128],
                        rhs=self.B3b[:, t, 512 + hd * 192:512 + (hd + 1) * 192], start=True, stop=True),
                        reads=[("B3", t, 0), ("B3", t, 1)] + vcells, writes=[pkc])
                for hh in range(2):
                    hd = pair * 2 + hh
                    add("dve", lambda e, pk=pk, hd=hd, hh=hh: e.tensor_tensor(
                        out=self.state[:, hd, :], in0=pk[:, hh * 192:(hh + 1) * 192], in1=self.state[:, hd, :], op=ALU.add),
                        reads=[pkc, ("state", hd)], writes=[("state", hd)])
                    add("dve", lambda e, hd=hd, par=par: e.tensor_scalar(out=self.state_bf[:, par, hd, :], in0=self.state[:, hd, :], scalar1=dec[hd],
                                                                        scalar2=None, op0=ALU.mult), reads=[("state", hd)], writes=[("state_bf", par, hd)])
                    add("dve", lambda e, hd=hd: e.tensor_scalar(out=self.state[:, hd, :], in0=self.state[:, hd, :], scalar1=dec[hd],
                                                               scalar2=None, op0=ALU.mult), reads=[("state", hd)], writes=[("state", hd)])

        def front(t):
            if t > 0:
                kvupd(t - 1)
            tok = slice(t * 128, (t + 1) * 128)
            ps, pc = self.psum()
            for hd in range(4):
                add("pe", lambda e, ps=ps, hd=hd, tok=tok: e.matmul(ps[:, hd * 128:(hd + 1) * 128], lhsT=self.B2[:, 4 + hd, tok],
                                                                   rhs=self.B2[:, hd, tok], start=True, stop=True),
                    reads=[("B2", 4 + hd, t), ("B2", hd, t)], writes=[pc])
            psl = self.rot("PT", 8)
            add("dve", lambda e, ps=ps, psl=psl: e.tensor_tensor(out=self.PT[:, psl, :], in0=ps[:, 0:512], in1=self.tri4[:], op=ALU.mult),
                reads=[pc, ("tri4",)], writes=[("PT", psl)])
            return psl, self.mem_front(t)

        def back(t, psl, mf):
            tok = slice(t * 128, (t + 1) * 128)
            vcells = [("B3", t, 2), ("B3", t, 3), ("B3", t, 4)]
            pos = []
            for pair in range(2):
                po_, poc = self.psum()
                pos.append((po_, poc))
                for hh in range(2):
                    hd = pair * 2 + hh
                    add("pe", lambda e, po_=po_, hd=hd, hh=hh, psl=psl, t=t: e.matmul(
                        po_[:, hh * 192:(hh + 1) * 192], lhsT=self.PT[:, psl, hd * 128:(hd + 1) * 128],
                        rhs=self.B3b[:, t, 512 + hd * 192:512 + (hd + 1) * 192], start=True, stop=False),
                        reads=[("PT", psl)] + vcells, writes=[poc])
                    add("pe", lambda e, po_=po_, hd=hd, hh=hh, tok=tok: e.matmul(
                        po_[:, hh * 192:(hh + 1) * 192], lhsT=self.B2[:, hd, tok], rhs=self.state_bf[:, (hf * NT + t) % 2, hd, :], start=False, stop=True),
                        reads=[("B2", hd, t), ("state_bf", (hf * NT + t) % 2, hd)], writes=[poc])
            st, stc = self.stat()
            for hd in range(4):
                po_, poc = pos[hd // 2]
                hh = hd % 2
                add("act", lambda e, po_=po_, hh=hh, hd=hd: e.activation(out=self.junk[:, 0:192], in_=po_[:, hh * 192:(hh + 1) * 192],
                                                                       func=AF.Square, accum_out=st[:, hd:hd + 1]),
                    reads=[poc], writes=[stc])
            st2, stc2 = self.stat()
            add("dve", lambda e: e.scalar_tensor_tensor(out=st2[:, 0:4], in0=st[:, 0:4], scalar=1.0 / 192.0, in1=self.retv[:, 4:8],
                                                        op0=ALU.mult, op1=ALU.add), reads=[stc, ("c", 2)], writes=[stc2])
            add("act", lambda e: e.activation(out=st2[:, 0:4], in_=st2[:, 0:4], func=AF.Sqrt), reads=[stc2], writes=[stc2])
            st3, stc3 = self.stat()
            add("dve", lambda e: e.reciprocal(out=st3[:, 0:4], in_=st2[:, 0:4]), reads=[stc2], writes=[stc3])
            for hd in range(4):
                po_, poc = pos[hd // 2]
                hh = hd % 2
                c0, c1 = hd * 192, (hd + 1) * 192
                add("dve", lambda e, po_=po_, hh=hh, hd=hd, c0=c0, c1=c1, t=t: e.scalar_tensor_tensor(
                    out=self.B1[:, t, c0:c1], in0=po_[:, hh * 192:(hh + 1) * 192], scalar=st3[:, hd:hd + 1],
                    in1=self.B3b[:, t, 1280 + c0:1280 + c1], op0=ALU.mult, op1=ALU.mult),
                    reads=[poc, stc3, ("B3", t, 5), ("B3", t, 6), ("B3", t, 7)],
                    writes=[("B1", t, j) for j in range(c0 // 128, (c1 - 1) // 128 + 1)])
            self.mem_back(t, mf)

        f = front(0)
        for t in range(NT):
            fn = front(t + 1) if t + 1 < NT else None
            back(t, *f)
            f = fn
        kvupd(NT - 1)
        if self.stop < 0.75: return
        self.cat_to_T()
        if self.stop < 0.85: return
        self.out_proj(self.w_out_a, self.g_post_mix[0])

    def layer1_mix(self, hf):
        add = self.add
        gsl = self.load_gain(self.kv_gain)
        self.norm_phase(gsl)
        W = self.w_kv
        for si in range(3):
            r = self.load_slab(W[:, si * 512:(si + 1) * 512])
            for j in range(4):
                col = si * 512 + j * 128
                if col >= 768:
                    continue
                c = col // 128
                for grp in range(2):
                    ps, pc = self.mm_B(r, j, self.B1, self.cellsB1_grp, grp)
                    for bb in range(2):
                        blk = hf * 4 + grp * 2 + bb
                        g0 = hf * T + grp * 512 + bb * 256
                        add("act", lambda e, ps=ps, c=c, g0=g0, bb=bb, blk=blk: e.activation(
                            out=self.KT[:, c, g0:g0 + 256], in_=ps[:, bb * 256:(bb + 1) * 256], func=AF.Copy,
                            accum_out=self.ksum[:, c, blk:blk + 1]),
                            reads=[pc], writes=[("KT", c, blk), ("ksum", c, blk)])
            v0 = max(si * 512, 768)
            v1 = (si + 1) * 512
            if v1 > v0:
                n = v1 - v0
                h0 = (v0 - 768) // 64
                nh = n // 64
                for t in range(NT):
                    gt = hf * NT + t
                    ps, pc = self.mm_A(r, v0 - si * 512, n, self.B1, self.cellsB1_t, t)
                    for j_ in range(nh):
                        hd_ = h0 + j_
                        add("dve", lambda e, ps=ps, gt=gt, hd_=hd_, j_=j_: e.tensor_scalar(
                            out=self.V[:, gt, hd_, 0:64], in0=ps[:, j_ * 64:(j_ + 1) * 64], scalar1=self.alibi[:, 192 + hd_:193 + hd_],
                            scalar2=None, op0=ALU.mult),
                            reads=[pc, ("c", 3)], writes=[("V", gt, hd_ // 4)])
        gsl = self.load_gain(self.g_pre_mix[1])
        self.norm_phase(gsl)
        self.mem_kv(1)
        W = self.w_in_b
        for si in range(2):
            r = self.load_slab(W[:, si * 512:(si + 1) * 512])
            for j in range(4):
                col = si * 512 + j * 128
                for grp in range(2):
                    ps, pc = self.mm_B(r, j, self.B1, self.cellsB1_grp, grp)
                    if col < 768:
                        c = col // 128
                        add("act", lambda e, ps=ps, c=c, grp=grp: e.copy(out=self.B2[:, c, grp * 512:(grp + 1) * 512], in_=ps[:, 0:512]),
                            reads=[pc], writes=[("B2", c, grp * 4 + i) for i in range(4)])
                    else:
                        c = (col - 768) // 128
                        add("act", lambda e, ps=ps, c=c, grp=grp: e.copy(out=self.qmT[:, c, grp * 512:(grp + 1) * 512], in_=ps[:, 0:512]),
                            reads=[pc], writes=[("qmT", c, grp * 4 + i) for i in range(4)])
        if hf == 1:
            kc_all = [("ksum", c, b) for c in range(6) for b in range(8)]
            add("act", lambda e: e.activation(out=self.kmT[:], in_=self.ksum[:], func=AF.Copy, scale=1.0 / 256.0),
                reads=kc_all, writes=[("kmT",)])
        prev = None
        for t in range(NT):
            gt = hf * NT + t
            qb = gt // 2
            if hf == 1:
                self.moba_gate(t, qb)
            mf = self.mem_front(t)
            for hd in range(12):
                cur = self.moba_front(hf, t, hd)
                if prev is not None:
                    self.moba_back(*prev)
                prev = (hf, t, hd, cur)
            self.mem_back(t, mf)
        self.moba_back(*prev)
        self.cat_to_T()
        self.out_proj(self.w_out_b, self.g_post_mix[1])

    def moba_gate(self, t, qb):
        add = self.add
        gv = self.gsb[:].rearrange("p (c two) b -> p c two b", two=2)
        for hh in range(2):
            ps, pc = self.psum()
            po = hh * 64
            for c in range(6):
                add("pe", lambda e, ps=ps, c=c, po=po: e.matmul(ps[:, c * 8:(c + 1) * 8], lhsT=self.B2[po:po + 64, c, t * 128:(t + 1) * 128],
                                                               rhs=self.kmT[po:po + 64, c, :], start=True, stop=True),
                    reads=[("B2", c, t), ("kmT",)], writes=[pc])
            add("act", lambda e, ps=ps, hh=hh: e.copy(out=gv[:, :, hh, :], in_=ps[:, 0:48].rearrange("p (c b) -> p c b", b=8)),
                reads=[pc], writes=[("gsb",)])
        if qb < 8:
            add("dve", lambda e: e.memset(self.gsb[:, :, qb:8], -1e30), reads=[("gsb",)], writes=[("gsb",)])
        for hd in range(12):
            add("dve", lambda e, hd=hd: e.max(out=self.top8[:, hd, :], in_=self.gsb[:, hd, :]), reads=[("gsb",)], writes=[("top8", hd)])
            add("dve", lambda e, hd=hd: e.tensor_scalar(out=self.sel[:, t % 2, hd, :], in0=self.gsb[:, hd, :], scalar1=self.top8[:, hd, 2:3],
                                                        scalar2=None, op0=ALU.is_ge), reads=[("gsb",), ("top8", hd)], writes=[("sel", t % 2, hd)])

    def moba_front(self, hf, t, hd):
        add = self.add
        gt = hf * NT + t
        qb = gt // 2
        c, po = hd // 2, (hd % 2) * 64
        qap = self.B2[po:po + 64, c, t * 128:(t + 1) * 128]
        kts = list(range(gt + 1))
        pt_of = {}
        for i0 in range(0, len(kts), 4):
            grp = kts[i0:i0 + 4]
            ps, pc = self.psum()
            for i, kt in enumerate(grp):
                add("pe", lambda e, ps=ps, i=i, kt=kt: e.matmul(ps[:, i * 128:(i + 1) * 128], lhsT=self.KT[po:po + 64, c, kt * 128:(kt + 1) * 128],
                                                               rhs=qap, start=True, stop=True),
                    reads=[("KT", c, kt // 2), ("B2", c, t)], writes=[pc])
            psl = self.rot("PT", 8)
            for i, kt in enumerate(grp):
                dd = gt - kt
                add("act", lambda e, ps=ps, i=i, psl=psl, dd=dd: e.activation(
                    out=self.PT[:, psl, i * 128:(i + 1) * 128], in_=ps[:, i * 128:(i + 1) * 128], func=AF.Exp,
                    bias=self.consts["dbias"][hd][dd], scale=0.125),
                    reads=[pc], writes=[("PT", psl)])
                if kt == gt:
                    add("dve", lambda e, psl=psl, i=i: e.tensor_tensor(out=self.PT[:, psl, i * 128:(i + 1) * 128],
                                                                       in0=self.PT[:, psl, i * 128:(i + 1) * 128], in1=self.tri4[:, 0:128], op=ALU.mult),
                        reads=[("PT", psl), ("tri4",)], writes=[("PT", psl)])
                pt_of[kt] = (psl, i)
        return pt_of

    def moba_back(self, hf, t, hd, pt_of):
        add = self.add
        gt = hf * NT + t
        qb = gt // 2
        kts = list(range(gt + 1))
        pso, poc = self.psum()
        asl = self.rot("acc", 2)
        if hf == 0:
            for n_, kt in enumerate(kts):
                psl, i = pt_of[kt]
                add("pe", lambda e, psl=psl, i=i, kt=kt, n_=n_: e.matmul(pso[:, 0:65], lhsT=self.PT[:, psl, i * 128:(i + 1) * 128],
                                                                        rhs=self.V[:, kt, hd, :], start=(n_ == 0), stop=(n_ == len(kts) - 1)),
                    reads=[("PT", psl), ("V", kt, hd // 4), ("Vones", kt)], writes=[poc])
            src = pso
            srcc = poc
            res = pso[:, 0:65]
        else:
            psB, pbc = self.psum()
            own = [kt for kt in kts if kt // 2 == qb]
            for n_, kt in enumerate(own):
                psl, i = pt_of[kt]
                add("pe", lambda e, psl=psl, i=i, kt=kt, n_=n_: e.matmul(psB[:, 0:65], lhsT=self.PT[:, psl, i * 128:(i + 1) * 128],
                                                                        rhs=self.V[:, kt, hd, :], start=(n_ == 0), stop=(n_ == len(own) - 1)),
                    reads=[("PT", psl), ("V", kt, hd // 4), ("Vones", kt)], writes=[pbc])
            for b in range(qb):
                for n_, kt in enumerate((2 * b, 2 * b + 1)):
                    psl, i = pt_of[kt]
                    add("pe", lambda e, psl=psl, i=i, kt=kt, n_=n_, b=b: e.matmul(pso[:, b * 65:(b + 1) * 65], lhsT=self.PT[:, psl, i * 128:(i + 1) * 128],
                                                                                 rhs=self.V[:, kt, hd, :], start=(n_ == 0), stop=(n_ == 1)),
                        reads=[("PT", psl), ("V", kt, hd // 4), ("Vones", kt)], writes=[poc])
            accap = self.acc[:, asl, :]
            add("dve", lambda e: e.tensor_copy(out=accap, in_=psB[:, 0:65]), reads=[pbc], writes=[("acc", asl)])
            for b in range(qb):
                add("dve", lambda e, b=b: e.scalar_tensor_tensor(out=accap, in0=pso[:, b * 65:(b + 1) * 65], scalar=self.sel[:, t % 2, hd, b:b + 1],
                                                                 in1=accap, op0=ALU.mult, op1=ALU.add),
                    reads=[poc, ("sel", t % 2, hd), ("acc", asl)], writes=[("acc", asl)])
            res = accap
            srcc = ("acc", asl)
        st, stc = self.stat()
        add("dve", lambda e: e.reciprocal(out=st[:, 0:1], in_=res[:, 64:65]), reads=[srcc], writes=[stc])
        add("dve", lambda e: e.tensor_scalar(out=self.B1[:, t, hd * 64:(hd + 1) * 64], in0=res[:, 0:64], scalar1=st[:, 0:1],
                                             scalar2=None, op0=ALU.mult), reads=[srcc, stc], writes=[("B1", t, hd // 2)])

    def build(self):
        add = self.add
        self.setup()
        self.epsc = self.sb("epsc", [128, 1], F32)
        add("dve", lambda e: e.memset(self.epsc[:], EPS), writes=[("epsc",)])
        for hf in range(NH):
            for t in range(NT):
                gt = hf * NT + t
                add("sp", lambda e, t=t, gt=gt: e.dma_start(out=self.h[:, t, :], in_=self.x[gt * 128:(gt + 1) * 128, :]),
                    writes=[("h", t)], dma="d_h%d" % t)
            if self.stop > 0:
                self.layer0_mix(hf)
            if self.stop >= 2:
                self.mlp(0)
            if self.stop >= 3:
                self.layer1_mix(hf)
            if self.stop >= 4:
                self.mlp(1)
            for t in range(NT):
                gt = hf * NT + t
                add("sp", lambda e, t=t, gt=gt: e.dma_start(out=self.out[gt * 128:(gt + 1) * 128, :], in_=self.h[:, t, :]),
                    reads=[("h", t)], dma="d_o%d" % t)
        add("sp", None, writes=[("h", t) for t in range(NT)])
        self.sc.emit(self.nc, self.es)
        self.es.close()
        return self.nc


_CACHE = {}


def get_nc(stop=99):
    if stop not in _CACHE:
        _CACHE[stop] = Builder(stop).build()
    return _CACHE[stop]


def kernel(stop=99, ncores=8, **inputs):
    stop = float(stop)
    nc = get_nc(stop)
    cs = make_consts()
    f = lambda a: np.ascontiguousarray(np.asarray(a, dtype=np.float32))
    shared = {
        "w_in_a": f(inputs["w_in_a"][0]), "ret_norm_gain": f(inputs["ret_norm_gain"][0]), "w_out_a": f(inputs["w_out_a"][0]),
        "kv_norm_gain": f(inputs["kv_norm_gain"]), "w_kv_shared": f(inputs["w_kv_shared"]), "w_in_b": f(inputs["w_in_b"][0]),
        "w_out_b": f(inputs["w_out_b"][0]), "w_mem_kv": f(inputs["w_mem_kv"]), "norm_pre_mix": f(inputs["norm_pre_mix"]),
        "norm_post_mix": f(inputs["norm_post_mix"]), "norm_pre_mlp": f(inputs["norm_pre_mlp"]),
        "norm_post_mlp": f(inputs["norm_post_mlp"]), "w_up": f(inputs["w_up"]), "w_down": f(inputs["w_down"]),
        "c_ident": cs["ident"], "c_tri4": cs["tri4"], "c_retv": cs["retv"], "c_alibi": cs["alibi"],
    }
    x = f(inputs["x"])
    mem = f(inputs["mem"])
    in_maps = []
    for b in range(ncores):
        m = dict(shared)
        m["x"] = x[b]
        m["mem"] = mem[b]
        in_maps.append(m)
    res = run_bass_kernel_spmd(nc, in_maps, core_ids=list(range(ncores)))
    return np.stack([np.asarray(r["out"], dtype=np.float32) for r in res.results], axis=0)
```

```python
import math
from contextlib import ExitStack
import numpy as np
import concourse.bass as bass
import concourse.mybir as mybir
from concourse.alu_op_type import AluOpType as ALU
from concourse.bass_utils import run_bass_kernel_spmd

F32 = mybir.dt.float32
BF16 = mybir.dt.bfloat16
AF = mybir.ActivationFunctionType
AX = mybir.AxisListType

D = 1024
S = 2048
T = 1024
NT = 8
NH = 2
DFF = 4096
EPS = 1e-6
NRING = 3
ENGS = ("pe", "act", "dve", "pool", "sp")


def alibi_slopes(n):
    def pow2(m):
        return [2.0 ** (-8.0 * (i + 1) / m) for i in range(m)]
    p = 2 ** int(math.floor(math.log2(n)))
    s = pow2(p)
    if p < n:
        s = s + pow2(2 * p)[0::2][: n - p]
    return np.asarray(s, dtype=np.float64)


class Ins:
    __slots__ = ("eng", "fn", "waits", "stream", "sidx", "milestone", "count", "is_dma")


class Sched:
    def __init__(self):
        self.cells = {}
        self.eng_list = {e: [] for e in ENGS}
        self.streams = {}
        self.seen = {e: {} for e in ENGS}

    def add(self, eng, fn, reads=(), writes=(), dma=None):
        ins = Ins()
        ins.eng = eng
        ins.fn = fn
        ins.is_dma = dma is not None
        ins.stream = dma if dma else eng
        need = {}

        def dep(d, war):
            if d is None:
                return
            if (not ins.is_dma) and (not d.is_dma) and d.eng == eng:
                if eng == "pe" or war:
                    return
            if need.get(d.stream, -1) < d.sidx:
                need[d.stream] = d.sidx

        for c in reads:
            cell = self.cells.get(c)
            if cell:
                dep(cell[0], False)
        for c in writes:
            cell = self.cells.get(c)
            if cell:
                dep(cell[0], False)
                for r in cell[1].values():
                    dep(r, True)
        waits = []
        seen = self.seen[eng]
        for s, i in need.items():
            if seen.get(s, -1) >= i:
                continue
            seen[s] = i
            waits.append((s, i))
            self.streams[s][i].milestone = True
        ins.waits = waits
        lst = self.streams.setdefault(ins.stream, [])
        ins.sidx = len(lst)
        lst.append(ins)
        ins.milestone = ins.is_dma
        self.eng_list[eng].append(ins)
        for c in writes:
            self.cells[c] = [ins, {}]
        for c in reads:
            cell = self.cells.setdefault(c, [None, {}])
            cell[1][ins.stream] = ins
        return ins

    def emit(self, nc, es):
        sems = {}
        for s, lst in self.streams.items():
            sems[s] = es.enter_context(nc.semaphore("s_" + s))
            c = 0
            for ins in lst:
                if ins.is_dma:
                    c += 16
                elif ins.milestone:
                    c += 1
                ins.count = c
        streams = self.streams

        def run(eng_name, eng):
            for ins in self.eng_list[eng_name]:
                for (s, i) in ins.waits:
                    eng.wait_ge(sems[s], streams[s][i].count)
                if ins.fn is not None:
                    h = ins.fn(eng)
                    if ins.is_dma:
                        h.then_inc(sems[ins.stream], 16)
                    elif ins.milestone:
                        h.then_inc(sems[ins.stream], 1)

        with nc.Block() as block:
            @block.tensor
            def _(e):
                run("pe", e)

            @block.scalar
            def _(e):
                run("act", e)

            @block.vector
            def _(e):
                run("dve", e)

            @block.gpsimd
            def _(e):
                run("pool", e)

            @block.sync
            def _(e):
                run("sp", e)


def make_consts():
    c = {}
    c["ident"] = np.eye(128, dtype=np.float32)
    k = np.arange(128)[:, None]
    q = np.arange(128)[None, :]
    tri = (q >= k).astype(np.float32)
    c["tri4"] = np.tile(tri, (1, 4)).astype(np.float32)
    hh = np.arange(4, dtype=np.float64)
    log_g = np.log1p(-np.exp2(-5.0 - hh))
    p = np.arange(128, dtype=np.float64)
    xi = np.exp(log_g[None, :] * (p[:, None] + 1.0))
    rv = np.zeros((128, 8), np.float32)
    rv[:, 0:4] = (1.0 / xi) * (128.0 ** -0.5)
    rv[:, 4:8] = EPS / (xi * xi)
    c["retv"] = rv
    c["ret_decay"] = [float(np.exp(log_g[i] * 128.0)) for i in range(4)]
    sl = alibi_slopes(12)
    ab = np.zeros((128, 12, 16), np.float32)
    for h in range(12):
        for d in range(16):
            ab[:, h, d] = sl[h] * (p - 64.0 - 128.0 * d)
    ef = np.exp(sl[None, :] * (p[:, None] - 64.0)).astype(np.float32)
    c["alibi"] = np.concatenate([ab.reshape(128, 192), ef], axis=1).astype(np.float32)
    c["dbias"] = [[float(-128.0 * sl[h] * d) for d in range(16)] for h in range(12)]
    return c


class Builder:
    def __init__(self, stop=99):
        self.stop = stop
        self.nc = bass.Bass("TRN2", target_bir_lowering=False, dynamic_dma_scratch_size=8192)
        self.sc = Sched()
        self.es = ExitStack()
        self.ps_i = 0
        self.ring_i = 0
        self.cnt = {}
        self.consts = make_consts()

    def sb(self, name, shape, dt):
        return self.es.enter_context(self.nc.sbuf_tensor(name, shape, dt))

    def dram(self, name, shape, dt=F32, kind="ExternalInput"):
        return self.nc.dram_tensor(name, shape, dt, kind=kind).ap()

    def rot(self, name, n):
        i = self.cnt.get(name, 0)
        self.cnt[name] = i + 1
        return i % n

    def psum(self):
        i = self.ps_i % 8
        self.ps_i += 1
        return self.PS[i], ("ps", i)

    def add(self, *a, **k):
        return self.sc.add(*a, **k)

    def setup(self):
        nc = self.nc
        d = self.dram
        self.x = d("x", [S, D])
        self.mem = d("mem", [256, D])
        self.w_in_a = d("w_in_a", [D, 2816])
        self.ret_gain = d("ret_norm_gain", [768])
        self.w_out_a = d("w_out_a", [D, D])
        self.kv_gain = d("kv_norm_gain", [D])
        self.w_kv = d("w_kv_shared", [D, 1536])
        self.w_in_b = d("w_in_b", [D, D])
        self.w_out_b = d("w_out_b", [D, D])
        self.w_mkv = d("w_mem_kv", [2, D, 512])
        self.g_pre_mix = d("norm_pre_mix", [2, D])
        self.g_post_mix = d("norm_post_mix", [2, D])
        self.g_pre_mlp = d("norm_pre_mlp", [2, D])
        self.g_post_mlp = d("norm_post_mlp", [2, D])
        self.w_up = d("w_up", [2, D, DFF])
        self.w_down = d("w_down", [2, DFF, D])
        self.c_ident = d("c_ident", [128, 128])
        self.c_tri4 = d("c_tri4", [128, 512])
        self.c_retv = d("c_retv", [128, 8])
        self.c_alibi = d("c_alibi", [128, 204])
        self.out = d("out", [S, D], kind="ExternalOutput")

        sb = self.sb
        self.h = sb("h", [128, NT, D], F32)
        self.KT = sb("KT", [128, 6, S], BF16)
        self.V = sb("V", [128, 16, 12, 65], BF16)
        self.ring = sb("ring", [128, NRING, 8, 512], BF16)
        self.B1 = sb("B1", [128, 8, 1024], BF16)
        self.B2 = sb("B2", [128, 8, 1024], BF16)
        self.B3 = sb("B3", [128, 8 * 1024], F32)
        self.y = self.B3[:].rearrange("p (t n) -> p t n", n=1024)
        self.B3b = self.B3[:].bitcast(BF16).rearrange("p (t n) -> p t n", n=2048)
        self.qmT = sb("qmT", [128, 2, 1024], BF16)
        self.memT = sb("memT", [128, 8, 256], BF16)
        self.mkT = sb("mkT", [128, 2, 256], BF16)
        self.mv = sb("mv", [128, 2, 4, 65], BF16)
        self.gain = sb("gain", [128, 2, 1024], F32)
        self.state = sb("state", [128, 4, 192], F32)
        self.state_bf = sb("state_bf", [128, 2, 4, 192], BF16)
        self.tmpf = sb("tmpf", [128, 1, 512], F32)
        self.hnb = sb("hnb", [128, 2, 1024], BF16)
        self.junk = sb("junk", [128, 192], BF16)
        self.PT = sb("PT", [128, 8, 512], BF16)
        self.PTm = sb("PTm", [128, 4, 512], BF16)
        self.ident = sb("ident", [128, 128], BF16)
        self.tri4 = sb("tri4", [128, 512], BF16)
        self.retv = sb("retv", [128, 8], F32)
        self.alibi = sb("alibi", [128, 204], F32)
        self.ksum = sb("ksum", [128, 6, 8], F32)
        self.kmT = sb("kmT", [128, 6, 8], BF16)
        self.st = sb("st", [128, 64], F32)
        self.gsb = sb("gsb", [128, 12, 8], F32)
        self.top8 = sb("top8", [128, 12, 8], F32)
        self.sel = sb("sel", [128, 2, 12, 8], F32)
        self.acc = sb("acc", [128, 2, 65], F32)
        self.PS = [self.es.enter_context(nc.psum_tensor("ps%d" % i, [128, 512], F32)) for i in range(8)]

        add = self.add
        ycell = lambda t: [("B3", t, s_) for s_ in range(8)]
        add("sp", lambda e: e.dma_start(out=self.y[:, 2, 0:128], in_=self.c_ident), writes=ycell(2), dma="d_c0")
        add("sp", lambda e: e.dma_start(out=self.y[:, 3, 0:512], in_=self.c_tri4), writes=ycell(3), dma="d_c1")
        add("sp", lambda e: e.dma_start(out=self.retv[:], in_=self.c_retv), writes=[("c", 2)], dma="d_c2")
        add("sp", lambda e: e.dma_start(out=self.alibi[:], in_=self.c_alibi), writes=[("c", 3)], dma="d_c3")
        add("dve", lambda e: e.tensor_copy(out=self.ident[:], in_=self.y[:, 2, 0:128]), reads=ycell(2), writes=[("ident",)])
        add("dve", lambda e: e.tensor_copy(out=self.tri4[:], in_=self.y[:, 3, 0:512]), reads=ycell(3), writes=[("tri4",)])
        for gt_ in range(16):
            add("dve", lambda e, gt_=gt_: e.tensor_copy(out=self.V[:, gt_, :, 64], in_=self.alibi[:, 192:204]),
                reads=[("c", 3)], writes=[("Vones", gt_)])
        add("dve", lambda e: e.memset(self.mv[:, :, :, 64:65], 1.0), writes=[("mvones",)])
        add("dve", lambda e: e.memset(self.state[:], 0.0), writes=[("state", i_) for i_ in range(4)])
        add("dve", lambda e: e.memset(self.state_bf[:], 0.0), writes=[("state_bf", p_, i_) for p_ in range(2) for i_ in range(4)])
        for mt in range(2):
            add("sp", lambda e, mt=mt: e.dma_start(out=self.y[:, mt, :], in_=self.mem[mt * 128:(mt + 1) * 128, :]),
                writes=ycell(mt), dma="d_mem%d" % mt)
            add("dve", lambda e, mt=mt: e.tensor_copy(out=self.hnb[:, 0, :], in_=self.y[:, mt, :]),
                reads=ycell(mt), writes=[("hnb", 0)])
            self.transpose8(self.hnb[:, 0, :], [("hnb", 0)], self.memT[:, :, mt * 128:(mt + 1) * 128], [("memT", mt)])

    def transpose8(self, src, src_cells, dst, dst_cells, eng="act"):
        ps, pc = self.psum()
        psb = ps[:].bitcast(BF16)
        for kc in range(8):
            self.add("pe", lambda e, kc=kc: e.transpose(out=psb[:, kc * 128:(kc + 1) * 128],
                                                        in_=src[:, kc * 128:(kc + 1) * 128], identity=self.ident[:]),
                     reads=list(src_cells) + [("ident",)], writes=[pc])
        pin = psb[:, 0:1024].rearrange("p (k c) -> p k c", c=128)
        if eng == "act":
            self.add("act", lambda e: e.copy(out=dst, in_=pin), reads=[pc], writes=dst_cells)
        else:
            self.add("dve", lambda e: e.tensor_copy(out=dst, in_=pin), reads=[pc], writes=dst_cells)

    def load_slab(self, wap, ncols=512):
        r = self.ring_i % NRING
        self.ring_i += 1
        src = wap.rearrange("(kc p) n -> p kc n", p=128)
        self.add("pool", lambda e: e.dma_start(out=self.ring[:, r, :, 0:ncols], in_=src),
                 writes=[("ring", r)], dma="d_ring%d" % r)
        return r

    def load_gain(self, gap, n=1024):
        sl = self.rot("gain", 2)
        self.add("sp", lambda e: e.dma_start(out=self.gain[:, sl, 0:n], in_=gap.partition_broadcast(128)),
                 writes=[("gain", sl)], dma="d_gain%d" % sl)
        return sl

    def stat(self):
        i = self.rot("st", 16)
        return self.st[:, i * 4:(i + 1) * 4], ("st", i)

    def norm_A(self, src, src_cells, gsl):
        add = self.add
        st, stc = self.stat()
        sl = self.rot("hnb", 2)
        add("act", lambda e: e.activation(out=self.hnb[:, sl, :], in_=src, func=AF.Square, accum_out=st[:, 0:1]),
            reads=src_cells, writes=[stc, ("hnb", sl)])
        add("act", lambda e: e.activation(out=st[:, 1:2], in_=st[:, 0:1], func=AF.Sqrt, bias=self.epsc[:, 0:1], scale=1.0 / D),
            reads=[stc, ("epsc",)], writes=[stc])
        add("dve", lambda e: e.reciprocal(out=st[:, 2:3], in_=st[:, 1:2]), reads=[stc], writes=[stc])
        add("dve", lambda e: e.scalar_tensor_tensor(out=self.hnb[:, sl, :], in0=src, scalar=st[:, 2:3],
                                                    in1=self.gain[:, gsl, :], op0=ALU.mult, op1=ALU.mult),
            reads=list(src_cells) + [stc, ("gain", gsl)], writes=[("hnb", sl)])
        return sl

    def norm_phase(self, gsl):
        sl = self.norm_A(self.h[:, 0, :], [("h", 0)], gsl)
        for t in range(NT):
            nsl = self.norm_A(self.h[:, t + 1, :], [("h", t + 1)], gsl) if t + 1 < NT else None
            self.transpose8(self.hnb[:, sl, :], [("hnb", sl)], self.B1[:, :, t * 128:(t + 1) * 128],
                            [("B1", kc, t) for kc in range(8)], eng=("act" if t % 2 == 0 else "dve"))
            sl = nsl

    def post_res(self, t, gsl):
        add = self.add
        st, stc = self.stat()
        ycells = [("B3", t, s) for s in range(8)]
        jsl = self.rot("hnb", 2)
        add("act", lambda e: e.activation(out=self.hnb[:, jsl, :], in_=self.y[:, t, :], func=AF.Square, accum_out=st[:, 0:1]),
            reads=ycells, writes=[stc, ("hnb", jsl)])
        add("act", lambda e: e.activation(out=st[:, 1:2], in_=st[:, 0:1], func=AF.Sqrt, bias=self.epsc[:, 0:1], scale=1.0 / D),
            reads=[stc, ("epsc",)], writes=[stc])
        add("dve", lambda e: e.reciprocal(out=st[:, 2:3], in_=st[:, 1:2]), reads=[stc], writes=[stc])
        add("dve", lambda e: e.scalar_tensor_tensor(out=self.y[:, t, :], in0=self.y[:, t, :], scalar=st[:, 2:3],
                                                    in1=self.gain[:, gsl, :], op0=ALU.mult, op1=ALU.mult),
            reads=ycells + [stc, ("gain", gsl)], writes=ycells)
        add("dve", lambda e: e.tensor_tensor(out=self.h[:, t, :], in0=self.h[:, t, :], in1=self.y[:, t, :], op=ALU.add),
            reads=[("h", t)] + ycells, writes=[("h", t)])

    def mm_B(self, r, j, xT, xcells_fn, grp, ncol=512, nk=8):
        ps, pc = self.psum()
        for kc in range(nk):
            self.add("pe", lambda e, kc=kc: e.matmul(ps[:, 0:ncol], lhsT=self.ring[:, r, kc, j * 128:(j + 1) * 128],
                                                     rhs=xT[:, kc, grp * ncol:(grp + 1) * ncol],
                                                     start=(kc == 0), stop=(kc == nk - 1)),
                     reads=[("ring", r)] + xcells_fn(kc, grp), writes=[pc])
        return ps, pc

    def mm_A(self, r, c0, n, xT, xcells_fn, t, nk=8):
        ps, pc = self.psum()
        for kc in range(nk):
            self.add("pe", lambda e, kc=kc: e.matmul(ps[:, 0:n], lhsT=xT[:, kc, t * 128:(t + 1) * 128],
                                                     rhs=self.ring[:, r, kc, c0:c0 + n],
                                                     start=(kc == 0), stop=(kc == nk - 1)),
                     reads=[("ring", r)] + xcells_fn(kc, t), writes=[pc])
        return ps, pc

    @staticmethod
    def cellsB1_grp(kc, grp):
        return [("B1", kc, grp * 4 + i) for i in range(4)]

    @staticmethod
    def cellsB1_t(kc, t):
        return [("B1", kc, t)]

    @staticmethod
    def cellsB2_grp(kc, grp):
        return [("B2", kc, grp * 4 + i) for i in range(4)]

    @staticmethod
    def cellsB2_t(kc, t):
        return [("B2", kc, t)]

    def mem_kv(self, l):
        add = self.add
        r = self.load_slab(self.w_mkv[l])
        for j in range(2):
            ps, pc = self.psum()
            for kc in range(8):
                add("pe", lambda e, kc=kc, ps=ps, j=j: e.matmul(ps[:, 0:256], lhsT=self.ring[:, r, kc, j * 128:(j + 1) * 128],
                                                               rhs=self.memT[:, kc, :], start=(kc == 0), stop=(kc == 7)),
                    reads=[("ring", r), ("memT", 0), ("memT", 1)], writes=[pc])
            add("act", lambda e, ps=ps, j=j: e.copy(out=self.mkT[:, j, :], in_=ps[:, 0:256]), reads=[pc], writes=[("mkT", j)])
        for mt in range(2):
            ps, pc = self.psum()
            for kc in range(8):
                add("pe", lambda e, kc=kc, ps=ps, mt=mt: e.matmul(ps[:, 0:256], lhsT=self.memT[:, kc, mt * 128:(mt + 1) * 128],
                                                                 rhs=self.ring[:, r, kc, 256:512], start=(kc == 0), stop=(kc == 7)),
                    reads=[("ring", r), ("memT", mt)], writes=[pc])
            add("act", lambda e, ps=ps, mt=mt: e.copy(out=self.mv[:, mt, :, 0:64],
                                                     in_=ps[:, 0:256].rearrange("p (h c) -> p h c", c=64)),
                reads=[pc, ("mvones",)], writes=[("mv", mt)])

    def mem_front(self, t):
        add = self.add
        pts = []
        for hh in range(2):
            ps, pc = self.psum()
            po = hh * 64
            for half in range(2):
                for mt in range(2):
                    sl = half * 2 + mt
                    add("pe", lambda e, ps=ps, sl=sl, po=po, half=half, mt=mt: e.matmul(
                        ps[:, sl * 128:(sl + 1) * 128], lhsT=self.mkT[po:po + 64, half, mt * 128:(mt + 1) * 128],
                        rhs=self.qmT[po:po + 64, half, t * 128:(t + 1) * 128], start=True, stop=True),
                        reads=[("mkT", half), ("qmT", half, t)], writes=[pc])
            psl = (t % 2) * 2 + hh
            add("act", lambda e, ps=ps, psl=psl: e.activation(out=self.PTm[:, psl, :], in_=ps[:, 0:512], func=AF.Exp, scale=0.125),
                reads=[pc], writes=[("PTm", psl)])
            pts.append(psl)
        return pts

    def mem_back(self, t, pts):
        add = self.add
        pso, poc = self.psum()
        for hd in range(4):
            psl = pts[hd % 2]
            for mt in range(2):
                sl = (hd // 2) * 2 + mt
                add("pe", lambda e, hd=hd, psl=psl, sl=sl, mt=mt: e.matmul(
                    pso[:, hd * 65:(hd + 1) * 65], lhsT=self.PTm[:, psl, sl * 128:(sl + 1) * 128],
                    rhs=self.mv[:, mt, hd, :], start=(mt == 0), stop=(mt == 1)),
                    reads=[("PTm", psl), ("mv", mt), ("mvones",)], writes=[poc])
        st, stc = self.stat()
        pv = pso[:, 0:260].rearrange("p (h c) -> p h c", c=65)
        add("dve", lambda e: e.reciprocal(out=st[:, 0:4], in_=pv[:, :, 64]), reads=[poc], writes=[stc])
        for hd in range(4):
            add("dve", lambda e, hd=hd: e.tensor_scalar(out=self.B1[:, t, 768 + hd * 64:768 + (hd + 1) * 64], in0=pv[:, hd, 0:64],
                                                        scalar1=st[:, hd:hd + 1], scalar2=None, op0=ALU.mult),
                reads=[poc, stc], writes=[("B1", t, 6 + hd // 2)])

    def mem_attn(self, t):
        self.mem_back(t, self.mem_front(t))

    def cat_to_T(self):
        for t in range(NT):
            self.transpose8(self.B1[:, t, :], [("B1", t, j) for j in range(8)],
                            self.B2[:, :, t * 128:(t + 1) * 128], [("B2", kc, t) for kc in range(8)])

    def out_proj(self, wap, g_post):
        add = self.add
        gsl = self.load_gain(g_post)
        for s in range(2):
            r = self.load_slab(wap[:, s * 512:(s + 1) * 512])
            for t in range(NT):
                ps, pc = self.mm_A(r, 0, 512, self.B2, self.cellsB2_t, t)
                add("act", lambda e, ps=ps, t=t, s=s: e.copy(out=self.y[:, t, s * 512:(s + 1) * 512], in_=ps[:, 0:512]),
                    reads=[pc], writes=[("B3", t, s * 4 + i) for i in range(4)])
                if s == 1:
                    if t >= 2:
                        self.post_res(t - 2, gsl)
            if s == 1:
                self.post_res(NT - 2, gsl)
                self.post_res(NT - 1, gsl)

    def mlp(self, l):
        add = self.add
        gsl = self.load_gain(self.g_pre_mlp[l])
        self.norm_phase(gsl)
        gpost = self.load_gain(self.g_post_mlp[l])
        for b in range(4):
            for s in range(2):
                r = self.load_slab(self.w_up[l][:, b * 1024 + s * 512: b * 1024 + (s + 1) * 512])
                for j in range(4):
                    for grp in range(2):
                        ps, pc = self.mm_B(r, j, self.B1, self.cellsB1_grp, grp)
                        dst = self.B2[:, s * 4 + j, grp * 512:(grp + 1) * 512]
                        dcells = [("B2", s * 4 + j, grp * 4 + i) for i in range(4)]
                        add("act", lambda e, ps=ps: e.activation(out=self.tmpf[:, 0, 0:512], in_=ps[:, 0:512], func=AF.Relu),
                            reads=[pc], writes=[("tmpf", 0)])
                        add("dve", lambda e, ps=ps, dst=dst: e.tensor_tensor(out=dst, in0=self.tmpf[:, 0, 0:512], in1=ps[:, 0:512], op=ALU.mult),
                            reads=[pc, ("tmpf", 0)], writes=dcells)
            for s in range(2):
                r = self.load_slab(self.w_down[l][b * 1024:(b + 1) * 1024, s * 512:(s + 1) * 512])
                for t in range(NT):
                    ps, pc = self.mm_A(r, 0, 512, self.B2, self.cellsB2_t, t)
                    yc = [("B3", t, s * 4 + i) for i in range(4)]
                    if b == 0:
                        add("act", lambda e, ps=ps, t=t, s=s: e.copy(out=self.y[:, t, s * 512:(s + 1) * 512], in_=ps[:, 0:512]),
                            reads=[pc], writes=yc)
                    else:
                        add("dve", lambda e, ps=ps, t=t, s=s: e.tensor_tensor(out=self.y[:, t, s * 512:(s + 1) * 512],
                                                                             in0=self.y[:, t, s * 512:(s + 1) * 512], in1=ps[:, 0:512], op=ALU.add),
                            reads=[pc] + yc, writes=yc)
                    if b == 3 and s == 1 and t >= 2:
                        self.post_res(t - 2, gpost)
                if b == 3 and s == 1:
                    self.post_res(NT - 2, gpost)
                    self.post_res(NT - 1, gpost)

    def layer0_mix(self, hf):
        add = self.add
        gsl = self.load_gain(self.g_pre_mix[0])
        self.norm_phase(gsl)
        if self.stop < 0.15: return
        self.mem_kv(0)
        if self.stop < 0.25: return
        W = self.w_in_a
        r = self.load_slab(W[:, 0:512])
        for j in range(4):
            for grp in range(2):
                ps, pc = self.mm_B(r, j, self.B1, self.cellsB1_grp, grp)
                add("act", lambda e, ps=ps, j=j, grp=grp: e.copy(out=self.B2[:, j, grp * 512:(grp + 1) * 512], in_=ps[:, 0:512]),
                    reads=[pc], writes=[("B2", j, grp * 4 + i) for i in range(4)])
        if self.stop < 0.35: return
        r = self.load_slab(W[:, 512:1024])
        for j in range(4):
            for grp in range(2):
                ps, pc = self.mm_B(r, j, self.B1, self.cellsB1_grp, grp)
                add("act", lambda e, ps=ps, j=j, grp=grp: e.copy(out=self.B2[:, 4 + j, grp * 512:(grp + 1) * 512], in_=ps[:, 0:512]),
                    reads=[pc], writes=[("B2", 4 + j, grp * 4 + i) for i in range(4)])
        for t in range(NT):
            ps, pc = self.mm_A(r, 0, 512, self.B1, self.cellsB1_t, t)
            add("dve", lambda e, ps=ps, t=t: e.tensor_copy(out=self.B3b[:, t, 0:512], in_=ps[:, 0:512]),
                reads=[pc], writes=[("B3", t, 0), ("B3", t, 1)])
        if self.stop < 0.45: return
        rgs = self.load_gain(self.ret_gain, 768)
        for si in range(2, 5):
            r = self.load_slab(W[:, si * 512:(si + 1) * 512])
            for t in range(NT):
                ps, pc = self.mm_A(r, 0, 512, self.B1, self.cellsB1_t, t)
                c0 = si * 512 - 1024
                bounds = sorted(set([c0, c0 + 512] + [b for b in range(0, 1537, 192) if c0 < b < c0 + 512]))
                for a, bnd in zip(bounds[:-1], bounds[1:]):
                    lo, hi = a - c0, bnd - c0
                    if a < 768:
                        hd = a // 192
                        add("act", lambda e, ps=ps, t=t, lo=lo, hi=hi, a=a, bnd=bnd, hd=hd: e.activation(
                            out=self.B3b[:, t, 512 + a:512 + bnd], in_=ps[:, lo:hi], func=AF.Copy, scale=self.retv[:, hd:hd + 1]),
                            reads=[pc, ("c", 2)], writes=[("B3", t, s) for s in range((512 + a) // 256, (512 + bnd - 1) // 256 + 1)])
                    else:
                        ga, gb = a - 768, bnd - 768
                        sl = 0
                        add("act", lambda e, ps=ps, lo=lo, hi=hi, sl=sl: e.activation(out=self.tmpf[:, sl, 0:hi - lo], in_=ps[:, lo:hi], func=AF.Silu),
                            reads=[pc], writes=[("tmpf", sl)])
                        add("dve", lambda e, t=t, ga=ga, gb=gb, sl=sl: e.tensor_tensor(
                            out=self.B3b[:, t, 1280 + ga:1280 + gb], in0=self.tmpf[:, sl, 0:gb - ga], in1=self.gain[:, rgs, ga:gb], op=ALU.mult),
                            reads=[("tmpf", sl), ("gain", rgs)], writes=[("B3", t, s) for s in range((1280 + ga) // 256, (1280 + gb - 1) // 256 + 1)])
        if self.stop < 0.55: return
        r = self.load_slab(W[:, 2560:2816], ncols=256)
        for j in range(2):
            for grp in range(2):
                ps, pc = self.mm_B(r, j, self.B1, self.cellsB1_grp, grp)
                add("act", lambda e, ps=ps, j=j, grp=grp: e.copy(out=self.qmT[:, j, grp * 512:(grp + 1) * 512], in_=ps[:, 0:512]),
                    reads=[pc], writes=[("qmT", j, grp * 4 + i) for i in range(4)])
        if self.stop < 0.65: return
        dec = self.consts["ret_decay"]
        def kvupd(t):
            par = (hf * NT + t + 1) % 2
            vcells = [("B3", t, 2), ("B3", t, 3), ("B3", t, 4)]
            for pair in range(2):
                pk, pkc = self.psum()
                for hh in range(2):
                    hd = pair * 2 + hh
                    add("pe", lambda e, pk=pk, hd=hd, hh=hh, t=t: e.matmul(
                        pk[:, hh * 192:(hh + 1) * 192], lhsT=self.B3b[:, t, hd * 128:(hd + 1) * 128],
                        rhs=self.B3b[:, t, 512 + hd * 192:512 + (hd + 1) * 192], start=True, stop=True),
                        reads=[("B3", t, 0), ("B3", t, 1)] + vcells, writes=[pkc])
                for hh in range(2):
                    hd = pair * 2 + hh
                    add("dve", lambda e, pk=pk, hd=hd, hh=hh: e.tensor_tensor(
                        out=self.state[:, hd, :], in0=pk[:, hh * 192:(hh + 1) * 192], in1=self.state[:, hd, :], op=ALU.add),
                        reads=[pkc, ("state", hd)], writes=[("state", hd)])
                    add("dve", lambda e, hd=hd, par=par: e.tensor_scalar(out=self.state_bf[:, par, hd, :], in0=self.state[:, hd, :], scalar1=dec[hd],
                                                                        scalar2=None, op0=ALU.mult), reads=[("state", hd)], writes=[("state_bf", par, hd)])
                    add("dve", lambda e, hd=hd: e.tensor_scalar(out=self.state[:, hd, :], in0=self.state[:, hd, :], scalar1=dec[hd],
                                                               scalar2=None, op0=ALU.mult), reads=[("state", hd)], writes=[("state", hd)])

        def front(t):
            if t > 0:
                kvupd(t - 1)
            tok = slice(t * 128, (t + 1) * 128)
            ps, pc = self.psum()
            for hd in range(4):
                add("pe", lambda e, ps=ps, hd=hd, tok=tok: e.matmul(ps[:, hd * 128:(hd + 1) * 128], lhsT=self.B2[:, 4 + hd, tok],
                                                                   rhs=self.B2[:, hd, tok], start=True, stop=True),
                    reads=[("B2", 4 + hd, t), ("B2", hd, t)], writes=[pc])
            psl = self.rot("PT", 8)
            add("dve", lambda e, ps=ps, psl=psl: e.tensor_tensor(out=self.PT[:, psl, :], in0=ps[:, 0:512], in1=self.tri4[:], op=ALU.mult),
                reads=[pc, ("tri4",)], writes=[("PT", psl)])
            return psl, self.mem_front(t)

        def back(t, psl, mf):
            tok = slice(t * 128, (t + 1) * 128)
            vcells = [("B3", t, 2), ("B3", t, 3), ("B3", t, 4)]
            pos = []
            for pair in range(2):
                po_, poc = self.psum()
                pos.append((po_, poc))
                for hh in range(2):
                    hd = pair * 2 + hh
                    add("pe", lambda e, po_=po_, hd=hd, hh=hh, psl=psl, t=t: e.matmul(
                        po_[:, hh * 192:(hh + 1) * 192], lhsT=self.PT[:, psl, hd * 128:(hd + 1) * 128],
                        rhs=self.B3b[:, t, 512 + hd * 192:512 + (hd + 1) * 192], start=True, stop=False),
                        reads=[("PT", psl)] + vcells, writes=[poc])
                    add("pe", lambda e, po_=po_, hd=hd, hh=hh, tok=tok: e.matmul(
                        po_[:, hh * 192:(hh + 1) * 192], lhsT=self.B2[:, hd, tok], rhs=self.state_bf[:, (hf * NT + t) % 2, hd, :], start=False, stop=True),
                        reads=[("B2", hd, t), ("state_bf", (hf * NT + t) % 2, hd)], writes=[poc])
            st, stc = self.stat()
            for hd in range(4):
                po_, poc = pos[hd // 2]
                hh = hd % 2
                add("act", lambda e, po_=po_, hh=hh, hd=hd: e.activation(out=self.junk[:, 0:192], in_=po_[:, hh * 192:(hh + 1) * 192],
                                                                       func=AF.Square, accum_out=st[:, hd:hd + 1]),
                    reads=[poc], writes=[stc])
            st2, stc2 = self.stat()
            add("dve", lambda e: e.scalar_tensor_tensor(out=st2[:, 0:4], in0=st[:, 0:4], scalar=1.0 / 192.0, in1=self.retv[:, 4:8],
                                                        op0=ALU.mult, op1=ALU.add), reads=[stc, ("c", 2)], writes=[stc2])
            add("act", lambda e: e.activation(out=st2[:, 0:4], in_=st2[:, 0:4], func=AF.Sqrt), reads=[stc2], writes=[stc2])
            st3, stc3 = self.stat()
            add("dve", lambda e: e.reciprocal(out=st3[:, 0:4], in_=st2[:, 0:4]), reads=[stc2], writes=[stc3])
            for hd in range(4):
                po_, poc = pos[hd // 2]
                hh = hd % 2
                c0, c1 = hd * 192, (hd + 1) * 192
                add("dve", lambda e, po_=po_, hh=hh, hd=hd, c0=c0, c1=c1, t=t: e.scalar_tensor_tensor(
                    out=self.B1[:, t, c0:c1], in0=po_[:, hh * 192:(hh + 1) * 192], scalar=st3[:, hd:hd + 1],
                    in1=self.B3b[:, t, 1280 + c0:1280 + c1], op0=ALU.mult, op1=ALU.mult),
                    reads=[poc, stc3, ("B3", t, 5), ("B3", t, 6), ("B3", t, 7)],
                    writes=[("B1", t, j) for j in range(c0 // 128, (c1 - 1) // 128 + 1)])
            self.mem_back(t, mf)

        f = front(0)
        for t in range(NT):
            fn = front(t + 1) if t + 1 < NT else None
            back(t, *f)
            f = fn
        kvupd(NT - 1)
        if self.stop < 0.75: return
        self.cat_to_T()
        if self.stop < 0.85: return
        self.out_proj(self.w_out_a, self.g_post_mix[0])

    def layer1_mix(self, hf):
        add = self.add
        gsl = self.load_gain(self.kv_gain)
        self.norm_phase(gsl)
        W = self.w_kv
        for si in range(3):
            r = self.load_slab(W[:, si * 512:(si + 1) * 512])
            for j in range(4):
                col = si * 512 + j * 128
                if col >= 768:
                    continue
                c = col // 128
                for grp in range(2):
                    ps, pc = self.mm_B(r, j, self.B1, self.cellsB1_grp, grp)
                    for bb in range(2):
                        blk = hf * 4 + grp * 2 + bb
                        g0 = hf * T + grp * 512 + bb * 256
                        add("act", lambda e, ps=ps, c=c, g0=g0, bb=bb, blk=blk: e.activation(
                            out=self.KT[:, c, g0:g0 + 256], in_=ps[:, bb * 256:(bb + 1) * 256], func=AF.Copy,
                            accum_out=self.ksum[:, c, blk:blk + 1]),
                            reads=[pc], writes=[("KT", c, blk), ("ksum", c, blk)])
            v0 = max(si * 512, 768)
            v1 = (si + 1) * 512
            if v1 > v0:
                n = v1 - v0
                h0 = (v0 - 768) // 64
                nh = n // 64
                for t in range(NT):
                    gt = hf * NT + t
                    ps, pc = self.mm_A(r, v0 - si * 512, n, self.B1, self.cellsB1_t, t)
                    for j_ in range(nh):
                        hd_ = h0 + j_
                        add("dve", lambda e, ps=ps, gt=gt, hd_=hd_, j_=j_: e.tensor_scalar(
                            out=self.V[:, gt, hd_, 0:64], in0=ps[:, j_ * 64:(j_ + 1) * 64], scalar1=self.alibi[:, 192 + hd_:193 + hd_],
                            scalar2=None, op0=ALU.mult),
                            reads=[pc, ("c", 3)], writes=[("V", gt, hd_ // 4)])
        gsl = self.load_gain(self.g_pre_mix[1])
        self.norm_phase(gsl)
        self.mem_kv(1)
        W = self.w_in_b
        for si in range(2):
            r = self.load_slab(W[:, si * 512:(si + 1) * 512])
            for j in range(4):
                col = si * 512 + j * 128
                for grp in range(2):
                    ps, pc = self.mm_B(r, j, self.B1, self.cellsB1_grp, grp)
                    if col < 768:
                        c = col // 128
                        add("act", lambda e, ps=ps, c=c, grp=grp: e.copy(out=self.B2[:, c, grp * 512:(grp + 1) * 512], in_=ps[:, 0:512]),
                            reads=[pc], writes=[("B2", c, grp * 4 + i) for i in range(4)])
                    else:
                        c = (col - 768) // 128
                        add("act", lambda e, ps=ps, c=c, grp=grp: e.copy(out=self.qmT[:, c, grp * 512:(grp + 1) * 512], in_=ps[:, 0:512]),
                            reads=[pc], writes=[("qmT", c, grp * 4 + i) for i in range(4)])
        if hf == 1:
            kc_all = [("ksum", c, b) for c in range(6) for b in range(8)]
            add("act", lambda e: e.activation(out=self.kmT[:], in_=self.ksum[:], func=AF.Copy, scale=1.0 / 256.0),
                reads=kc_all, writes=[("kmT",)])
        prev = None
        for t in range(NT):
            gt = hf * NT + t
            qb = gt // 2
            if hf == 1:
                self.moba_gate(t, qb)
            mf = self.mem_front(t)
            for hd in range(12):
                cur = self.moba_front(hf, t, hd)
                if prev is not None:
                    self.moba_back(*prev)
                prev = (hf, t, hd, cur)
            self.mem_back(t, mf)
        self.moba_back(*prev)
        self.cat_to_T()
        self.out_proj(self.w_out_b, self.g_post_mix[1])

    def moba_gate(self, t, qb):
        add = self.add
        gv = self.gsb[:].rearrange("p (c two) b -> p c two b", two=2)
        for hh in range(2):
            ps, pc = self.psum()
            po = hh * 64
            for c in range(6):
                add("pe", lambda e, ps=ps, c=c, po=po: e.matmul(ps[:, c * 8:(c + 1) * 8], lhsT=self.B2[po:po + 64, c, t * 128:(t + 1) * 128],
                                                               rhs=self.kmT[po:po + 64, c, :], start=True, stop=True),
                    reads=[("B2", c, t), ("kmT",)], writes=[pc])
            add("act", lambda e, ps=ps, hh=hh: e.copy(out=gv[:, :, hh, :], in_=ps[:, 0:48].rearrange("p (c b) -> p c b", b=8)),
                reads=[pc], writes=[("gsb",)])
        if qb < 8:
            add("dve", lambda e: e.memset(self.gsb[:, :, qb:8], -1e30), reads=[("gsb",)], writes=[("gsb",)])
        for hd in range(12):
            add("dve", lambda e, hd=hd: e.max(out=self.top8[:, hd, :], in_=self.gsb[:, hd, :]), reads=[("gsb",)], writes=[("top8", hd)])
            add("dve", lambda e, hd=hd: e.tensor_scalar(out=self.sel[:, t % 2, hd, :], in0=self.gsb[:, hd, :], scalar1=self.top8[:, hd, 2:3],
                                                        scalar2=None, op0=ALU.is_ge), reads=[("gsb",), ("top8", hd)], writes=[("sel", t % 2, hd)])

    def moba_front(self, hf, t, hd):
        add = self.add
        gt = hf * NT + t
        qb = gt // 2
        c, po = hd // 2, (hd % 2) * 64
        qap = self.B2[po:po + 64, c, t * 128:(t + 1) * 128]
        kts = list(range(gt + 1))
        pt_of = {}
        for i0 in range(0, len(kts), 4):
            grp = kts[i0:i0 + 4]
            ps, pc = self.psum()
            for i, kt in enumerate(grp):
                add("pe", lambda e, ps=ps, i=i, kt=kt: e.matmul(ps[:, i * 128:(i + 1) * 128], lhsT=self.KT[po:po + 64, c, kt * 128:(kt + 1) * 128],
                                                               rhs=qap, start=True, stop=True),
                    reads=[("KT", c, kt // 2), ("B2", c, t)], writes=[pc])
            psl = self.rot("PT", 8)
            for i, kt in enumerate(grp):
                dd = gt - kt
                add("act", lambda e, ps=ps, i=i, psl=psl, dd=dd: e.activation(
                    out=self.PT[:, psl, i * 128:(i + 1) * 128], in_=ps[:, i * 128:(i + 1) * 128], func=AF.Exp,
                    bias=self.consts["dbias"][hd][dd], scale=0.125),
                    reads=[pc], writes=[("PT", psl)])
                if kt == gt:
                    add("dve", lambda e, psl=psl, i=i: e.tensor_tensor(out=self.PT[:, psl, i * 128:(i + 1) * 128],
                                                                       in0=self.PT[:, psl, i * 128:(i + 1) * 128], in1=self.tri4[:, 0:128], op=ALU.mult),
                        reads=[("PT", psl), ("tri4",)], writes=[("PT", psl)])
                pt_of[kt] = (psl, i)
        return pt_of

    def moba_back(self, hf, t, hd, pt_of):
        add = self.add
        gt = hf * NT + t
        qb = gt // 2
        kts = list(range(gt + 1))
        pso, poc = self.psum()
        asl = self.rot("acc", 2)
        if hf == 0:
            for n_, kt in enumerate(kts):
                psl, i = pt_of[kt]
                add("pe", lambda e, psl=psl, i=i, kt=kt, n_=n_: e.matmul(pso[:, 0:65], lhsT=self.PT[:, psl, i * 128:(i + 1) * 128],
                                                                        rhs=self.V[:, kt, hd, :], start=(n_ == 0), stop=(n_ == len(kts) - 1)),
                    reads=[("PT", psl), ("V", kt, hd // 4), ("Vones", kt)], writes=[poc])
            src = pso
            srcc = poc
            res = pso[:, 0:65]
        else:
            psB, pbc = self.psum()
            own = [kt for kt in kts if kt // 2 == qb]
            for n_, kt in enumerate(own):
                psl, i = pt_of[kt]
                add("pe", lambda e, psl=psl, i=i, kt=kt, n_=n_: e.matmul(psB[:, 0:65], lhsT=self.PT[:, psl, i * 128:(i + 1) * 128],
                                                                        rhs=self.V[:, kt, hd, :], start=(n_ == 0), stop=(n_ == len(own) - 1)),
                    reads=[("PT", psl), ("V", kt, hd // 4), ("Vones", kt)], writes=[pbc])
            for b in range(qb):
                for n_, kt in enumerate((2 * b, 2 * b + 1)):
                    psl, i = pt_of[kt]
                    add("pe", lambda e, psl=psl, i=i, kt=kt, n_=n_, b=b: e.matmul(pso[:, b * 65:(b + 1) * 65], lhsT=self.PT[:, psl, i * 128:(i + 1) * 128],
                                                                                 rhs=self.V[:, kt, hd, :], start=(n_ == 0), stop=(n_ == 1)),
                        reads=[("PT", psl), ("V", kt, hd // 4), ("Vones", kt)], writes=[poc])
            accap = self.acc[:, asl, :]
            add("dve", lambda e: e.tensor_copy(out=accap, in_=psB[:, 0:65]), reads=[pbc], writes=[("acc", asl)])
            for b in range(qb):
                add("dve", lambda e, b=b: e.scalar_tensor_tensor(out=accap, in0=pso[:, b * 65:(b + 1) * 65], scalar=self.sel[:, t % 2, hd, b:b + 1],
                                                                 in1=accap, op0=ALU.mult, op1=ALU.add),
                    reads=[poc, ("sel", t % 2, hd), ("acc", asl)], writes=[("acc", asl)])
            res = accap
            srcc = ("acc", asl)
        st, stc = self.stat()
        add("dve", lambda e: e.reciprocal(out=st[:, 0:1], in_=res[:, 64:65]), reads=[srcc], writes=[stc])
        add("dve", lambda e: e.tensor_scalar(out=self.B1[:, t, hd * 64:(hd + 1) * 64], in0=res[:, 0:64], scalar1=st[:, 0:1],
                                             scalar2=None, op0=ALU.mult), reads=[srcc, stc], writes=[("B1", t, hd // 2)])

    def build(self):
        add = self.add
        self.setup()
        self.epsc = self.sb("epsc", [128, 1], F32)
        add("dve", lambda e: e.memset(self.epsc[:], EPS), writes=[("epsc",)])
        for hf in range(NH):
            for t in range(NT):
                gt = hf * NT + t
                add("sp", lambda e, t=t, gt=gt: e.dma_start(out=self.h[:, t, :], in_=self.x[gt * 128:(gt + 1) * 128, :]),
                    writes=[("h", t)], dma="d_h%d" % t)
            if self.stop > 0:
                self.layer0_mix(hf)
            if self.stop >= 2:
                self.mlp(0)
            if self.stop >= 3:
                self.layer1_mix(hf)
            if self.stop >= 4:
                self.mlp(1)
            for t in range(NT):
                gt = hf * NT + t
                add("sp", lambda e, t=t, gt=gt: e.dma_start(out=self.out[gt * 128:(gt + 1) * 128, :], in_=self.h[:, t, :]),
                    reads=[("h", t)], dma="d_o%d" % t)
        add("sp", None, writes=[("h", t) for t in range(NT)])
        self.sc.emit(self.nc, self.es)
        self.es.close()
        return self.nc


_CACHE = {}


def get_nc(stop=99):
    if stop not in _CACHE:
        _CACHE[stop] = Builder(stop).build()
    return _CACHE[stop]


def kernel(stop=99, ncores=8, **inputs):
    stop = float(stop)
    nc = get_nc(stop)
    cs = make_consts()
    f = lambda a: np.ascontiguousarray(np.asarray(a, dtype=np.float32))
    shared = {
        "w_in_a": f(inputs["w_in_a"][0]), "ret_norm_gain": f(inputs["ret_norm_gain"][0]), "w_out_a": f(inputs["w_out_a"][0]),
        "kv_norm_gain": f(inputs["kv_norm_gain"]), "w_kv_shared": f(inputs["w_kv_shared"]), "w_in_b": f(inputs["w_in_b"][0]),
        "w_out_b": f(inputs["w_out_b"][0]), "w_mem_kv": f(inputs["w_mem_kv"]), "norm_pre_mix": f(inputs["norm_pre_mix"]),
        "norm_post_mix": f(inputs["norm_post_mix"]), "norm_pre_mlp": f(inputs["norm_pre_mlp"]),
        "norm_post_mlp": f(inputs["norm_post_mlp"]), "w_up": f(inputs["w_up"]), "w_down": f(inputs["w_down"]),
        "c_ident": cs["ident"], "c_tri4": cs["tri4"], "c_retv": cs["retv"], "c_alibi": cs["alibi"],
    }
    x = f(inputs["x"])
    mem = f(inputs["mem"])
    in_maps = []
    for b in range(ncores):
        m = dict(shared)
        m["x"] = x[b]
        m["mem"] = mem[b]
        in_maps.append(m)
    res = run_bass_kernel_spmd(nc, in_maps, core_ids=list(range(ncores)))
    return np.stack([np.asarray(r["out"], dtype=np.float32) for r in res.results], axis=0)
```

```python
import math
from contextlib import ExitStack
import numpy as np
import concourse.bass as bass
import concourse.mybir as mybir
from concourse.alu_op_type import AluOpType as ALU
from concourse.bass_utils import run_bass_kernel_spmd

F32 = mybir.dt.float32
BF16 = mybir.dt.bfloat16
AF = mybir.ActivationFunctionType
AX = mybir.AxisListType

D = 1024
S = 2048
T = 1024
NT = 8
NH = 2
DFF = 4096
EPS = 1e-6
NRING = 3
ENGS = ("pe", "act", "dve", "pool", "sp")


def alibi_slopes(n):
    def pow2(m):
        return [2.0 ** (-8.0 * (i + 1) / m) for i in range(m)]
    p = 2 ** int(math.floor(math.log2(n)))
    s = pow2(p)
    if p < n:
        s = s + pow2(2 * p)[0::2][: n - p]
    return np.asarray(s, dtype=np.float64)


class Ins:
    __slots__ = ("eng", "fn", "waits", "stream", "sidx", "milestone", "count", "is_dma")


class Sched:
    def __init__(self):
        self.cells = {}
        self.eng_list = {e: [] for e in ENGS}
        self.streams = {}
        self.seen = {e: {} for e in ENGS}

    def add(self, eng, fn, reads=(), writes=(), dma=None):
        ins = Ins()
        ins.eng = eng
        ins.fn = fn
        ins.is_dma = dma is not None
        ins.stream = dma if dma else eng
        need = {}

        def dep(d, war):
            if d is None:
                return
            if (not ins.is_dma) and (not d.is_dma) and d.eng == eng:
                if eng == "pe" or war:
                    return
            if need.get(d.stream, -1) < d.sidx:
                need[d.stream] = d.sidx

        for c in reads:
            cell = self.cells.get(c)
            if cell:
                dep(cell[0], False)
        for c in writes:
            cell = self.cells.get(c)
            if cell:
                dep(cell[0], False)
                for r in cell[1].values():
                    dep(r, True)
        waits = []
        seen = self.seen[eng]
        for s, i in need.items():
            if seen.get(s, -1) >= i:
                continue
            seen[s] = i
            waits.append((s, i))
            self.streams[s][i].milestone = True
        ins.waits = waits
        lst = self.streams.setdefault(ins.stream, [])
        ins.sidx = len(lst)
        lst.append(ins)
        ins.milestone = ins.is_dma
        self.eng_list[eng].append(ins)
        for c in writes:
            self.cells[c] = [ins, {}]
        for c in reads:
            cell = self.cells.setdefault(c, [None, {}])
            cell[1][ins.stream] = ins
        return ins

    def emit(self, nc, es):
        sems = {}
        for s, lst in self.streams.items():
            sems[s] = es.enter_context(nc.semaphore("s_" + s))
            c = 0
            for ins in lst:
                if ins.is_dma:
                    c += 16
                elif ins.milestone:
                    c += 1
                ins.count = c
        streams = self.streams

        def run(eng_name, eng):
            for ins in self.eng_list[eng_name]:
                for (s, i) in ins.waits:
                    eng.wait_ge(sems[s], streams[s][i].count)
                if ins.fn is not None:
                    h = ins.fn(eng)
                    if ins.is_dma:
                        h.then_inc(sems[ins.stream], 16)
                    elif ins.milestone:
                        h.then_inc(sems[ins.stream], 1)

        with nc.Block() as block:
            @block.tensor
            def _(e):
                run("pe", e)

            @block.scalar
            def _(e):
                run("act", e)

            @block.vector
            def _(e):
                run("dve", e)

            @block.gpsimd
            def _(e):
                run("pool", e)

            @block.sync
            def _(e):
                run("sp", e)


def make_consts():
    c = {}
    c["ident"] = np.eye(128, dtype=np.float32)
    k = np.arange(128)[:, None]
    q = np.arange(128)[None, :]
    tri = (q >= k).astype(np.float32)
    c["tri4"] = np.tile(tri, (1, 4)).astype(np.float32)
    hh = np.arange(4, dtype=np.float64)
    log_g = np.log1p(-np.exp2(-5.0 - hh))
    p = np.arange(128, dtype=np.float64)
    xi = np.exp(log_g[None, :] * (p[:, None] + 1.0))
    rv = np.zeros((128, 8), np.float32)
    rv[:, 0:4] = (1.0 / xi) * (128.0 ** -0.5)
    rv[:, 4:8] = EPS / (xi * xi)
    c["retv"] = rv
    c["ret_decay"] = [float(np.exp(log_g[i] * 128.0)) for i in range(4)]
    sl = alibi_slopes(12)
    ab = np.zeros((128, 12, 16), np.float32)
    for h in range(12):
        for d in range(16):
            ab[:, h, d] = sl[h] * (p - 64.0 - 128.0 * d)
    ef = np.exp(sl[None, :] * (p[:, None] - 64.0)).astype(np.float32)
    c["alibi"] = np.concatenate([ab.reshape(128, 192), ef], axis=1).astype(np.float32)
    c["dbias"] = [[float(-128.0 * sl[h] * d) for d in range(16)] for h in range(12)]
    return c


class Builder:
    def __init__(self, stop=99):
        self.stop = stop
        self.nc = bass.Bass("TRN2", target_bir_lowering=False, dynamic_dma_scratch_size=8192)
        self.sc = Sched()
        self.es = ExitStack()
        self.ps_i = 0
        self.ring_i = 0
        self.cnt = {}
        self.consts = make_consts()

    def sb(self, name, shape, dt):
        return self.es.enter_context(self.nc.sbuf_tensor(name, shape, dt))

    def dram(self, name, shape, dt=F32, kind="ExternalInput"):
        return self.nc.dram_tensor(name, shape, dt, kind=kind).ap()

    def rot(self, name, n):
        i = self.cnt.get(name, 0)
        self.cnt[name] = i + 1
        return i % n

    def psum(self):
        i = self.ps_i % 8
        self.ps_i += 1
        return self.PS[i], ("ps", i)

    def add(self, *a, **k):
        return self.sc.add(*a, **k)

    def setup(self):
        nc = self.nc
        d = self.dram
        self.x = d("x", [S, D])
        self.mem = d("mem", [256, D])
        self.w_in_a = d("w_in_a", [D, 2816])
        self.ret_gain = d("ret_norm_gain", [768])
        self.w_out_a = d("w_out_a", [D, D])
        self.kv_gain = d("kv_norm_gain", [D])
        self.w_kv = d("w_kv_shared", [D, 1536])
        self.w_in_b = d("w_in_b", [D, D])
        self.w_out_b = d("w_out_b", [D, D])
        self.w_mkv = d("w_mem_kv", [2, D, 512])
        self.g_pre_mix = d("norm_pre_mix", [2, D])
        self.g_post_mix = d("norm_post_mix", [2, D])
        self.g_pre_mlp = d("norm_pre_mlp", [2, D])
        self.g_post_mlp = d("norm_post_mlp", [2, D])
        self.w_up = d("w_up", [2, D, DFF])
        self.w_down = d("w_down", [2, DFF, D])
        self.c_ident = d("c_ident", [128, 128])
        self.c_tri4 = d("c_tri4", [128, 512])
        self.c_retv = d("c_retv", [128, 8])
        self.c_alibi = d("c_alibi", [128, 204])
        self.out = d("out", [S, D], kind="ExternalOutput")

        sb = self.sb
        self.h = sb("h", [128, NT, D], F32)
        self.KT = sb("KT", [128, 6, S], BF16)
        self.V = sb("V", [128, 16, 12, 65], BF16)
        self.ring = sb("ring", [128, NRING, 8, 512], BF16)
        self.B1 = sb("B1", [128, 8, 1024], BF16)
        self.B2 = sb("B2", [128, 8, 1024], BF16)
        self.B3 = sb("B3", [128, 8 * 1024], F32)
        self.y = self.B3[:].rearrange("p (t n) -> p t n", n=1024)
        self.B3b = self.B3[:].bitcast(BF16).rearrange("p (t n) -> p t n", n=2048)
        self.qmT = sb("qmT", [128, 2, 1024], BF16)
        self.memT = sb("memT", [128, 8, 256], BF16)
        self.mkT = sb("mkT", [128, 2, 256], BF16)
        self.mv = sb("mv", [128, 2, 4, 65], BF16)
        self.gain = sb("gain", [128, 2, 1024], F32)
        self.state = sb("state", [128, 4, 192], F32)
        self.state_bf = sb("state_bf", [128, 2, 4, 192], BF16)
        self.tmpf = sb("tmpf", [128, 1, 512], F32)
        self.hnb = sb("hnb", [128, 2, 1024], BF16)
        self.junk = sb("junk", [128, 192], BF16)
        self.PT = sb("PT", [128, 8, 512], BF16)
        self.PTm = sb("PTm", [128, 4, 512], BF16)
        self.ident = sb("ident", [128, 128], BF16)
        self.tri4 = sb("tri4", [128, 512], BF16)
        self.retv = sb("retv", [128, 8], F32)
        self.alibi = sb("alibi", [128, 204], F32)
        self.ksum = sb("ksum", [128, 6, 8], F32)
        self.kmT = sb("kmT", [128, 6, 8], BF16)
        self.st = sb("st", [128, 64], F32)
        self.gsb = sb("gsb", [128, 12, 8], F32)
        self.top8 = sb("top8", [128, 12, 8], F32)
        self.sel = sb("sel", [128, 2, 12, 8], F32)
        self.acc = sb("acc", [128, 2, 65], F32)
        self.PS = [self.es.enter_context(nc.psum_tensor("ps%d" % i, [128, 512], F32)) for i in range(8)]

        add = self.add
        ycell = lambda t: [("B3", t, s_) for s_ in range(8)]
        add("sp", lambda e: e.dma_start(out=self.y[:, 2, 0:128], in_=self.c_ident), writes=ycell(2), dma="d_c0")
        add("sp", lambda e: e.dma_start(out=self.y[:, 3, 0:512], in_=self.c_tri4), writes=ycell(3), dma="d_c1")
        add("sp", lambda e: e.dma_start(out=self.retv[:], in_=self.c_retv), writes=[("c", 2)], dma="d_c2")
        add("sp", lambda e: e.dma_start(out=self.alibi[:], in_=self.c_alibi), writes=[("c", 3)], dma="d_c3")
        add("dve", lambda e: e.tensor_copy(out=self.ident[:], in_=self.y[:, 2, 0:128]), reads=ycell(2), writes=[("ident",)])
        add("dve", lambda e: e.tensor_copy(out=self.tri4[:], in_=self.y[:, 3, 0:512]), reads=ycell(3), writes=[("tri4",)])
        for gt_ in range(16):
            add("dve", lambda e, gt_=gt_: e.tensor_copy(out=self.V[:, gt_, :, 64], in_=self.alibi[:, 192:204]),
                reads=[("c", 3)], writes=[("Vones", gt_)])
        add("dve", lambda e: e.memset(self.mv[:, :, :, 64:65], 1.0), writes=[("mvones",)])
        add("dve", lambda e: e.memset(self.state[:], 0.0), writes=[("state", i_) for i_ in range(4)])
        add("dve", lambda e: e.memset(self.state_bf[:], 0.0), writes=[("state_bf", p_, i_) for p_ in range(2) for i_ in range(4)])
        for mt in range(2):
            add("sp", lambda e, mt=mt: e.dma_start(out=self.y[:, mt, :], in_=self.mem[mt * 128:(mt + 1) * 128, :]),
                writes=ycell(mt), dma="d_mem%d" % mt)
            add("dve", lambda e, mt=mt: e.tensor_copy(out=self.hnb[:, 0, :], in_=self.y[:, mt, :]),
                reads=ycell(mt), writes=[("hnb", 0)])
            self.transpose8(self.hnb[:, 0, :], [("hnb", 0)], self.memT[:, :, mt * 128:(mt + 1) * 128], [("memT", mt)])

    def transpose8(self, src, src_cells, dst, dst_cells, eng="act"):
        ps, pc = self.psum()
        psb = ps[:].bitcast(BF16)
        for kc in range(8):
            self.add("pe", lambda e, kc=kc: e.transpose(out=psb[:, kc * 128:(kc + 1) * 128],
                                                        in_=src[:, kc * 128:(kc + 1) * 128], identity=self.ident[:]),
                     reads=list(src_cells) + [("ident",)], writes=[pc])
        pin = psb[:, 0:1024].rearrange("p (k c) -> p k c", c=128)
        if eng == "act":
            self.add("act", lambda e: e.copy(out=dst, in_=pin), reads=[pc], writes=dst_cells)
        else:
            self.add("dve", lambda e: e.tensor_copy(out=dst, in_=pin), reads=[pc], writes=dst_cells)

    def load_slab(self, wap, ncols=512):
        r = self.ring_i % NRING
        self.ring_i += 1
        src = wap.rearrange("(kc p) n -> p kc n", p=128)
        self.add("pool", lambda e: e.dma_start(out=self.ring[:, r, :, 0:ncols], in_=src),
                 writes=[("ring", r)], dma="d_ring%d" % r)
        return r

    def load_gain(self, gap, n=1024):
        sl = self.rot("gain", 2)
        self.add("sp", lambda e: e.dma_start(out=self.gain[:, sl, 0:n], in_=gap.partition_broadcast(128)),
                 writes=[("gain", sl)], dma="d_gain%d" % sl)
        return sl

    def stat(self):
        i = self.rot("st", 16)
        return self.st[:, i * 4:(i + 1) * 4], ("st", i)

    def norm_A(self, src, src_cells, gsl):
        add = self.add
        st, stc = self.stat()
        sl = self.rot("hnb", 2)
        add("act", lambda e: e.activation(out=self.hnb[:, sl, :], in_=src, func=AF.Square, accum_out=st[:, 0:1]),
            reads=src_cells, writes=[stc, ("hnb", sl)])
        add("act", lambda e: e.activation(out=st[:, 1:2], in_=st[:, 0:1], func=AF.Sqrt, bias=self.epsc[:, 0:1], scale=1.0 / D),
            reads=[stc, ("epsc",)], writes=[stc])
        add("dve", lambda e: e.reciprocal(out=st[:, 2:3], in_=st[:, 1:2]), reads=[stc], writes=[stc])
        add("dve", lambda e: e.scalar_tensor_tensor(out=self.hnb[:, sl, :], in0=src, scalar=st[:, 2:3],
                                                    in1=self.gain[:, gsl, :], op0=ALU.mult, op1=ALU.mult),
            reads=list(src_cells) + [stc, ("gain", gsl)], writes=[("hnb", sl)])
        return sl

    def norm_phase(self, gsl):
        sl = self.norm_A(self.h[:, 0, :], [("h", 0)], gsl)
        for t in range(NT):
            nsl = self.norm_A(self.h[:, t + 1, :], [("h", t + 1)], gsl) if t + 1 < NT else None
            self.transpose8(self.hnb[:, sl, :], [("hnb", sl)], self.B1[:, :, t * 128:(t + 1) * 128],
                            [("B1", kc, t) for kc in range(8)], eng=("act" if t % 2 == 0 else "dve"))
            sl = nsl

    def post_res(self, t, gsl):
        add = self.add
        st, stc = self.stat()
        ycells = [("B3", t, s) for s in range(8)]
        jsl = self.rot("hnb", 2)
        add("act", lambda e: e.activation(out=self.hnb[:, jsl, :], in_=self.y[:, t, :], func=AF.Square, accum_out=st[:, 0:1]),
            reads=ycells, writes=[stc, ("hnb", jsl)])
        add("act", lambda e: e.activation(out=st[:, 1:2], in_=st[:, 0:1], func=AF.Sqrt, bias=self.epsc[:, 0:1], scale=1.0 / D),
            reads=[stc, ("epsc",)], writes=[stc])
        add("dve", lambda e: e.reciprocal(out=st[:, 2:3], in_=st[:, 1:2]), reads=[stc], writes=[stc])
        add("dve", lambda e: e.scalar_tensor_tensor(out=self.y[:, t, :], in0=self.y[:, t, :], scalar=st[:, 2:3],
                                                    in1=self.gain[:, gsl, :], op0=ALU.mult, op1=ALU.mult),
            reads=ycells + [stc, ("gain", gsl)], writes=ycells)
        add("dve", lambda e: e.tensor_tensor(out=self.h[:, t, :], in0=self.h[:, t, :], in1=self.y[:, t, :], op=ALU.add),
            reads=[("h", t)] + ycells, writes=[("h", t)])

    def mm_B(self, r, j, xT, xcells_fn, grp, ncol=512, nk=8):
        ps, pc = self.psum()
        for kc in range(nk):
            self.add("pe", lambda e, kc=kc: e.matmul(ps[:, 0:ncol], lhsT=self.ring[:, r, kc, j * 128:(j + 1) * 128],
                                                     rhs=xT[:, kc, grp * ncol:(grp + 1) * ncol],
                                                     start=(kc == 0), stop=(kc == nk - 1)),
                     reads=[("ring", r)] + xcells_fn(kc, grp), writes=[pc])
        return ps, pc

    def mm_A(self, r, c0, n, xT, xcells_fn, t, nk=8):
        ps, pc = self.psum()
        for kc in range(nk):
            self.add("pe", lambda e, kc=kc: e.matmul(ps[:, 0:n], lhsT=xT[:, kc, t * 128:(t + 1) * 128],
                                                     rhs=self.ring[:, r, kc, c0:c0 + n],
                                                     start=(kc == 0), stop=(kc == nk - 1)),
                     reads=[("ring", r)] + xcells_fn(kc, t), writes=[pc])
        return ps, pc

    @staticmethod
    def cellsB1_grp(kc, grp):
        return [("B1", kc, grp * 4 + i) for i in range(4)]

    @staticmethod
    def cellsB1_t(kc, t):
        return [("B1", kc, t)]

    @staticmethod
    def cellsB2_grp(kc, grp):
        return [("B2", kc, grp * 4 + i) for i in range(4)]

    @staticmethod
    def cellsB2_t(kc, t):
        return [("B2", kc, t)]

    def mem_kv(self, l):
        add = self.add
        r = self.load_slab(self.w_mkv[l])
        for j in range(2):
            ps, pc = self.psum()
            for kc in range(8):
                add("pe", lambda e, kc=kc, ps=ps, j=j: e.matmul(ps[:, 0:256], lhsT=self.ring[:, r, kc, j * 128:(j + 1) * 128],
                                                               rhs=self.memT[:, kc, :], start=(kc == 0), stop=(kc == 7)),
                    reads=[("ring", r), ("memT", 0), ("memT", 1)], writes=[pc])
            add("act", lambda e, ps=ps, j=j: e.copy(out=self.mkT[:, j, :], in_=ps[:, 0:256]), reads=[pc], writes=[("mkT", j)])
        for mt in range(2):
            ps, pc = self.psum()
            for kc in range(8):
                add("pe", lambda e, kc=kc, ps=ps, mt=mt: e.matmul(ps[:, 0:256], lhsT=self.memT[:, kc, mt * 128:(mt + 1) * 128],
                                                                 rhs=self.ring[:, r, kc, 256:512], start=(kc == 0), stop=(kc == 7)),
                    reads=[("ring", r), ("memT", mt)], writes=[pc])
            add("act", lambda e, ps=ps, mt=mt: e.copy(out=self.mv[:, mt, :, 0:64],
                                                     in_=ps[:, 0:256].rearrange("p (h c) -> p h c", c=64)),
                reads=[pc, ("mvones",)], writes=[("mv", mt)])

    def mem_front(self, t):
        add = self.add
        pts = []
        for hh in range(2):
            ps, pc = self.psum()
            po = hh * 64
            for half in range(2):
                for mt in range(2):
                    sl = half * 2 + mt
                    add("pe", lambda e, ps=ps, sl=sl, po=po, half=half, mt=mt: e.matmul(
                        ps[:, sl * 128:(sl + 1) * 128], lhsT=self.mkT[po:po + 64, half, mt * 128:(mt + 1) * 128],
                        rhs=self.qmT[po:po + 64, half, t * 128:(t + 1) * 128], start=True, stop=True),
                        reads=[("mkT", half), ("qmT", half, t)], writes=[pc])
            psl = (t % 2) * 2 + hh
            add("act", lambda e, ps=ps, psl=psl: e.activation(out=self.PTm[:, psl, :], in_=ps[:, 0:512], func=AF.Exp, scale=0.125),
                reads=[pc], writes=[("PTm", psl)])
            pts.append(psl)
        return pts

    def mem_back(self, t, pts, on_act=False):
        add = self.add
        pso, poc = self.psum()
        for hd in range(4):
            psl = pts[hd % 2]
            for mt in range(2):
                sl = (hd // 2) * 2 + mt
                add("pe", lambda e, hd=hd, psl=psl, sl=sl, mt=mt: e.matmul(
                    pso[:, hd * 65:(hd + 1) * 65], lhsT=self.PTm[:, psl, sl * 128:(sl + 1) * 128],
                    rhs=self.mv[:, mt, hd, :], start=(mt == 0), stop=(mt == 1)),
                    reads=[("PTm", psl), ("mv", mt), ("mvones",)], writes=[poc])
        st, stc = self.stat()
        pv = pso[:, 0:260].rearrange("p (h c) -> p h c", c=65)
        add("dve", lambda e: e.reciprocal(out=st[:, 0:4], in_=pv[:, :, 64]), reads=[poc], writes=[stc])
        for hd in range(4):
            if on_act:
                add("act", lambda e, hd=hd: e.activation(out=self.B1[:, t, 768 + hd * 64:768 + (hd + 1) * 64], in_=pv[:, hd, 0:64],
                                                         func=AF.Copy, scale=st[:, hd:hd + 1]),
                    reads=[poc, stc], writes=[("B1", t, 6 + hd // 2)])
            else:
                add("dve", lambda e, hd=hd: e.tensor_scalar(out=self.B1[:, t, 768 + hd * 64:768 + (hd + 1) * 64], in0=pv[:, hd, 0:64],
                                                            scalar1=st[:, hd:hd + 1], scalar2=None, op0=ALU.mult),
                    reads=[poc, stc], writes=[("B1", t, 6 + hd // 2)])

    def mem_attn(self, t):
        self.mem_back(t, self.mem_front(t))

    def cat_to_T(self):
        for t in range(NT):
            self.transpose8(self.B1[:, t, :], [("B1", t, j) for j in range(8)],
                            self.B2[:, :, t * 128:(t + 1) * 128], [("B2", kc, t) for kc in range(8)])

    def out_proj(self, wap, g_post):
        add = self.add
        gsl = self.load_gain(g_post)
        for s in range(2):
            r = self.load_slab(wap[:, s * 512:(s + 1) * 512])
            for t in range(NT):
                ps, pc = self.mm_A(r, 0, 512, self.B2, self.cellsB2_t, t)
                add("act", lambda e, ps=ps, t=t, s=s: e.copy(out=self.y[:, t, s * 512:(s + 1) * 512], in_=ps[:, 0:512]),
                    reads=[pc], writes=[("B3", t, s * 4 + i) for i in range(4)])
                if s == 1:
                    if t >= 2:
                        self.post_res(t - 2, gsl)
            if s == 1:
                self.post_res(NT - 2, gsl)
                self.post_res(NT - 1, gsl)

    def mlp(self, l):
        add = self.add
        gsl = self.load_gain(self.g_pre_mlp[l])
        self.norm_phase(gsl)
        gpost = self.load_gain(self.g_post_mlp[l])
        for b in range(4):
            for s in range(2):
                r = self.load_slab(self.w_up[l][:, b * 1024 + s * 512: b * 1024 + (s + 1) * 512])
                for j in range(4):
                    for grp in range(2):
                        ps, pc = self.mm_B(r, j, self.B1, self.cellsB1_grp, grp)
                        dst = self.B2[:, s * 4 + j, grp * 512:(grp + 1) * 512]
                        dcells = [("B2", s * 4 + j, grp * 4 + i) for i in range(4)]
                        add("act", lambda e, ps=ps: e.activation(out=self.tmpf[:, 0, 0:512], in_=ps[:, 0:512], func=AF.Relu),
                            reads=[pc], writes=[("tmpf", 0)])
                        add("dve", lambda e, ps=ps, dst=dst: e.tensor_tensor(out=dst, in0=self.tmpf[:, 0, 0:512], in1=ps[:, 0:512], op=ALU.mult),
                            reads=[pc, ("tmpf", 0)], writes=dcells)
            for s in range(2):
                r = self.load_slab(self.w_down[l][b * 1024:(b + 1) * 1024, s * 512:(s + 1) * 512])
                for t in range(NT):
                    ps, pc = self.mm_A(r, 0, 512, self.B2, self.cellsB2_t, t)
                    yc = [("B3", t, s * 4 + i) for i in range(4)]
                    if b == 0:
                        add("act", lambda e, ps=ps, t=t, s=s: e.copy(out=self.y[:, t, s * 512:(s + 1) * 512], in_=ps[:, 0:512]),
                            reads=[pc], writes=yc)
                    else:
                        add("dve", lambda e, ps=ps, t=t, s=s: e.tensor_tensor(out=self.y[:, t, s * 512:(s + 1) * 512],
                                                                             in0=self.y[:, t, s * 512:(s + 1) * 512], in1=ps[:, 0:512], op=ALU.add),
                            reads=[pc] + yc, writes=yc)
                    if b == 3 and s == 1 and t >= 2:
                        self.post_res(t - 2, gpost)
                if b == 3 and s == 1:
                    self.post_res(NT - 2, gpost)
                    self.post_res(NT - 1, gpost)

    def layer0_mix(self, hf):
        add = self.add
        gsl = self.load_gain(self.g_pre_mix[0])
        self.norm_phase(gsl)
        if self.stop < 0.15: return
        self.mem_kv(0)
        if self.stop < 0.25: return
        W = self.w_in_a
        r = self.load_slab(W[:, 0:512])
        for j in range(4):
            for grp in range(2):
                ps, pc = self.mm_B(r, j, self.B1, self.cellsB1_grp, grp)
                add("act", lambda e, ps=ps, j=j, grp=grp: e.copy(out=self.B2[:, j, grp * 512:(grp + 1) * 512], in_=ps[:, 0:512]),
                    reads=[pc], writes=[("B2", j, grp * 4 + i) for i in range(4)])
        if self.stop < 0.35: return
        r = self.load_slab(W[:, 512:1024])
        for j in range(4):
            for grp in range(2):
                ps, pc = self.mm_B(r, j, self.B1, self.cellsB1_grp, grp)
                add("act", lambda e, ps=ps, j=j, grp=grp: e.copy(out=self.B2[:, 4 + j, grp * 512:(grp + 1) * 512], in_=ps[:, 0:512]),
                    reads=[pc], writes=[("B2", 4 + j, grp * 4 + i) for i in range(4)])
        for t in range(NT):
            ps, pc = self.mm_A(r, 0, 512, self.B1, self.cellsB1_t, t)
            add("dve", lambda e, ps=ps, t=t: e.tensor_copy(out=self.B3b[:, t, 0:512], in_=ps[:, 0:512]),
                reads=[pc], writes=[("B3", t, 0), ("B3", t, 1)])
        if self.stop < 0.45: return
        rgs = self.load_gain(self.ret_gain, 768)
        for si in range(2, 5):
            r = self.load_slab(W[:, si * 512:(si + 1) * 512])
            for t in range(NT):
                ps, pc = self.mm_A(r, 0, 512, self.B1, self.cellsB1_t, t)
                c0 = si * 512 - 1024
                bounds = sorted(set([c0, c0 + 512] + [b for b in range(0, 1537, 192) if c0 < b < c0 + 512]))
                for a, bnd in zip(bounds[:-1], bounds[1:]):
                    lo, hi = a - c0, bnd - c0
                    if a < 768:
                        hd = a // 192
                        add("act", lambda e, ps=ps, t=t, lo=lo, hi=hi, a=a, bnd=bnd, hd=hd: e.activation(
                            out=self.B3b[:, t, 512 + a:512 + bnd], in_=ps[:, lo:hi], func=AF.Copy, scale=self.retv[:, hd:hd + 1]),
                            reads=[pc, ("c", 2)], writes=[("B3", t, s) for s in range((512 + a) // 256, (512 + bnd - 1) // 256 + 1)])
                    else:
                        ga, gb = a - 768, bnd - 768
                        sl = 0
                        add("act", lambda e, ps=ps, lo=lo, hi=hi, sl=sl: e.activation(out=self.tmpf[:, sl, 0:hi - lo], in_=ps[:, lo:hi], func=AF.Silu),
                            reads=[pc], writes=[("tmpf", sl)])
                        add("dve", lambda e, t=t, ga=ga, gb=gb, sl=sl: e.tensor_tensor(
                            out=self.B3b[:, t, 1280 + ga:1280 + gb], in0=self.tmpf[:, sl, 0:gb - ga], in1=self.gain[:, rgs, ga:gb], op=ALU.mult),
                            reads=[("tmpf", sl), ("gain", rgs)], writes=[("B3", t, s) for s in range((1280 + ga) // 256, (1280 + gb - 1) // 256 + 1)])
        if self.stop < 0.55: return
        r = self.load_slab(W[:, 2560:2816], ncols=256)
        for j in range(2):
            for grp in range(2):
                ps, pc = self.mm_B(r, j, self.B1, self.cellsB1_grp, grp)
                add("act", lambda e, ps=ps, j=j, grp=grp: e.copy(out=self.qmT[:, j, grp * 512:(grp + 1) * 512], in_=ps[:, 0:512]),
                    reads=[pc], writes=[("qmT", j, grp * 4 + i) for i in range(4)])
        if self.stop < 0.65: return
        dec = self.consts["ret_decay"]
        def kvupd(t):
            par = (hf * NT + t + 1) % 2
            vcells = [("B3", t, 2), ("B3", t, 3), ("B3", t, 4)]
            for pair in range(2):
                pk, pkc = self.psum()
                for hh in range(2):
                    hd = pair * 2 + hh
                    add("pe", lambda e, pk=pk, hd=hd, hh=hh, t=t: e.matmul(
                        pk[:, hh * 192:(hh + 1) * 192], lhsT=self.B3b[:, t, hd * 128:(hd + 1) * 128],
                        rhs=self.B3b[:, t, 512 + hd * 192:512 + (hd + 1) * 192], start=True, stop=True),
                        reads=[("B3", t, 0), ("B3", t, 1)] + vcells, writes=[pkc])
                for hh in range(2):
                    hd = pair * 2 + hh
                    add("dve", lambda e, pk=pk, hd=hd, hh=hh: e.tensor_tensor(
                        out=self.state[:, hd, :], in0=pk[:, hh * 192:(hh + 1) * 192], in1=self.state[:, hd, :], op=ALU.add),
                        reads=[pkc, ("state", hd)], writes=[("state", hd)])
                    add("dve", lambda e, hd=hd, par=par: e.tensor_scalar(out=self.state_bf[:, par, hd, :], in0=self.state[:, hd, :], scalar1=dec[hd],
                                                                        scalar2=None, op0=ALU.mult), reads=[("state", hd)], writes=[("state_bf", par, hd)])
                    add("dve", lambda e, hd=hd: e.tensor_scalar(out=self.state[:, hd, :], in0=self.state[:, hd, :], scalar1=dec[hd],
                                                               scalar2=None, op0=ALU.mult), reads=[("state", hd)], writes=[("state", hd)])

        def front(t):
            if t > 0:
                kvupd(t - 1)
            tok = slice(t * 128, (t + 1) * 128)
            ps, pc = self.psum()
            for hd in range(4):
                add("pe", lambda e, ps=ps, hd=hd, tok=tok: e.matmul(ps[:, hd * 128:(hd + 1) * 128], lhsT=self.B2[:, 4 + hd, tok],
                                                                   rhs=self.B2[:, hd, tok], start=True, stop=True),
                    reads=[("B2", 4 + hd, t), ("B2", hd, t)], writes=[pc])
            psl = self.rot("PT", 8)
            add("dve", lambda e, ps=ps, psl=psl: e.tensor_tensor(out=self.PT[:, psl, :], in0=ps[:, 0:512], in1=self.tri4[:], op=ALU.mult),
                reads=[pc, ("tri4",)], writes=[("PT", psl)])
            return psl, self.mem_front(t)

        def back(t, psl, mf):
            tok = slice(t * 128, (t + 1) * 128)
            vcells = [("B3", t, 2), ("B3", t, 3), ("B3", t, 4)]
            pos = []
            for pair in range(2):
                po_, poc = self.psum()
                pos.append((po_, poc))
                for hh in range(2):
                    hd = pair * 2 + hh
                    add("pe", lambda e, po_=po_, hd=hd, hh=hh, psl=psl, t=t: e.matmul(
                        po_[:, hh * 192:(hh + 1) * 192], lhsT=self.PT[:, psl, hd * 128:(hd + 1) * 128],
                        rhs=self.B3b[:, t, 512 + hd * 192:512 + (hd + 1) * 192], start=True, stop=False),
                        reads=[("PT", psl)] + vcells, writes=[poc])
                    add("pe", lambda e, po_=po_, hd=hd, hh=hh, tok=tok: e.matmul(
                        po_[:, hh * 192:(hh + 1) * 192], lhsT=self.B2[:, hd, tok], rhs=self.state_bf[:, (hf * NT + t) % 2, hd, :], start=False, stop=True),
                        reads=[("B2", hd, t), ("state_bf", (hf * NT + t) % 2, hd)], writes=[poc])
            st, stc = self.stat()
            for hd in range(4):
                po_, poc = pos[hd // 2]
                hh = hd % 2
                add("act", lambda e, po_=po_, hh=hh, hd=hd: e.activation(out=self.junk[:, 0:192], in_=po_[:, hh * 192:(hh + 1) * 192],
                                                                       func=AF.Square, accum_out=st[:, hd:hd + 1]),
                    reads=[poc], writes=[stc])
            st2, stc2 = self.stat()
            add("dve", lambda e: e.scalar_tensor_tensor(out=st2[:, 0:4], in0=st[:, 0:4], scalar=1.0 / 192.0, in1=self.retv[:, 4:8],
                                                        op0=ALU.mult, op1=ALU.add), reads=[stc, ("c", 2)], writes=[stc2])
            st3, stc3 = self.stat()
            add("act", lambda e: e.activation(out=st3[:, 0:4], in_=st2[:, 0:4], func=AF.Ln), reads=[stc2], writes=[stc3])
            add("act", lambda e: e.activation(out=st3[:, 0:4], in_=st3[:, 0:4], func=AF.Exp, scale=-0.5), reads=[stc3], writes=[stc3])
            for hd in range(4):
                po_, poc = pos[hd // 2]
                hh = hd % 2
                c0, c1 = hd * 192, (hd + 1) * 192
                add("dve", lambda e, po_=po_, hh=hh, hd=hd, c0=c0, c1=c1, t=t: e.scalar_tensor_tensor(
                    out=self.B1[:, t, c0:c1], in0=po_[:, hh * 192:(hh + 1) * 192], scalar=st3[:, hd:hd + 1],
                    in1=self.B3b[:, t, 1280 + c0:1280 + c1], op0=ALU.mult, op1=ALU.mult),
                    reads=[poc, stc3, ("B3", t, 5), ("B3", t, 6), ("B3", t, 7)],
                    writes=[("B1", t, j) for j in range(c0 // 128, (c1 - 1) // 128 + 1)])
            self.mem_back(t, mf, on_act=True)

        f = front(0)
        for t in range(NT):
            fn = front(t + 1) if t + 1 < NT else None
            back(t, *f)
            f = fn
        kvupd(NT - 1)
        if self.stop < 0.75: return
        self.cat_to_T()
        if self.stop < 0.85: return
        self.out_proj(self.w_out_a, self.g_post_mix[0])

    def layer1_mix(self, hf):
        add = self.add
        gsl = self.load_gain(self.kv_gain)
        self.norm_phase(gsl)
        W = self.w_kv
        for si in range(3):
            r = self.load_slab(W[:, si * 512:(si + 1) * 512])
            for j in range(4):
                col = si * 512 + j * 128
                if col >= 768:
                    continue
                c = col // 128
                for grp in range(2):
                    ps, pc = self.mm_B(r, j, self.B1, self.cellsB1_grp, grp)
                    for bb in range(2):
                        blk = hf * 4 + grp * 2 + bb
                        g0 = hf * T + grp * 512 + bb * 256
                        add("act", lambda e, ps=ps, c=c, g0=g0, bb=bb, blk=blk: e.activation(
                            out=self.KT[:, c, g0:g0 + 256], in_=ps[:, bb * 256:(bb + 1) * 256], func=AF.Copy,
                            accum_out=self.ksum[:, c, blk:blk + 1]),
                            reads=[pc], writes=[("KT", c, blk), ("ksum", c, blk)])
            v0 = max(si * 512, 768)
            v1 = (si + 1) * 512
            if v1 > v0:
                n = v1 - v0
                h0 = (v0 - 768) // 64
                nh = n // 64
                for t in range(NT):
                    gt = hf * NT + t
                    ps, pc = self.mm_A(r, v0 - si * 512, n, self.B1, self.cellsB1_t, t)
                    for j_ in range(nh):
                        hd_ = h0 + j_
                        add("dve", lambda e, ps=ps, gt=gt, hd_=hd_, j_=j_: e.tensor_scalar(
                            out=self.V[:, gt, hd_, 0:64], in0=ps[:, j_ * 64:(j_ + 1) * 64], scalar1=self.alibi[:, 192 + hd_:193 + hd_],
                            scalar2=None, op0=ALU.mult),
                            reads=[pc, ("c", 3)], writes=[("V", gt, hd_ // 4)])
        gsl = self.load_gain(self.g_pre_mix[1])
        self.norm_phase(gsl)
        self.mem_kv(1)
        W = self.w_in_b
        for si in range(2):
            r = self.load_slab(W[:, si * 512:(si + 1) * 512])
            for j in range(4):
                col = si * 512 + j * 128
                for grp in range(2):
                    ps, pc = self.mm_B(r, j, self.B1, self.cellsB1_grp, grp)
                    if col < 768:
                        c = col // 128
                        add("act", lambda e, ps=ps, c=c, grp=grp: e.copy(out=self.B2[:, c, grp * 512:(grp + 1) * 512], in_=ps[:, 0:512]),
                            reads=[pc], writes=[("B2", c, grp * 4 + i) for i in range(4)])
                    else:
                        c = (col - 768) // 128
                        add("act", lambda e, ps=ps, c=c, grp=grp: e.copy(out=self.qmT[:, c, grp * 512:(grp + 1) * 512], in_=ps[:, 0:512]),
                            reads=[pc], writes=[("qmT", c, grp * 4 + i) for i in range(4)])
        if hf == 1:
            kc_all = [("ksum", c, b) for c in range(6) for b in range(8)]
            add("act", lambda e: e.activation(out=self.kmT[:], in_=self.ksum[:], func=AF.Copy, scale=1.0 / 256.0),
                reads=kc_all, writes=[("kmT",)])
        prev = None
        for t in range(NT):
            gt = hf * NT + t
            qb = gt // 2
            if hf == 1:
                self.moba_gate(t, qb)
            mf = self.mem_front(t)
            for hd in range(12):
                cur = self.moba_front(hf, t, hd)
                if prev is not None:
                    self.moba_back(*prev)
                prev = (hf, t, hd, cur)
            self.mem_back(t, mf)
        self.moba_back(*prev)
        self.cat_to_T()
        self.out_proj(self.w_out_b, self.g_post_mix[1])

    def moba_gate(self, t, qb):
        add = self.add
        gv = self.gsb[:].rearrange("p (c two) b -> p c two b", two=2)
        for hh in range(2):
            ps, pc = self.psum()
            po = hh * 64
            for c in range(6):
                add("pe", lambda e, ps=ps, c=c, po=po: e.matmul(ps[:, c * 8:(c + 1) * 8], lhsT=self.B2[po:po + 64, c, t * 128:(t + 1) * 128],
                                                               rhs=self.kmT[po:po + 64, c, :], start=True, stop=True),
                    reads=[("B2", c, t), ("kmT",)], writes=[pc])
            add("act", lambda e, ps=ps, hh=hh: e.copy(out=gv[:, :, hh, :], in_=ps[:, 0:48].rearrange("p (c b) -> p c b", b=8)),
                reads=[pc], writes=[("gsb",)])
        if qb < 8:
            add("dve", lambda e: e.memset(self.gsb[:, :, qb:8], -1e30), reads=[("gsb",)], writes=[("gsb",)])
        for hd in range(12):
            add("dve", lambda e, hd=hd: e.max(out=self.top8[:, hd, :], in_=self.gsb[:, hd, :]), reads=[("gsb",)], writes=[("top8", hd)])
            add("dve", lambda e, hd=hd: e.tensor_scalar(out=self.sel[:, t % 2, hd, :], in0=self.gsb[:, hd, :], scalar1=self.top8[:, hd, 2:3],
                                                        scalar2=None, op0=ALU.is_ge), reads=[("gsb",), ("top8", hd)], writes=[("sel", t % 2, hd)])

    def moba_front(self, hf, t, hd):
        add = self.add
        gt = hf * NT + t
        qb = gt // 2
        c, po = hd // 2, (hd % 2) * 64
        qap = self.B2[po:po + 64, c, t * 128:(t + 1) * 128]
        kts = list(range(gt + 1))
        pt_of = {}
        for i0 in range(0, len(kts), 4):
            grp = kts[i0:i0 + 4]
            ps, pc = self.psum()
            for i, kt in enumerate(grp):
                add("pe", lambda e, ps=ps, i=i, kt=kt: e.matmul(ps[:, i * 128:(i + 1) * 128], lhsT=self.KT[po:po + 64, c, kt * 128:(kt + 1) * 128],
                                                               rhs=qap, start=True, stop=True),
                    reads=[("KT", c, kt // 2), ("B2", c, t)], writes=[pc])
            psl = self.rot("PT", 8)
            for i, kt in enumerate(grp):
                dd = gt - kt
                add("act", lambda e, ps=ps, i=i, psl=psl, dd=dd: e.activation(
                    out=self.PT[:, psl, i * 128:(i + 1) * 128], in_=ps[:, i * 128:(i + 1) * 128], func=AF.Exp,
                    bias=self.consts["dbias"][hd][dd], scale=0.125),
                    reads=[pc], writes=[("PT", psl)])
                if kt == gt:
                    add("dve", lambda e, psl=psl, i=i: e.tensor_tensor(out=self.PT[:, psl, i * 128:(i + 1) * 128],
                                                                       in0=self.PT[:, psl, i * 128:(i + 1) * 128], in1=self.tri4[:, 0:128], op=ALU.mult),
                        reads=[("PT", psl), ("tri4",)], writes=[("PT", psl)])
                pt_of[kt] = (psl, i)
        return pt_of

    def moba_back(self, hf, t, hd, pt_of):
        add = self.add
        gt = hf * NT + t
        qb = gt // 2
        kts = list(range(gt + 1))
        pso, poc = self.psum()
        asl = self.rot("acc", 2)
        if hf == 0:
            for n_, kt in enumerate(kts):
                psl, i = pt_of[kt]
                add("pe", lambda e, psl=psl, i=i, kt=kt, n_=n_: e.matmul(pso[:, 0:65], lhsT=self.PT[:, psl, i * 128:(i + 1) * 128],
                                                                        rhs=self.V[:, kt, hd, :], start=(n_ == 0), stop=(n_ == len(kts) - 1)),
                    reads=[("PT", psl), ("V", kt, hd // 4), ("Vones", kt)], writes=[poc])
            src = pso
            srcc = poc
            res = pso[:, 0:65]
        else:
            psB, pbc = self.psum()
            own = [kt for kt in kts if kt // 2 == qb]
            for n_, kt in enumerate(own):
                psl, i = pt_of[kt]
                add("pe", lambda e, psl=psl, i=i, kt=kt, n_=n_: e.matmul(psB[:, 0:65], lhsT=self.PT[:, psl, i * 128:(i + 1) * 128],
                                                                        rhs=self.V[:, kt, hd, :], start=(n_ == 0), stop=(n_ == len(own) - 1)),
                    reads=[("PT", psl), ("V", kt, hd // 4), ("Vones", kt)], writes=[pbc])
            for b in range(qb):
                for n_, kt in enumerate((2 * b, 2 * b + 1)):
                    psl, i = pt_of[kt]
                    add("pe", lambda e, psl=psl, i=i, kt=kt, n_=n_, b=b: e.matmul(pso[:, b * 65:(b + 1) * 65], lhsT=self.PT[:, psl, i * 128:(i + 1) * 128],
                                                                                 rhs=self.V[:, kt, hd, :], start=(n_ == 0), stop=(n_ == 1)),
                        reads=[("PT", psl), ("V", kt, hd // 4), ("Vones", kt)], writes=[poc])
            accap = self.acc[:, asl, :]
            add("dve", lambda e: e.tensor_copy(out=accap, in_=psB[:, 0:65]), reads=[pbc], writes=[("acc", asl)])
            for b in range(qb):
                add("dve", lambda e, b=b: e.scalar_tensor_tensor(out=accap, in0=pso[:, b * 65:(b + 1) * 65], scalar=self.sel[:, t % 2, hd, b:b + 1],
                                                                 in1=accap, op0=ALU.mult, op1=ALU.add),
                    reads=[poc, ("sel", t % 2, hd), ("acc", asl)], writes=[("acc", asl)])
            res = accap
            srcc = ("acc", asl)
        st, stc = self.stat()
        add("dve", lambda e: e.reciprocal(out=st[:, 0:1], in_=res[:, 64:65]), reads=[srcc], writes=[stc])
        add("dve", lambda e: e.tensor_scalar(out=self.B1[:, t, hd * 64:(hd + 1) * 64], in0=res[:, 0:64], scalar1=st[:, 0:1],
                                             scalar2=None, op0=ALU.mult), reads=[srcc, stc], writes=[("B1", t, hd // 2)])

    def build(self):
        add = self.add
        self.setup()
        self.epsc = self.sb("epsc", [128, 1], F32)
        add("dve", lambda e: e.memset(self.epsc[:], EPS), writes=[("epsc",)])
        for hf in range(NH):
            for t in range(NT):
                gt = hf * NT + t
                add("sp", lambda e, t=t, gt=gt: e.dma_start(out=self.h[:, t, :], in_=self.x[gt * 128:(gt + 1) * 128, :]),
                    writes=[("h", t)], dma="d_h%d" % t)
            if self.stop > 0:
                self.layer0_mix(hf)
            if self.stop >= 2:
                self.mlp(0)
            if self.stop >= 3:
                self.layer1_mix(hf)
            if self.stop >= 4:
                self.mlp(1)
            for t in range(NT):
                gt = hf * NT + t
                add("sp", lambda e, t=t, gt=gt: e.dma_start(out=self.out[gt * 128:(gt + 1) * 128, :], in_=self.h[:, t, :]),
                    reads=[("h", t)], dma="d_o%d" % t)
        add("sp", None, writes=[("h", t) for t in range(NT)])
        self.sc.emit(self.nc, self.es)
        self.es.close()
        return self.nc


_CACHE = {}


def get_nc(stop=99):
    if stop not in _CACHE:
        _CACHE[stop] = Builder(stop).build()
    return _CACHE[stop]


def kernel(stop=99, ncores=8, **inputs):
    stop = float(stop)
    nc = get_nc(stop)
    cs = make_consts()
    f = lambda a: np.ascontiguousarray(np.asarray(a, dtype=np.float32))
    shared = {
        "w_in_a": f(inputs["w_in_a"][0]), "ret_norm_gain": f(inputs["ret_norm_gain"][0]), "w_out_a": f(inputs["w_out_a"][0]),
        "kv_norm_gain": f(inputs["kv_norm_gain"]), "w_kv_shared": f(inputs["w_kv_shared"]), "w_in_b": f(inputs["w_in_b"][0]),
        "w_out_b": f(inputs["w_out_b"][0]), "w_mem_kv": f(inputs["w_mem_kv"]), "norm_pre_mix": f(inputs["norm_pre_mix"]),
        "norm_post_mix": f(inputs["norm_post_mix"]), "norm_pre_mlp": f(inputs["norm_pre_mlp"]),
        "norm_post_mlp": f(inputs["norm_post_mlp"]), "w_up": f(inputs["w_up"]), "w_down": f(inputs["w_down"]),
        "c_ident": cs["ident"], "c_tri4": cs["tri4"], "c_retv": cs["retv"], "c_alibi": cs["alibi"],
    }
    x = f(inputs["x"])
    mem = f(inputs["mem"])
    in_maps = []
    for b in range(ncores):
        m = dict(shared)
        m["x"] = x[b]
        m["mem"] = mem[b]
        in_maps.append(m)
    res = run_bass_kernel_spmd(nc, in_maps, core_ids=list(range(ncores)))
    return np.stack([np.asarray(r["out"], dtype=np.float32) for r in res.results], axis=0)
```

```python
import math
from contextlib import ExitStack
import numpy as np
import concourse.bass as bass
import concourse.mybir as mybir
from concourse.alu_op_type import AluOpType as ALU
from concourse.bass_utils import run_bass_kernel_spmd

F32 = mybir.dt.float32
BF16 = mybir.dt.bfloat16
AF = mybir.ActivationFunctionType
AX = mybir.AxisListType

D = 1024
S = 2048
T = 1024
NT = 8
NH = 2
DFF = 4096
EPS = 1e-6
NRING = 3
ENGS = ("pe", "act", "dve", "pool", "sp")


def alibi_slopes(n):
    def pow2(m):
        return [2.0 ** (-8.0 * (i + 1) / m) for i in range(m)]
    p = 2 ** int(math.floor(math.log2(n)))
    s = pow2(p)
    if p < n:
        s = s + pow2(2 * p)[0::2][: n - p]
    return np.asarray(s, dtype=np.float64)


class Ins:
    __slots__ = ("eng", "fn", "waits", "stream", "sidx", "milestone", "count", "is_dma")


class Sched:
    def __init__(self):
        self.cells = {}
        self.eng_list = {e: [] for e in ENGS}
        self.streams = {}
        self.seen = {e: {} for e in ENGS}

    def add(self, eng, fn, reads=(), writes=(), dma=None):
        ins = Ins()
        ins.eng = eng
        ins.fn = fn
        ins.is_dma = dma is not None
        ins.stream = dma if dma else eng
        need = {}

        def dep(d, war):
            if d is None:
                return
            if (not ins.is_dma) and (not d.is_dma) and d.eng == eng:
                if eng == "pe" or war:
                    return
            if need.get(d.stream, -1) < d.sidx:
                need[d.stream] = d.sidx

        for c in reads:
            cell = self.cells.get(c)
            if cell:
                dep(cell[0], False)
        for c in writes:
            cell = self.cells.get(c)
            if cell:
                dep(cell[0], False)
                for r in cell[1].values():
                    dep(r, True)
        waits = []
        seen = self.seen[eng]
        for s, i in need.items():
            if seen.get(s, -1) >= i:
                continue
            seen[s] = i
            waits.append((s, i))
            self.streams[s][i].milestone = True
        ins.waits = waits
        lst = self.streams.setdefault(ins.stream, [])
        ins.sidx = len(lst)
        lst.append(ins)
        ins.milestone = ins.is_dma
        self.eng_list[eng].append(ins)
        for c in writes:
            self.cells[c] = [ins, {}]
        for c in reads:
            cell = self.cells.setdefault(c, [None, {}])
            cell[1][ins.stream] = ins
        return ins

    def emit(self, nc, es):
        sems = {}
        for s, lst in self.streams.items():
            sems[s] = es.enter_context(nc.semaphore("s_" + s))
            c = 0
            for ins in lst:
                if ins.is_dma:
                    c += 16
                elif ins.milestone:
                    c += 1
                ins.count = c
        streams = self.streams

        def run(eng_name, eng):
            for ins in self.eng_list[eng_name]:
                for (s, i) in ins.waits:
                    eng.wait_ge(sems[s], streams[s][i].count)
                if ins.fn is not None:
                    h = ins.fn(eng)
                    if ins.is_dma:
                        h.then_inc(sems[ins.stream], 16)
                    elif ins.milestone:
                        h.then_inc(sems[ins.stream], 1)

        with nc.Block() as block:
            @block.tensor
            def _(e):
                run("pe", e)

            @block.scalar
            def _(e):
                run("act", e)

            @block.vector
            def _(e):
                run("dve", e)

            @block.gpsimd
            def _(e):
                run("pool", e)

            @block.sync
            def _(e):
                run("sp", e)


def make_consts():
    c = {}
    c["ident"] = np.eye(128, dtype=np.float32)
    k = np.arange(128)[:, None]
    q = np.arange(128)[None, :]
    tri = (q >= k).astype(np.float32)
    c["tri4"] = np.tile(tri, (1, 4)).astype(np.float32)
    hh = np.arange(4, dtype=np.float64)
    log_g = np.log1p(-np.exp2(-5.0 - hh))
    p = np.arange(128, dtype=np.float64)
    xi = np.exp(log_g[None, :] * (p[:, None] + 1.0))
    rv = np.zeros((128, 8), np.float32)
    rv[:, 0:4] = (1.0 / xi) * (128.0 ** -0.5)
    rv[:, 4:8] = EPS / (xi * xi)
    c["retv"] = rv
    c["ret_decay"] = [float(np.exp(log_g[i] * 128.0)) for i in range(4)]
    sl = alibi_slopes(12)
    ab = np.zeros((128, 12, 16), np.float32)
    for h in range(12):
        for d in range(16):
            ab[:, h, d] = sl[h] * (p - 64.0 - 128.0 * d)
    ef = np.exp(sl[None, :] * (p[:, None] - 64.0)).astype(np.float32)
    c["alibi"] = np.concatenate([ab.reshape(128, 192), ef], axis=1).astype(np.float32)
    c["dbias"] = [[float(-128.0 * sl[h] * d) for d in range(16)] for h in range(12)]
    return c


class Builder:
    def __init__(self, stop=99):
        self.stop = stop
        self.nc = bass.Bass("TRN2", target_bir_lowering=False, dynamic_dma_scratch_size=8192)
        self.sc = Sched()
        self.es = ExitStack()
        self.ps_i = 0
        self.ring_i = 0
        self.cnt = {}
        self.consts = make_consts()

    def sb(self, name, shape, dt):
        return self.es.enter_context(self.nc.sbuf_tensor(name, shape, dt))

    def dram(self, name, shape, dt=F32, kind="ExternalInput"):
        return self.nc.dram_tensor(name, shape, dt, kind=kind).ap()

    def rot(self, name, n):
        i = self.cnt.get(name, 0)
        self.cnt[name] = i + 1
        return i % n

    def psum(self):
        i = self.ps_i % 8
        self.ps_i += 1
        return self.PS[i], ("ps", i)

    def add(self, *a, **k):
        return self.sc.add(*a, **k)

    def setup(self):
        nc = self.nc
        d = self.dram
        self.x = d("x", [S, D])
        self.mem = d("mem", [256, D])
        self.w_in_a = d("w_in_a", [D, 2816])
        self.ret_gain = d("ret_norm_gain", [768])
        self.w_out_a = d("w_out_a", [D, D])
        self.kv_gain = d("kv_norm_gain", [D])
        self.w_kv = d("w_kv_shared", [D, 1536])
        self.w_in_b = d("w_in_b", [D, D])
        self.w_out_b = d("w_out_b", [D, D])
        self.w_mkv = d("w_mem_kv", [2, D, 512])
        self.g_pre_mix = d("norm_pre_mix", [2, D])
        self.g_post_mix = d("norm_post_mix", [2, D])
        self.g_pre_mlp = d("norm_pre_mlp", [2, D])
        self.g_post_mlp = d("norm_post_mlp", [2, D])
        self.w_up = d("w_up", [2, D, DFF])
        self.w_down = d("w_down", [2, DFF, D])
        self.c_ident = d("c_ident", [128, 128])
        self.c_tri4 = d("c_tri4", [128, 512])
        self.c_retv = d("c_retv", [128, 8])
        self.c_alibi = d("c_alibi", [128, 204])
        self.out = d("out", [S, D], kind="ExternalOutput")

        sb = self.sb
        self.h = sb("h", [128, NT, D], F32)
        self.KT = sb("KT", [128, 6, S], BF16)
        self.V = sb("V", [128, 16, 12, 65], BF16)
        self.ring = sb("ring", [128, NRING, 8, 512], BF16)
        self.B1 = sb("B1", [128, 8, 1024], BF16)
        self.B2 = sb("B2", [128, 8, 1024], BF16)
        self.B3 = sb("B3", [128, 8 * 1024], F32)
        self.y = self.B3[:].rearrange("p (t n) -> p t n", n=1024)
        self.B3b = self.B3[:].bitcast(BF16).rearrange("p (t n) -> p t n", n=2048)
        self.qmT = sb("qmT", [128, 2, 1024], BF16)
        self.memT = sb("memT", [128, 8, 256], BF16)
        self.mkT = sb("mkT", [128, 2, 256], BF16)
        self.mv = sb("mv", [128, 2, 4, 65], BF16)
        self.gain = sb("gain", [128, 2, 1024], F32)
        self.state = sb("state", [128, 4, 192], F32)
        self.state_bf = sb("state_bf", [128, 2, 4, 192], BF16)
        self.tmpf = sb("tmpf", [128, 1, 512], F32)
        self.hnb = sb("hnb", [128, 2, 1024], BF16)
        self.junk = sb("junk", [128, 192], BF16)
        self.PT = sb("PT", [128, 8, 512], BF16)
        self.PTm = sb("PTm", [128, 4, 512], BF16)
        self.ident = sb("ident", [128, 128], BF16)
        self.tri4 = sb("tri4", [128, 512], BF16)
        self.retv = sb("retv", [128, 8], F32)
        self.alibi = sb("alibi", [128, 204], F32)
        self.ksum = sb("ksum", [128, 6, 8], F32)
        self.kmT = sb("kmT", [128, 6, 8], BF16)
        self.st = sb("st", [128, 64], F32)
        self.gsb = sb("gsb", [128, 12, 8], F32)
        self.top8 = sb("top8", [128, 12, 8], F32)
        self.sel = sb("sel", [128, 2, 12, 8], F32)
        self.acc = sb("acc", [128, 2, 65], F32)
        self.PS = [self.es.enter_context(nc.psum_tensor("ps%d" % i, [128, 512], F32)) for i in range(8)]

        add = self.add
        ycell = lambda t: [("B3", t, s_) for s_ in range(8)]
        add("sp", lambda e: e.dma_start(out=self.y[:, 2, 0:128], in_=self.c_ident), writes=ycell(2), dma="d_c0")
        add("sp", lambda e: e.dma_start(out=self.y[:, 3, 0:512], in_=self.c_tri4), writes=ycell(3), dma="d_c1")
        add("sp", lambda e: e.dma_start(out=self.retv[:], in_=self.c_retv), writes=[("c", 2)], dma="d_c2")
        add("sp", lambda e: e.dma_start(out=self.alibi[:], in_=self.c_alibi), writes=[("c", 3)], dma="d_c3")
        add("dve", lambda e: e.tensor_copy(out=self.ident[:], in_=self.y[:, 2, 0:128]), reads=ycell(2), writes=[("ident",)])
        add("dve", lambda e: e.tensor_copy(out=self.tri4[:], in_=self.y[:, 3, 0:512]), reads=ycell(3), writes=[("tri4",)])
        for gt_ in range(16):
            add("dve", lambda e, gt_=gt_: e.tensor_copy(out=self.V[:, gt_, :, 64], in_=self.alibi[:, 192:204]),
                reads=[("c", 3)], writes=[("Vones", gt_)])
        add("dve", lambda e: e.memset(self.mv[:, :, :, 64:65], 1.0), writes=[("mvones",)])
        add("dve", lambda e: e.memset(self.state[:], 0.0), writes=[("state", i_) for i_ in range(4)])
        add("dve", lambda e: e.memset(self.state_bf[:], 0.0), writes=[("state_bf", p_, i_) for p_ in range(2) for i_ in range(4)])
        for mt in range(2):
            add("sp", lambda e, mt=mt: e.dma_start(out=self.y[:, mt, :], in_=self.mem[mt * 128:(mt + 1) * 128, :]),
                writes=ycell(mt), dma="d_mem%d" % mt)
            add("dve", lambda e, mt=mt: e.tensor_copy(out=self.hnb[:, 0, :], in_=self.y[:, mt, :]),
                reads=ycell(mt), writes=[("hnb", 0)])
            self.transpose8(self.hnb[:, 0, :], [("hnb", 0)], self.memT[:, :, mt * 128:(mt + 1) * 128], [("memT", mt)])

    def transpose8(self, src, src_cells, dst, dst_cells, eng="act"):
        ps, pc = self.psum()
        psb = ps[:].bitcast(BF16)
        for kc in range(8):
            self.add("pe", lambda e, kc=kc: e.transpose(out=psb[:, kc * 128:(kc + 1) * 128],
                                                        in_=src[:, kc * 128:(kc + 1) * 128], identity=self.ident[:]),
                     reads=list(src_cells) + [("ident",)], writes=[pc])
        pin = psb[:, 0:1024].rearrange("p (k c) -> p k c", c=128)
        if eng == "act":
            self.add("act", lambda e: e.copy(out=dst, in_=pin), reads=[pc], writes=dst_cells)
        else:
            self.add("dve", lambda e: e.tensor_copy(out=dst, in_=pin), reads=[pc], writes=dst_cells)

    def load_slab(self, wap, ncols=512):
        r = self.ring_i % NRING
        self.ring_i += 1
        src = wap.rearrange("(kc p) n -> p kc n", p=128)
        self.add("pool", lambda e: e.dma_start(out=self.ring[:, r, :, 0:ncols], in_=src),
                 writes=[("ring", r)], dma="d_ring%d" % r)
        return r

    def load_gain(self, gap, n=1024):
        sl = self.rot("gain", 2)
        self.add("sp", lambda e: e.dma_start(out=self.gain[:, sl, 0:n], in_=gap.partition_broadcast(128)),
                 writes=[("gain", sl)], dma="d_gain%d" % sl)
        return sl

    def stat(self):
        i = self.rot("st", 16)
        return self.st[:, i * 4:(i + 1) * 4], ("st", i)

    def norm_A(self, src, src_cells, gsl):
        add = self.add
        st, stc = self.stat()
        sl = self.rot("hnb", 2)
        add("act", lambda e: e.activation(out=self.hnb[:, sl, :], in_=src, func=AF.Square, accum_out=st[:, 0:1]),
            reads=src_cells, writes=[stc, ("hnb", sl)])
        add("act", lambda e: e.activation(out=st[:, 1:2], in_=st[:, 0:1], func=AF.Sqrt, bias=self.epsc[:, 0:1], scale=1.0 / D),
            reads=[stc, ("epsc",)], writes=[stc])
        add("dve", lambda e: e.reciprocal(out=st[:, 2:3], in_=st[:, 1:2]), reads=[stc], writes=[stc])
        add("dve", lambda e: e.scalar_tensor_tensor(out=self.hnb[:, sl, :], in0=src, scalar=st[:, 2:3],
                                                    in1=self.gain[:, gsl, :], op0=ALU.mult, op1=ALU.mult),
            reads=list(src_cells) + [stc, ("gain", gsl)], writes=[("hnb", sl)])
        return sl

    def norm_phase(self, gsl):
        sl = self.norm_A(self.h[:, 0, :], [("h", 0)], gsl)
        for t in range(NT):
            nsl = self.norm_A(self.h[:, t + 1, :], [("h", t + 1)], gsl) if t + 1 < NT else None
            self.transpose8(self.hnb[:, sl, :], [("hnb", sl)], self.B1[:, :, t * 128:(t + 1) * 128],
                            [("B1", kc, t) for kc in range(8)], eng=("act" if t % 2 == 0 else "dve"))
            sl = nsl

    def post_res(self, t, gsl):
        add = self.add
        st, stc = self.stat()
        ycells = [("B3", t, s) for s in range(8)]
        jsl = self.rot("hnb", 2)
        add("act", lambda e: e.activation(out=self.hnb[:, jsl, :], in_=self.y[:, t, :], func=AF.Square, accum_out=st[:, 0:1]),
            reads=ycells, writes=[stc, ("hnb", jsl)])
        add("act", lambda e: e.activation(out=st[:, 1:2], in_=st[:, 0:1], func=AF.Sqrt, bias=self.epsc[:, 0:1], scale=1.0 / D),
            reads=[stc, ("epsc",)], writes=[stc])
        add("dve", lambda e: e.reciprocal(out=st[:, 2:3], in_=st[:, 1:2]), reads=[stc], writes=[stc])
        add("dve", lambda e: e.scalar_tensor_tensor(out=self.y[:, t, :], in0=self.y[:, t, :], scalar=st[:, 2:3],
                                                    in1=self.gain[:, gsl, :], op0=ALU.mult, op1=ALU.mult),
            reads=ycells + [stc, ("gain", gsl)], writes=ycells)
        add("dve", lambda e: e.tensor_tensor(out=self.h[:, t, :], in0=self.h[:, t, :], in1=self.y[:, t, :], op=ALU.add),
            reads=[("h", t)] + ycells, writes=[("h", t)])

    def mm_B(self, r, j, xT, xcells_fn, grp, ncol=512, nk=8):
        ps, pc = self.psum()
        for kc in range(nk):
            self.add("pe", lambda e, kc=kc: e.matmul(ps[:, 0:ncol], lhsT=self.ring[:, r, kc, j * 128:(j + 1) * 128],
                                                     rhs=xT[:, kc, grp * ncol:(grp + 1) * ncol],
                                                     start=(kc == 0), stop=(kc == nk - 1)),
                     reads=[("ring", r)] + xcells_fn(kc, grp), writes=[pc])
        return ps, pc

    def mm_A(self, r, c0, n, xT, xcells_fn, t, nk=8):
        ps, pc = self.psum()
        for kc in range(nk):
            self.add("pe", lambda e, kc=kc: e.matmul(ps[:, 0:n], lhsT=xT[:, kc, t * 128:(t + 1) * 128],
                                                     rhs=self.ring[:, r, kc, c0:c0 + n],
                                                     start=(kc == 0), stop=(kc == nk - 1)),
                     reads=[("ring", r)] + xcells_fn(kc, t), writes=[pc])
        return ps, pc

    @staticmethod
    def cellsB1_grp(kc, grp):
        return [("B1", kc, grp * 4 + i) for i in range(4)]

    @staticmethod
    def cellsB1_t(kc, t):
        return [("B1", kc, t)]

    @staticmethod
    def cellsB2_grp(kc, grp):
        return [("B2", kc, grp * 4 + i) for i in range(4)]

    @staticmethod
    def cellsB2_t(kc, t):
        return [("B2", kc, t)]

    def mem_kv(self, l):
        add = self.add
        r = self.load_slab(self.w_mkv[l])
        for j in range(2):
            ps, pc = self.psum()
            for kc in range(8):
                add("pe", lambda e, kc=kc, ps=ps, j=j: e.matmul(ps[:, 0:256], lhsT=self.ring[:, r, kc, j * 128:(j + 1) * 128],
                                                               rhs=self.memT[:, kc, :], start=(kc == 0), stop=(kc == 7)),
                    reads=[("ring", r), ("memT", 0), ("memT", 1)], writes=[pc])
            add("act", lambda e, ps=ps, j=j: e.copy(out=self.mkT[:, j, :], in_=ps[:, 0:256]), reads=[pc], writes=[("mkT", j)])
        for mt in range(2):
            ps, pc = self.psum()
            for kc in range(8):
                add("pe", lambda e, kc=kc, ps=ps, mt=mt: e.matmul(ps[:, 0:256], lhsT=self.memT[:, kc, mt * 128:(mt + 1) * 128],
                                                                 rhs=self.ring[:, r, kc, 256:512], start=(kc == 0), stop=(kc == 7)),
                    reads=[("ring", r), ("memT", mt)], writes=[pc])
            add("act", lambda e, ps=ps, mt=mt: e.copy(out=self.mv[:, mt, :, 0:64],
                                                     in_=ps[:, 0:256].rearrange("p (h c) -> p h c", c=64)),
                reads=[pc, ("mvones",)], writes=[("mv", mt)])

    def mem_front(self, t):
        add = self.add
        pts = []
        for hh in range(2):
            ps, pc = self.psum()
            po = hh * 64
            for half in range(2):
                for mt in range(2):
                    sl = half * 2 + mt
                    add("pe", lambda e, ps=ps, sl=sl, po=po, half=half, mt=mt: e.matmul(
                        ps[:, sl * 128:(sl + 1) * 128], lhsT=self.mkT[po:po + 64, half, mt * 128:(mt + 1) * 128],
                        rhs=self.qmT[po:po + 64, half, t * 128:(t + 1) * 128], start=True, stop=True),
                        reads=[("mkT", half), ("qmT", half, t)], writes=[pc])
            psl = (t % 2) * 2 + hh
            add("act", lambda e, ps=ps, psl=psl: e.activation(out=self.PTm[:, psl, :], in_=ps[:, 0:512], func=AF.Exp, scale=0.125),
                reads=[pc], writes=[("PTm", psl)])
            pts.append(psl)
        return pts

    def mem_back(self, t, pts, on_act=False):
        add = self.add
        pso, poc = self.psum()
        for hd in range(4):
            psl = pts[hd % 2]
            for mt in range(2):
                sl = (hd // 2) * 2 + mt
                add("pe", lambda e, hd=hd, psl=psl, sl=sl, mt=mt: e.matmul(
                    pso[:, hd * 65:(hd + 1) * 65], lhsT=self.PTm[:, psl, sl * 128:(sl + 1) * 128],
                    rhs=self.mv[:, mt, hd, :], start=(mt == 0), stop=(mt == 1)),
                    reads=[("PTm", psl), ("mv", mt), ("mvones",)], writes=[poc])
        st, stc = self.stat()
        pv = pso[:, 0:260].rearrange("p (h c) -> p h c", c=65)
        add("dve", lambda e: e.reciprocal(out=st[:, 0:4], in_=pv[:, :, 64]), reads=[poc], writes=[stc])
        for hd in range(4):
            if on_act:
                add("act", lambda e, hd=hd: e.activation(out=self.B1[:, t, 768 + hd * 64:768 + (hd + 1) * 64], in_=pv[:, hd, 0:64],
                                                         func=AF.Copy, scale=st[:, hd:hd + 1]),
                    reads=[poc, stc], writes=[("B1", t, 6 + hd // 2)])
            else:
                add("dve", lambda e, hd=hd: e.tensor_scalar(out=self.B1[:, t, 768 + hd * 64:768 + (hd + 1) * 64], in0=pv[:, hd, 0:64],
                                                            scalar1=st[:, hd:hd + 1], scalar2=None, op0=ALU.mult),
                    reads=[poc, stc], writes=[("B1", t, 6 + hd // 2)])

    def mem_attn(self, t):
        self.mem_back(t, self.mem_front(t))

    def cat_to_T(self):
        for t in range(NT):
            self.transpose8(self.B1[:, t, :], [("B1", t, j) for j in range(8)],
                            self.B2[:, :, t * 128:(t + 1) * 128], [("B2", kc, t) for kc in range(8)])

    def out_proj(self, wap, g_post):
        add = self.add
        gsl = self.load_gain(g_post)
        for s in range(2):
            r = self.load_slab(wap[:, s * 512:(s + 1) * 512])
            for t in range(NT):
                ps, pc = self.mm_A(r, 0, 512, self.B2, self.cellsB2_t, t)
                add("act", lambda e, ps=ps, t=t, s=s: e.copy(out=self.y[:, t, s * 512:(s + 1) * 512], in_=ps[:, 0:512]),
                    reads=[pc], writes=[("B3", t, s * 4 + i) for i in range(4)])
                if s == 1:
                    if t >= 2:
                        self.post_res(t - 2, gsl)
            if s == 1:
                self.post_res(NT - 2, gsl)
                self.post_res(NT - 1, gsl)

    def mlp(self, l):
        add = self.add
        gsl = self.load_gain(self.g_pre_mlp[l])
        self.norm_phase(gsl)
        gpost = self.load_gain(self.g_post_mlp[l])
        for b in range(4):
            for s in range(2):
                r = self.load_slab(self.w_up[l][:, b * 1024 + s * 512: b * 1024 + (s + 1) * 512])
                for j in range(4):
                    for grp in range(2):
                        ps, pc = self.mm_B(r, j, self.B1, self.cellsB1_grp, grp)
                        dst = self.B2[:, s * 4 + j, grp * 512:(grp + 1) * 512]
                        dcells = [("B2", s * 4 + j, grp * 4 + i) for i in range(4)]
                        add("act", lambda e, ps=ps: e.activation(out=self.tmpf[:, 0, 0:512], in_=ps[:, 0:512], func=AF.Relu),
                            reads=[pc], writes=[("tmpf", 0)])
                        add("dve", lambda e, ps=ps, dst=dst: e.tensor_tensor(out=dst, in0=self.tmpf[:, 0, 0:512], in1=ps[:, 0:512], op=ALU.mult),
                            reads=[pc, ("tmpf", 0)], writes=dcells)
            for s in range(2):
                r = self.load_slab(self.w_down[l][b * 1024:(b + 1) * 1024, s * 512:(s + 1) * 512])
                for t in range(NT):
                    ps, pc = self.mm_A(r, 0, 512, self.B2, self.cellsB2_t, t)
                    yc = [("B3", t, s * 4 + i) for i in range(4)]
                    if b == 0:
                        add("act", lambda e, ps=ps, t=t, s=s: e.copy(out=self.y[:, t, s * 512:(s + 1) * 512], in_=ps[:, 0:512]),
                            reads=[pc], writes=yc)
                    else:
                        add("dve", lambda e, ps=ps, t=t, s=s: e.tensor_tensor(out=self.y[:, t, s * 512:(s + 1) * 512],
                                                                             in0=self.y[:, t, s * 512:(s + 1) * 512], in1=ps[:, 0:512], op=ALU.add),
                            reads=[pc] + yc, writes=yc)
                    if b == 3 and s == 1 and t >= 2:
                        self.post_res(t - 2, gpost)
                if b == 3 and s == 1:
                    self.post_res(NT - 2, gpost)
                    self.post_res(NT - 1, gpost)

    def layer0_mix(self, hf):
        add = self.add
        gsl = self.load_gain(self.g_pre_mix[0])
        self.norm_phase(gsl)
        if self.stop < 0.15: return
        self.mem_kv(0)
        if self.stop < 0.25: return
        W = self.w_in_a
        r = self.load_slab(W[:, 0:512])
        for j in range(4):
            for grp in range(2):
                ps, pc = self.mm_B(r, j, self.B1, self.cellsB1_grp, grp)
                add("act", lambda e, ps=ps, j=j, grp=grp: e.copy(out=self.B2[:, j, grp * 512:(grp + 1) * 512], in_=ps[:, 0:512]),
                    reads=[pc], writes=[("B2", j, grp * 4 + i) for i in range(4)])
        if self.stop < 0.35: return
        r = self.load_slab(W[:, 512:1024])
        for j in range(4):
            for grp in range(2):
                ps, pc = self.mm_B(r, j, self.B1, self.cellsB1_grp, grp)
                add("act", lambda e, ps=ps, j=j, grp=grp: e.copy(out=self.B2[:, 4 + j, grp * 512:(grp + 1) * 512], in_=ps[:, 0:512]),
                    reads=[pc], writes=[("B2", 4 + j, grp * 4 + i) for i in range(4)])
        for t in range(NT):
            ps, pc = self.mm_A(r, 0, 512, self.B1, self.cellsB1_t, t)
            add("dve", lambda e, ps=ps, t=t: e.tensor_copy(out=self.B3b[:, t, 0:512], in_=ps[:, 0:512]),
                reads=[pc], writes=[("B3", t, 0), ("B3", t, 1)])
        if self.stop < 0.45: return
        rgs = self.load_gain(self.ret_gain, 768)
        for si in range(2, 5):
            r = self.load_slab(W[:, si * 512:(si + 1) * 512])
            for t in range(NT):
                ps, pc = self.mm_A(r, 0, 512, self.B1, self.cellsB1_t, t)
                c0 = si * 512 - 1024
                bounds = sorted(set([c0, c0 + 512] + [b for b in range(0, 1537, 192) if c0 < b < c0 + 512]))
                for a, bnd in zip(bounds[:-1], bounds[1:]):
                    lo, hi = a - c0, bnd - c0
                    if a < 768:
                        hd = a // 192
                        add("act", lambda e, ps=ps, t=t, lo=lo, hi=hi, a=a, bnd=bnd, hd=hd: e.activation(
                            out=self.B3b[:, t, 512 + a:512 + bnd], in_=ps[:, lo:hi], func=AF.Copy, scale=self.retv[:, hd:hd + 1]),
                            reads=[pc, ("c", 2)], writes=[("B3", t, s) for s in range((512 + a) // 256, (512 + bnd - 1) // 256 + 1)])
                    else:
                        ga, gb = a - 768, bnd - 768
                        sl = 0
                        add("act", lambda e, ps=ps, lo=lo, hi=hi, sl=sl: e.activation(out=self.tmpf[:, sl, 0:hi - lo], in_=ps[:, lo:hi], func=AF.Silu),
                            reads=[pc], writes=[("tmpf", sl)])
                        add("dve", lambda e, t=t, ga=ga, gb=gb, sl=sl: e.tensor_tensor(
                            out=self.B3b[:, t, 1280 + ga:1280 + gb], in0=self.tmpf[:, sl, 0:gb - ga], in1=self.gain[:, rgs, ga:gb], op=ALU.mult),
                            reads=[("tmpf", sl), ("gain", rgs)], writes=[("B3", t, s) for s in range((1280 + ga) // 256, (1280 + gb - 1) // 256 + 1)])
        if self.stop < 0.55: return
        r = self.load_slab(W[:, 2560:2816], ncols=256)
        for j in range(2):
            for grp in range(2):
                ps, pc = self.mm_B(r, j, self.B1, self.cellsB1_grp, grp)
                add("act", lambda e, ps=ps, j=j, grp=grp: e.copy(out=self.qmT[:, j, grp * 512:(grp + 1) * 512], in_=ps[:, 0:512]),
                    reads=[pc], writes=[("qmT", j, grp * 4 + i) for i in range(4)])
        if self.stop < 0.65: return
        dec = self.consts["ret_decay"]
        def kvupd(t):
            par = (hf * NT + t + 1) % 2
            vcells = [("B3", t, 2), ("B3", t, 3), ("B3", t, 4)]
            for pair in range(2):
                pk, pkc = self.psum()
                for hh in range(2):
                    hd = pair * 2 + hh
                    add("pe", lambda e, pk=pk, hd=hd, hh=hh, t=t: e.matmul(
                        pk[:, hh * 192:(hh + 1) * 192], lhsT=self.B3b[:, t, hd * 128:(hd + 1) * 128],
                        rhs=self.B3b[:, t, 512 + hd * 192:512 + (hd + 1) * 192], start=True, stop=True),
                        reads=[("B3", t, 0), ("B3", t, 1)] + vcells, writes=[pkc])
                for hh in range(2):
                    hd = pair * 2 + hh
                    add("dve", lambda e, pk=pk, hd=hd, hh=hh: e.tensor_tensor(
                        out=self.state[:, hd, :], in0=pk[:, hh * 192:(hh + 1) * 192], in1=self.state[:, hd, :], op=ALU.add),
                        reads=[pkc, ("state", hd)], writes=[("state", hd)])
                    add("dve", lambda e, hd=hd, par=par: e.tensor_scalar(out=self.state_bf[:, par, hd, :], in0=self.state[:, hd, :], scalar1=dec[hd],
                                                                        scalar2=None, op0=ALU.mult), reads=[("state", hd)], writes=[("state_bf", par, hd)])
                    add("dve", lambda e, hd=hd: e.tensor_scalar(out=self.state[:, hd, :], in0=self.state[:, hd, :], scalar1=dec[hd],
                                                               scalar2=None, op0=ALU.mult), reads=[("state", hd)], writes=[("state", hd)])

        def front(t):
            if t > 0:
                kvupd(t - 1)
            tok = slice(t * 128, (t + 1) * 128)
            ps, pc = self.psum()
            for hd in range(4):
                add("pe", lambda e, ps=ps, hd=hd, tok=tok: e.matmul(ps[:, hd * 128:(hd + 1) * 128], lhsT=self.B2[:, 4 + hd, tok],
                                                                   rhs=self.B2[:, hd, tok], start=True, stop=True),
                    reads=[("B2", 4 + hd, t), ("B2", hd, t)], writes=[pc])
            psl = self.rot("PT", 8)
            add("dve", lambda e, ps=ps, psl=psl: e.tensor_tensor(out=self.PT[:, psl, :], in0=ps[:, 0:512], in1=self.tri4[:], op=ALU.mult),
                reads=[pc, ("tri4",)], writes=[("PT", psl)])
            return psl, self.mem_front(t)

        def back(t, psl, mf):
            tok = slice(t * 128, (t + 1) * 128)
            vcells = [("B3", t, 2), ("B3", t, 3), ("B3", t, 4)]
            pos = []
            for pair in range(2):
                po_, poc = self.psum()
                pos.append((po_, poc))
                for hh in range(2):
                    hd = pair * 2 + hh
                    add("pe", lambda e, po_=po_, hd=hd, hh=hh, psl=psl, t=t: e.matmul(
                        po_[:, hh * 192:(hh + 1) * 192], lhsT=self.PT[:, psl, hd * 128:(hd + 1) * 128],
                        rhs=self.B3b[:, t, 512 + hd * 192:512 + (hd + 1) * 192], start=True, stop=False),
                        reads=[("PT", psl)] + vcells, writes=[poc])
                    add("pe", lambda e, po_=po_, hd=hd, hh=hh, tok=tok: e.matmul(
                        po_[:, hh * 192:(hh + 1) * 192], lhsT=self.B2[:, hd, tok], rhs=self.state_bf[:, (hf * NT + t) % 2, hd, :], start=False, stop=True),
                        reads=[("B2", hd, t), ("state_bf", (hf * NT + t) % 2, hd)], writes=[poc])
            st, stc = self.stat()
            for hd in range(4):
                po_, poc = pos[hd // 2]
                hh = hd % 2
                add("act", lambda e, po_=po_, hh=hh, hd=hd: e.activation(out=self.junk[:, 0:192], in_=po_[:, hh * 192:(hh + 1) * 192],
                                                                       func=AF.Square, accum_out=st[:, hd:hd + 1]),
                    reads=[poc], writes=[stc])
            st2, stc2 = self.stat()
            add("dve", lambda e: e.scalar_tensor_tensor(out=st2[:, 0:4], in0=st[:, 0:4], scalar=1.0 / 192.0, in1=self.retv[:, 4:8],
                                                        op0=ALU.mult, op1=ALU.add), reads=[stc, ("c", 2)], writes=[stc2])
            st3, stc3 = self.stat()
            add("act", lambda e: e.activation(out=st3[:, 0:4], in_=st2[:, 0:4], func=AF.Ln), reads=[stc2], writes=[stc3])
            add("act", lambda e: e.activation(out=st3[:, 0:4], in_=st3[:, 0:4], func=AF.Exp, scale=-0.5), reads=[stc3], writes=[stc3])
            for hd in range(4):
                po_, poc = pos[hd // 2]
                hh = hd % 2
                c0, c1 = hd * 192, (hd + 1) * 192
                add("dve", lambda e, po_=po_, hh=hh, hd=hd, c0=c0, c1=c1, t=t: e.scalar_tensor_tensor(
                    out=self.B1[:, t, c0:c1], in0=po_[:, hh * 192:(hh + 1) * 192], scalar=st3[:, hd:hd + 1],
                    in1=self.B3b[:, t, 1280 + c0:1280 + c1], op0=ALU.mult, op1=ALU.mult),
                    reads=[poc, stc3, ("B3", t, 5), ("B3", t, 6), ("B3", t, 7)],
                    writes=[("B1", t, j) for j in range(c0 // 128, (c1 - 1) // 128 + 1)])
            self.mem_back(t, mf, on_act=True)

        f = front(0)
        for t in range(NT):
            fn = front(t + 1) if t + 1 < NT else None
            back(t, *f)
            f = fn
        kvupd(NT - 1)
        if self.stop < 0.75: return
        self.cat_to_T()
        if self.stop < 0.85: return
        self.out_proj(self.w_out_a, self.g_post_mix[0])

    def layer1_mix(self, hf):
        add = self.add
        gsl = self.load_gain(self.kv_gain)
        self.norm_phase(gsl)
        W = self.w_kv
        for si in range(3):
            r = self.load_slab(W[:, si * 512:(si + 1) * 512])
            for j in range(4):
                col = si * 512 + j * 128
                if col >= 768:
                    continue
                c = col // 128
                for grp in range(2):
                    ps, pc = self.mm_B(r, j, self.B1, self.cellsB1_grp, grp)
                    for bb in range(2):
                        blk = hf * 4 + grp * 2 + bb
                        g0 = hf * T + grp * 512 + bb * 256
                        add("act", lambda e, ps=ps, c=c, g0=g0, bb=bb, blk=blk: e.activation(
                            out=self.KT[:, c, g0:g0 + 256], in_=ps[:, bb * 256:(bb + 1) * 256], func=AF.Copy,
                            accum_out=self.ksum[:, c, blk:blk + 1]),
                            reads=[pc], writes=[("KT", c, blk), ("ksum", c, blk)])
            v0 = max(si * 512, 768)
            v1 = (si + 1) * 512
            if v1 > v0:
                n = v1 - v0
                h0 = (v0 - 768) // 64
                nh = n // 64
                for t in range(NT):
                    gt = hf * NT + t
                    ps, pc = self.mm_A(r, v0 - si * 512, n, self.B1, self.cellsB1_t, t)
                    for j_ in range(nh):
                        hd_ = h0 + j_
                        add("dve", lambda e, ps=ps, gt=gt, hd_=hd_, j_=j_: e.tensor_scalar(
                            out=self.V[:, gt, hd_, 0:64], in0=ps[:, j_ * 64:(j_ + 1) * 64], scalar1=self.alibi[:, 192 + hd_:193 + hd_],
                            scalar2=None, op0=ALU.mult),
                            reads=[pc, ("c", 3)], writes=[("V", gt, hd_ // 4)])
        gsl = self.load_gain(self.g_pre_mix[1])
        self.norm_phase(gsl)
        self.mem_kv(1)
        W = self.w_in_b
        for si in range(2):
            r = self.load_slab(W[:, si * 512:(si + 1) * 512])
            for j in range(4):
                col = si * 512 + j * 128
                for grp in range(2):
                    ps, pc = self.mm_B(r, j, self.B1, self.cellsB1_grp, grp)
                    if col < 768:
                        c = col // 128
                        add("act", lambda e, ps=ps, c=c, grp=grp: e.copy(out=self.B2[:, c, grp * 512:(grp + 1) * 512], in_=ps[:, 0:512]),
                            reads=[pc], writes=[("B2", c, grp * 4 + i) for i in range(4)])
                    else:
                        c = (col - 768) // 128
                        add("act", lambda e, ps=ps, c=c, grp=grp: e.copy(out=self.qmT[:, c, grp * 512:(grp + 1) * 512], in_=ps[:, 0:512]),
                            reads=[pc], writes=[("qmT", c, grp * 4 + i) for i in range(4)])
        if hf == 1:
            kc_all = [("ksum", c, b) for c in range(6) for b in range(8)]
            add("act", lambda e: e.activation(out=self.kmT[:], in_=self.ksum[:], func=AF.Copy, scale=1.0 / 256.0),
                reads=kc_all, writes=[("kmT",)])
        prev = None
        for t in range(NT):
            gt = hf * NT + t
            qb = gt // 2
            if hf == 1:
                self.moba_gate(t, qb)
            mf = self.mem_front(t)
            for hd in range(12):
                cur = self.moba_front(hf, t, hd)
                if prev is not None:
                    self.moba_back(*prev)
                prev = (hf, t, hd, cur)
            self.mem_back(t, mf)
        self.moba_back(*prev)
        self.cat_to_T()
        self.out_proj(self.w_out_b, self.g_post_mix[1])

    def moba_gate(self, t, qb):
        add = self.add
        gv = self.gsb[:].rearrange("p (c two) b -> p c two b", two=2)
        for hh in range(2):
            ps, pc = self.psum()
            po = hh * 64
            for c in range(6):
                add("pe", lambda e, ps=ps, c=c, po=po: e.matmul(ps[:, c * 8:(c + 1) * 8], lhsT=self.B2[po:po + 64, c, t * 128:(t + 1) * 128],
                                                               rhs=self.kmT[po:po + 64, c, :], start=True, stop=True),
                    reads=[("B2", c, t), ("kmT",)], writes=[pc])
            add("act", lambda e, ps=ps, hh=hh: e.copy(out=gv[:, :, hh, :], in_=ps[:, 0:48].rearrange("p (c b) -> p c b", b=8)),
                reads=[pc], writes=[("gsb",)])
        if qb < 8:
            add("dve", lambda e: e.memset(self.gsb[:, :, qb:8], -1e30), reads=[("gsb",)], writes=[("gsb",)])
        for hd in range(12):
            add("dve", lambda e, hd=hd: e.max(out=self.top8[:, hd, :], in_=self.gsb[:, hd, :]), reads=[("gsb",)], writes=[("top8", hd)])
            add("dve", lambda e, hd=hd: e.tensor_scalar(out=self.sel[:, t % 2, hd, :], in0=self.gsb[:, hd, :], scalar1=self.top8[:, hd, 2:3],
                                                        scalar2=None, op0=ALU.is_ge), reads=[("gsb",), ("top8", hd)], writes=[("sel", t % 2, hd)])

    def moba_front(self, hf, t, hd):
        add = self.add
        gt = hf * NT + t
        qb = gt // 2
        c, po = hd // 2, (hd % 2) * 64
        qap = self.B2[po:po + 64, c, t * 128:(t + 1) * 128]
        kts = list(range(gt + 1))
        pt_of = {}
        for i0 in range(0, len(kts), 4):
            grp = kts[i0:i0 + 4]
            ps, pc = self.psum()
            for i, kt in enumerate(grp):
                add("pe", lambda e, ps=ps, i=i, kt=kt: e.matmul(ps[:, i * 128:(i + 1) * 128], lhsT=self.KT[po:po + 64, c, kt * 128:(kt + 1) * 128],
                                                               rhs=qap, start=True, stop=True),
                    reads=[("KT", c, kt // 2), ("B2", c, t)], writes=[pc])
            psl = self.rot("PT", 8)
            for i, kt in enumerate(grp):
                dd = gt - kt
                add("act", lambda e, ps=ps, i=i, psl=psl, dd=dd: e.activation(
                    out=self.PT[:, psl, i * 128:(i + 1) * 128], in_=ps[:, i * 128:(i + 1) * 128], func=AF.Exp,
                    bias=self.consts["dbias"][hd][dd], scale=0.125),
                    reads=[pc], writes=[("PT", psl)])
                if kt == gt:
                    add("dve", lambda e, psl=psl, i=i: e.tensor_tensor(out=self.PT[:, psl, i * 128:(i + 1) * 128],
                                                                       in0=self.PT[:, psl, i * 128:(i + 1) * 128], in1=self.tri4[:, 0:128], op=ALU.mult),
                        reads=[("PT", psl), ("tri4",)], writes=[("PT", psl)])
                pt_of[kt] = (psl, i)
        return pt_of

    def moba_back(self, hf, t, hd, pt_of):
        add = self.add
        gt = hf * NT + t
        qb = gt // 2
        kts = list(range(gt + 1))
        pso, poc = self.psum()
        asl = self.rot("acc", 2)
        if hf == 0:
            for n_, kt in enumerate(kts):
                psl, i = pt_of[kt]
                add("pe", lambda e, psl=psl, i=i, kt=kt, n_=n_: e.matmul(pso[:, 0:65], lhsT=self.PT[:, psl, i * 128:(i + 1) * 128],
                                                                        rhs=self.V[:, kt, hd, :], start=(n_ == 0), stop=(n_ == len(kts) - 1)),
                    reads=[("PT", psl), ("V", kt, hd // 4), ("Vones", kt)], writes=[poc])
            src = pso
            srcc = poc
            res = pso[:, 0:65]
        else:
            psB, pbc = self.psum()
            own = [kt for kt in kts if kt // 2 == qb]
            for n_, kt in enumerate(own):
                psl, i = pt_of[kt]
                add("pe", lambda e, psl=psl, i=i, kt=kt, n_=n_: e.matmul(psB[:, 0:65], lhsT=self.PT[:, psl, i * 128:(i + 1) * 128],
                                                                        rhs=self.V[:, kt, hd, :], start=(n_ == 0), stop=(n_ == len(own) - 1)),
                    reads=[("PT", psl), ("V", kt, hd // 4), ("Vones", kt)], writes=[pbc])
            for b in range(qb):
                for n_, kt in enumerate((2 * b, 2 * b + 1)):
                    psl, i = pt_of[kt]
                    add("pe", lambda e, psl=psl, i=i, kt=kt, n_=n_, b=b: e.matmul(pso[:, b * 65:(b + 1) * 65], lhsT=self.PT[:, psl, i * 128:(i + 1) * 128],
                                                                                 rhs=self.V[:, kt, hd, :], start=(n_ == 0), stop=(n_ == 1)),
                        reads=[("PT", psl), ("V", kt, hd // 4), ("Vones", kt)], writes=[poc])
            accap = self.acc[:, asl, :]
            tv = self.tmpf[:, 0, 0:455].rearrange("p (b c) -> p b c", c=65)
            for b in range(qb):
                add("dve", lambda e, b=b: e.tensor_scalar(out=tv[:, b, :], in0=pso[:, b * 65:(b + 1) * 65],
                                                          scalar1=self.sel[:, t % 2, hd, b:b + 1], scalar2=None, op0=ALU.mult),
                    reads=[poc, ("sel", t % 2, hd), ("tmpf", 0)], writes=[("tmpfb", b)])
            rv = self.tmpf[:, 0, 0:qb * 65].rearrange("p (b c) -> p c b", c=65)
            add("dve", lambda e: e.tensor_reduce(out=accap, in_=rv, axis=AX.X, op=ALU.add),
                reads=[("tmpfb", b) for b in range(qb)], writes=[("acc", asl)])
            add("dve", lambda e: e.tensor_tensor(out=accap, in0=psB[:, 0:65], in1=accap, op=ALU.add),
                reads=[pbc, ("acc", asl)], writes=[("acc", asl)])
            res = accap
            srcc = ("acc", asl)
        st, stc = self.stat()
        add("dve", lambda e: e.reciprocal(out=st[:, 0:1], in_=res[:, 64:65]), reads=[srcc], writes=[stc])
        add("dve", lambda e: e.tensor_scalar(out=self.B1[:, t, hd * 64:(hd + 1) * 64], in0=res[:, 0:64], scalar1=st[:, 0:1],
                                             scalar2=None, op0=ALU.mult), reads=[srcc, stc], writes=[("B1", t, hd // 2)])

    def build(self):
        add = self.add
        self.setup()
        self.epsc = self.sb("epsc", [128, 1], F32)
        add("dve", lambda e: e.memset(self.epsc[:], EPS), writes=[("epsc",)])
        for hf in range(NH):
            for t in range(NT):
                gt = hf * NT + t
                add("sp", lambda e, t=t, gt=gt: e.dma_start(out=self.h[:, t, :], in_=self.x[gt * 128:(gt + 1) * 128, :]),
                    writes=[("h", t)], dma="d_h%d" % t)
            if self.stop > 0:
                self.layer0_mix(hf)
            if self.stop >= 2:
                self.mlp(0)
            if self.stop >= 3:
                self.layer1_mix(hf)
            if self.stop >= 4:
                self.mlp(1)
            for t in range(NT):
                gt = hf * NT + t
                add("sp", lambda e, t=t, gt=gt: e.dma_start(out=self.out[gt * 128:(gt + 1) * 128, :], in_=self.h[:, t, :]),
                    reads=[("h", t)], dma="d_o%d" % t)
        add("sp", None, writes=[("h", t) for t in range(NT)])
        self.sc.emit(self.nc, self.es)
        self.es.close()
        return self.nc


_CACHE = {}


def get_nc(stop=99):
    if stop not in _CACHE:
        _CACHE[stop] = Builder(stop).build()
    return _CACHE[stop]


def kernel(stop=99, ncores=8, **inputs):
    stop = float(stop)
    nc = get_nc(stop)
    cs = make_consts()
    f = lambda a: np.ascontiguousarray(np.asarray(a, dtype=np.float32))
    shared = {
        "w_in_a": f(inputs["w_in_a"][0]), "ret_norm_gain": f(inputs["ret_norm_gain"][0]), "w_out_a": f(inputs["w_out_a"][0]),
        "kv_norm_gain": f(inputs["kv_norm_gain"]), "w_kv_shared": f(inputs["w_kv_shared"]), "w_in_b": f(inputs["w_in_b"][0]),
        "w_out_b": f(inputs["w_out_b"][0]), "w_mem_kv": f(inputs["w_mem_kv"]), "norm_pre_mix": f(inputs["norm_pre_mix"]),
        "norm_post_mix": f(inputs["norm_post_mix"]), "norm_pre_mlp": f(inputs["norm_pre_mlp"]),
        "norm_post_mlp": f(inputs["norm_post_mlp"]), "w_up": f(inputs["w_up"]), "w_down": f(inputs["w_down"]),
        "c_ident": cs["ident"], "c_tri4": cs["tri4"], "c_retv": cs["retv"], "c_alibi": cs["alibi"],
    }
    x = f(inputs["x"])
    mem = f(inputs["mem"])
    in_maps = []
    for b in range(ncores):
        m = dict(shared)
        m["x"] = x[b]
        m["mem"] = mem[b]
        in_maps.append(m)
    res = run_bass_kernel_spmd(nc, in_maps, core_ids=list(range(ncores)))
    return np.stack([np.asarray(r["out"], dtype=np.float32) for r in res.results], axis=0)
```
